# Optimizing a Trainium2 kernel written in Bass

```python
import math
import jax, jax.numpy as jnp
from jax import lax
import numpy as np

D_MODEL = 1024
BATCH = 8
SEQ = 4096
DEPTH = 1

CTX_LEN = 256
GRID_W = 64
D_MIX = D_MODEL
GLA_WIDTH = D_MIX // 2
GLA_HEADS = 4
GLA_DK = 64
GLA_DV = GLA_WIDTH // GLA_HEADS
GLA_QK = GLA_HEADS * GLA_DK
GLA_RANK = 16
GLA_GATE_NORM = 16.0
GLA_CHUNK = 64
S5_WIDTH = D_MIX - GLA_WIDTH
S5_CH = 16
S5_GROUPS = S5_WIDTH // S5_CH
S5_STATE = 64
N_EXPERTS = 32
TOP_K = 4
D_FF = D_MODEL
SWIGLU_ALPHA = 1.702
SWIGLU_LIMIT = 7.0
MOE_BLOCK = 128
EPS = 1e-6
IN_SPLITS = (GLA_QK, 2 * GLA_QK, 2 * GLA_QK + GLA_WIDTH, 2 * GLA_QK + 2 * GLA_WIDTH,
             2 * GLA_QK + 2 * GLA_WIDTH + 2 * GLA_RANK)
IN_COLS = IN_SPLITS[-1] + S5_WIDTH

kernel_name = "hybrid_gla_s5_moe_prefix_dit"


def rms_norm(x, w):
    xf = x.astype(jnp.float32)
    y = xf * lax.rsqrt(jnp.mean(xf * xf, axis=-1, keepdims=True) + EPS)
    return (y * w.astype(jnp.float32)).astype(x.dtype)


def modulate(h, shift, scale):
    return h * (1 + scale) + shift


def to_cols(t):
    b, l, ch = t.shape
    rows = l // GRID_W
    return t.reshape(b, rows, GRID_W, ch).transpose(0, 2, 1, 3).reshape(b, l, ch)


def from_cols(t):
    b, l, ch = t.shape
    rows = l // GRID_W
    return t.reshape(b, GRID_W, rows, ch).transpose(0, 2, 1, 3).reshape(b, l, ch)


def gla_chunked(q, k, v, log_a, s0):
    b, h, l, _ = q.shape
    dv = v.shape[-1]
    n = l // GLA_CHUNK

    def chunks(t):
        return jnp.moveaxis(t.reshape(b, h, n, GLA_CHUNK, t.shape[-1]), 2, 0)

    mask = jnp.tril(jnp.ones((GLA_CHUNK, GLA_CHUNK), dtype=bool))[:, :, None]

    def step(S, inp):
        qc, kc, vc, lac = inp
        cum = jnp.cumsum(lac, axis=2)
        inter = jnp.einsum('bhid,bhde->bhie', qc * jnp.exp(cum), S)
        diff = cum[:, :, :, None, :] - cum[:, :, None, :, :]
        decay = jnp.exp(jnp.where(mask, diff, -jnp.inf))
        scores = jnp.sum(qc[:, :, :, None, :] * kc[:, :, None, :, :] * decay, axis=-1)
        intra = jnp.einsum('bhij,bhje->bhie', scores, vc)
        last = cum[:, :, -1:, :]
        S_new = jnp.exp(last[:, :, 0, :])[..., None] * S + jnp.einsum(
            'bhjd,bhje->bhde', kc * jnp.exp(last - cum), vc)
        return S_new, inter + intra

    s_fin, o = lax.scan(step, s0, (chunks(q), chunks(k), chunks(v), chunks(log_a)))
    return jnp.moveaxis(o, 0, 2).reshape(b, h, l, dv), s_fin


def gla_prep(p, lr_up, lr_bias):
    q, k, v, r, lr, u = jnp.split(p, IN_SPLITS, axis=-1)
    b, l, _ = p.shape

    def heads(t, d):
        return t.reshape(b, l, GLA_HEADS, d).transpose(0, 2, 1, 3).astype(jnp.float32)

    g = jnp.einsum('blzr,zrk->zblk', lr.reshape(b, l, 2, GLA_RANK), lr_up) + lr_bias[:, None, None, :]
    log_a = jax.nn.log_sigmoid(g.astype(jnp.float32)) / GLA_GATE_NORM
    log_a = log_a.reshape(2, b, l, GLA_HEADS, GLA_DK).transpose(0, 1, 3, 2, 4)
    return heads(q, GLA_DK) * (GLA_DK ** -0.5), heads(k, GLA_DK), heads(v, GLA_DV), log_a, r, u


def gla_bidir(q, k, v, log_a, s0_f, s0_b):
    o_f, s_f = gla_chunked(q, k, v, log_a[0], s0_f)
    o_b, s_b = gla_chunked(jnp.flip(q, 2), jnp.flip(k, 2), jnp.flip(v, 2), jnp.flip(log_a[1], 2), s0_b)
    return o_f + jnp.flip(o_b, 2), s_f, s_b


def gla_output(o, r, norm_w):
    on = o * lax.rsqrt(jnp.mean(o * o, axis=-1, keepdims=True) + EPS) * norm_w.astype(jnp.float32)
    b, h, l, dv = o.shape
    on = on.transpose(0, 2, 1, 3).reshape(b, l, h * dv).astype(r.dtype)
    return on * jax.nn.silu(r)


def s5_discretize(lam_re, lam_im, log_dt, b_re, b_im, c_re, c_im):
    lam = lax.complex(lam_re.astype(jnp.float32), lam_im.astype(jnp.float32))
    dt = jnp.exp(log_dt.astype(jnp.float32))[:, None]
    lam_bar = jnp.exp(lam * dt)
    b_mat = lax.complex(b_re.astype(jnp.float32), b_im.astype(jnp.float32))
    b_bar = ((lam_bar - 1) / lam)[..., None] * b_mat
    c_mat = lax.complex(c_re.astype(jnp.float32), c_im.astype(jnp.float32))
    return lam_bar, b_bar, c_mat


def s5_states(u, lam_bar, b_bar, h0):
    b, l, _ = u.shape
    uc = u.astype(jnp.float32).reshape(b, l, S5_GROUPS, S5_CH).astype(jnp.complex64)
    bu = jnp.einsum('blgh,gph->lbgp', uc, b_bar)
    bu = bu.at[0].add(lam_bar * h0)
    a = jnp.broadcast_to(lam_bar[None, None], (l, 1) + lam_bar.shape)

    def combine(e1, e2):
        a1, b1 = e1
        a2, b2 = e2
        return a1 * a2, a2 * b1 + b2

    _, states = lax.associative_scan(combine, (a, bu), axis=0)
    return states


def s5_readout(states, c_mat):
    l, b = states.shape[:2]
    return jnp.real(jnp.einsum('lbgp,ghp->blgh', states, c_mat)).reshape(b, l, S5_WIDTH)


def s5_glu(y, u, d, glu_w, glu_b):
    y = y.astype(u.dtype) + d * u
    y = jax.nn.gelu(y)
    return y * jax.nn.sigmoid(y @ glu_w + glu_b)


def token_mixers(h_lat, h_ctx, w_in, lr_up, lr_bias, gla_norm_w, lam_re, lam_im, log_dt,
                 b_re, b_im, c_re, c_im, s5_d, glu_w, glu_b, w_out, need_ctx_out):
    q, k, v, la, r, u = gla_prep(h_lat @ w_in, lr_up, lr_bias)
    qc, kc, vc, lac, rc, uc = gla_prep(h_ctx @ w_in, lr_up, lr_bias)
    b = h_ctx.shape[0]

    z = jnp.zeros((b, GLA_HEADS, GLA_DK, GLA_DV), jnp.float32)
    o_c, sc_f, sc_b = gla_bidir(qc, kc, vc, lac, z, z)
    o_l, _, _ = gla_bidir(q, k, v, la, sc_f, sc_b)
    gla_lat = gla_output(o_l, r, gla_norm_w)

    disc = [s5_discretize(lam_re[d], lam_im[d], log_dt[d], b_re[d], b_im[d], c_re[d], c_im[d])
            for d in range(2)]
    h0 = jnp.zeros((b, S5_GROUPS, S5_STATE), jnp.complex64)
    st_cf = s5_states(uc, disc[0][0], disc[0][1], h0)
    st_cb = s5_states(jnp.flip(uc, 1), disc[1][0], disc[1][1], h0)
    ul = to_cols(u)
    st_f = s5_states(ul, disc[0][0], disc[0][1], st_cf[-1])
    st_b = s5_states(jnp.flip(ul, 1), disc[1][0], disc[1][1], st_cb[-1])
    y = s5_readout(st_f, disc[0][2]) + jnp.flip(s5_readout(st_b, disc[1][2]), 1)
    s5_lat = from_cols(s5_glu(y, ul, s5_d, glu_w, glu_b))

    mix_lat = jnp.concatenate([gla_lat, s5_lat], axis=-1) @ w_out
    if not need_ctx_out:
        return mix_lat, None
    gla_ctx = gla_output(o_c, rc, gla_norm_w)
    yc = s5_readout(st_cf, disc[0][2]) + jnp.flip(s5_readout(st_cb, disc[1][2]), 1)
    s5_ctx = s5_glu(yc, uc, s5_d, glu_w, glu_b)
    mix_ctx = jnp.concatenate([gla_ctx, s5_ctx], axis=-1) @ w_out
    return mix_lat, mix_ctx


def moe_ffn(h, router_w, router_b, w_gu, b_gu, w_down, b_down):
    n = h.shape[0]
    logits = (h @ router_w + router_b).astype(jnp.float32)
    top_val, top_idx = lax.top_k(logits, TOP_K)
    gate = jax.nn.softmax(top_val, axis=-1).astype(h.dtype)
    flat_e = top_idx.reshape(-1)
    flat_tok = jnp.repeat(jnp.arange(n, dtype=jnp.int32), TOP_K)
    flat_w = gate.reshape(-1)
    order = jnp.argsort(flat_e)
    se, stok, sw = flat_e[order], flat_tok[order], flat_w[order]
    counts = jnp.bincount(flat_e, length=N_EXPERTS)
    start = jnp.cumsum(counts) - counts
    padded = ((counts + MOE_BLOCK - 1) // MOE_BLOCK) * MOE_BLOCK
    pend = jnp.cumsum(padded)
    pstart = pend - padded
    dest = pstart[se] + (jnp.arange(n * TOP_K) - start[se])
    n_blocks = -(-(n * TOP_K) // MOE_BLOCK) + N_EXPERTS
    p_rows = n_blocks * MOE_BLOCK
    tok_buf = jnp.zeros((p_rows,), jnp.int32).at[dest].set(stok)
    w_buf = jnp.zeros((p_rows,), h.dtype).at[dest].set(sw)
    blk_expert = jnp.minimum(
        jnp.searchsorted(pend, jnp.arange(n_blocks) * MOE_BLOCK, side='right'), N_EXPERTS - 1)

    def expert_block(args):
        tok, e = args
        xb = h[tok]
        gu = xb @ w_gu[e] + b_gu[e]
        g, lin = jnp.split(gu, 2, axis=-1)
        g = jnp.minimum(g, SWIGLU_LIMIT)
        lin = jnp.clip(lin, -SWIGLU_LIMIT, SWIGLU_LIMIT)
        act = (lin + 1) * (g * jax.nn.sigmoid(SWIGLU_ALPHA * g))
        return act @ w_down[e] + b_down[e]

    ys = lax.map(expert_block, (tok_buf.reshape(n_blocks, MOE_BLOCK), blk_expert))
    return jnp.zeros_like(h).at[tok_buf].add(ys.reshape(p_rows, -1) * w_buf[:, None])


def setup_inputs(seed: int = 0) -> dict:
    key = jax.random.key(seed)
    ks = jax.random.split(key, 32)
    f32 = jnp.float32
    D, L, G, P, H = DEPTH, D_MODEL, S5_GROUPS, S5_STATE, S5_CH
    nrm = lambda k, shape, s: (jax.random.normal(k, shape, f32) * s)
    gain = lambda k, shape: 1.0 + 0.01 * jax.random.normal(k, shape, f32)
    lam_im = jnp.pi * jnp.arange(P, dtype=f32)
    return {
        'x': nrm(ks[0], (BATCH, SEQ, L), 1.0),
        'c': nrm(ks[1], (BATCH, L), 1.0),
        'ctx': nrm(ks[2], (BATCH, CTX_LEN, L), 1.0),
        'c_ctx': nrm(ks[3], (L,), 1.0),
        'ada_w': nrm(ks[4], (D, L, 6 * L), 0.5 * L ** -0.5),
        'ada_b': nrm(ks[5], (D, 6 * L), 0.02),
        'norm_mix_w': gain(ks[6], (D, L)),
        'w_in': nrm(ks[7], (D, L, IN_COLS), L ** -0.5),
        'gla_lr_up': nrm(ks[8], (D, 2, GLA_RANK, GLA_QK), GLA_RANK ** -0.5),
        'gla_lr_bias': nrm(ks[9], (D, 2, GLA_QK), 0.1),
        'gla_norm_w': gain(ks[10], (D, GLA_DV)),
        's5_lam_re': -0.5 + 0.01 * jax.random.normal(ks[11], (D, 2, G, P), f32),
        's5_lam_im': lam_im + 0.01 * jax.random.normal(ks[12], (D, 2, G, P), f32),
        's5_log_dt': jax.random.uniform(ks[13], (D, 2, G), f32, math.log(1e-3), math.log(1e-1)),
        's5_b_re': nrm(ks[14], (D, 2, G, P, H), (2 * H) ** -0.5),
        's5_b_im': nrm(ks[15], (D, 2, G, P, H), (2 * H) ** -0.5),
        's5_c_re': nrm(ks[16], (D, 2, G, H, P), (2 * P) ** -0.5),
        's5_c_im': nrm(ks[17], (D, 2, G, H, P), (2 * P) ** -0.5),
        's5_d': nrm(ks[18], (D, S5_WIDTH), 1.0),
        'glu_w': nrm(ks[19], (D, S5_WIDTH, S5_WIDTH), S5_WIDTH ** -0.5),
        'glu_b': nrm(ks[20], (D, S5_WIDTH), 0.01),
        'w_out': nrm(ks[21], (D, D_MIX, L), D_MIX ** -0.5),
        'norm_ffn_w': gain(ks[22], (D, L)),
        'router_w': nrm(ks[23], (D, L, N_EXPERTS), L ** -0.5),
        'router_b': nrm(ks[24], (D, N_EXPERTS), 0.01),
        'exp_w_gu': nrm(ks[25], (D, N_EXPERTS, L, 2 * D_FF), L ** -0.5),
        'exp_b_gu': nrm(ks[26], (D, N_EXPERTS, 2 * D_FF), 0.01),
        'exp_w_down': nrm(ks[27], (D, N_EXPERTS, D_FF, L), D_FF ** -0.5),
        'exp_b_down': nrm(ks[28], (D, N_EXPERTS, L), 0.01),
        'final_norm_w': gain(ks[29], (L,)),
    }


def reference(x, c, ctx, c_ctx, ada_w, ada_b, norm_mix_w, w_in, gla_lr_up, gla_lr_bias, gla_norm_w,
              s5_lam_re, s5_lam_im, s5_log_dt, s5_b_re, s5_b_im, s5_c_re, s5_c_im, s5_d, glu_w, glu_b,
              w_out, norm_ffn_w, router_w, router_b, exp_w_gu, exp_b_gu, exp_w_down, exp_b_down,
              final_norm_w):
    bsz, seq, dm = x.shape
    for layer in range(DEPTH):
        need_ctx = layer < DEPTH - 1
        mod = jax.nn.silu(c) @ ada_w[layer] + ada_b[layer]
        mod_c = jax.nn.silu(c_ctx) @ ada_w[layer] + ada_b[layer]
        sh1, sc1, g1, sh2, sc2, g2 = jnp.split(mod[:, None, :], 6, axis=-1)
        csh1, csc1, cg1, csh2, csc2, cg2 = jnp.split(mod_c, 6, axis=-1)

        h = modulate(rms_norm(x, norm_mix_w[layer]), sh1, sc1)
        hc = modulate(rms_norm(ctx, norm_mix_w[layer]), csh1, csc1)
        mix, mix_c = token_mixers(h, hc, w_in[layer], gla_lr_up[layer], gla_lr_bias[layer],
                                  gla_norm_w[layer], s5_lam_re[layer], s5_lam_im[layer],
                                  s5_log_dt[layer], s5_b_re[layer], s5_b_im[layer], s5_c_re[layer],
                                  s5_c_im[layer], s5_d[layer], glu_w[layer], glu_b[layer],
                                  w_out[layer], need_ctx)
        x = x + g1 * mix
        h2 = modulate(rms_norm(x, norm_ffn_w[layer]), sh2, sc2)
        ff = moe_ffn(h2.reshape(bsz * seq, dm), router_w[layer], router_b[layer], exp_w_gu[layer],
                     exp_b_gu[layer], exp_w_down[layer], exp_b_down[layer])
        x = x + g2 * ff.reshape(bsz, seq, dm)
        if need_ctx:
            ctx = ctx + cg1 * mix_c
            hc2 = modulate(rms_norm(ctx, norm_ffn_w[layer]), csh2, csc2)
            ffc = moe_ffn(hc2.reshape(-1, dm), router_w[layer], router_b[layer], exp_w_gu[layer],
                          exp_b_gu[layer], exp_w_down[layer], exp_b_down[layer])
            ctx = ctx + cg2 * ffc.reshape(ctx.shape)
    return rms_norm(x, final_norm_w)
```

```python
import numpy as np
import concourse.bass as bass
import concourse.mybir as mybir
from concourse.bass_utils import run_bass_kernel_spmd
from contextlib import ExitStack

F32 = mybir.dt.float32
BF16 = mybir.dt.bfloat16
U32 = mybir.dt.uint32
I32 = mybir.dt.int32
ACT = mybir.ActivationFunctionType
ALU = mybir.AluOpType
AX = mybir.AxisListType
PoolE = mybir.EngineType.Pool

D = 1024
NL = 4096
NCX = 256
NT = NL + NCX
NE = 32
EPS = 1e-6


class Dep:
    __slots__ = ("w", "r", "name")

    def __init__(self, name=""):
        self.w = None
        self.r = []
        self.name = name


class KB:
    NSLOT = 8

    def __init__(self, nc, es):
        self.nc = nc
        self.engs = {"pe": nc.tensor, "dve": nc.vector, "act": nc.scalar,
                     "pool": nc.gpsimd, "sp": nc.sync}
        self.sem = {}
        self.cnt = {}
        for k in self.engs:
            self.sem[k] = es.enter_context(nc.semaphore("s_" + k))
            self.cnt[k] = 0
        self.slots = {}
        self.slot_rr = {}
        for q in ("sp", "act", "pool"):
            self.slots[q] = [[es.enter_context(nc.semaphore(f"d_{q}{i}")), 0]
                             for i in range(self.NSLOT)]
            self.slot_rr[q] = 0
        self.seen = {k: {} for k in self.engs}
        self.ninst = 0
        self.nwaits = 0

    def _wait(self, eng, tok):
        if tok is None:
            return
        sem, val, key = tok
        if eng == "pe" and key == "pe":
            return
        s = self.seen[eng]
        if s.get(key, 0) >= val:
            return
        self.engs[eng].wait_ge(sem, val)
        s[key] = val
        self.nwaits += 1

    def _deps(self, eng, reads, writes):
        for d in reads:
            self._wait(eng, d.w)
        for d in writes:
            self._wait(eng, d.w)
            for t in d.r:
                self._wait(eng, t)

    def _commit(self, tok, reads, writes):
        for d in reads:
            d.r.append(tok)
            if len(d.r) > 16:
                best = {}
                for t in d.r:
                    if t[2] not in best or best[t[2]][1] < t[1]:
                        best[t[2]] = t
                d.r = list(best.values())
        for d in writes:
            d.w = tok
            d.r = []

    dead = False

    def op(self, eng, fn, reads=(), writes=()):
        if KB.dead:
            return None
        self._deps(eng, reads, writes)
        inst = fn()
        self.cnt[eng] += 1
        inst.then_inc(self.sem[eng], 1)
        tok = (self.sem[eng], self.cnt[eng], eng)
        self._commit(tok, reads, writes)
        self.ninst += 1
        return tok

    def dma(self, q, fn, reads=(), writes=()):
        if KB.dead:
            return None
        i = self.slot_rr[q]
        self.slot_rr[q] = (i + 1) % self.NSLOT
        slot = self.slots[q][i]
        key = f"d_{q}{i}"
        if slot[1] > 0:
            self._wait(q, (slot[0], slot[1], key))
        self._deps(q, reads, writes)
        inst = fn()
        slot[1] += 16
        inst.then_inc(slot[0], 16)
        tok = (slot[0], slot[1], key)
        self._commit(tok, reads, writes)
        self.ninst += 1
        return tok

    def wait_all(self, eng, deps):
        for d in deps:
            self._wait(eng, d.w)
            for t in d.r:
                self._wait(eng, t)

    def barrier(self):
        for e in self.engs:
            self.finish(e)

    def finish(self, eng="sp"):
        for k in self.engs:
            if self.cnt[k] > 0:
                self._wait(eng, (self.sem[k], self.cnt[k], k))
        for q in self.slots:
            for i, s in enumerate(self.slots[q]):
                if s[1] > 0:
                    self._wait(eng, (s[0], s[1], f"d_{q}{i}"))


class T:
    _ctr = [0]

    def __init__(self, kb, es, shape, dtype, name, psum=False, nd=1):
        nc = kb.nc
        T._ctr[0] += 1
        name = f"{name}_{T._ctr[0]}"
        if psum:
            self.t = es.enter_context(nc.psum_tensor(name, list(shape), dtype))
        else:
            self.t = es.enter_context(nc.sbuf_tensor(name, list(shape), dtype))
        self.ds = [Dep(f"{name}.{i}") for i in range(nd)]
        self.d = self.ds[0]
        self.shape = shape

    def __getitem__(self, k):
        return self.t[k]


class Ctx:
    pass


class StopBuild(Exception):
    pass


def ckpt(name):
    if STOP == name:
        KB.dead = True


STOP = None


def build(dbg=(), stop=None):
    global STOP
    STOP = stop
    KB.dead = False
    nc = bass.Bass("TRN2", target_bir_lowering=False)
    g = Ctx()
    g.nc = nc
    g.dbg = set(dbg)
    g.outs = {}
    g.out_d = Dep("out")

    def din(name, shape, dt=F32):
        return nc.dram_tensor(name, list(shape), dt, kind="ExternalInput")

    H = Ctx()
    g.H = H
    H.x = din("x", [NL, D])
    H.ctx = din("ctx", [NCX, D])
    H.c = din("c", [1, D])
    H.c_ctx = din("c_ctx", [D])
    H.ada_w = din("ada_w", [1, D, 6 * D])
    H.ada_b = din("ada_b", [1, 6 * D])
    H.norm_mix_w = din("norm_mix_w", [1, D])
    H.w_in = din("w_in", [1, D, 2080])
    H.gla_lr_up = din("gla_lr_up", [1, 2, 16, 256])
    H.gla_lr_bias = din("gla_lr_bias", [1, 2, 256])
    H.gla_norm_w = din("gla_norm_w", [1, 128])
    H.s5_lam_re = din("s5_lam_re", [1, 2, 32, 64])
    H.s5_lam_im = din("s5_lam_im", [1, 2, 32, 64])
    H.s5_log_dt = din("s5_log_dt", [1, 2, 32])
    H.s5_b_re = din("s5_b_re", [1, 2, 32, 64, 16])
    H.s5_b_im = din("s5_b_im", [1, 2, 32, 64, 16])
    H.s5_c_re = din("s5_c_re", [1, 2, 32, 16, 64])
    H.s5_c_im = din("s5_c_im", [1, 2, 32, 16, 64])
    H.s5_d = din("s5_d", [1, 512])
    H.glu_w = din("glu_w", [1, 512, 512])
    H.glu_b = din("glu_b", [1, 512])
    H.w_out = din("w_out", [1, D, D])
    H.norm_ffn_w = din("norm_ffn_w", [1, D])
    H.router_w = din("router_w", [1, D, NE])
    H.router_b = din("router_b", [1, NE])
    H.exp_w_gu = din("exp_w_gu", [1, NE, D, 2 * D])
    H.exp_b_gu = din("exp_b_gu", [1, NE, 2 * D])
    H.exp_w_down = din("exp_w_down", [1, NE, D, D])
    H.exp_b_down = din("exp_b_down", [1, NE, D])
    H.final_norm_w = din("final_norm_w", [D])
    H.out = nc.dram_tensor("out", [NL, D], F32, kind="ExternalOutput")

    S = Ctx()
    g.S = S

    def scr(name, shape, dt):
        if name in g.dbg:
            h = nc.dram_tensor(name, list(shape), dt, kind="ExternalOutput")
        else:
            h = nc.dram_tensor(name, list(shape), dt)
        return h, Dep(name)

    S.mod, S.mod_d = scr("mod_s", [2, 6 * D], F32)
    S.qT, S.qT_d = scr("qT_s", [256, NT], BF16)
    S.kT, S.kT_d = scr("kT_s", [256, NT], BF16)
    S.rT, S.rT_d = scr("rT_s", [512, NL], BF16)
    S.lrT, S.lrT_d = scr("lrT_s", [2, 16, NT], F32)
    S.v, S.v_d = scr("v_s", [NT, 512], BF16)
    S.uT, S.uT_d = scr("uT_s", [512, NT], BF16)
    S.u, S.u_d = scr("u_s", [NT, 512], BF16)
    S.glaT, S.glaT_d = scr("glaT_s", [512, NL], BF16)
    S.y0, S.y0_d = scr("y0_s", [32, 128, 512], F32)
    S.Ut, S.Ut_d = scr("Ut_s", [32, 128, NT // 8], BF16)
    S.x2, S.x2_d = scr("x2_s", [NL, D], F32)
    S.h2, S.h2_d = scr("h2_s", [NL, D], BF16)
    S.xg, S.xg_d = scr("xg_s", [64 * 512, D], BF16)
    S.yg, S.yg_d = scr("yg_s", [64 * 512, D], BF16)
    S.bguT, S.bguT_d = scr("bguT_s", [NE, 128, 16], F32)

    with ExitStack() as es:
        kb = KB(nc, es)
        g.kb = kb
        g.es = es
        g.ident = T(kb, es, [128, 128], F32, "ident")
        g.identb = T(kb, es, [128, 128], BF16, "identb")
        io = T(kb, es, [128, 128], F32, "iota0")
        kb.op("pool", lambda: nc.gpsimd.iota(io[:], pattern=[[1, 128]], base=0, channel_multiplier=-1,
                                              allow_small_or_imprecise_dtypes=True), writes=[io.d])
        kb.op("dve", lambda: nc.vector.tensor_single_scalar(g.ident[:], io[:], 0.0, op=ALU.is_equal),
              reads=[io.d], writes=[g.ident.d])
        kb.op("dve", lambda: nc.vector.tensor_copy(g.identb[:], g.ident[:]), reads=[g.ident.d], writes=[g.identb.d])
        g.iota = io
        g.ps = [T(kb, es, [128, 512], F32, f"ps{i}", psum=True) for i in range(6)]
        g.psb = [T(kb, es, [128, 1024], BF16, f"psb{i}", psum=True) for i in range(2)]
        g.ps_rr = 0
        R = Ctx()
        g.R = R
        R.mask = T(kb, es, [128, 32, NE], F32, "r_mask")
        R.idxf = T(kb, es, [128, 32, 4], F32, "r_idxf")
        R.gate = T(kb, es, [128, 32, 4], F32, "r_gate")
        R.slot = T(kb, es, [128, 32, 4], I32, "r_slot", nd=32)
        R.segexp = T(kb, es, [128, 64], I32, "r_segexp")

        phase_a(g)
        kb.barrier()
        if stop != 'a':
            phase_b(g)
            kb.barrier()
            if stop != 'b':
                if stop not in ('d_only', 'e_only'):
                    phase_c(g)
                    kb.barrier()
                if stop not in ('c', 'c0', 'c1', 'c2', 'c3'):
                    if stop != 'e_only':
                        phase_d(g)
                        kb.barrier()
                    if stop not in ('d', 'd_only'):
                        phase_e(g)
                        kb.barrier()
                        if stop != 'e':
                            phase_f(g)
                            kb.barrier()
                            phase_g(g)
                            kb.barrier()

        kb.finish("sp")
        print("instructions", kb.ninst, "waits", kb.nwaits)
    return nc, g


def next_ps(g):
    p = g.ps[g.ps_rr]
    g.ps_rr = (g.ps_rr + 1) % len(g.ps)
    return p


def dbg_out(g, name, src_ap, deps, shape, dt=F32, q="sp"):
    if name not in g.dbg:
        return
    nc, kb = g.nc, g.kb
    o = nc.dram_tensor("dbg_" + name, list(shape), dt, kind="ExternalOutput")
    g.outs[name] = o
    kb.dma(q, lambda: nc.sync.dma_start(out=o.ap(), in_=src_ap), reads=deps)


def phase_a(g):
    nc, kb, H, S = g.nc, g.kb, g.H, g.S
    with ExitStack() as es:
        cc = T(kb, es, [128, 8, 2], F32, "cc")
        kb.dma("sp", lambda: nc.sync.dma_start(out=cc[:, :, 0], in_=H.c[0, :].rearrange("(k p) -> p k", p=128),
                                               allow_slow_non_contiguous=True), writes=[cc.d])
        kb.dma("sp", lambda: nc.sync.dma_start(out=cc[:, :, 1], in_=H.c_ctx.ap().rearrange("(k p) -> p k", p=128),
                                               allow_slow_non_contiguous=True), writes=[cc.d])
        kb.op("act", lambda: nc.scalar.activation(cc[:], cc[:], ACT.Silu), reads=[cc.d], writes=[cc.d])
        ab = T(kb, es, [2, 6 * D], F32, "ab")
        kb.dma("sp", lambda: nc.sync.dma_start(out=ab[0:1, :], in_=H.ada_b[0:1, :]), writes=[ab.d])
        kb.dma("sp", lambda: nc.sync.dma_start(out=ab[1:2, :], in_=H.ada_b[0:1, :]), writes=[ab.d])
        modsb = T(kb, es, [2, 6 * D], F32, "modsb")
        aw = [T(kb, es, [128, 8, 512], F32, f"aw{i}") for i in range(2)]
        for j in range(12):
            a = aw[j % 2]
            kb.dma("sp" if j % 2 == 0 else "act",
                   (lambda a=a, j=j: (nc.sync if j % 2 == 0 else nc.scalar).dma_start(
                       out=a[:], in_=H.ada_w[0, :, j * 512:(j + 1) * 512].rearrange("(k p) n -> p k n", p=128))),
                   writes=[a.d])
            ps = next_ps(g)
            for k in range(8):
                kb.op("pe", lambda k=k: nc.tensor.matmul(ps[0:2, :], lhsT=cc[:, k, :], rhs=a[:, k, :],
                                                         start=(k == 0), stop=(k == 7)),
                      reads=[cc.d, a.d], writes=[ps.d])
            kb.op("dve", lambda j=j: nc.vector.tensor_tensor(modsb[:, j * 512:(j + 1) * 512], ps[0:2, :],
                                                            ab[:, j * 512:(j + 1) * 512], op=ALU.add),
                  reads=[ps.d, ab.d], writes=[modsb.d])
        kb.dma("sp", lambda: nc.sync.dma_start(out=S.mod.ap(), in_=modsb[:]), reads=[modsb.d], writes=[S.mod_d])


def load_fm_vec(g, es, name, src_ap_1d):
    nc, kb = g.nc, g.kb
    t = T(kb, es, [128, 8], F32, name)
    kb.dma("sp", lambda: nc.sync.dma_start(out=t[:], in_=src_ap_1d.rearrange("(k p) -> p k", p=128),
                                           allow_slow_non_contiguous=True), reads=[g.S.mod_d], writes=[t.d])
    return t


def s5_cols(hT, k, s0, n):
    if s0 < NCX:
        assert s0 + n <= NCX
        return hT[:, k, s0:s0 + n]
    c0 = (s0 - NCX) // 64
    ncol = n // 64
    v = hT[:, k, NCX:NT].rearrange("p (row col) -> p col row", col=64)
    return v[:, c0:c0 + ncol, :]


def phase_b(g):
    nc, kb, H, S = g.nc, g.kb, g.H, g.S
    with ExitStack() as es:
        sh1 = load_fm_vec(g, es, "sh1", S.mod[0, 0:D])
        sc1 = load_fm_vec(g, es, "sc1", S.mod[0, D:2 * D])
        csh1 = load_fm_vec(g, es, "csh1", S.mod[1, 0:D])
        csc1 = load_fm_vec(g, es, "csc1", S.mod[1, D:2 * D])
        nmw = load_fm_vec(g, es, "nmw", H.norm_mix_w[0, :])
        g1f = T(kb, es, [128, 8], F32, "g1f")
        cg1f = T(kb, es, [128, 8], F32, "cg1f")
        kb.op("dve", lambda: nc.vector.scalar_tensor_tensor(g1f[:], sc1[:], 1.0, nmw[:], op0=ALU.add, op1=ALU.mult),
              reads=[sc1.d, nmw.d], writes=[g1f.d])
        kb.op("dve", lambda: nc.vector.scalar_tensor_tensor(cg1f[:], csc1[:], 1.0, nmw[:], op0=ALU.add, op1=ALU.mult),
              reads=[csc1.d, nmw.d], writes=[cg1f.d])
        wi = T(kb, es, [128, 8, 2080], BF16, "wi")
        for (a, b) in ((0, 1040), (1040, 2080)):
            kb.dma("pool", lambda a=a, b=b: nc.gpsimd.dma_start(
                out=wi[:, :, a:b], in_=H.w_in[0, :, a:b].rearrange("(k p) n -> p k n", p=128)), writes=[wi.d])
        hT = T(kb, es, [128, 8, NT], BF16, "hT", nd=9)
        xg = [T(kb, es, [128, 4, D], F32, f"xg{i}") for i in range(2)]
        junk = T(kb, es, [128, D], BF16, "junkb")
        groups = [("ctx", 0, 2)] + [("lat", gi, 4) for gi in range(8)]
        for gi, (kind, idx, ntile) in enumerate(groups):
            xt = xg[gi % 2]
            ntok = ntile * 128
            if kind == "ctx":
                src = H.ctx[0:ntok, :]
                col0 = 0
                gsc, gsh = cg1f, csh1
            else:
                src = H.x[idx * 512:(idx + 1) * 512, :]
                col0 = NCX + idx * 512
                gsc, gsh = g1f, sh1
            q = "sp" if gi % 2 == 0 else "act"
            kb.dma(q, lambda xt=xt, src=src, ntile=ntile, q=q: (nc.sync if q == "sp" else nc.scalar).dma_start(
                out=xt[:, 0:ntile, :], in_=src.rearrange("(j p) d -> p j d", p=128)), writes=[xt.d])
            ss = T(kb, es, [128, 4], F32, f"ss{gi}")
            rs = T(kb, es, [128, 4], F32, f"rs{gi}")
            for j in range(ntile):
                kb.op("act", lambda j=j: nc.scalar.activation(junk[:], xt[:, j, :], ACT.Square,
                                                              accum_out=ss[:, j:j + 1]),
                      reads=[xt.d], writes=[junk.d, ss.d])
            kb.op("dve", lambda: nc.vector.tensor_scalar(rs[:, 0:ntile], ss[:, 0:ntile], 1.0 / D, EPS,
                                                         op0=ALU.mult, op1=ALU.add), reads=[ss.d], writes=[rs.d])
            kb.op("act", lambda: nc.scalar.activation(rs[:, 0:ntile], rs[:, 0:ntile], ACT.Sqrt),
                  reads=[rs.d], writes=[rs.d])
            kb.op("dve", lambda: nc.vector.reciprocal(rs[:, 0:ntile], rs[:, 0:ntile]), reads=[rs.d], writes=[rs.d])
            for j in range(ntile):
                kb.op("dve", lambda j=j: nc.vector.tensor_scalar(xt[:, j, :], xt[:, j, :], rs[:, j:j + 1], None,
                                                                 op0=ALU.mult), reads=[xt.d, rs.d], writes=[xt.d])
            for k in range(8):
                ps = next_ps(g)
                for j in range(ntile):
                    kb.op("pe", lambda j=j, k=k: nc.tensor.transpose(ps[:, j * 128:(j + 1) * 128],
                                                                     xt[:, j, k * 128:(k + 1) * 128], g.ident[:]),
                          reads=[xt.d, g.ident.d], writes=[ps.d])
                kb.op("act", lambda k=k, ps=ps: nc.scalar.activation(
                    hT[:, k, col0:col0 + ntok], ps[:, 0:ntok], ACT.Identity,
                    bias=gsh[:, k:k + 1], scale=gsc[:, k:k + 1]),
                    reads=[ps.d, gsh.d, gsc.d], writes=[hT.ds[gi]])
        hall = hT.ds
        if "hT" in g.dbg:
            dbg_out(g, "hT", hT[:], hall, [128, 8, NT], BF16)
        stg = [T(kb, es, [128, NT], BF16, f"stg{i}") for i in range(2)]
        stgf = T(kb, es, [16, NT], F32, "stgf")
        rr = [0]

        def fm_proj(c0, m, dst_fn, t0, t1, cols_fn, f32=False):
            st = stgf if f32 else stg[rr[0] % 2]
            rr[0] += 0 if f32 else 1
            t = t0
            while t < t1:
                n = min(512, t1 - t)
                if t < NCX:
                    n = min(n, NCX - t)
                ps = next_ps(g)
                for k in range(8):
                    kb.op("pe", lambda k=k, t=t, n=n: nc.tensor.matmul(
                        ps[0:m, 0:n], lhsT=wi[:, k, c0:c0 + m], rhs=cols_fn(k, t, n),
                        start=(k == 0), stop=(k == 7)), reads=[wi.d] + hall, writes=[ps.d])
                eng = "act" if (t // 512) % 2 == 0 else "dve"
                if eng == "act":
                    kb.op("act", lambda t=t, n=n, ps=ps: nc.scalar.copy(st[0:m, t:t + n], ps[0:m, 0:n]),
                          reads=[ps.d], writes=[st.d])
                else:
                    kb.op("dve", lambda t=t, n=n, ps=ps: nc.vector.tensor_copy(st[0:m, t:t + n], ps[0:m, 0:n]),
                          reads=[ps.d], writes=[st.d])
                t += n
            dst, dd = dst_fn()
            kb.dma("sp", lambda: nc.sync.dma_start(out=dst, in_=st[0:m, t0:t1]), reads=[st.d], writes=[dd])

        raster = lambda k, t, n: hT[:, k, t:t + n]
        s5t = lambda k, t, n: s5_cols(hT, k, t, n)
        for mt in range(2):
            fm_proj(mt * 128, 128, lambda mt=mt: (S.qT[mt * 128:(mt + 1) * 128, :], S.qT_d), 0, NT, raster)
        for mt in range(2):
            fm_proj(256 + mt * 128, 128, lambda mt=mt: (S.kT[mt * 128:(mt + 1) * 128, :], S.kT_d), 0, NT, raster)
        for mt in range(4):
            fm_proj(1024 + mt * 128, 128, lambda mt=mt: (S.rT[mt * 128:(mt + 1) * 128, :], S.rT_d), NCX, NT, raster)
        for z in range(2):
            fm_proj(1536 + z * 16, 16, lambda z=z: (S.lrT[z, :, :], S.lrT_d), 0, NT, raster, f32=True)
        for mt in range(4):
            fm_proj(1568 + mt * 128, 128, lambda mt=mt: (S.uT[mt * 128:(mt + 1) * 128, :], S.uT_d), 0, NT, s5t)
        st4 = [T(kb, es, [128, 4, 512], BF16, f"st4_{i}") for i in range(2)]
        ngrp = 0
        for (c0, dst, dd, s5) in ((512, S.v, S.v_d, False), (1568, S.u, S.u_d, True)):
            for t0 in list(range(0, NCX, 512)) + list(range(NCX, NT, 512)):
                nt = 2 if t0 < NCX else 4
                st = st4[ngrp % 2]
                ngrp += 1
                for j in range(nt):
                    ps = next_ps(g)
                    tt = t0 + j * 128
                    if s5 and tt >= NCX:
                        for hf in range(2):
                            col = (tt - NCX) // 64 + hf
                            for k in range(8):
                                lh = hT[:, k, NCX + col:NT:64]
                                kb.op("pe", lambda k=k, lh=lh, ps=ps, hf=hf: nc.tensor.matmul(
                                    ps[hf * 64:(hf + 1) * 64, :], lhsT=lh, rhs=wi[:, k, c0:c0 + 512],
                                    start=(k == 0), stop=(k == 7)), reads=[wi.d] + hall, writes=[ps.d])
                    else:
                        for k in range(8):
                            lh = hT[:, k, tt:tt + 128]
                            kb.op("pe", lambda k=k, lh=lh, ps=ps: nc.tensor.matmul(
                                ps[:, :], lhsT=lh, rhs=wi[:, k, c0:c0 + 512], start=(k == 0), stop=(k == 7)),
                                reads=[wi.d] + hall, writes=[ps.d])
                    if j % 2 == 0:
                        kb.op("act", lambda j=j, ps=ps, st=st: nc.scalar.copy(st[:, j, :], ps[:, :]),
                              reads=[ps.d], writes=[st.d])
                    else:
                        kb.op("dve", lambda j=j, ps=ps, st=st: nc.vector.tensor_copy(st[:, j, :], ps[:, :]),
                              reads=[ps.d], writes=[st.d])
                kb.dma("sp", lambda st=st, t0=t0, nt=nt, dst=dst: nc.sync.dma_start(
                    out=dst[t0:t0 + nt * 128, :].rearrange("(j p) e -> p j e", p=128), in_=st[:, 0:nt, :]),
                    reads=[st.d], writes=[dd])


def phase_c(g):
    nc, kb, H, S = g.nc, g.kb, g.H, g.S
    NCH = NT // 64
    with ExitStack() as es:
        psb = g.psb[0]
        lup = T(kb, es, [16, 2, 256], F32, "lup")
        kb.dma("sp", lambda: nc.sync.dma_start(out=lup[:], in_=H.gla_lr_up[0].rearrange("z r f -> r z f")),
               writes=[lup.d])
        nb = T(kb, es, [128, 2, 2], F32, "nbias")
        kb.dma("sp", lambda: nc.sync.dma_start(out=nb[:], in_=H.gla_lr_bias[0].rearrange("z (t p) -> p z t", p=128),
                                               allow_slow_non_contiguous=True), writes=[nb.d])
        kb.op("dve", lambda: nc.vector.tensor_scalar(nb[:], nb[:], -1.0, None, op0=ALU.mult), reads=[nb.d], writes=[nb.d])
        nw = T(kb, es, [128, 1], F32, "gnw")
        kb.dma("sp", lambda: nc.sync.dma_start(out=nw[:], in_=H.gla_norm_w[0, :].rearrange("(p o) -> p o", o=1)),
               writes=[nw.d])
        ones = T(kb, es, [128, 128], BF16, "onesb")
        kb.op("pool", lambda: nc.gpsimd.memset(ones[:], 1.0), writes=[ones.d])
        io = g.iota
        mk = []
        for z in range(2):
            m4 = T(kb, es, [128, 4, 64], BF16, f"tri{z}")
            op = ALU.is_ge if z == 0 else ALU.is_le
            for h in range(4):
                kb.op("dve", lambda h=h, m4=m4, op=op: nc.vector.tensor_single_scalar(
                    m4[0:64, h, :], io[0:64, 0:64], 0.0, op=op), reads=[io.d], writes=[m4.d])
                kb.op("dve", lambda h=h, m4=m4, op=op: nc.vector.tensor_single_scalar(
                    m4[64:128, h, :], io[64:128, 0:64], -64.0, op=op), reads=[io.d], writes=[m4.d])
            mk.append(m4)
        qt = [T(kb, es, [128, 2, NT], BF16, f"qtl{z}") for z in range(2)]
        kt = [T(kb, es, [128, 2, NT], BF16, f"ktl{z}") for z in range(2)]
        elast = [T(kb, es, [128, 2, NCH], F32, f"elast{z}") for z in range(2)]
        if STOP == 'c0':
            return
        with ExitStack() as es2:
            mask = T(kb, es2, [128, NT + 1], BF16, "cmask")
            kb.op("pool", lambda: nc.gpsimd.memset(mask[:], 1.0), writes=[mask.d])
            kb.op("pool", lambda: nc.gpsimd.memset(mask[:, 0:NT + 1:64], 0.0), writes=[mask.d])
            lrtz = T(kb, es2, [16, NT], F32, "lrt")
            A = T(kb, es2, [128, NT], F32, "scrA")
            B = T(kb, es2, [128, NT], F32, "scrB")
            raw = [T(kb, es2, [128, NT], BF16, f"raw{i}") for i in range(2)]
            for z in range(2):
                kb.dma("sp", lambda z=z: nc.sync.dma_start(out=lrtz[:], in_=S.lrT[z, :, :]),
                       reads=[S.lrT_d], writes=[lrtz.d])
                for pt in range(2):
                    if STOP == 'c1a' and (z, pt) != (0, 0):
                        continue
                    for t0 in range(0, NT, 512):
                        n = min(512, NT - t0)
                        ps = next_ps(g)
                        kb.op("pe", lambda t0=t0, n=n, ps=ps: nc.tensor.matmul(
                            ps[:, 0:n], lhsT=lup[:, z, pt * 128:(pt + 1) * 128], rhs=lrtz[:, t0:t0 + n],
                            start=True, stop=True), reads=[lup.d, lrtz.d], writes=[ps.d])
                        kb.op("act", lambda t0=t0, n=n, ps=ps: nc.scalar.activation(
                            A[:, t0:t0 + n], ps[:, 0:n], ACT.Exp, bias=nb[:, z, pt:pt + 1], scale=-1.0),
                            reads=[ps.d, nb.d], writes=[A.d])
                    ckpt("k1")
                    kb.op("act", lambda: nc.scalar.activation(A[:], A[:], ACT.Ln, bias=1.0, scale=1.0),
                          reads=[A.d], writes=[A.d])
                    ckpt("k2")
                    if z == 0:
                        kb.op("dve", lambda: nc.vector.tensor_tensor_scan(B[:], mask[:, 0:NT], A[:], 0.0,
                                                                          ALU.mult, ALU.add),
                              reads=[mask.d, A.d], writes=[B.d])
                    else:
                        kb.op("dve", lambda: nc.vector.tensor_tensor_scan(B[:, NT - 1::-1] if False else B[:, ::-1],
                                                                          mask[:, NT:0:-1], A[:, ::-1], 0.0,
                                                                          ALU.mult, ALU.add),
                              reads=[mask.d, A.d], writes=[B.d])
                    ckpt("k3")
                    kb.op("act", lambda: nc.scalar.activation(A[:], B[:], ACT.Exp, scale=-1.0 / 16.0),
                          reads=[B.d], writes=[A.d])
                    kb.op("act", lambda: nc.scalar.activation(B[:], B[:], ACT.Exp, scale=1.0 / 16.0),
                          reads=[B.d], writes=[B.d])
                    ckpt("k4")
                    rq, rk = raw
                    kb.dma("sp", lambda: nc.sync.dma_start(out=rq[:], in_=S.qT[pt * 128:(pt + 1) * 128, :]),
                           reads=[S.qT_d], writes=[rq.d])
                    kb.dma("sp", lambda: nc.sync.dma_start(out=rk[:], in_=S.kT[pt * 128:(pt + 1) * 128, :]),
                           reads=[S.kT_d], writes=[rk.d])
                    ckpt("k5")
                    kb.op("dve", lambda: nc.vector.scalar_tensor_tensor(qt[z][:, pt, :], rq[:], 0.125, A[:],
                                                                        op0=ALU.mult, op1=ALU.mult),
                          reads=[rq.d, A.d], writes=[qt[z].d])
                    ckpt("k6")
                    kb.op("pool", lambda: nc.gpsimd.tensor_tensor(kt[z][:, pt, :], rk[:], B[:], op=ALU.mult),
                          reads=[rk.d, B.d], writes=[kt[z].d])
                    ckpt("k7")
                    e0 = 63 if z == 0 else 0
                    kb.op("dve", lambda: nc.vector.tensor_copy(elast[z][:, pt, :], A[:, e0:NT:64]),
                          reads=[A.d], writes=[elast[z].d])
        kb.barrier()
        if STOP == 'c1':
            return
        vt = T(kb, es, [128, NT // 128, 512], BF16, "vt")
        kb.dma("act", lambda: nc.scalar.dma_start(out=vt[:], in_=S.v.ap().rearrange("(n p) e -> p n e", p=128)),
               reads=[S.v_d], writes=[vt.d])
        ktm = [T(kb, es, [128, NT // 128, 256], BF16, f"ktm{z}") for z in range(2)]
        oT = T(kb, es, [128, 4, NL], BF16, "oT", nd=NL // 64)
        for z in range(2):
            for n2 in range(0, NT // 128, 2):
                for a in range(2):
                    for pt in range(2):
                        kb.op("pe", lambda a=a, pt=pt, n2=n2: nc.tensor.transpose(
                            psb[:, (a * 2 + pt) * 128:(a * 2 + pt + 1) * 128],
                            kt[z][:, pt, (n2 + a) * 128:(n2 + a + 1) * 128], g.identb[:]),
                            reads=[kt[z].d, g.identb.d], writes=[psb.d])
                kb.op("act", lambda n2=n2: nc.scalar.copy(
                    ktm[z][:, n2:n2 + 2, :], psb[:, 0:512].rearrange("p (a f) -> p a f", a=2)),
                    reads=[psb.d], writes=[ktm[z].d])
        if "qtl" in g.dbg:
            dbg_out(g, "qtl0", qt[0][:], [qt[0].d], [128, 2, NT], BF16)
            dbg_out(g, "ktl1", kt[1][:], [kt[1].d], [128, 2, NT], BF16)
            dbg_out(g, "ktm1", ktm[1][:], [ktm[1].d], [128, NT // 128, 256], BF16)
            dbg_out(g, "elast1", elast[1][:], [elast[1].d], [128, 2, NCH])
        if STOP == 'c2':
            return
        Sst = [T(kb, es, [128, 2, 128], F32, f"Sst{z}") for z in range(2)]
        SbfZ = [T(kb, es, [128, 4, 128], BF16, f"SbfZ{z}") for z in range(2)]
        tmpS = [T(kb, es, [128, 2, 128], F32, f"tmpS{z}") for z in range(2)]
        smZ = [[T(kb, es, [128, 4, 64], BF16, f"smZ{z}_{i}") for i in range(2)] for z in range(2)]
        for z in range(2):
            kb.op("pool", lambda z=z: nc.gpsimd.memset(Sst[z][:], 0.0), writes=[Sst[z].d])
            kb.op("pool", lambda z=z: nc.gpsimd.memset(SbfZ[z][:], 0.0), writes=[SbfZ[z].d])
            for i in range(2):
                kb.op("pool", lambda z=z, i=i: nc.gpsimd.memset(smZ[z][i][:], 0.0), writes=[smZ[z][i].d])
        order = [list(range(NCH)), [3, 2, 1, 0] + list(range(NCH - 1, 3, -1))]
        written = set()
        for i in range(NCH):
            for z in range(2):
                n = order[z][i]
                t0 = 64 * n
                nt = n // 2
                jo = (n % 2) * 64
                if n >= 4:
                    smt = smZ[z][n % 2]
                    for par in range(2):
                        ho = par * 64
                        ps_s = next_ps(g)
                        for hh in range(2):
                            h = hh * 2 + par
                            pt = hh
                            kb.op("pe", lambda h=h, pt=pt, ho=ho, ps_s=ps_s, hh=hh: nc.tensor.matmul(
                                ps_s[jo:jo + 64, hh * 64:(hh + 1) * 64], lhsT=kt[z][ho:ho + 64, pt, t0:t0 + 64],
                                rhs=qt[z][ho:ho + 64, pt, t0:t0 + 64], start=True, stop=True),
                                reads=[kt[z].d, qt[z].d], writes=[ps_s.d])
                        kb.op("dve", lambda ps_s=ps_s, smt=smt, par=par: nc.vector.tensor_tensor(
                            smt[jo:jo + 64, par:4:2, :], ps_s[jo:jo + 64, 0:128].rearrange("p (h i) -> p h i", h=2),
                            mk[z][jo:jo + 64, 0:2, :], op=ALU.mult), reads=[ps_s.d, mk[z].d], writes=[smt.d])
                    ps_o = next_ps(g)
                    for h in range(4):
                        pt = h // 2
                        kb.op("pe", lambda h=h, ps_o=ps_o, smt=smt: nc.tensor.matmul(
                            ps_o[:, h * 64:(h + 1) * 64], lhsT=vt[:, nt, h * 128:(h + 1) * 128],
                            rhs=smt[:, h, :], start=True, stop=False),
                            reads=[vt.d, smt.d], writes=[ps_o.d])
                        kb.op("pe", lambda h=h, pt=pt, ps_o=ps_o: nc.tensor.matmul(
                            ps_o[:, h * 64:(h + 1) * 64], lhsT=SbfZ[z][:, h, :],
                            rhs=qt[z][:, pt, t0:t0 + 64], start=False, stop=True),
                            reads=[SbfZ[z].d, qt[z].d], writes=[ps_o.d])
                    tl = t0 - NCX
                    od = oT.ds[tl // 64]
                    osl = oT[:, :, tl:tl + 64]
                    pv = ps_o[:, 0:256].rearrange("p (h i) -> p h i", h=4)
                    if n not in written:
                        written.add(n)
                        kb.op("act", lambda osl=osl, pv=pv: nc.scalar.copy(osl, pv), reads=[ps_o.d], writes=[od])
                    else:
                        kb.op("dve", lambda osl=osl, pv=pv: nc.vector.tensor_tensor(osl, pv, osl, op=ALU.add),
                              reads=[ps_o.d, od], writes=[od])
                ps_kv = next_ps(g)
                for h in range(4):
                    pt, ho = h // 2, (h % 2) * 64
                    kb.op("pe", lambda h=h, pt=pt, ho=ho, ps_kv=ps_kv: nc.tensor.matmul(
                        ps_kv[ho:ho + 64, pt * 128:(pt + 1) * 128], lhsT=ktm[z][jo:jo + 64, nt, h * 64:(h + 1) * 64],
                        rhs=vt[jo:jo + 64, nt, h * 128:(h + 1) * 128], start=True, stop=True),
                        reads=[ktm[z].d, vt.d], writes=[ps_kv.d])
                kb.op("dve", lambda ps_kv=ps_kv: nc.vector.tensor_tensor(
                    tmpS[z][:], ps_kv[:, 0:256].rearrange("p (t e) -> p t e", t=2), Sst[z][:], op=ALU.add),
                    reads=[ps_kv.d, Sst[z].d], writes=[tmpS[z].d])
                kb.op("dve", lambda n=n: nc.vector.tensor_tensor(
                    Sst[z][:], tmpS[z][:], elast[z][:, :, n:n + 1].to_broadcast([128, 2, 128]), op=ALU.mult),
                    reads=[tmpS[z].d, elast[z].d], writes=[Sst[z].d])
                for par in range(2):
                    ho = par * 64
                    kb.op("act", lambda par=par, ho=ho: nc.scalar.copy(SbfZ[z][ho:ho + 64, par:4:2, :],
                                                                       Sst[z][ho:ho + 64, :, :]),
                          reads=[Sst[z].d], writes=[SbfZ[z].d])
        if "oT" in g.dbg:
            dbg_out(g, "oT", oT[:], oT.ds, [128, 4, NL], BF16)
        if STOP == 'c3':
            return
        sq = T(kb, es, [128, 512], BF16, "gsq")
        rstd = T(kb, es, [128, 512], F32, "grstd")
        rt = [T(kb, es, [128, 4, 512], BF16, f"grt{i}") for i in range(2)]
        gl = [T(kb, es, [128, 4, 512], BF16, f"ggl{i}") for i in range(2)]
        tmpb = T(kb, es, [128, 512], BF16, "gtmp")
        for sp in range(NL // 512):
            c0 = sp * 512
            r_t, g_t = rt[sp % 2], gl[sp % 2]
            kb.dma("sp", lambda r_t=r_t, c0=c0: nc.sync.dma_start(
                out=r_t[:], in_=S.rT[:, c0:c0 + 512].rearrange("(m p) t -> p m t", p=128)),
                reads=[S.rT_d], writes=[r_t.d])
            kb.op("act", lambda r_t=r_t: nc.scalar.activation(r_t[:], r_t[:], ACT.Silu), reads=[r_t.d], writes=[r_t.d])
            ods = oT.ds[c0 // 64:(c0 + 512) // 64]
            for h in range(4):
                kb.op("dve", lambda h=h: nc.vector.tensor_tensor(sq[:], oT[:, h, c0:c0 + 512], oT[:, h, c0:c0 + 512],
                                                                 op=ALU.mult), reads=ods, writes=[sq.d])
                ps = next_ps(g)
                kb.op("pe", lambda ps=ps: nc.tensor.matmul(ps[:, :], lhsT=ones[:], rhs=sq[:], start=True, stop=True),
                      reads=[ones.d, sq.d], writes=[ps.d])
                kb.op("dve", lambda ps=ps: nc.vector.tensor_scalar(rstd[:], ps[:, :], 1.0 / 128.0, EPS,
                                                                   op0=ALU.mult, op1=ALU.add),
                      reads=[ps.d], writes=[rstd.d])
                kb.op("act", lambda: nc.scalar.activation(rstd[:], rstd[:], ACT.Sqrt), reads=[rstd.d], writes=[rstd.d])
                kb.op("dve", lambda: nc.vector.reciprocal(rstd[:], rstd[:]), reads=[rstd.d], writes=[rstd.d])
                kb.op("dve", lambda h=h: nc.vector.scalar_tensor_tensor(
                    tmpb[:], oT[:, h, c0:c0 + 512], nw[:, 0:1], rstd[:], op0=ALU.mult, op1=ALU.mult),
                    reads=ods + [nw.d, rstd.d], writes=[tmpb.d])
                kb.op("pool", lambda h=h, g_t=g_t, r_t=r_t: nc.gpsimd.tensor_tensor(
                    g_t[:, h, :], tmpb[:], r_t[:, h, :], op=ALU.mult), reads=[tmpb.d, r_t.d], writes=[g_t.d])
            kb.dma("act", lambda g_t=g_t, c0=c0: nc.scalar.dma_start(
                out=S.glaT[:, c0:c0 + 512].rearrange("(m p) t -> p m t", p=128), in_=g_t[:]),
                reads=[g_t.d], writes=[S.glaT_d])


def cmul(g, out_r, out_i, ar, ai, br, bi, tmp, deps_in, dep_out, sl=None):
    nc, kb = g.nc, g.kb
    kb.op("dve", lambda: nc.vector.tensor_tensor(tmp, ai, bi, op=ALU.mult), reads=deps_in, writes=[dep_out])
    kb.op("dve", lambda: nc.vector.tensor_tensor(out_r, ar, br, op=ALU.mult), reads=deps_in, writes=[dep_out])
    kb.op("dve", lambda: nc.vector.tensor_tensor(out_r, out_r, tmp, op=ALU.subtract), reads=[dep_out], writes=[dep_out])
    kb.op("dve", lambda: nc.vector.tensor_tensor(tmp, ai, br, op=ALU.mult), reads=deps_in, writes=[dep_out])
    kb.op("dve", lambda: nc.vector.tensor_tensor(out_i, ar, bi, op=ALU.mult), reads=deps_in, writes=[dep_out])
    kb.op("dve", lambda: nc.vector.tensor_tensor(out_i, out_i, tmp, op=ALU.add), reads=[dep_out], writes=[dep_out])


def phase_d(g):
    nc, kb, H, S = g.nc, g.kb, g.H, g.S
    NCK = NT // 8
    NMAC = NCK // 16
    io = g.iota
    with ExitStack() as es:
        psb = g.psb[0]
        pd = Dep("s5par")
        P0 = T(kb, es, [128, 24, 64], F32, "s5p0")
        pl = lambda i: P0[:, i, :]
        LRE, LIM, DT, MAG, CS, SN, T1, T2, T3, LBR, LBI, CR, CI, IR, II = range(15)
        for half in range(2):
            rows = slice(half * 64, half * 64 + 64)
            kb.dma("sp", lambda rows=rows: nc.sync.dma_start(
                out=P0[rows, LRE, :], in_=H.s5_lam_re[0].rearrange("z g p -> p (z g)"),
                allow_slow_non_contiguous=True), writes=[pd])
            kb.dma("act", lambda rows=rows: nc.scalar.dma_start(
                out=P0[rows, LIM, :], in_=H.s5_lam_im[0].rearrange("z g p -> p (z g)"),
                allow_slow_non_contiguous=True), writes=[pd])
        kb.dma("sp", lambda: nc.sync.dma_start(
            out=pl(DT), in_=H.s5_log_dt[0:1, :, :].rearrange("o z g -> o (z g)").partition_broadcast(128)),
            writes=[pd])
        D1 = [pd]

        def v(fn):
            kb.op("dve", fn, reads=D1, writes=D1)

        def a(fn):
            kb.op("act", fn, reads=D1, writes=D1)

        a(lambda: nc.scalar.activation(pl(DT), pl(DT), ACT.Exp))
        v(lambda: nc.vector.tensor_tensor(pl(MAG), pl(LRE), pl(DT), op=ALU.mult))
        a(lambda: nc.scalar.activation(pl(MAG), pl(MAG), ACT.Exp))
        v(lambda: nc.vector.tensor_tensor(pl(T1), pl(LIM), pl(DT), op=ALU.mult))
        a(lambda: nc.scalar.activation(pl(SN), pl(T1), ACT.Sin, scale=1.0 / 16.0))
        a(lambda: nc.scalar.activation(pl(CS), pl(T1), ACT.Sin, bias=float(np.pi / 2), scale=1.0 / 16.0))
        for _ in range(4):
            v(lambda: nc.vector.tensor_tensor(pl(T2), pl(CS), pl(CS), op=ALU.mult))
            v(lambda: nc.vector.tensor_tensor(pl(T3), pl(SN), pl(SN), op=ALU.mult))
            v(lambda: nc.vector.scalar_tensor_tensor(pl(SN), pl(CS), 2.0, pl(SN), op0=ALU.mult, op1=ALU.mult))
            v(lambda: nc.vector.tensor_tensor(pl(CS), pl(T2), pl(T3), op=ALU.subtract))
        v(lambda: nc.vector.tensor_tensor(pl(LBR), pl(MAG), pl(CS), op=ALU.mult))
        v(lambda: nc.vector.tensor_tensor(pl(LBI), pl(MAG), pl(SN), op=ALU.mult))
        v(lambda: nc.vector.tensor_tensor(pl(T1), pl(LRE), pl(LRE), op=ALU.mult))
        v(lambda: nc.vector.tensor_tensor(pl(T2), pl(LIM), pl(LIM), op=ALU.mult))
        v(lambda: nc.vector.tensor_tensor(pl(T1), pl(T1), pl(T2), op=ALU.add))
        v(lambda: nc.vector.reciprocal(pl(T1), pl(T1)))
        v(lambda: nc.vector.tensor_scalar(pl(T2), pl(LBR), -1.0, None, op0=ALU.add))
        v(lambda: nc.vector.tensor_tensor(pl(CR), pl(T2), pl(LRE), op=ALU.mult))
        v(lambda: nc.vector.tensor_tensor(pl(T3), pl(LBI), pl(LIM), op=ALU.mult))
        v(lambda: nc.vector.tensor_tensor(pl(CR), pl(CR), pl(T3), op=ALU.add))
        v(lambda: nc.vector.tensor_tensor(pl(CR), pl(CR), pl(T1), op=ALU.mult))
        v(lambda: nc.vector.tensor_tensor(pl(CI), pl(LBI), pl(LRE), op=ALU.mult))
        v(lambda: nc.vector.tensor_tensor(pl(T3), pl(T2), pl(LIM), op=ALU.mult))
        v(lambda: nc.vector.tensor_tensor(pl(CI), pl(CI), pl(T3), op=ALU.subtract))
        v(lambda: nc.vector.tensor_tensor(pl(CI), pl(CI), pl(T1), op=ALU.mult))
        v(lambda: nc.vector.tensor_tensor(pl(T1), pl(LBR), pl(LBR), op=ALU.mult))
        v(lambda: nc.vector.tensor_tensor(pl(T2), pl(LBI), pl(LBI), op=ALU.mult))
        v(lambda: nc.vector.tensor_tensor(pl(T1), pl(T1), pl(T2), op=ALU.add))
        v(lambda: nc.vector.reciprocal(pl(T1), pl(T1)))
        v(lambda: nc.vector.tensor_tensor(pl(IR), pl(LBR), pl(T1), op=ALU.mult))
        v(lambda: nc.vector.scalar_tensor_tensor(pl(II), pl(LBI), -1.0, pl(T1), op0=ALU.mult, op1=ALU.mult))
        PW = T(kb, es, [128, 9, 2, 64], F32, "s5pw")
        NW = T(kb, es, [128, 8, 2, 64], F32, "s5nw")
        for W, br_, bi_, n in ((PW, LBR, LBI, 9), (NW, IR, II, 8)):
            v(lambda W=W: nc.vector.memset(W[:, 0, 0, :], 1.0))
            v(lambda W=W: nc.vector.memset(W[:, 0, 1, :], 0.0))
            for k in range(1, n):
                cmul(g, W[:, k, 0, :], W[:, k, 1, :], W[:, k - 1, 0, :], W[:, k - 1, 1, :], pl(br_), pl(bi_),
                     pl(T3), D1, pd)
        P128 = T(kb, es, [128, 2, 64], F32, "s5p128")
        v(lambda: nc.vector.tensor_copy(P128[:], PW[:, 8, :, :]))
        for _ in range(4):
            cmul(g, pl(T1), pl(T2), P128[:, 0, :], P128[:, 1, :], P128[:, 0, :], P128[:, 1, :], pl(T3), D1, pd)
            v(lambda: nc.vector.tensor_copy(P128[:, 0, :], pl(T1)))
            v(lambda: nc.vector.tensor_copy(P128[:, 1, :], pl(T2)))
        WN = T(kb, es, [128, 8, 2, 64], F32, "s5wn")
        WP = T(kb, es, [128, 8, 2, 64], F32, "s5wp")
        for k in range(8):
            cmul(g, WN[:, k, 0, :], WN[:, k, 1, :], NW[:, k, 0, :], NW[:, k, 1, :], pl(CR), pl(CI), pl(T3), D1, pd)
            cmul(g, WP[:, k, 0, :], WP[:, k, 1, :], PW[:, k, 0, :], PW[:, k, 1, :], pl(CR), pl(CI), pl(T3), D1, pd)
        v(lambda: nc.vector.tensor_scalar(PW[64:128, :, 0, :], PW[64:128, :, 0, :], -1.0, None, op0=ALU.mult))
        v(lambda: nc.vector.tensor_scalar(WN[0:64, :, 1, :], WN[0:64, :, 1, :], -1.0, None, op0=ALU.mult))
        v(lambda: nc.vector.tensor_scalar(WP[0:64, :, 1, :], WP[0:64, :, 1, :], -1.0, None, op0=ALU.mult))
        A8s = T(kb, es, [128, 2, 64], F32, "s5a8")
        v(lambda: nc.vector.tensor_copy(A8s[:, 1, :], PW[:, 8, 1, :]))
        v(lambda: nc.vector.tensor_copy(A8s[0:64, 0, :], PW[0:64, 8, 0, :]))
        v(lambda: nc.vector.tensor_scalar(A8s[64:128, 0, :], PW[64:128, 8, 0, :], -1.0, None, op0=ALU.mult))
        Jt = T(kb, es, [128, 128], F32, "s5jt")
        v(lambda: nc.vector.tensor_single_scalar(Jt[:], io[:], 64.0, op=ALU.is_equal))
        v(lambda: nc.vector.tensor_single_scalar(pl(T1)[:, 0:64], io[:, 0:64], -64.0, op=ALU.is_equal))
        v(lambda: nc.vector.tensor_tensor(Jt[:, 0:64], Jt[:, 0:64], pl(T1)[:, 0:64], op=ALU.subtract))
        bm = []
        for z in range(2):
            m = T(kb, es, [128, 128], F32, f"s5bm{z}")
            v(lambda m=m: nc.vector.memset(m[:], 0.0))
            for j in range(0, 8, 2):
                for jj in range(2):
                    pass
            bm.append(m)
        rowb = T(kb, es, [128, 1], F32, "s5rowb")
        pcol = T(kb, es, [128, 1], F32, "s5pcol")
        kb.op("pool", lambda: nc.gpsimd.iota(pcol[:], pattern=[[0, 1]], base=0, channel_multiplier=1,
                                              allow_small_or_imprecise_dtypes=True), writes=[pd])
        v(lambda: nc.vector.memset(rowb[:], 0.0))
        for t in range(1, 8):
            v(lambda t=t: nc.vector.tensor_scalar(pl(T1)[:, 0:1], pcol[:], float(16 * t), 16.0, op0=ALU.is_ge, op1=ALU.mult))
            v(lambda: nc.vector.tensor_tensor(rowb[:], rowb[:], pl(T1)[:, 0:1], op=ALU.add))
        colf = T(kb, es, [128, 128], F32, "s5colf")
        kb.op("pool", lambda: nc.gpsimd.iota(colf[:], pattern=[[1, 128]], base=0, channel_multiplier=0,
                                              allow_small_or_imprecise_dtypes=True), writes=[pd])
        v(lambda: nc.vector.tensor_scalar(bm[0][:], colf[:], rowb[:, 0:1], 0.0, op0=ALU.subtract, op1=ALU.is_ge))
        v(lambda: nc.vector.tensor_scalar(bm[1][:], colf[:], rowb[:, 0:1], 15.0, op0=ALU.subtract, op1=ALU.is_le))
        CC = T(kb, es, [128, 64, 16], F32, "s5cc")
        CCs = T(kb, es, [128, 64, 16], F32, "s5ccs")
        BB = T(kb, es, [128, 64, 16], F32, "s5bb")
        BBs = T(kb, es, [128, 64, 16], F32, "s5bbs")
        kb.dma("sp", lambda: nc.sync.dma_start(out=BB[0:64, :, :], in_=H.s5_b_re[0].rearrange("z g p h -> p (z g) h")),
               writes=[pd])
        kb.dma("act", lambda: nc.scalar.dma_start(out=BB[64:128, :, :], in_=H.s5_b_im[0].rearrange("z g p h -> p (z g) h")),
               writes=[pd])
        kb.dma("sp", lambda: nc.sync.dma_start(out=BBs[0:64, :, :], in_=H.s5_b_im[0].rearrange("z g p h -> p (z g) h")),
               writes=[pd])
        kb.dma("act", lambda: nc.scalar.dma_start(out=BBs[64:128, :, :], in_=H.s5_b_re[0].rearrange("z g p h -> p (z g) h")),
               writes=[pd])
        with ExitStack() as esx:
            xc = [T(kb, esx, [128, 8, 128], F32, f"s5xc{i}") for i in range(2)]
            for i, (aa, bb) in enumerate(((H.s5_c_re, H.s5_c_im), (H.s5_c_im, H.s5_c_re))):
                kb.dma("sp", lambda aa=aa, i=i: nc.sync.dma_start(
                    out=xc[i][:, :, 0:64], in_=aa[0].rearrange("z g h p -> (z g h) p").rearrange("(t r) p -> r t p", r=128)),
                    writes=[pd])
                kb.dma("act", lambda bb=bb, i=i: nc.scalar.dma_start(
                    out=xc[i][:, :, 64:128], in_=bb[0].rearrange("z g h p -> (z g h) p").rearrange("(t r) p -> r t p", r=128)),
                    writes=[pd])
            for i, dst in enumerate((CC, CCs)):
                for t in range(8):
                    ps = next_ps(g)
                    kb.op("pe", lambda t=t, i=i, ps=ps: nc.tensor.transpose(ps[:, 0:128], xc[i][:, t, :], g.ident[:]),
                          reads=[pd, g.ident.d], writes=[ps.d])
                    kb.op("act", lambda t=t, dst=dst, ps=ps: nc.scalar.copy(
                        dst[:, t * 8:(t + 1) * 8, :], ps[:, 0:128].rearrange("p (a h) -> p a h", a=8)),
                        reads=[ps.d], writes=[pd])
            kb.barrier()
        with ExitStack() as esu:
            U8 = [T(kb, esu, [128, 8, 512], BF16, f"s5u8_{i}") for i in range(2)]
            U8g = [T(kb, esu, [128, 32, 128], BF16, f"s5u8g_{i}") for i in range(2)]
            utst = [T(kb, esu, [128, 4, 128], BF16, f"s5utst_{i}") for i in range(2)]
            blocks = [(0, 32)] + [(32 + 128 * b, 128) for b in range(4)]
            for bi_, (c0, ncb) in enumerate(blocks):
                u8, u8g = U8[bi_ % 2], U8g[bi_ % 2]
                kb.dma("sp", lambda u8=u8, c0=c0, ncb=ncb: nc.sync.dma_start(
                    out=u8[0:ncb, :, :], in_=S.u[c0 * 8:(c0 + ncb) * 8, :].rearrange("(c j) f -> c j f", j=8)),
                    reads=[S.u_d], writes=[u8.d])
                kb.op("pool", lambda u8=u8, u8g=u8g, ncb=ncb: nc.gpsimd.tensor_copy(
                    u8g[0:ncb, :, :].rearrange("c g (j h) -> c g j h", j=8),
                    u8[0:ncb, :, :].rearrange("c j (g h) -> c g j h", g=32)), reads=[u8.d], writes=[u8g.d])
                for g4 in range(0, 32, 4):
                    for gg in range(4):
                        kb.op("pe", lambda gg=gg, g4=g4, u8g=u8g, ncb=ncb: nc.tensor.transpose(
                            psb[:, gg * 128:gg * 128 + ncb], u8g[0:ncb, g4 + gg, :], g.identb[0:ncb, 0:ncb]),
                            reads=[u8g.d, g.identb.d], writes=[psb.d])
                    ust = utst[(g4 // 4) % 2]
                    kb.op("act", lambda g4=g4, c0=c0, ncb=ncb, ust=ust: nc.scalar.copy(
                        ust[:, :, 0:ncb], psb[:, 0:512].rearrange("p (a c) -> p a c", a=4)[:, :, 0:ncb]),
                        reads=[psb.d], writes=[ust.d])
                    kb.dma("act", lambda g4=g4, c0=c0, ncb=ncb, ust=ust: nc.scalar.dma_start(
                        out=S.Ut[g4:g4 + 4, :, c0:c0 + ncb].rearrange("a p c -> p a c"), in_=ust[:, :, 0:ncb]),
                        reads=[ust.d], writes=[S.Ut_d])
            kb.barrier()
        for z in range(2):
            gs = slice(z * 32, z * 32 + 32)
            with ExitStack() as ez:
                MT = T(kb, ez, [128, 32, 128], BF16, "s5mt")
                RT = T(kb, ez, [128, 32, 128], BF16, "s5rt")
                OTb = T(kb, ez, [128, 32, 128], BF16, "s5otb")
                A8T = T(kb, ez, [128, 32, 128], F32, "s5a8t")
                with ExitStack() as ep:
                    Gall = T(kb, ep, [128, 32, 9, 16], F32, "s5gall")
                    Kn = T(kb, ep, [128, 32, 8, 16], F32, "s5kn")
                    Kp = T(kb, ep, [128, 32, 8, 16], F32, "s5kp")
                    tmpk = T(kb, ep, [128, 32, 16], F32, "s5tmpk")
                    bc = lambda ap2: ap2.unsqueeze(2).to_broadcast([128, 32, 16])
                    for k in range(9):
                        i = k if z == 0 else 8 - k
                        v(lambda k=k, i=i: nc.vector.tensor_tensor(Gall[:, :, i, :], CC[:, gs, :], bc(PW[:, k, 0, gs]),
                                                                  op=ALU.mult))
                        v(lambda k=k: nc.vector.tensor_tensor(tmpk[:], CCs[:, gs, :], bc(PW[:, k, 1, gs]), op=ALU.mult))
                        v(lambda i=i: nc.vector.tensor_tensor(Gall[:, :, i, :], Gall[:, :, i, :], tmpk[:], op=ALU.subtract))
                    for k in range(8):
                        j = k if z == 0 else 7 - k
                        v(lambda k=k, j=j: nc.vector.tensor_tensor(Kn[:, :, j, :], BB[:, gs, :], bc(WN[:, k, 0, gs]),
                                                                  op=ALU.mult))
                        v(lambda k=k: nc.vector.tensor_tensor(tmpk[:], BBs[:, gs, :], bc(WN[:, k, 1, gs]), op=ALU.mult))
                        v(lambda j=j: nc.vector.tensor_tensor(Kn[:, :, j, :], Kn[:, :, j, :], tmpk[:], op=ALU.add))
                        j2 = 7 - k if z == 0 else k
                        v(lambda k=k, j2=j2: nc.vector.tensor_tensor(Kp[:, :, j2, :], BB[:, gs, :], bc(WP[:, k, 0, gs]),
                                                                    op=ALU.mult))
                        v(lambda k=k: nc.vector.tensor_tensor(tmpk[:], BBs[:, gs, :], bc(WP[:, k, 1, gs]), op=ALU.mult))
                        v(lambda j2=j2: nc.vector.tensor_tensor(Kp[:, :, j2, :], Kp[:, :, j2, :], tmpk[:], op=ALU.add))
                    q0 = 0 if z == 0 else 1
                    o0 = 1 if z == 0 else 0
                    for gi in range(32):
                        ps = next_ps(g)
                        kb.op("pe", lambda gi=gi, ps=ps: nc.tensor.matmul(
                            ps[:, 0:128], lhsT=Kn[:, gi, :, :].rearrange("p j h -> p (j h)"),
                            rhs=Gall[:, gi, q0:q0 + 8, :].rearrange("p s h -> p (s h)"), start=True, stop=True),
                            reads=D1, writes=[ps.d])
                        kb.op("dve", lambda gi=gi, ps=ps: nc.vector.tensor_tensor(MT[:, gi, :], ps[:, 0:128], bm[z][:],
                                                                                 op=ALU.mult),
                              reads=[ps.d] + D1, writes=[MT.d])
                        ps2 = next_ps(g)
                        kb.op("pe", lambda gi=gi, ps2=ps2: nc.tensor.transpose(
                            ps2[:, 0:128], Kp[:, gi, :, :].rearrange("p j h -> p (j h)"), g.ident[:]),
                            reads=D1 + [g.ident.d], writes=[ps2.d])
                        kb.op("act", lambda gi=gi, ps2=ps2: nc.scalar.copy(RT[:, gi, :], ps2[:, 0:128]),
                              reads=[ps2.d], writes=[RT.d])
                        kb.op("act", lambda gi=gi: nc.scalar.copy(
                            OTb[:, gi, :], Gall[:, gi, o0:o0 + 8, :].rearrange("p s h -> p (s h)")),
                            reads=D1, writes=[OTb.d])
                        kb.op("pool", lambda gi=gi: nc.gpsimd.tensor_scalar(
                            A8T[:, gi, :], g.ident[:], A8s[:, 0, z * 32 + gi:z * 32 + gi + 1], None, op0=ALU.mult),
                            reads=D1 + [g.ident.d], writes=[A8T.d])
                        kb.op("dve", lambda gi=gi: nc.vector.scalar_tensor_tensor(
                            A8T[:, gi, :], Jt[:], A8s[:, 1, z * 32 + gi:z * 32 + gi + 1], A8T[:, gi, :],
                            op0=ALU.mult, op1=ALU.add), reads=D1 + [A8T.d], writes=[A8T.d])
                    kb.barrier()
                if f"s5mat{z}" in g.dbg:
                    dbg_out(g, f"MT{z}", MT[:], [MT.d], [128, 32, 128], BF16)
                    dbg_out(g, f"RT{z}", RT[:], [RT.d], [128, 32, 128], BF16)
                    dbg_out(g, f"OTb{z}", OTb[:], [OTb.d], [128, 32, 128], BF16)
                    dbg_out(g, f"A8T{z}", A8T[:], [A8T.d], [128, 32, 128], F32)
                X = T(kb, ez, [128, 32, NCK], F32, "s5x", nd=32)
                utg = [T(kb, ez, [128, NCK], BF16, f"s5utg{i}") for i in range(3)]
                for gi in range(32):
                    ug = utg[gi % 3]
                    kb.dma("sp", lambda gi=gi, ug=ug: nc.sync.dma_start(out=ug[:], in_=S.Ut[gi, :, :]),
                           reads=[S.Ut_d], writes=[ug.d])
                    for (c0, n) in ((0, 512), (512, NCK - 512)):
                        ps = next_ps(g)
                        kb.op("pe", lambda gi=gi, c0=c0, n=n, ps=ps: nc.tensor.matmul(
                            ps[:, 0:n], lhsT=RT[:, gi, :], rhs=ug[:, c0:c0 + n], start=True, stop=True),
                            reads=[RT.d, ug.d], writes=[ps.d])
                        eng = "act" if gi % 2 == 0 else "dve"
                        if eng == "act":
                            kb.op("act", lambda gi=gi, c0=c0, n=n, ps=ps: nc.scalar.copy(X[:, gi, c0:c0 + n], ps[:, 0:n]),
                                  reads=[ps.d], writes=[X.ds[gi]])
                        else:
                            kb.op("dve", lambda gi=gi, c0=c0, n=n, ps=ps: nc.vector.tensor_copy(X[:, gi, c0:c0 + n], ps[:, 0:n]),
                                  reads=[ps.d], writes=[X.ds[gi]])
                cur = [T(kb, ez, [128, 32, NMAC], F32, f"s5cur{i}") for i in range(2)]
                Gm = T(kb, ez, [128, 32, NMAC], F32, "s5gm")
                gv = T(kb, ez, [128, 32], F32, "s5gv")
                tu = T(kb, ez, [128, 32], F32, "s5tu")
                tt = T(kb, ez, [128, 32], F32, "s5tt")
                xall = X.ds
                colsel = (lambda i: i) if z == 0 else (lambda i: 15 - i)
                gbanks = ((0, 15), (15, 30), (30, 32))

                def step(src, dst, i, store):
                    col = colsel(i)
                    pss = []
                    for (g0, g1) in gbanks:
                        ps = next_ps(g)
                        pss.append(ps)
                        for gi in range(g0, g1):
                            kb.op("pe", lambda gi=gi, ps=ps, g0=g0: nc.tensor.matmul(
                                ps[:, (gi - g0) * NMAC:(gi - g0 + 1) * NMAC], lhsT=A8T[:, gi, :], rhs=src[:, gi, :],
                                start=True, stop=True), reads=[A8T.d, src.d], writes=[ps.d])
                    for (g0, g1), ps in zip(gbanks, pss):
                        kb.op("dve", lambda g0=g0, g1=g1, ps=ps: nc.vector.tensor_tensor(
                            dst[:, g0:g1, :], ps[:, 0:(g1 - g0) * NMAC].rearrange("p (a m) -> p a m", m=NMAC),
                            X[:, g0:g1, col:NCK:16], op=ALU.add), reads=[ps.d] + xall, writes=[dst.d])
                    if store:
                        kb.op("pool", lambda: nc.gpsimd.tensor_copy(X[:, :, col:NCK:16], src[:]),
                              reads=[src.d] + xall, writes=xall)

                kb.op("pool", lambda: nc.gpsimd.memset(cur[0][:], 0.0), writes=[cur[0].d])
                for i in range(16):
                    step(cur[i % 2], cur[(i + 1) % 2], i, False)
                Em = cur[0]
                qorder = list(range(NMAC)) if z == 0 else [1, 0] + list(range(NMAC - 1, 1, -1))
                kb.op("pool", lambda: nc.gpsimd.memset(gv[:], 0.0), writes=[gv.d])
                for q in qorder:
                    kb.op("act", lambda q=q: nc.scalar.copy(Gm[:, :, q], gv[:]), reads=[gv.d], writes=[Gm.d])
                    ps = next_ps(g)
                    kb.op("pe", lambda ps=ps: nc.tensor.matmul(ps[:, 0:32], lhsT=Jt[:], rhs=gv[:], start=True, stop=True),
                          reads=D1 + [gv.d], writes=[ps.d])
                    kb.op("dve", lambda ps=ps: nc.vector.tensor_tensor(tt[:], ps[:, 0:32], P128[:, 1, gs], op=ALU.mult),
                          reads=[ps.d] + D1, writes=[tt.d])
                    kb.op("pool", lambda q=q: nc.gpsimd.tensor_tensor(tu[:], gv[:], P128[:, 0, gs], op=ALU.mult),
                          reads=[gv.d] + D1, writes=[tu.d])
                    kb.op("pool", lambda q=q: nc.gpsimd.tensor_tensor(tu[:], tu[:], Em[:, :, q], op=ALU.add),
                          reads=[tu.d, Em.d], writes=[tu.d])
                    kb.op("dve", lambda: nc.vector.tensor_tensor(gv[:], tu[:], tt[:], op=ALU.add),
                          reads=[tu.d, tt.d], writes=[gv.d])
                kb.op("dve", lambda: nc.vector.tensor_copy(cur[0][:], Gm[:]), reads=[Gm.d], writes=[cur[0].d])
                for i in range(16):
                    step(cur[i % 2], cur[(i + 1) % 2], i, True)
                if f"s5x{z}" in g.dbg:
                    dbg_out(g, f"X{z}", X[:], xall, [128, 32, NCK], F32)
                xb = [T(kb, ez, [128, 512], BF16, f"s5xb{i}") for i in range(2)]
                yo = [T(kb, ez, [128, 512], F32, f"s5yo{i}") for i in range(2)]
                yfl = [T(kb, ez, [128, 512], F32, f"s5yfl{i}") for i in range(2)]
                for gi in range(32):
                    xbt = xb[gi % 2]
                    ug = utg[gi % 3]
                    kb.dma("sp", lambda gi=gi, ug=ug: nc.sync.dma_start(out=ug[:], in_=S.Ut[gi, :, :]),
                           reads=[S.Ut_d], writes=[ug.d])
                    kb.op("act", lambda gi=gi, xbt=xbt: nc.scalar.copy(xbt[:], X[:, gi, 32:NCK]), reads=xall, writes=[xbt.d])
                    ps = next_ps(g)
                    kb.op("pe", lambda gi=gi, ps=ps, ug=ug: nc.tensor.matmul(ps[:, :], lhsT=MT[:, gi, :], rhs=ug[:, 32:NCK],
                                                                             start=True, stop=False),
                          reads=[MT.d, ug.d], writes=[ps.d])
                    kb.op("pe", lambda gi=gi, ps=ps, xbt=xbt: nc.tensor.matmul(ps[:, :], lhsT=OTb[:, gi, :], rhs=xbt[:],
                                                                               start=False, stop=True),
                          reads=[OTb.d, xbt.d], writes=[ps.d])
                    yt = yo[gi % 2]
                    if z == 0:
                        kb.op("dve", lambda ps=ps, yt=yt: nc.vector.tensor_copy(yt[:], ps[:, :]),
                              reads=[ps.d], writes=[yt.d])
                    else:
                        yf = yfl[gi % 2]
                        kb.dma("act", lambda gi=gi, yf=yf: nc.scalar.dma_start(out=yf[:], in_=S.y0[gi, :, :]),
                               reads=[S.y0_d], writes=[yf.d])
                        kb.op("dve", lambda ps=ps, yt=yt, yf=yf: nc.vector.tensor_tensor(yt[:], ps[:, :], yf[:], op=ALU.add),
                              reads=[ps.d, yf.d], writes=[yt.d])
                    kb.dma("sp", lambda gi=gi, yt=yt: nc.sync.dma_start(out=S.y0[gi, :, :], in_=yt[:]),
                           reads=[yt.d], writes=[S.y0_d])
                kb.barrier()


SEG = 512
NSEG = 64


def bc_tile(g, es, name, src_row_ap, n, reads=()):
    nc, kb = g.nc, g.kb
    t = T(kb, es, [128, n], F32, name)
    kb.dma("sp", lambda: nc.sync.dma_start(out=t[:], in_=src_row_ap.partition_broadcast(128)),
           reads=list(reads), writes=[t.d])
    return t


def phase_e(g):
    nc, kb, H, S = g.nc, g.kb, g.H, g.S
    R = g.R
    with ExitStack() as es:
        psb = g.psb[0]
        s5T = T(kb, es, [128, 4, NL], BF16, "s5T", nd=4)
        with ExitStack() as e1:
            glw = T(kb, e1, [128, 4, 512], BF16, "glw")
            kb.dma("pool", lambda: nc.gpsimd.dma_start(out=glw[:], in_=H.glu_w[0].rearrange("(k p) n -> p k n", p=128)),
                   writes=[glw.d])
            glb = T(kb, e1, [128, 4], F32, "glb")
            kb.dma("sp", lambda: nc.sync.dma_start(out=glb[:], in_=H.glu_b[0, :].rearrange("(k p) -> p k", p=128),
                                                   allow_slow_non_contiguous=True), writes=[glb.d])
            s5d = T(kb, e1, [128, 4], F32, "s5d")
            kb.dma("sp", lambda: nc.sync.dma_start(out=s5d[:], in_=H.s5_d[0, :].rearrange("(k p) -> p k", p=128),
                                                   allow_slow_non_contiguous=True), writes=[s5d.d])
            Yg = T(kb, e1, [128, 32, 128], F32, "Yg")
            Ytm = T(kb, e1, [128, 8, 512], F32, "Ytm")
            yT = T(kb, e1, [128, 4, 1024], F32, "yTt")
            uTt = T(kb, e1, [128, 4, 1024], BF16, "uTt")
            t1 = T(kb, e1, [128, 4, 1024], F32, "glt1")
            glg = T(kb, e1, [128, 4, 1024], BF16, "glg")
            sg = T(kb, e1, [128, 512], BF16, "glsg")
            for cb in range(4):
                kb.dma("sp", lambda cb=cb: nc.sync.dma_start(
                    out=Yg[:], in_=S.y0[:, :, cb * 128:(cb + 1) * 128].rearrange("g p c -> p g c")),
                    reads=[S.y0_d], writes=[Yg.d])
                kb.dma("act", lambda cb=cb: nc.scalar.dma_start(
                    out=uTt[:], in_=S.uT[:, NCX + cb * 1024:NCX + (cb + 1) * 1024].rearrange("(k p) t -> p k t", p=128)),
                    reads=[S.uT_d], writes=[uTt.d])
                for g4 in range(0, 32, 4):
                    ps = next_ps(g)
                    for gg in range(4):
                        kb.op("pe", lambda gg=gg, g4=g4, ps=ps: nc.tensor.transpose(
                            ps[:, gg * 128:(gg + 1) * 128], Yg[:, g4 + gg, :], g.ident[:]),
                            reads=[Yg.d, g.ident.d], writes=[ps.d])
                    kb.op("act", lambda g4=g4, ps=ps: nc.scalar.copy(
                        Ytm[:, :, g4 * 16:g4 * 16 + 64].rearrange("c s (a h) -> c a s h", a=4),
                        ps[:, :].rearrange("c (a s h) -> c a s h", a=4, s=8)), reads=[ps.d], writes=[Ytm.d])
                for s_ in range(8):
                    ps = next_ps(g)
                    for cc in range(4):
                        kb.op("pe", lambda cc=cc, s_=s_, ps=ps: nc.tensor.transpose(
                            ps[:, cc * 128:(cc + 1) * 128], Ytm[:, s_, cc * 128:(cc + 1) * 128], g.ident[:]),
                            reads=[Ytm.d, g.ident.d], writes=[ps.d])
                    kb.op("dve", lambda s_=s_, ps=ps: nc.vector.tensor_copy(
                        yT[:, :, s_:1024:8], ps[:, :].rearrange("p (a c) -> p a c", a=4)), reads=[ps.d], writes=[yT.d])
                for cc in range(4):
                    kb.op("dve", lambda cc=cc: nc.vector.scalar_tensor_tensor(
                        yT[:, cc, :], uTt[:, cc, :], s5d[:, cc:cc + 1], yT[:, cc, :], op0=ALU.mult, op1=ALU.add),
                        reads=[uTt.d, s5d.d, yT.d], writes=[yT.d])
                kb.op("pool", lambda: nc.gpsimd.tensor_tensor(t1[:], yT[:], yT[:], op=ALU.mult), reads=[yT.d], writes=[t1.d])
                kb.op("dve", lambda: nc.vector.tensor_scalar(t1[:], t1[:], 0.044715, 1.0, op0=ALU.mult, op1=ALU.add),
                      reads=[t1.d], writes=[t1.d])
                kb.op("pool", lambda: nc.gpsimd.tensor_tensor(t1[:], t1[:], yT[:], op=ALU.mult), reads=[t1.d, yT.d], writes=[t1.d])
                kb.op("act", lambda: nc.scalar.activation(t1[:], t1[:], ACT.Sigmoid, scale=1.5957691216057308),
                      reads=[t1.d], writes=[t1.d])
                kb.op("dve", lambda: nc.vector.tensor_tensor(glg[:], t1[:], yT[:], op=ALU.mult), reads=[t1.d, yT.d], writes=[glg.d])
                for nn in range(4):
                    for th in range(2):
                        ps = next_ps(g)
                        for kc in range(4):
                            kb.op("pe", lambda nn=nn, th=th, kc=kc, ps=ps: nc.tensor.matmul(
                                ps[:, :], lhsT=glw[:, kc, nn * 128:(nn + 1) * 128], rhs=glg[:, kc, th * 512:(th + 1) * 512],
                                start=(kc == 0), stop=(kc == 3)), reads=[glw.d, glg.d], writes=[ps.d])
                        kb.op("act", lambda nn=nn, ps=ps: nc.scalar.activation(sg[:], ps[:, :], ACT.Sigmoid,
                                                                              bias=glb[:, nn:nn + 1], scale=1.0),
                              reads=[ps.d, glb.d], writes=[sg.d])
                        kb.op("dve", lambda nn=nn, th=th, cb=cb: nc.vector.tensor_tensor(
                            s5T[:, nn, cb * 1024 + th * 512:cb * 1024 + (th + 1) * 512], sg[:],
                            glg[:, nn, th * 512:(th + 1) * 512], op=ALU.mult), reads=[sg.d, glg.d], writes=[s5T.ds[nn]])
            kb.barrier()
        if "s5T" in g.dbg:
            dbg_out(g, "s5T", s5T[:], s5T.ds, [128, 4, NL], BF16)
        glaT = T(kb, es, [128, 4, NL], BF16, "glaTt")
        kb.dma("sp", lambda: nc.sync.dma_start(out=glaT[:], in_=S.glaT.ap().rearrange("(k p) t -> p k t", p=128)),
               reads=[S.glaT_d], writes=[glaT.d])
        wo = T(kb, es, [128, 8, D], BF16, "wo")
        kb.dma("pool", lambda: nc.gpsimd.dma_start(out=wo[:], in_=H.w_out[0].rearrange("(k p) n -> p k n", p=128)),
               writes=[wo.d])
        g1b = bc_tile(g, es, "g1b", S.mod[0:1, 2 * D:3 * D], D, [S.mod_d])
        sh2b = bc_tile(g, es, "sh2b", S.mod[0:1, 3 * D:4 * D], D, [S.mod_d])
        g2e = bc_tile(g, es, "g2e", S.mod[0:1, 4 * D:5 * D], D, [S.mod_d])
        nfw = bc_tile(g, es, "nfw", H.norm_ffn_w[0:1, :], D)
        kb.op("dve", lambda: nc.vector.scalar_tensor_tensor(g2e[:], g2e[:], 1.0, nfw[:], op0=ALU.add, op1=ALU.mult),
              reads=[g2e.d, nfw.d], writes=[g2e.d])
        rw = T(kb, es, [128, 8, NE], F32, "rw")
        kb.dma("sp", lambda: nc.sync.dma_start(out=rw[:], in_=H.router_w[0].rearrange("(k p) e -> p k e", p=128)),
               writes=[rw.d])
        rbb = bc_tile(g, es, "rbb", H.router_b[0:1, :], NE)
        xt = [T(kb, es, [128, D], F32, f"ext{i}") for i in range(2)]
        x2t = [T(kb, es, [128, D], F32, f"ex2{i}") for i in range(2)]
        tmp = T(kb, es, [128, D], F32, "etmp")
        h2 = T(kb, es, [128, D], F32, "eh2")
        h2b = [T(kb, es, [128, D], BF16, f"eh2b{i}") for i in range(2)]
        hT2 = T(kb, es, [128, 8, 128], F32, "ehT2")
        junk = T(kb, es, [128, D], BF16, "ejunk")
        lg = T(kb, es, [128, NE], F32, "elg")
        mx8 = T(kb, es, [128, 8], F32, "emx8")
        ix8 = T(kb, es, [128, 8], U32, "eix8")
        sm1 = T(kb, es, [128, 4], F32, "esm")
        for tl in range(NL // 128):
            t0 = tl * 128
            x_t, x2 = xt[tl % 2], x2t[tl % 2]
            kb.dma("act", lambda x_t=x_t, t0=t0: nc.scalar.dma_start(out=x_t[:], in_=H.x[t0:t0 + 128, :]), writes=[x_t.d])
            for nh in range(2):
                ps = next_ps(g)
                for r in range(2):
                    row = tl * 2 + r
                    for kc in range(8):
                        if kc < 4:
                            lh = glaT[:, kc, row * 64:(row + 1) * 64]
                            rd = [glaT.d]
                        else:
                            lh = s5T[:, kc - 4, row:NL:64]
                            rd = s5T.ds
                        kb.op("pe", lambda lh=lh, r=r, kc=kc, nh=nh, ps=ps: nc.tensor.matmul(
                            ps[r * 64:(r + 1) * 64, :], lhsT=lh, rhs=wo[:, kc, nh * 512:(nh + 1) * 512],
                            start=(kc == 0), stop=(kc == 7)), reads=rd + [wo.d], writes=[ps.d])
                kb.op("dve", lambda nh=nh, ps=ps: nc.vector.tensor_tensor(
                    tmp[:, nh * 512:(nh + 1) * 512], ps[:, :], g1b[:, nh * 512:(nh + 1) * 512], op=ALU.mult),
                    reads=[ps.d, g1b.d], writes=[tmp.d])
            kb.op("pool", lambda x2=x2, x_t=x_t: nc.gpsimd.tensor_tensor(x2[:], tmp[:], x_t[:], op=ALU.add),
                  reads=[tmp.d, x_t.d], writes=[x2.d])
            kb.dma("sp", lambda x2=x2, t0=t0: nc.sync.dma_start(out=S.x2[t0:t0 + 128, :], in_=x2[:]),
                   reads=[x2.d], writes=[S.x2_d])
            kb.op("act", lambda x2=x2: nc.scalar.activation(junk[:], x2[:], ACT.Square, accum_out=sm1[:, 0:1]),
                  reads=[x2.d], writes=[junk.d, sm1.d])
            kb.op("dve", lambda: nc.vector.tensor_scalar(sm1[:, 1:2], sm1[:, 0:1], 1.0 / D, EPS, op0=ALU.mult, op1=ALU.add),
                  reads=[sm1.d], writes=[sm1.d])
            kb.op("act", lambda: nc.scalar.activation(sm1[:, 1:2], sm1[:, 1:2], ACT.Sqrt), reads=[sm1.d], writes=[sm1.d])
            kb.op("dve", lambda: nc.vector.reciprocal(sm1[:, 2:3], sm1[:, 1:2]), reads=[sm1.d], writes=[sm1.d])
            kb.op("dve", lambda x2=x2: nc.vector.scalar_tensor_tensor(tmp[:], x2[:], sm1[:, 2:3], g2e[:],
                                                                      op0=ALU.mult, op1=ALU.mult),
                  reads=[x2.d, sm1.d, g2e.d], writes=[tmp.d])
            kb.op("pool", lambda: nc.gpsimd.tensor_tensor(h2[:], tmp[:], sh2b[:], op=ALU.add),
                  reads=[tmp.d, sh2b.d], writes=[h2.d])
            hb = h2b[tl % 2]
            kb.op("act", lambda hb=hb: nc.scalar.copy(hb[:], h2[:]), reads=[h2.d], writes=[hb.d])
            kb.dma("sp", lambda hb=hb, t0=t0: nc.sync.dma_start(out=S.h2[t0:t0 + 128, :], in_=hb[:]),
                   reads=[hb.d], writes=[S.h2_d])
            for hf in range(2):
                ps = next_ps(g)
                for kk in range(4):
                    kc = hf * 4 + kk
                    kb.op("pe", lambda kc=kc, kk=kk, ps=ps: nc.tensor.transpose(
                        ps[:, kk * 128:(kk + 1) * 128], h2[:, kc * 128:(kc + 1) * 128], g.ident[:]),
                        reads=[h2.d, g.ident.d], writes=[ps.d])
                kb.op("act", lambda hf=hf, ps=ps: nc.scalar.copy(
                    hT2[:, hf * 4:(hf + 1) * 4, :], ps[:, :].rearrange("p (a t) -> p a t", a=4)),
                    reads=[ps.d], writes=[hT2.d])
            ps = next_ps(g)
            for kc in range(8):
                kb.op("pe", lambda kc=kc, ps=ps: nc.tensor.matmul(ps[:, 0:NE], lhsT=hT2[:, kc, :], rhs=rw[:, kc, :],
                                                                  start=(kc == 0), stop=(kc == 7)),
                      reads=[hT2.d, rw.d], writes=[ps.d])
            kb.op("dve", lambda ps=ps: nc.vector.tensor_tensor(lg[:], ps[:, 0:NE], rbb[:], op=ALU.add),
                  reads=[ps.d, rbb.d], writes=[lg.d])
            kb.op("dve", lambda: nc.vector.max(mx8[:], lg[:]), reads=[lg.d], writes=[mx8.d])
            kb.op("dve", lambda: nc.vector.max_index(ix8[:], mx8[:], lg[:]), reads=[lg.d, mx8.d], writes=[ix8.d])
            kb.op("dve", lambda tl=tl: nc.vector.tensor_copy(R.idxf[:, tl, :], ix8[:, 0:4]), reads=[ix8.d], writes=[R.idxf.d])
            kb.op("dve", lambda tl=tl: nc.vector.tensor_scalar(R.mask[:, tl, :], lg[:], mx8[:, 3:4], None, op0=ALU.is_ge),
                  reads=[lg.d, mx8.d], writes=[R.mask.d])
            kb.op("dve", lambda: nc.vector.tensor_scalar(sm1[:, 3:4], mx8[:, 0:1], -1.0, None, op0=ALU.mult),
                  reads=[mx8.d, sm1.d], writes=[sm1.d])
            kb.op("act", lambda tl=tl: nc.scalar.activation(R.gate[:, tl, :], mx8[:, 0:4], ACT.Exp, bias=sm1[:, 3:4],
                                                            scale=1.0, accum_out=sm1[:, 0:1]),
                  reads=[mx8.d, sm1.d], writes=[R.gate.d, sm1.d])
            kb.op("dve", lambda: nc.vector.reciprocal(sm1[:, 1:2], sm1[:, 0:1]), reads=[sm1.d], writes=[sm1.d])
            kb.op("dve", lambda tl=tl: nc.vector.tensor_scalar(R.gate[:, tl, :], R.gate[:, tl, :], sm1[:, 1:2], None,
                                                               op0=ALU.mult), reads=[R.gate.d, sm1.d], writes=[R.gate.d])
        if "logits" in g.dbg:
            dbg_out(g, "gate", R.gate[:], [R.gate.d], [128, 32, 4])
            dbg_out(g, "idxf", R.idxf[:], [R.idxf.d], [128, 32, 4])
        onesf = T(kb, es, [128, 128], F32, "onesf")
        kb.op("pool", lambda: nc.gpsimd.memset(onesf[:], 1.0), writes=[onesf.d])
        triu = T(kb, es, [128, 128], F32, "triu")
        kb.op("dve", lambda: nc.vector.tensor_single_scalar(triu[:], g.iota[:], 0.0, op=ALU.is_gt),
              reads=[g.iota.d], writes=[triu.d])
        ps = next_ps(g)
        for tl in range(32):
            kb.op("pe", lambda tl=tl, ps=ps: nc.tensor.matmul(ps[:, 0:NE], lhsT=onesf[:], rhs=R.mask[:, tl, :],
                                                              start=(tl == 0), stop=(tl == 31)),
                  reads=[onesf.d, R.mask.d], writes=[ps.d])
        cnt = T(kb, es, [128, NE], F32, "cnt")
        nsg = T(kb, es, [128, NE], F32, "nsg")
        pend = T(kb, es, [128, NE], F32, "pend")
        pst = T(kb, es, [128, NE], F32, "pst")
        t32 = T(kb, es, [128, NE], F32, "t32")
        kb.op("dve", lambda ps=ps: nc.vector.tensor_copy(cnt[:], ps[:, 0:NE]), reads=[ps.d], writes=[cnt.d])
        kb.op("dve", lambda: nc.vector.memset(nsg[:], 0.0), writes=[nsg.d])
        for k in range(8):
            kb.op("dve", lambda k=k: nc.vector.tensor_scalar(t32[:], cnt[:], float(SEG * k) + 0.5, None, op0=ALU.is_ge),
                  reads=[cnt.d], writes=[t32.d])
            kb.op("dve", lambda: nc.vector.tensor_tensor(nsg[:], nsg[:], t32[:], op=ALU.add), reads=[nsg.d, t32.d], writes=[nsg.d])
        kb.op("dve", lambda: nc.vector.tensor_tensor_scan(pend[:], onesf[:, 0:NE], nsg[:], 0.0, ALU.mult, ALU.add),
              reads=[onesf.d, nsg.d], writes=[pend.d])
        kb.op("dve", lambda: nc.vector.tensor_tensor(pst[:], pend[:], nsg[:], op=ALU.subtract), reads=[pend.d, nsg.d], writes=[pst.d])
        kb.op("dve", lambda: nc.vector.tensor_scalar(pst[:], pst[:], float(SEG), None, op0=ALU.mult), reads=[pst.d], writes=[pst.d])
        sidx = T(kb, es, [128, NSEG], F32, "sidx")
        kb.op("pool", lambda: nc.gpsimd.iota(sidx[:], pattern=[[1, NSEG]], base=0, channel_multiplier=0,
                                              allow_small_or_imprecise_dtypes=True), writes=[sidx.d])
        cmp3 = T(kb, es, [128, NSEG, NE], F32, "cmp3")
        kb.op("dve", lambda: nc.vector.tensor_tensor(cmp3[:], pend[:].unsqueeze(1).to_broadcast([128, NSEG, NE]),
                                                     sidx[:].unsqueeze(2).to_broadcast([128, NSEG, NE]), op=ALU.is_le),
              reads=[pend.d, sidx.d], writes=[cmp3.d])
        sef = T(kb, es, [128, NSEG], F32, "sef")
        kb.op("dve", lambda: nc.vector.tensor_reduce(sef[:], cmp3[:], axis=AX.X, op=ALU.add), reads=[cmp3.d], writes=[sef.d])
        kb.op("dve", lambda: nc.vector.tensor_scalar(sef[:], sef[:], float(NE - 1), None, op0=ALU.min), reads=[sef.d], writes=[sef.d])
        kb.op("dve", lambda: nc.vector.tensor_copy(R.segexp[:], sef[:]), reads=[sef.d], writes=[R.segexp.d])
        if "logits" in g.dbg:
            dbg_out(g, "cnt", cnt[:], [cnt.d], [128, NE])
            dbg_out(g, "segexp", R.segexp[:], [R.segexp.d], [128, NSEG], I32)
        carry = T(kb, es, [128, NE], F32, "carry")
        kb.op("dve", lambda: nc.vector.tensor_copy(carry[:], pst[:]), reads=[pst.d], writes=[carry.d])
        ief = T(kb, es, [128, NE], F32, "ief")
        kb.op("pool", lambda: nc.gpsimd.iota(ief[:], pattern=[[1, NE]], base=0, channel_multiplier=0,
                                              allow_small_or_imprecise_dtypes=True), writes=[ief.d])
        slf = T(kb, es, [128, NE], F32, "slf")
        slk = T(kb, es, [128, 4], F32, "slk")
        hld = [T(kb, es, [128, D], BF16, f"hld{i}") for i in range(2)]
        for tl in range(32):
            t0 = tl * 128
            psA = next_ps(g)
            kb.op("pe", lambda tl=tl, psA=psA: nc.tensor.matmul(psA[:, 0:NE], lhsT=triu[:], rhs=R.mask[:, tl, :],
                                                                start=True, stop=True),
                  reads=[triu.d, R.mask.d], writes=[psA.d])
            kb.op("dve", lambda psA=psA: nc.vector.tensor_tensor(slf[:], psA[:, 0:NE], carry[:], op=ALU.add),
                  reads=[psA.d, carry.d], writes=[slf.d])
            psB = next_ps(g)
            kb.op("pe", lambda tl=tl, psB=psB: nc.tensor.matmul(psB[:, 0:NE], lhsT=onesf[:], rhs=R.mask[:, tl, :],
                                                                start=True, stop=True),
                  reads=[onesf.d, R.mask.d], writes=[psB.d])
            kb.op("dve", lambda psB=psB: nc.vector.tensor_tensor(carry[:], carry[:], psB[:, 0:NE], op=ALU.add),
                  reads=[psB.d, carry.d, slf.d], writes=[carry.d])
            for k in range(4):
                kb.op("dve", lambda k=k, tl=tl: nc.vector.scalar_tensor_tensor(
                    t32[:], ief[:], R.idxf[:, tl, k:k + 1], slf[:], op0=ALU.is_equal, op1=ALU.mult,
                    accum_out=slk[:, k:k + 1]), reads=[ief.d, R.idxf.d, slf.d], writes=[t32.d, slk.d])
            kb.op("dve", lambda tl=tl: nc.vector.tensor_copy(R.slot[:, tl, :], slk[:]), reads=[slk.d], writes=[R.slot.ds[tl]])
            hl = hld[tl % 2]
            kb.dma("sp", lambda hl=hl, t0=t0: nc.sync.dma_start(out=hl[:], in_=S.h2[t0:t0 + 128, :]),
                   reads=[S.h2_d], writes=[hl.d])
            for k in range(4):
                kb.dma("pool", lambda hl=hl, tl=tl, k=k: nc.gpsimd.indirect_dma_start(
                    out=S.xg[:, :], out_offset=bass.IndirectOffsetOnAxis(ap=R.slot[:, tl, k:k + 1], axis=0),
                    in_=hl[:, :], in_offset=None), reads=[hl.d, R.slot.ds[tl]], writes=[S.xg_d])
        if "logits" in g.dbg:
            dbg_out(g, "slot", R.slot[:], R.slot.ds, [128, 32, 4], I32)
        bg = T(kb, es, [NE, 2 * D], F32, "bgrow")
        kb.dma("sp", lambda: nc.sync.dma_start(out=bg[:], in_=H.exp_b_gu[0]), writes=[bg.d])
        bgt = T(kb, es, [128, 16, NE], F32, "bgt")
        for c4 in range(0, 16, 4):
            ps = next_ps(g)
            for cc in range(4):
                kb.op("pe", lambda cc=cc, c4=c4, ps=ps: nc.tensor.transpose(
                    ps[:, cc * NE:(cc + 1) * NE], bg[:, (c4 + cc) * 128:(c4 + cc + 1) * 128], g.ident[0:NE, 0:NE]),
                    reads=[bg.d, g.ident.d], writes=[ps.d])
            kb.op("act", lambda c4=c4, ps=ps: nc.scalar.copy(
                bgt[:, c4:c4 + 4, :], ps[:, 0:4 * NE].rearrange("p (a e) -> p a e", a=4)), reads=[ps.d], writes=[bgt.d])
        kb.dma("sp", lambda: nc.sync.dma_start(out=S.bguT.ap().rearrange("e p c -> p c e"), in_=bgt[:],
                                               allow_slow_non_contiguous=True), reads=[bgt.d], writes=[S.bguT_d])


def phase_f(g):
    nc, kb, H, S = g.nc, g.kb, g.H, g.S
    R = g.R
    with ExitStack() as es:
        wgu = [T(kb, es, [128, 8, 2 * D], BF16, f"wgu{i}") for i in range(2)]
        wdn = [T(kb, es, [128, 8, D], BF16, f"wdn{i}") for i in range(2)]
        bgu = [T(kb, es, [128, 16], F32, f"bgu{i}") for i in range(2)]
        bdn = [T(kb, es, [128, D], F32, f"bdn{i}") for i in range(2)]
        xrow = [T(kb, es, [128, 4, D], BF16, f"xrow{i}") for i in range(2)]
        xT = [T(kb, es, [128, 8, SEG], BF16, f"xT{i}") for i in range(2)]
        actT = [T(kb, es, [128, 8, SEG], BF16, f"actT{i}", nd=8) for i in range(2)]
        yrow = [T(kb, es, [128, 4, D], BF16, f"yrow{i}") for i in range(2)]
        L0 = [T(kb, es, [128, SEG], F32, f"fL0_{i}") for i in range(2)]
        Gc = [T(kb, es, [128, SEG], F32, f"fGc_{i}") for i in range(2)]
        sg = [T(kb, es, [128, SEG], F32, f"fsg_{i}") for i in range(2)]
        tt = [T(kb, es, [128, SEG], F32, f"ftt_{i}") for i in range(2)]
        sef = T(kb, es, [128, NSEG], F32, "f_sef")
        kb.op("dve", lambda: nc.vector.tensor_copy(sef[:], R.segexp[:]), reads=[R.segexp.d], writes=[sef.d])
        pk = T(kb, es, [128, 8], F32, "f_pk")
        kb.op("pool", lambda: nc.gpsimd.iota(pk[:], pattern=[[128, 8]], base=0, channel_multiplier=1,
                                              allow_small_or_imprecise_dtypes=True), writes=[pk.d])
        idxwf = T(kb, es, [128, NSEG, 8], F32, "f_idxwf")
        kb.op("dve", lambda: nc.vector.tensor_scalar(idxwf[:], sef[:].unsqueeze(2).to_broadcast([128, NSEG, 8]),
                                                     float(D), None, op0=ALU.mult), reads=[sef.d], writes=[idxwf.d])
        kb.op("dve", lambda: nc.vector.tensor_tensor(idxwf[:], idxwf[:], pk[:].unsqueeze(1).to_broadcast([128, NSEG, 8]),
                                                     op=ALU.add), reads=[idxwf.d, pk.d], writes=[idxwf.d])
        idxw = T(kb, es, [128, NSEG, 8], I32, "f_idxw")
        kb.op("dve", lambda: nc.vector.tensor_copy(idxw[:], idxwf[:]), reads=[idxwf.d], writes=[idxw.d])
        idxbf = T(kb, es, [128, NSEG], F32, "f_idxbf")
        kb.op("dve", lambda: nc.vector.scalar_tensor_tensor(idxbf[:], sef[:], 128.0, pk[:, 0:1].to_broadcast([128, NSEG]),
                                                            op0=ALU.mult, op1=ALU.add), reads=[sef.d, pk.d], writes=[idxbf.d])
        idxb = T(kb, es, [128, NSEG], I32, "f_idxb")
        kb.op("dve", lambda: nc.vector.tensor_copy(idxb[:], idxbf[:]), reads=[idxbf.d], writes=[idxb.d])
        wgu_rows = H.exp_w_gu[0].rearrange("e r n -> (e r) n")
        wdn_rows = H.exp_w_down[0].rearrange("e r n -> (e r) n")
        bgu_rows = S.bguT.ap().rearrange("e p c -> (e p) c")
        nseg = g.nseg_limit if getattr(g, "nseg_limit", None) else NSEG
        for s in range(nseg):
            b = s % 2
            for k in range(8):
                kb.dma("pool", lambda k=k: nc.gpsimd.indirect_dma_start(
                    out=wgu[b][:, k, :], out_offset=None, in_=wgu_rows,
                    in_offset=bass.IndirectOffsetOnAxis(ap=idxw[:, s, k:k + 1], axis=0)),
                    reads=[idxw.d], writes=[wgu[b].d])
            for k in range(8):
                kb.dma("pool", lambda k=k: nc.gpsimd.indirect_dma_start(
                    out=wdn[b][:, k, :], out_offset=None, in_=wdn_rows,
                    in_offset=bass.IndirectOffsetOnAxis(ap=idxw[:, s, k:k + 1], axis=0)),
                    reads=[idxw.d], writes=[wdn[b].d])
            kb.dma("pool", lambda: nc.gpsimd.indirect_dma_start(
                out=bgu[b][:, :], out_offset=None, in_=bgu_rows,
                in_offset=bass.IndirectOffsetOnAxis(ap=idxb[:, s:s + 1], axis=0)),
                reads=[idxb.d, S.bguT_d], writes=[bgu[b].d])
            kb.dma("pool", lambda: nc.gpsimd.indirect_dma_start(
                out=bdn[b][:, :], out_offset=None, in_=H.exp_b_down[0],
                in_offset=bass.IndirectOffsetOnAxis(ap=R.segexp[:, s:s + 1], axis=0)),
                reads=[R.segexp.d], writes=[bdn[b].d])
            kb.dma("sp", lambda: nc.sync.dma_start(
                out=xrow[b][:], in_=S.xg[s * SEG:(s + 1) * SEG, :].rearrange("(j p) d -> p j d", p=128)),
                reads=[S.xg_d], writes=[xrow[b].d])
            for kc in range(8):
                pb = g.psb[kc % 2]
                for j in range(4):
                    kb.op("pe", lambda j=j, kc=kc, pb=pb: nc.tensor.transpose(
                        pb[:, j * 128:(j + 1) * 128], xrow[b][:, j, kc * 128:(kc + 1) * 128], g.identb[:]),
                        reads=[xrow[b].d, g.identb.d], writes=[pb.d])
                if kc % 2 == 0:
                    kb.op("act", lambda kc=kc, pb=pb: nc.scalar.copy(xT[b][:, kc, :], pb[:, 0:SEG]),
                          reads=[pb.d], writes=[xT[b].d])
                else:
                    kb.op("dve", lambda kc=kc, pb=pb: nc.vector.tensor_copy(xT[b][:, kc, :], pb[:, 0:SEG]),
                          reads=[pb.d], writes=[xT[b].d])
            for c in range(8):
                i2 = c % 2
                pg = next_ps(g)
                for kc in range(8):
                    kb.op("pe", lambda kc=kc, c=c, pg=pg: nc.tensor.matmul(
                        pg[:, :], lhsT=wgu[b][:, kc, c * 128:(c + 1) * 128], rhs=xT[b][:, kc, :],
                        start=(kc == 0), stop=(kc == 7)), reads=[wgu[b].d, xT[b].d], writes=[pg.d])
                pl_ = next_ps(g)
                for kc in range(8):
                    kb.op("pe", lambda kc=kc, c=c, pl_=pl_: nc.tensor.matmul(
                        pl_[:, :], lhsT=wgu[b][:, kc, D + c * 128:D + (c + 1) * 128], rhs=xT[b][:, kc, :],
                        start=(kc == 0), stop=(kc == 7)), reads=[wgu[b].d, xT[b].d], writes=[pl_.d])
                kb.op("dve", lambda c=c, pg=pg: nc.vector.tensor_scalar(Gc[i2][:], pg[:, :], bgu[b][:, c:c + 1], 7.0,
                                                                      op0=ALU.add, op1=ALU.min),
                      reads=[pg.d, bgu[b].d], writes=[Gc[i2].d])
                kb.op("act", lambda: nc.scalar.activation(sg[i2][:], Gc[i2][:], ACT.Sigmoid, scale=1.702),
                      reads=[Gc[i2].d], writes=[sg[i2].d])
                kb.op("act", lambda c=c, pl_=pl_: nc.scalar.activation(L0[i2][:], pl_[:, :], ACT.Identity,
                                                                      bias=bgu[b][:, 8 + c:9 + c], scale=1.0),
                      reads=[pl_.d, bgu[b].d], writes=[L0[i2].d])
                kb.op("dve", lambda: nc.vector.tensor_scalar(L0[i2][:], L0[i2][:], 7.0, -7.0, op0=ALU.min, op1=ALU.max),
                      reads=[L0[i2].d], writes=[L0[i2].d])
                kb.op("pool", lambda: nc.gpsimd.tensor_tensor(tt[i2][:], Gc[i2][:], sg[i2][:], op=ALU.mult),
                      reads=[Gc[i2].d, sg[i2].d], writes=[tt[i2].d])
                kb.op("dve", lambda c=c: nc.vector.scalar_tensor_tensor(actT[b][:, c, :], L0[i2][:], 1.0, tt[i2][:],
                                                                       op0=ALU.add, op1=ALU.mult),
                      reads=[L0[i2].d, tt[i2].d], writes=[actT[b].ds[c]])
            for j in range(4):
                for nh in range(2):
                    ps = next_ps(g)
                    for c in range(8):
                        kb.op("pe", lambda c=c, j=j, nh=nh, ps=ps: nc.tensor.matmul(
                            ps[:, :], lhsT=actT[b][:, c, j * 128:(j + 1) * 128], rhs=wdn[b][:, c, nh * 512:(nh + 1) * 512],
                            start=(c == 0), stop=(c == 7)), reads=[actT[b].ds[c], wdn[b].d], writes=[ps.d])
                    kb.op("dve", lambda j=j, nh=nh, ps=ps: nc.vector.tensor_tensor(
                        yrow[b][:, j, nh * 512:(nh + 1) * 512], ps[:, :], bdn[b][:, nh * 512:(nh + 1) * 512], op=ALU.add),
                        reads=[ps.d, bdn[b].d], writes=[yrow[b].d])
            kb.dma("act", lambda: nc.scalar.dma_start(
                out=S.yg[s * SEG:(s + 1) * SEG, :].rearrange("(j p) d -> p j d", p=128), in_=yrow[b][:]),
                reads=[yrow[b].d], writes=[S.yg_d])


def phase_g(g):
    nc, kb, H, S = g.nc, g.kb, g.H, g.S
    R = g.R
    with ExitStack() as es:
        g2b = bc_tile(g, es, "g2b", S.mod[0:1, 5 * D:6 * D], D, [S.mod_d])
        fnw = bc_tile(g, es, "fnw", H.final_norm_w.ap().rearrange("(o d) -> o d", o=1), D)
        yk = [[T(kb, es, [128, D], BF16, f"yk{i}_{k}") for k in range(4)] for i in range(2)]
        x2t = [T(kb, es, [128, D], F32, f"gx2{i}") for i in range(2)]
        acc = T(kb, es, [128, D], F32, "gacc")
        x3 = T(kb, es, [128, D], F32, "gx3")
        ot = [T(kb, es, [128, D], F32, f"got{i}") for i in range(2)]
        junk = T(kb, es, [128, D], BF16, "gjunk")
        sm1 = T(kb, es, [128, 4], F32, "gsm")
        for tl in range(NL // 128):
            t0 = tl * 128
            b = tl % 2
            for k in range(4):
                kb.dma("pool", lambda k=k: nc.gpsimd.indirect_dma_start(
                    out=yk[b][k][:, :], out_offset=None, in_=S.yg[:, :],
                    in_offset=bass.IndirectOffsetOnAxis(ap=R.slot[:, tl, k:k + 1], axis=0)),
                    reads=[S.yg_d, R.slot.ds[tl]], writes=[yk[b][k].d])
            kb.dma("sp", lambda: nc.sync.dma_start(out=x2t[b][:], in_=S.x2[t0:t0 + 128, :]), reads=[S.x2_d], writes=[x2t[b].d])
            kb.op("dve", lambda: nc.vector.tensor_scalar(acc[:], yk[b][0][:], R.gate[:, tl, 0:1], None, op0=ALU.mult),
                  reads=[yk[b][0].d, R.gate.d], writes=[acc.d])
            for k in range(1, 4):
                kb.op("dve", lambda k=k: nc.vector.scalar_tensor_tensor(acc[:], yk[b][k][:], R.gate[:, tl, k:k + 1], acc[:],
                                                                       op0=ALU.mult, op1=ALU.add),
                      reads=[yk[b][k].d, R.gate.d, acc.d], writes=[acc.d])
            kb.op("pool", lambda: nc.gpsimd.tensor_tensor(acc[:], acc[:], g2b[:], op=ALU.mult), reads=[acc.d, g2b.d], writes=[acc.d])
            kb.op("pool", lambda: nc.gpsimd.tensor_tensor(x3[:], acc[:], x2t[b][:], op=ALU.add), reads=[acc.d, x2t[b].d], writes=[x3.d])
            kb.op("act", lambda: nc.scalar.activation(junk[:], x3[:], ACT.Square, accum_out=sm1[:, 0:1]),
                  reads=[x3.d], writes=[junk.d, sm1.d])
            kb.op("dve", lambda: nc.vector.tensor_scalar(sm1[:, 1:2], sm1[:, 0:1], 1.0 / D, EPS, op0=ALU.mult, op1=ALU.add),
                  reads=[sm1.d], writes=[sm1.d])
            kb.op("act", lambda: nc.scalar.activation(sm1[:, 1:2], sm1[:, 1:2], ACT.Sqrt), reads=[sm1.d], writes=[sm1.d])
            kb.op("dve", lambda: nc.vector.reciprocal(sm1[:, 2:3], sm1[:, 1:2]), reads=[sm1.d], writes=[sm1.d])
            kb.op("dve", lambda: nc.vector.scalar_tensor_tensor(ot[b][:], x3[:], sm1[:, 2:3], fnw[:], op0=ALU.mult, op1=ALU.mult),
                  reads=[x3.d, sm1.d, fnw.d], writes=[ot[b].d])
            kb.dma("act", lambda: nc.scalar.dma_start(out=H.out[t0:t0 + 128, :], in_=ot[b][:]), reads=[ot[b].d], writes=[g.out_d])


_SHARED = ("c_ctx", "ada_w", "ada_b", "norm_mix_w", "w_in", "gla_lr_up", "gla_lr_bias", "gla_norm_w",
           "s5_lam_re", "s5_lam_im", "s5_log_dt", "s5_b_re", "s5_b_im", "s5_c_re", "s5_c_im", "s5_d",
           "glu_w", "glu_b", "w_out", "norm_ffn_w", "router_w", "router_b", "exp_w_gu", "exp_b_gu",
           "exp_w_down", "exp_b_down", "final_norm_w")


def kernel(x, c, ctx, c_ctx, ada_w, ada_b, norm_mix_w, w_in, gla_lr_up, gla_lr_bias, gla_norm_w,
           s5_lam_re, s5_lam_im, s5_log_dt, s5_b_re, s5_b_im, s5_c_re, s5_c_im, s5_d, glu_w, glu_b,
           w_out, norm_ffn_w, router_w, router_b, exp_w_gu, exp_b_gu, exp_w_down, exp_b_down,
           final_norm_w):
    loc = locals()
    shared = {k: np.ascontiguousarray(np.asarray(loc[k], dtype=np.float32)) for k in _SHARED}
    x = np.asarray(x, dtype=np.float32)
    c = np.asarray(c, dtype=np.float32)
    ctx = np.asarray(ctx, dtype=np.float32)
    nb = x.shape[0]
    nc, _ = build()
    in_maps = []
    for b in range(nb):
        m = dict(shared)
        m["x"] = np.ascontiguousarray(x[b])
        m["ctx"] = np.ascontiguousarray(ctx[b])
        m["c"] = np.ascontiguousarray(c[b:b + 1])
        in_maps.append(m)
    res = run_bass_kernel_spmd(nc, in_maps, core_ids=list(range(nb)))
    return np.stack([np.asarray(r["out"], dtype=np.float32) for r in res.results], axis=0)
```

```python
import numpy as np
import concourse.bass as bass
import concourse.mybir as mybir
from concourse.bass_utils import run_bass_kernel_spmd
from contextlib import ExitStack

F32 = mybir.dt.float32
BF16 = mybir.dt.bfloat16
U32 = mybir.dt.uint32
I32 = mybir.dt.int32
ACT = mybir.ActivationFunctionType
ALU = mybir.AluOpType
AX = mybir.AxisListType
PoolE = mybir.EngineType.Pool

D = 1024
NL = 4096
NCX = 256
NT = NL + NCX
NE = 32
EPS = 1e-6


class Dep:
    __slots__ = ("w", "r", "name")

    def __init__(self, name=""):
        self.w = None
        self.r = []
        self.name = name


class KB:
    NSLOT = 8

    def __init__(self, nc, es):
        self.nc = nc
        self.engs = {"pe": nc.tensor, "dve": nc.vector, "act": nc.scalar,
                     "pool": nc.gpsimd, "sp": nc.sync}
        self.sem = {}
        self.cnt = {}
        for k in self.engs:
            self.sem[k] = es.enter_context(nc.semaphore("s_" + k))
            self.cnt[k] = 0
        self.slots = {}
        self.slot_rr = {}
        for q in ("sp", "act", "pool"):
            self.slots[q] = [[es.enter_context(nc.semaphore(f"d_{q}{i}")), 0]
                             for i in range(self.NSLOT)]
            self.slot_rr[q] = 0
        self.slots["conv"] = [[es.enter_context(nc.semaphore(f"d_conv{i}")), 0] for i in range(6)]
        self.slot_rr["conv"] = 0
        self.pending = []
        self.seen = {k: {} for k in self.engs}
        self.ninst = 0
        self.nwaits = 0

    def _wait(self, eng, tok):
        if tok is None:
            return
        sem, val, key = tok
        if eng == "pe" and key == "pe":
            return
        s = self.seen[eng]
        if s.get(key, 0) >= val:
            return
        self.engs[eng].wait_ge(sem, val)
        s[key] = val
        self.nwaits += 1

    def _deps(self, eng, reads, writes):
        for d in reads:
            self._wait(eng, d.w)
        for d in writes:
            self._wait(eng, d.w)
            for t in d.r:
                self._wait(eng, t)

    def _commit(self, tok, reads, writes):
        for d in reads:
            d.r.append(tok)
            if len(d.r) > 16:
                best = {}
                for t in d.r:
                    if t[2] not in best or best[t[2]][1] < t[1]:
                        best[t[2]] = t
                d.r = list(best.values())
        for d in writes:
            d.w = tok
            d.r = []

    dead = False

    def op(self, eng, fn, reads=(), writes=()):
        if KB.dead:
            return None
        self._deps(eng, reads, writes)
        inst = fn()
        self.cnt[eng] += 1
        inst.then_inc(self.sem[eng], 1)
        tok = (self.sem[eng], self.cnt[eng], eng)
        self._commit(tok, reads, writes)
        self.ninst += 1
        return tok

    def pump(self, n=1):
        for _ in range(n):
            if not self.pending:
                return
            fn, reads, writes = self.pending.pop(0)
            self.dma("pool", fn, reads=reads, writes=writes, grp="conv")

    def dma(self, q, fn, reads=(), writes=(), grp=None):
        if KB.dead:
            return None
        grp = grp or q
        i = self.slot_rr[grp]
        self.slot_rr[grp] = (i + 1) % len(self.slots[grp])
        slot = self.slots[grp][i]
        key = f"d_{grp}{i}"
        if slot[1] > 0:
            self._wait(q, (slot[0], slot[1], key))
        self._deps(q, reads, writes)
        inst = fn()
        slot[1] += 16
        inst.then_inc(slot[0], 16)
        tok = (slot[0], slot[1], key)
        self._commit(tok, reads, writes)
        self.ninst += 1
        return tok

    def wait_all(self, eng, deps):
        for d in deps:
            self._wait(eng, d.w)
            for t in d.r:
                self._wait(eng, t)

    def barrier(self, conv=False):
        for e in self.engs:
            self.finish(e, conv)

    def finish(self, eng="sp", conv=True):
        for k in self.engs:
            if self.cnt[k] > 0:
                self._wait(eng, (self.sem[k], self.cnt[k], k))
        for q in self.slots:
            if q == "conv" and not conv:
                continue
            for i, s in enumerate(self.slots[q]):
                if s[1] > 0:
                    self._wait(eng, (s[0], s[1], f"d_{q}{i}"))


class T:
    _ctr = [0]

    def __init__(self, kb, es, shape, dtype, name, psum=False, nd=1):
        nc = kb.nc
        T._ctr[0] += 1
        name = f"{name}_{T._ctr[0]}"
        if psum:
            self.t = es.enter_context(nc.psum_tensor(name, list(shape), dtype))
        else:
            self.t = es.enter_context(nc.sbuf_tensor(name, list(shape), dtype))
        self.ds = [Dep(f"{name}.{i}") for i in range(nd)]
        self.d = self.ds[0]
        self.shape = shape

    def __getitem__(self, k):
        return self.t[k]


class Ctx:
    pass


class StopBuild(Exception):
    pass


def ckpt(name):
    if STOP == name:
        KB.dead = True


STOP = None


def build(dbg=(), stop=None):
    global STOP
    STOP = stop
    KB.dead = False
    nc = bass.Bass("TRN2", target_bir_lowering=False)
    g = Ctx()
    g.nc = nc
    g.dbg = set(dbg)
    g.outs = {}
    g.out_d = Dep("out")

    def din(name, shape, dt=F32):
        return nc.dram_tensor(name, list(shape), dt, kind="ExternalInput")

    H = Ctx()
    g.H = H
    H.x = din("x", [NL, D])
    H.ctx = din("ctx", [NCX, D])
    H.c = din("c", [1, D])
    H.c_ctx = din("c_ctx", [D])
    H.ada_w = din("ada_w", [1, D, 6 * D])
    H.ada_b = din("ada_b", [1, 6 * D])
    H.norm_mix_w = din("norm_mix_w", [1, D])
    H.w_in = din("w_in", [1, D, 2080])
    H.gla_lr_up = din("gla_lr_up", [1, 2, 16, 256])
    H.gla_lr_bias = din("gla_lr_bias", [1, 2, 256])
    H.gla_norm_w = din("gla_norm_w", [1, 128])
    H.s5_lam_re = din("s5_lam_re", [1, 2, 32, 64])
    H.s5_lam_im = din("s5_lam_im", [1, 2, 32, 64])
    H.s5_log_dt = din("s5_log_dt", [1, 2, 32])
    H.s5_b_re = din("s5_b_re", [1, 2, 32, 64, 16])
    H.s5_b_im = din("s5_b_im", [1, 2, 32, 64, 16])
    H.s5_c_re = din("s5_c_re", [1, 2, 32, 16, 64])
    H.s5_c_im = din("s5_c_im", [1, 2, 32, 16, 64])
    H.s5_d = din("s5_d", [1, 512])
    H.glu_w = din("glu_w", [1, 512, 512])
    H.glu_b = din("glu_b", [1, 512])
    H.w_out = din("w_out", [1, D, D])
    H.norm_ffn_w = din("norm_ffn_w", [1, D])
    H.router_w = din("router_w", [1, D, NE])
    H.router_b = din("router_b", [1, NE])
    H.exp_w_gu = din("exp_w_gu", [1, NE, D, 2 * D])
    H.exp_b_gu = din("exp_b_gu", [1, NE, 2 * D])
    H.exp_w_down = din("exp_w_down", [1, NE, D, D])
    H.exp_b_down = din("exp_b_down", [1, NE, D])
    H.final_norm_w = din("final_norm_w", [D])
    H.out = nc.dram_tensor("out", [NL, D], F32, kind="ExternalOutput")

    S = Ctx()
    g.S = S

    def scr(name, shape, dt):
        if name in g.dbg:
            h = nc.dram_tensor(name, list(shape), dt, kind="ExternalOutput")
        else:
            h = nc.dram_tensor(name, list(shape), dt)
        return h, Dep(name)

    S.mod, S.mod_d = scr("mod_s", [2, 6 * D], F32)
    S.qT, S.qT_d = scr("qT_s", [256, NT], BF16)
    S.kT, S.kT_d = scr("kT_s", [256, NT], BF16)
    S.rT, S.rT_d = scr("rT_s", [512, NL], BF16)
    S.lrT, S.lrT_d = scr("lrT_s", [2, 16, NT], F32)
    S.v, S.v_d = scr("v_s", [NT, 512], BF16)
    S.uT, S.uT_d = scr("uT_s", [512, NT], BF16)
    S.u, S.u_d = scr("u_s", [NT, 512], BF16)
    S.glaT, S.glaT_d = scr("glaT_s", [512, NL], BF16)
    S.y0, S.y0_d = scr("y0_s", [32, 128, 512], F32)
    S.Ut, S.Ut_d = scr("Ut_s", [32, 128, NT // 8], BF16)
    S.x2, S.x2_d = scr("x2_s", [NL, D], F32)
    S.h2, S.h2_d = scr("h2_s", [NL, D], BF16)
    S.xg, S.xg_d = scr("xg_s", [64 * 512, D], BF16)
    S.yg, S.yg_d = scr("yg_s", [64 * 512, D], BF16)
    S.bguT, S.bguT_d = scr("bguT_s", [NE, 128, 16], F32)
    S.wg, S.wg_d = scr("wg_s", [NE, 128, 8 * 2 * D], BF16)
    S.wd, S.wd_d = scr("wd_s", [NE, 128, 8 * D], BF16)

    with ExitStack() as es:
        kb = KB(nc, es)
        g.kb = kb
        g.es = es
        g.ident = T(kb, es, [128, 128], F32, "ident")
        g.identb = T(kb, es, [128, 128], BF16, "identb")
        io = T(kb, es, [128, 128], F32, "iota0")
        kb.op("pool", lambda: nc.gpsimd.iota(io[:], pattern=[[1, 128]], base=0, channel_multiplier=-1,
                                              allow_small_or_imprecise_dtypes=True), writes=[io.d])
        kb.op("dve", lambda: nc.vector.tensor_single_scalar(g.ident[:], io[:], 0.0, op=ALU.is_equal),
              reads=[io.d], writes=[g.ident.d])
        kb.op("dve", lambda: nc.vector.tensor_copy(g.identb[:], g.ident[:]), reads=[g.ident.d], writes=[g.identb.d])
        g.iota = io
        g.ps = [T(kb, es, [128, 512], F32, f"ps{i}", psum=True) for i in range(6)]
        g.psb = [T(kb, es, [128, 1024], BF16, f"psb{i}", psum=True) for i in range(2)]
        g.ps_rr = 0
        R = Ctx()
        g.R = R
        R.mask = T(kb, es, [128, 32, NE], F32, "r_mask")
        R.idxf = T(kb, es, [128, 32, 4], F32, "r_idxf")
        R.gate = T(kb, es, [128, 32, 4], F32, "r_gate")
        R.slot = T(kb, es, [128, 32, 4], I32, "r_slot", nd=32)
        R.segexp = T(kb, es, [128, 64], I32, "r_segexp")
        R.segidx = T(kb, es, [128, 64], I32, "r_segidx")
        R.segrow = T(kb, es, [128, 64], I32, "r_segrow")

        if stop is not None and str(stop).startswith("benchf"):
            kb.op("pool", lambda: nc.gpsimd.iota(R.segidx[:], pattern=[[0, 64]], base=0, channel_multiplier=1),
                  writes=[R.segidx.d])
            kb.op("pool", lambda: nc.gpsimd.memset(R.segrow[:], 0), writes=[R.segrow.d])
            g.bench = stop
            phase_f(g)
            kb.barrier()
            kb.finish("sp")
            print("instructions", kb.ninst, "waits", kb.nwaits)
            return nc, g
        phase_a(g)
        kb.barrier()
        for e in range(NE):
            kb.pending.append((lambda e=e: nc.gpsimd.dma_start(
                out=S.wg[e, :, :].rearrange("p (k n) -> p k n", k=8),
                in_=H.exp_w_gu[0, e, :, :].rearrange("(k p) n -> p k n", p=128)), [], [S.wg_d]))
            kb.pending.append((lambda e=e: nc.gpsimd.dma_start(
                out=S.wd[e, :, :].rearrange("p (k n) -> p k n", k=8),
                in_=H.exp_w_down[0, e, :, :].rearrange("(k p) n -> p k n", p=128)), [], [S.wd_d]))
        kb.pump(6)
        if stop != 'a':
            phase_b(g)
            kb.barrier()
            if stop != 'b':
                if stop not in ('d_only', 'e_only'):
                    phase_c(g)
                    kb.barrier()
                if stop not in ('c', 'c0', 'c1', 'c2', 'c3'):
                    if stop != 'e_only':
                        phase_d(g)
                        kb.barrier()
                    if stop not in ('d', 'd_only'):
                        phase_e(g)
                        kb.barrier()
                        if stop != 'e':
                            kb.pump(1000)
                            kb.barrier(conv=True)
                            phase_f(g)
                            kb.barrier()
                            phase_g(g)
                            kb.barrier()

        kb.finish("sp")
        print("instructions", kb.ninst, "waits", kb.nwaits)
    return nc, g


def next_ps(g):
    p = g.ps[g.ps_rr]
    g.ps_rr = (g.ps_rr + 1) % len(g.ps)
    return p


def dbg_out(g, name, src_ap, deps, shape, dt=F32, q="sp"):
    if name not in g.dbg:
        return
    nc, kb = g.nc, g.kb
    o = nc.dram_tensor("dbg_" + name, list(shape), dt, kind="ExternalOutput")
    g.outs[name] = o
    kb.dma(q, lambda: nc.sync.dma_start(out=o.ap(), in_=src_ap), reads=deps)


def phase_a(g):
    nc, kb, H, S = g.nc, g.kb, g.H, g.S
    with ExitStack() as es:
        cc = T(kb, es, [128, 8, 2], F32, "cc")
        kb.dma("sp", lambda: nc.sync.dma_start(out=cc[:, :, 0], in_=H.c[0, :].rearrange("(k p) -> p k", p=128),
                                               allow_slow_non_contiguous=True), writes=[cc.d])
        kb.dma("sp", lambda: nc.sync.dma_start(out=cc[:, :, 1], in_=H.c_ctx.ap().rearrange("(k p) -> p k", p=128),
                                               allow_slow_non_contiguous=True), writes=[cc.d])
        kb.op("act", lambda: nc.scalar.activation(cc[:], cc[:], ACT.Silu), reads=[cc.d], writes=[cc.d])
        ab = T(kb, es, [2, 6 * D], F32, "ab")
        kb.dma("sp", lambda: nc.sync.dma_start(out=ab[0:1, :], in_=H.ada_b[0:1, :]), writes=[ab.d])
        kb.dma("sp", lambda: nc.sync.dma_start(out=ab[1:2, :], in_=H.ada_b[0:1, :]), writes=[ab.d])
        modsb = T(kb, es, [2, 6 * D], F32, "modsb")
        aw = [T(kb, es, [128, 8, 512], F32, f"aw{i}") for i in range(2)]
        for j in range(12):
            a = aw[j % 2]
            kb.dma("sp" if j % 2 == 0 else "act",
                   (lambda a=a, j=j: (nc.sync if j % 2 == 0 else nc.scalar).dma_start(
                       out=a[:], in_=H.ada_w[0, :, j * 512:(j + 1) * 512].rearrange("(k p) n -> p k n", p=128))),
                   writes=[a.d])
            ps = next_ps(g)
            for k in range(8):
                kb.op("pe", lambda k=k: nc.tensor.matmul(ps[0:2, :], lhsT=cc[:, k, :], rhs=a[:, k, :],
                                                         start=(k == 0), stop=(k == 7)),
                      reads=[cc.d, a.d], writes=[ps.d])
            kb.op("dve", lambda j=j: nc.vector.tensor_tensor(modsb[:, j * 512:(j + 1) * 512], ps[0:2, :],
                                                            ab[:, j * 512:(j + 1) * 512], op=ALU.add),
                  reads=[ps.d, ab.d], writes=[modsb.d])
        kb.dma("sp", lambda: nc.sync.dma_start(out=S.mod.ap(), in_=modsb[:]), reads=[modsb.d], writes=[S.mod_d])


def load_fm_vec(g, es, name, src_ap_1d):
    nc, kb = g.nc, g.kb
    t = T(kb, es, [128, 8], F32, name)
    kb.dma("sp", lambda: nc.sync.dma_start(out=t[:], in_=src_ap_1d.rearrange("(k p) -> p k", p=128),
                                           allow_slow_non_contiguous=True), reads=[g.S.mod_d], writes=[t.d])
    return t


def s5_cols(hT, k, s0, n):
    if s0 < NCX:
        assert s0 + n <= NCX
        return hT[:, k, s0:s0 + n]
    c0 = (s0 - NCX) // 64
    ncol = n // 64
    v = hT[:, k, NCX:NT].rearrange("p (row col) -> p col row", col=64)
    return v[:, c0:c0 + ncol, :]


def phase_b(g):
    nc, kb, H, S = g.nc, g.kb, g.H, g.S
    with ExitStack() as es:
        sh1 = load_fm_vec(g, es, "sh1", S.mod[0, 0:D])
        sc1 = load_fm_vec(g, es, "sc1", S.mod[0, D:2 * D])
        csh1 = load_fm_vec(g, es, "csh1", S.mod[1, 0:D])
        csc1 = load_fm_vec(g, es, "csc1", S.mod[1, D:2 * D])
        nmw = load_fm_vec(g, es, "nmw", H.norm_mix_w[0, :])
        g1f = T(kb, es, [128, 8], F32, "g1f")
        cg1f = T(kb, es, [128, 8], F32, "cg1f")
        kb.op("dve", lambda: nc.vector.scalar_tensor_tensor(g1f[:], sc1[:], 1.0, nmw[:], op0=ALU.add, op1=ALU.mult),
              reads=[sc1.d, nmw.d], writes=[g1f.d])
        kb.op("dve", lambda: nc.vector.scalar_tensor_tensor(cg1f[:], csc1[:], 1.0, nmw[:], op0=ALU.add, op1=ALU.mult),
              reads=[csc1.d, nmw.d], writes=[cg1f.d])
        wi = T(kb, es, [128, 8, 2080], BF16, "wi")
        for (a, b) in ((0, 1040), (1040, 2080)):
            kb.dma("pool", lambda a=a, b=b: nc.gpsimd.dma_start(
                out=wi[:, :, a:b], in_=H.w_in[0, :, a:b].rearrange("(k p) n -> p k n", p=128)), writes=[wi.d])
        hT = T(kb, es, [128, 8, NT], BF16, "hT", nd=9)
        xg = [T(kb, es, [128, 4, D], F32, f"xg{i}") for i in range(2)]
        junk = T(kb, es, [128, D], BF16, "junkb")
        groups = [("ctx", 0, 2)] + [("lat", gi, 4) for gi in range(8)]
        for gi, (kind, idx, ntile) in enumerate(groups):
            kb.pump(1)
            xt = xg[gi % 2]
            ntok = ntile * 128
            if kind == "ctx":
                src = H.ctx[0:ntok, :]
                col0 = 0
                gsc, gsh = cg1f, csh1
            else:
                src = H.x[idx * 512:(idx + 1) * 512, :]
                col0 = NCX + idx * 512
                gsc, gsh = g1f, sh1
            q = "sp" if gi % 2 == 0 else "act"
            kb.dma(q, lambda xt=xt, src=src, ntile=ntile, q=q: (nc.sync if q == "sp" else nc.scalar).dma_start(
                out=xt[:, 0:ntile, :], in_=src.rearrange("(j p) d -> p j d", p=128)), writes=[xt.d])
            ss = T(kb, es, [128, 4], F32, f"ss{gi}")
            rs = T(kb, es, [128, 4], F32, f"rs{gi}")
            for j in range(ntile):
                kb.op("act", lambda j=j: nc.scalar.activation(junk[:], xt[:, j, :], ACT.Square,
                                                              accum_out=ss[:, j:j + 1]),
                      reads=[xt.d], writes=[junk.d, ss.d])
            kb.op("dve", lambda: nc.vector.tensor_scalar(rs[:, 0:ntile], ss[:, 0:ntile], 1.0 / D, EPS,
                                                         op0=ALU.mult, op1=ALU.add), reads=[ss.d], writes=[rs.d])
            kb.op("act", lambda: nc.scalar.activation(rs[:, 0:ntile], rs[:, 0:ntile], ACT.Sqrt),
                  reads=[rs.d], writes=[rs.d])
            kb.op("dve", lambda: nc.vector.reciprocal(rs[:, 0:ntile], rs[:, 0:ntile]), reads=[rs.d], writes=[rs.d])
            for j in range(ntile):
                kb.op("dve", lambda j=j: nc.vector.tensor_scalar(xt[:, j, :], xt[:, j, :], rs[:, j:j + 1], None,
                                                                 op0=ALU.mult), reads=[xt.d, rs.d], writes=[xt.d])
            for k in range(8):
                ps = next_ps(g)
                for j in range(ntile):
                    kb.op("pe", lambda j=j, k=k: nc.tensor.transpose(ps[:, j * 128:(j + 1) * 128],
                                                                     xt[:, j, k * 128:(k + 1) * 128], g.ident[:]),
                          reads=[xt.d, g.ident.d], writes=[ps.d])
                kb.op("act", lambda k=k, ps=ps: nc.scalar.activation(
                    hT[:, k, col0:col0 + ntok], ps[:, 0:ntok], ACT.Identity,
                    bias=gsh[:, k:k + 1], scale=gsc[:, k:k + 1]),
                    reads=[ps.d, gsh.d, gsc.d], writes=[hT.ds[gi]])
        hall = hT.ds
        if "hT" in g.dbg:
            dbg_out(g, "hT", hT[:], hall, [128, 8, NT], BF16)
        stg = [T(kb, es, [128, NT], BF16, f"stg{i}") for i in range(2)]
        stgf = T(kb, es, [16, NT], F32, "stgf")
        rr = [0]

        def fm_proj(c0, m, dst_fn, t0, t1, cols_fn, f32=False):
            kb.pump(1)
            st = stgf if f32 else stg[rr[0] % 2]
            rr[0] += 0 if f32 else 1
            t = t0
            while t < t1:
                n = min(512, t1 - t)
                if t < NCX:
                    n = min(n, NCX - t)
                ps = next_ps(g)
                for k in range(8):
                    kb.op("pe", lambda k=k, t=t, n=n: nc.tensor.matmul(
                        ps[0:m, 0:n], lhsT=wi[:, k, c0:c0 + m], rhs=cols_fn(k, t, n),
                        start=(k == 0), stop=(k == 7)), reads=[wi.d] + hall, writes=[ps.d])
                eng = "act" if (t // 512) % 2 == 0 else "dve"
                if eng == "act":
                    kb.op("act", lambda t=t, n=n, ps=ps: nc.scalar.copy(st[0:m, t:t + n], ps[0:m, 0:n]),
                          reads=[ps.d], writes=[st.d])
                else:
                    kb.op("dve", lambda t=t, n=n, ps=ps: nc.vector.tensor_copy(st[0:m, t:t + n], ps[0:m, 0:n]),
                          reads=[ps.d], writes=[st.d])
                t += n
            dst, dd = dst_fn()
            kb.dma("sp", lambda: nc.sync.dma_start(out=dst, in_=st[0:m, t0:t1]), reads=[st.d], writes=[dd])

        raster = lambda k, t, n: hT[:, k, t:t + n]
        s5t = lambda k, t, n: s5_cols(hT, k, t, n)
        for mt in range(2):
            fm_proj(mt * 128, 128, lambda mt=mt: (S.qT[mt * 128:(mt + 1) * 128, :], S.qT_d), 0, NT, raster)
        for mt in range(2):
            fm_proj(256 + mt * 128, 128, lambda mt=mt: (S.kT[mt * 128:(mt + 1) * 128, :], S.kT_d), 0, NT, raster)
        for mt in range(4):
            fm_proj(1024 + mt * 128, 128, lambda mt=mt: (S.rT[mt * 128:(mt + 1) * 128, :], S.rT_d), NCX, NT, raster)
        for z in range(2):
            fm_proj(1536 + z * 16, 16, lambda z=z: (S.lrT[z, :, :], S.lrT_d), 0, NT, raster, f32=True)
        for mt in range(4):
            fm_proj(1568 + mt * 128, 128, lambda mt=mt: (S.uT[mt * 128:(mt + 1) * 128, :], S.uT_d), 0, NT, s5t)
        st4 = [T(kb, es, [128, 4, 512], BF16, f"st4_{i}") for i in range(2)]
        ngrp = 0
        for (c0, dst, dd, s5) in ((512, S.v, S.v_d, False), (1568, S.u, S.u_d, True)):
            for t0 in list(range(0, NCX, 512)) + list(range(NCX, NT, 512)):
                nt = 2 if t0 < NCX else 4
                st = st4[ngrp % 2]
                ngrp += 1
                for j in range(nt):
                    ps = next_ps(g)
                    tt = t0 + j * 128
                    if s5 and tt >= NCX:
                        for hf in range(2):
                            col = (tt - NCX) // 64 + hf
                            for k in range(8):
                                lh = hT[:, k, NCX + col:NT:64]
                                kb.op("pe", lambda k=k, lh=lh, ps=ps, hf=hf: nc.tensor.matmul(
                                    ps[hf * 64:(hf + 1) * 64, :], lhsT=lh, rhs=wi[:, k, c0:c0 + 512],
                                    start=(k == 0), stop=(k == 7)), reads=[wi.d] + hall, writes=[ps.d])
                    else:
                        for k in range(8):
                            lh = hT[:, k, tt:tt + 128]
                            kb.op("pe", lambda k=k, lh=lh, ps=ps: nc.tensor.matmul(
                                ps[:, :], lhsT=lh, rhs=wi[:, k, c0:c0 + 512], start=(k == 0), stop=(k == 7)),
                                reads=[wi.d] + hall, writes=[ps.d])
                    if j % 2 == 0:
                        kb.op("act", lambda j=j, ps=ps, st=st: nc.scalar.copy(st[:, j, :], ps[:, :]),
                              reads=[ps.d], writes=[st.d])
                    else:
                        kb.op("dve", lambda j=j, ps=ps, st=st: nc.vector.tensor_copy(st[:, j, :], ps[:, :]),
                              reads=[ps.d], writes=[st.d])
                kb.dma("sp", lambda st=st, t0=t0, nt=nt, dst=dst: nc.sync.dma_start(
                    out=dst[t0:t0 + nt * 128, :].rearrange("(j p) e -> p j e", p=128), in_=st[:, 0:nt, :]),
                    reads=[st.d], writes=[dd])


def phase_c(g):
    nc, kb, H, S = g.nc, g.kb, g.H, g.S
    NCH = NT // 64
    with ExitStack() as es:
        psb = g.psb[0]
        lup = T(kb, es, [16, 2, 256], F32, "lup")
        kb.dma("sp", lambda: nc.sync.dma_start(out=lup[:], in_=H.gla_lr_up[0].rearrange("z r f -> r z f")),
               writes=[lup.d])
        nb = T(kb, es, [128, 2, 2], F32, "nbias")
        kb.dma("sp", lambda: nc.sync.dma_start(out=nb[:], in_=H.gla_lr_bias[0].rearrange("z (t p) -> p z t", p=128),
                                               allow_slow_non_contiguous=True), writes=[nb.d])
        kb.op("dve", lambda: nc.vector.tensor_scalar(nb[:], nb[:], -1.0, None, op0=ALU.mult), reads=[nb.d], writes=[nb.d])
        nw = T(kb, es, [128, 1], F32, "gnw")
        kb.dma("sp", lambda: nc.sync.dma_start(out=nw[:], in_=H.gla_norm_w[0, :].rearrange("(p o) -> p o", o=1)),
               writes=[nw.d])
        ones = T(kb, es, [128, 128], BF16, "onesb")
        kb.op("pool", lambda: nc.gpsimd.memset(ones[:], 1.0), writes=[ones.d])
        io = g.iota
        mk = []
        for z in range(2):
            m4 = T(kb, es, [128, 4, 64], BF16, f"tri{z}")
            op = ALU.is_ge if z == 0 else ALU.is_le
            for h in range(4):
                kb.op("dve", lambda h=h, m4=m4, op=op: nc.vector.tensor_single_scalar(
                    m4[0:64, h, :], io[0:64, 0:64], 0.0, op=op), reads=[io.d], writes=[m4.d])
                kb.op("dve", lambda h=h, m4=m4, op=op: nc.vector.tensor_single_scalar(
                    m4[64:128, h, :], io[64:128, 0:64], -64.0, op=op), reads=[io.d], writes=[m4.d])
            mk.append(m4)
        qt = [T(kb, es, [128, 2, NT], BF16, f"qtl{z}") for z in range(2)]
        kt = [T(kb, es, [128, 2, NT], BF16, f"ktl{z}") for z in range(2)]
        elast = [T(kb, es, [128, 2, NCH], F32, f"elast{z}") for z in range(2)]
        if STOP == 'c0':
            return
        with ExitStack() as es2:
            mask = T(kb, es2, [128, NT + 1], BF16, "cmask")
            kb.op("pool", lambda: nc.gpsimd.memset(mask[:], 1.0), writes=[mask.d])
            kb.op("pool", lambda: nc.gpsimd.memset(mask[:, 0:NT + 1:64], 0.0), writes=[mask.d])
            lrtz = T(kb, es2, [16, NT], F32, "lrt")
            A = T(kb, es2, [128, NT], F32, "scrA")
            B = T(kb, es2, [128, NT], F32, "scrB")
            raw = [T(kb, es2, [128, NT], BF16, f"raw{i}") for i in range(2)]
            for z in range(2):
                kb.dma("sp", lambda z=z: nc.sync.dma_start(out=lrtz[:], in_=S.lrT[z, :, :]),
                       reads=[S.lrT_d], writes=[lrtz.d])
                for pt in range(2):
                    if STOP == 'c1a' and (z, pt) != (0, 0):
                        continue
                    for t0 in range(0, NT, 512):
                        n = min(512, NT - t0)
                        ps = next_ps(g)
                        kb.op("pe", lambda t0=t0, n=n, ps=ps: nc.tensor.matmul(
                            ps[:, 0:n], lhsT=lup[:, z, pt * 128:(pt + 1) * 128], rhs=lrtz[:, t0:t0 + n],
                            start=True, stop=True), reads=[lup.d, lrtz.d], writes=[ps.d])
                        kb.op("act", lambda t0=t0, n=n, ps=ps: nc.scalar.activation(
                            A[:, t0:t0 + n], ps[:, 0:n], ACT.Exp, bias=nb[:, z, pt:pt + 1], scale=-1.0),
                            reads=[ps.d, nb.d], writes=[A.d])
                    ckpt("k1")
                    kb.op("act", lambda: nc.scalar.activation(A[:], A[:], ACT.Ln, bias=1.0, scale=1.0),
                          reads=[A.d], writes=[A.d])
                    ckpt("k2")
                    if z == 0:
                        kb.op("dve", lambda: nc.vector.tensor_tensor_scan(B[:], mask[:, 0:NT], A[:], 0.0,
                                                                          ALU.mult, ALU.add),
                              reads=[mask.d, A.d], writes=[B.d])
                    else:
                        kb.op("dve", lambda: nc.vector.tensor_tensor_scan(B[:, NT - 1::-1] if False else B[:, ::-1],
                                                                          mask[:, NT:0:-1], A[:, ::-1], 0.0,
                                                                          ALU.mult, ALU.add),
                              reads=[mask.d, A.d], writes=[B.d])
                    ckpt("k3")
                    kb.op("act", lambda: nc.scalar.activation(A[:], B[:], ACT.Exp, scale=-1.0 / 16.0),
                          reads=[B.d], writes=[A.d])
                    kb.op("act", lambda: nc.scalar.activation(B[:], B[:], ACT.Exp, scale=1.0 / 16.0),
                          reads=[B.d], writes=[B.d])
                    ckpt("k4")
                    rq, rk = raw
                    kb.dma("sp", lambda: nc.sync.dma_start(out=rq[:], in_=S.qT[pt * 128:(pt + 1) * 128, :]),
                           reads=[S.qT_d], writes=[rq.d])
                    kb.dma("sp", lambda: nc.sync.dma_start(out=rk[:], in_=S.kT[pt * 128:(pt + 1) * 128, :]),
                           reads=[S.kT_d], writes=[rk.d])
                    ckpt("k5")
                    kb.op("dve", lambda: nc.vector.scalar_tensor_tensor(qt[z][:, pt, :], rq[:], 0.125, A[:],
                                                                        op0=ALU.mult, op1=ALU.mult),
                          reads=[rq.d, A.d], writes=[qt[z].d])
                    ckpt("k6")
                    kb.op("pool", lambda: nc.gpsimd.tensor_tensor(kt[z][:, pt, :], rk[:], B[:], op=ALU.mult),
                          reads=[rk.d, B.d], writes=[kt[z].d])
                    ckpt("k7")
                    e0 = 63 if z == 0 else 0
                    kb.op("dve", lambda: nc.vector.tensor_copy(elast[z][:, pt, :], A[:, e0:NT:64]),
                          reads=[A.d], writes=[elast[z].d])
        kb.barrier()
        if STOP == 'c1':
            return
        vt = T(kb, es, [128, NT // 128, 512], BF16, "vt")
        kb.dma("act", lambda: nc.scalar.dma_start(out=vt[:], in_=S.v.ap().rearrange("(n p) e -> p n e", p=128)),
               reads=[S.v_d], writes=[vt.d])
        ktm = [T(kb, es, [128, NT // 128, 256], BF16, f"ktm{z}") for z in range(2)]
        oT = T(kb, es, [128, 4, NL], BF16, "oT", nd=NL // 64)
        for z in range(2):
            for n2 in range(0, NT // 128, 2):
                for a in range(2):
                    for pt in range(2):
                        kb.op("pe", lambda a=a, pt=pt, n2=n2: nc.tensor.transpose(
                            psb[:, (a * 2 + pt) * 128:(a * 2 + pt + 1) * 128],
                            kt[z][:, pt, (n2 + a) * 128:(n2 + a + 1) * 128], g.identb[:]),
                            reads=[kt[z].d, g.identb.d], writes=[psb.d])
                kb.op("act", lambda n2=n2: nc.scalar.copy(
                    ktm[z][:, n2:n2 + 2, :], psb[:, 0:512].rearrange("p (a f) -> p a f", a=2)),
                    reads=[psb.d], writes=[ktm[z].d])
        if "qtl" in g.dbg:
            dbg_out(g, "qtl0", qt[0][:], [qt[0].d], [128, 2, NT], BF16)
            dbg_out(g, "ktl1", kt[1][:], [kt[1].d], [128, 2, NT], BF16)
            dbg_out(g, "ktm1", ktm[1][:], [ktm[1].d], [128, NT // 128, 256], BF16)
            dbg_out(g, "elast1", elast[1][:], [elast[1].d], [128, 2, NCH])
        if STOP == 'c2':
            return
        Sst = [T(kb, es, [128, 2, 128], F32, f"Sst{z}") for z in range(2)]
        SbfZ = [T(kb, es, [128, 4, 128], BF16, f"SbfZ{z}") for z in range(2)]
        tmpS = [T(kb, es, [128, 2, 128], F32, f"tmpS{z}") for z in range(2)]
        smZ = [[T(kb, es, [128, 4, 64], BF16, f"smZ{z}_{i}") for i in range(2)] for z in range(2)]
        for z in range(2):
            kb.op("pool", lambda z=z: nc.gpsimd.memset(Sst[z][:], 0.0), writes=[Sst[z].d])
            kb.op("pool", lambda z=z: nc.gpsimd.memset(SbfZ[z][:], 0.0), writes=[SbfZ[z].d])
            for i in range(2):
                kb.op("pool", lambda z=z, i=i: nc.gpsimd.memset(smZ[z][i][:], 0.0), writes=[smZ[z][i].d])
        order = [list(range(NCH)), [3, 2, 1, 0] + list(range(NCH - 1, 3, -1))]
        written = set()
        for i in range(NCH):
            if i % 4 == 0:
                kb.pump(1)
            for z in range(2):
                n = order[z][i]
                t0 = 64 * n
                nt = n // 2
                jo = (n % 2) * 64
                if n >= 4:
                    smt = smZ[z][n % 2]
                    for par in range(2):
                        ho = par * 64
                        ps_s = next_ps(g)
                        for hh in range(2):
                            h = hh * 2 + par
                            pt = hh
                            kb.op("pe", lambda h=h, pt=pt, ho=ho, ps_s=ps_s, hh=hh: nc.tensor.matmul(
                                ps_s[jo:jo + 64, hh * 64:(hh + 1) * 64], lhsT=kt[z][ho:ho + 64, pt, t0:t0 + 64],
                                rhs=qt[z][ho:ho + 64, pt, t0:t0 + 64], start=True, stop=True),
                                reads=[kt[z].d, qt[z].d], writes=[ps_s.d])
                        kb.op("dve", lambda ps_s=ps_s, smt=smt, par=par: nc.vector.tensor_tensor(
                            smt[jo:jo + 64, par:4:2, :], ps_s[jo:jo + 64, 0:128].rearrange("p (h i) -> p h i", h=2),
                            mk[z][jo:jo + 64, 0:2, :], op=ALU.mult), reads=[ps_s.d, mk[z].d], writes=[smt.d])
                    ps_o = next_ps(g)
                    for h in range(4):
                        pt = h // 2
                        kb.op("pe", lambda h=h, ps_o=ps_o, smt=smt: nc.tensor.matmul(
                            ps_o[:, h * 64:(h + 1) * 64], lhsT=vt[:, nt, h * 128:(h + 1) * 128],
                            rhs=smt[:, h, :], start=True, stop=False),
                            reads=[vt.d, smt.d], writes=[ps_o.d])
                        kb.op("pe", lambda h=h, pt=pt, ps_o=ps_o: nc.tensor.matmul(
                            ps_o[:, h * 64:(h + 1) * 64], lhsT=SbfZ[z][:, h, :],
                            rhs=qt[z][:, pt, t0:t0 + 64], start=False, stop=True),
                            reads=[SbfZ[z].d, qt[z].d], writes=[ps_o.d])
                    tl = t0 - NCX
                    od = oT.ds[tl // 64]
                    osl = oT[:, :, tl:tl + 64]
                    pv = ps_o[:, 0:256].rearrange("p (h i) -> p h i", h=4)
                    if n not in written:
                        written.add(n)
                        kb.op("act", lambda osl=osl, pv=pv: nc.scalar.copy(osl, pv), reads=[ps_o.d], writes=[od])
                    else:
                        kb.op("dve", lambda osl=osl, pv=pv: nc.vector.tensor_tensor(osl, pv, osl, op=ALU.add),
                              reads=[ps_o.d, od], writes=[od])
                ps_kv = next_ps(g)
                for h in range(4):
                    pt, ho = h // 2, (h % 2) * 64
                    kb.op("pe", lambda h=h, pt=pt, ho=ho, ps_kv=ps_kv: nc.tensor.matmul(
                        ps_kv[ho:ho + 64, pt * 128:(pt + 1) * 128], lhsT=ktm[z][jo:jo + 64, nt, h * 64:(h + 1) * 64],
                        rhs=vt[jo:jo + 64, nt, h * 128:(h + 1) * 128], start=True, stop=True),
                        reads=[ktm[z].d, vt.d], writes=[ps_kv.d])
                kb.op("dve", lambda ps_kv=ps_kv: nc.vector.tensor_tensor(
                    tmpS[z][:], ps_kv[:, 0:256].rearrange("p (t e) -> p t e", t=2), Sst[z][:], op=ALU.add),
                    reads=[ps_kv.d, Sst[z].d], writes=[tmpS[z].d])
                kb.op("dve", lambda n=n: nc.vector.tensor_tensor(
                    Sst[z][:], tmpS[z][:], elast[z][:, :, n:n + 1].to_broadcast([128, 2, 128]), op=ALU.mult),
                    reads=[tmpS[z].d, elast[z].d], writes=[Sst[z].d])
                for par in range(2):
                    ho = par * 64
                    kb.op("act", lambda par=par, ho=ho: nc.scalar.copy(SbfZ[z][ho:ho + 64, par:4:2, :],
                                                                       Sst[z][ho:ho + 64, :, :]),
                          reads=[Sst[z].d], writes=[SbfZ[z].d])
        if "oT" in g.dbg:
            dbg_out(g, "oT", oT[:], oT.ds, [128, 4, NL], BF16)
        if STOP == 'c3':
            return
        sq = T(kb, es, [128, 512], BF16, "gsq")
        rstd = T(kb, es, [128, 512], F32, "grstd")
        rt = [T(kb, es, [128, 4, 512], BF16, f"grt{i}") for i in range(2)]
        gl = [T(kb, es, [128, 4, 512], BF16, f"ggl{i}") for i in range(1)]
        tmpb = T(kb, es, [128, 512], BF16, "gtmp")
        for sp in range(NL // 512):
            c0 = sp * 512
            r_t, g_t = rt[sp % 2], gl[0]
            kb.dma("sp", lambda r_t=r_t, c0=c0: nc.sync.dma_start(
                out=r_t[:], in_=S.rT[:, c0:c0 + 512].rearrange("(m p) t -> p m t", p=128)),
                reads=[S.rT_d], writes=[r_t.d])
            kb.op("act", lambda r_t=r_t: nc.scalar.activation(r_t[:], r_t[:], ACT.Silu), reads=[r_t.d], writes=[r_t.d])
            ods = oT.ds[c0 // 64:(c0 + 512) // 64]
            for h in range(4):
                kb.op("dve", lambda h=h: nc.vector.tensor_tensor(sq[:], oT[:, h, c0:c0 + 512], oT[:, h, c0:c0 + 512],
                                                                 op=ALU.mult), reads=ods, writes=[sq.d])
                ps = next_ps(g)
                kb.op("pe", lambda ps=ps: nc.tensor.matmul(ps[:, :], lhsT=ones[:], rhs=sq[:], start=True, stop=True),
                      reads=[ones.d, sq.d], writes=[ps.d])
                kb.op("dve", lambda ps=ps: nc.vector.tensor_scalar(rstd[:], ps[:, :], 1.0 / 128.0, EPS,
                                                                   op0=ALU.mult, op1=ALU.add),
                      reads=[ps.d], writes=[rstd.d])
                kb.op("act", lambda: nc.scalar.activation(rstd[:], rstd[:], ACT.Sqrt), reads=[rstd.d], writes=[rstd.d])
                kb.op("dve", lambda: nc.vector.reciprocal(rstd[:], rstd[:]), reads=[rstd.d], writes=[rstd.d])
                kb.op("dve", lambda h=h: nc.vector.scalar_tensor_tensor(
                    tmpb[:], oT[:, h, c0:c0 + 512], nw[:, 0:1], rstd[:], op0=ALU.mult, op1=ALU.mult),
                    reads=ods + [nw.d, rstd.d], writes=[tmpb.d])
                kb.op("pool", lambda h=h, g_t=g_t, r_t=r_t: nc.gpsimd.tensor_tensor(
                    g_t[:, h, :], tmpb[:], r_t[:, h, :], op=ALU.mult), reads=[tmpb.d, r_t.d], writes=[g_t.d])
            kb.dma("act", lambda g_t=g_t, c0=c0: nc.scalar.dma_start(
                out=S.glaT[:, c0:c0 + 512].rearrange("(m p) t -> p m t", p=128), in_=g_t[:]),
                reads=[g_t.d], writes=[S.glaT_d])


def cmul(g, out_r, out_i, ar, ai, br, bi, tmp, deps_in, dep_out, sl=None):
    nc, kb = g.nc, g.kb
    kb.op("dve", lambda: nc.vector.tensor_tensor(tmp, ai, bi, op=ALU.mult), reads=deps_in, writes=[dep_out])
    kb.op("dve", lambda: nc.vector.tensor_tensor(out_r, ar, br, op=ALU.mult), reads=deps_in, writes=[dep_out])
    kb.op("dve", lambda: nc.vector.tensor_tensor(out_r, out_r, tmp, op=ALU.subtract), reads=[dep_out], writes=[dep_out])
    kb.op("dve", lambda: nc.vector.tensor_tensor(tmp, ai, br, op=ALU.mult), reads=deps_in, writes=[dep_out])
    kb.op("dve", lambda: nc.vector.tensor_tensor(out_i, ar, bi, op=ALU.mult), reads=deps_in, writes=[dep_out])
    kb.op("dve", lambda: nc.vector.tensor_tensor(out_i, out_i, tmp, op=ALU.add), reads=[dep_out], writes=[dep_out])


def phase_d(g):
    nc, kb, H, S = g.nc, g.kb, g.H, g.S
    NCK = NT // 8
    NMAC = NCK // 16
    io = g.iota
    with ExitStack() as es:
        psb = g.psb[0]
        pd = Dep("s5par")
        P0 = T(kb, es, [128, 24, 64], F32, "s5p0")
        pl = lambda i: P0[:, i, :]
        LRE, LIM, DT, MAG, CS, SN, T1, T2, T3, LBR, LBI, CR, CI, IR, II = range(15)
        for half in range(2):
            rows = slice(half * 64, half * 64 + 64)
            kb.dma("sp", lambda rows=rows: nc.sync.dma_start(
                out=P0[rows, LRE, :], in_=H.s5_lam_re[0].rearrange("z g p -> p (z g)"),
                allow_slow_non_contiguous=True), writes=[pd])
            kb.dma("act", lambda rows=rows: nc.scalar.dma_start(
                out=P0[rows, LIM, :], in_=H.s5_lam_im[0].rearrange("z g p -> p (z g)"),
                allow_slow_non_contiguous=True), writes=[pd])
        kb.dma("sp", lambda: nc.sync.dma_start(
            out=pl(DT), in_=H.s5_log_dt[0:1, :, :].rearrange("o z g -> o (z g)").partition_broadcast(128)),
            writes=[pd])
        D1 = [pd]

        def v(fn):
            kb.op("dve", fn, reads=D1, writes=D1)

        def a(fn):
            kb.op("act", fn, reads=D1, writes=D1)

        a(lambda: nc.scalar.activation(pl(DT), pl(DT), ACT.Exp))
        v(lambda: nc.vector.tensor_tensor(pl(MAG), pl(LRE), pl(DT), op=ALU.mult))
        a(lambda: nc.scalar.activation(pl(MAG), pl(MAG), ACT.Exp))
        v(lambda: nc.vector.tensor_tensor(pl(T1), pl(LIM), pl(DT), op=ALU.mult))
        a(lambda: nc.scalar.activation(pl(SN), pl(T1), ACT.Sin, scale=1.0 / 16.0))
        a(lambda: nc.scalar.activation(pl(CS), pl(T1), ACT.Sin, bias=float(np.pi / 2), scale=1.0 / 16.0))
        for _ in range(4):
            v(lambda: nc.vector.tensor_tensor(pl(T2), pl(CS), pl(CS), op=ALU.mult))
            v(lambda: nc.vector.tensor_tensor(pl(T3), pl(SN), pl(SN), op=ALU.mult))
            v(lambda: nc.vector.scalar_tensor_tensor(pl(SN), pl(CS), 2.0, pl(SN), op0=ALU.mult, op1=ALU.mult))
            v(lambda: nc.vector.tensor_tensor(pl(CS), pl(T2), pl(T3), op=ALU.subtract))
        v(lambda: nc.vector.tensor_tensor(pl(LBR), pl(MAG), pl(CS), op=ALU.mult))
        v(lambda: nc.vector.tensor_tensor(pl(LBI), pl(MAG), pl(SN), op=ALU.mult))
        v(lambda: nc.vector.tensor_tensor(pl(T1), pl(LRE), pl(LRE), op=ALU.mult))
        v(lambda: nc.vector.tensor_tensor(pl(T2), pl(LIM), pl(LIM), op=ALU.mult))
        v(lambda: nc.vector.tensor_tensor(pl(T1), pl(T1), pl(T2), op=ALU.add))
        v(lambda: nc.vector.reciprocal(pl(T1), pl(T1)))
        v(lambda: nc.vector.tensor_scalar(pl(T2), pl(LBR), -1.0, None, op0=ALU.add))
        v(lambda: nc.vector.tensor_tensor(pl(CR), pl(T2), pl(LRE), op=ALU.mult))
        v(lambda: nc.vector.tensor_tensor(pl(T3), pl(LBI), pl(LIM), op=ALU.mult))
        v(lambda: nc.vector.tensor_tensor(pl(CR), pl(CR), pl(T3), op=ALU.add))
        v(lambda: nc.vector.tensor_tensor(pl(CR), pl(CR), pl(T1), op=ALU.mult))
        v(lambda: nc.vector.tensor_tensor(pl(CI), pl(LBI), pl(LRE), op=ALU.mult))
        v(lambda: nc.vector.tensor_tensor(pl(T3), pl(T2), pl(LIM), op=ALU.mult))
        v(lambda: nc.vector.tensor_tensor(pl(CI), pl(CI), pl(T3), op=ALU.subtract))
        v(lambda: nc.vector.tensor_tensor(pl(CI), pl(CI), pl(T1), op=ALU.mult))
        v(lambda: nc.vector.tensor_tensor(pl(T1), pl(LBR), pl(LBR), op=ALU.mult))
        v(lambda: nc.vector.tensor_tensor(pl(T2), pl(LBI), pl(LBI), op=ALU.mult))
        v(lambda: nc.vector.tensor_tensor(pl(T1), pl(T1), pl(T2), op=ALU.add))
        v(lambda: nc.vector.reciprocal(pl(T1), pl(T1)))
        v(lambda: nc.vector.tensor_tensor(pl(IR), pl(LBR), pl(T1), op=ALU.mult))
        v(lambda: nc.vector.scalar_tensor_tensor(pl(II), pl(LBI), -1.0, pl(T1), op0=ALU.mult, op1=ALU.mult))
        PW = T(kb, es, [128, 9, 2, 64], F32, "s5pw")
        NW = T(kb, es, [128, 8, 2, 64], F32, "s5nw")
        for W, br_, bi_, n in ((PW, LBR, LBI, 9), (NW, IR, II, 8)):
            v(lambda W=W: nc.vector.memset(W[:, 0, 0, :], 1.0))
            v(lambda W=W: nc.vector.memset(W[:, 0, 1, :], 0.0))
            for k in range(1, n):
                cmul(g, W[:, k, 0, :], W[:, k, 1, :], W[:, k - 1, 0, :], W[:, k - 1, 1, :], pl(br_), pl(bi_),
                     pl(T3), D1, pd)
        P128 = T(kb, es, [128, 2, 64], F32, "s5p128")
        v(lambda: nc.vector.tensor_copy(P128[:], PW[:, 8, :, :]))
        for _ in range(4):
            cmul(g, pl(T1), pl(T2), P128[:, 0, :], P128[:, 1, :], P128[:, 0, :], P128[:, 1, :], pl(T3), D1, pd)
            v(lambda: nc.vector.tensor_copy(P128[:, 0, :], pl(T1)))
            v(lambda: nc.vector.tensor_copy(P128[:, 1, :], pl(T2)))
        WN = T(kb, es, [128, 8, 2, 64], F32, "s5wn")
        WP = T(kb, es, [128, 8, 2, 64], F32, "s5wp")
        for k in range(8):
            cmul(g, WN[:, k, 0, :], WN[:, k, 1, :], NW[:, k, 0, :], NW[:, k, 1, :], pl(CR), pl(CI), pl(T3), D1, pd)
            cmul(g, WP[:, k, 0, :], WP[:, k, 1, :], PW[:, k, 0, :], PW[:, k, 1, :], pl(CR), pl(CI), pl(T3), D1, pd)
        v(lambda: nc.vector.tensor_scalar(PW[64:128, :, 0, :], PW[64:128, :, 0, :], -1.0, None, op0=ALU.mult))
        v(lambda: nc.vector.tensor_scalar(WN[0:64, :, 1, :], WN[0:64, :, 1, :], -1.0, None, op0=ALU.mult))
        v(lambda: nc.vector.tensor_scalar(WP[0:64, :, 1, :], WP[0:64, :, 1, :], -1.0, None, op0=ALU.mult))
        A8s = T(kb, es, [128, 2, 64], F32, "s5a8")
        v(lambda: nc.vector.tensor_copy(A8s[:, 1, :], PW[:, 8, 1, :]))
        v(lambda: nc.vector.tensor_copy(A8s[0:64, 0, :], PW[0:64, 8, 0, :]))
        v(lambda: nc.vector.tensor_scalar(A8s[64:128, 0, :], PW[64:128, 8, 0, :], -1.0, None, op0=ALU.mult))
        Jt = T(kb, es, [128, 128], F32, "s5jt")
        v(lambda: nc.vector.tensor_single_scalar(Jt[:], io[:], 64.0, op=ALU.is_equal))
        v(lambda: nc.vector.tensor_single_scalar(pl(T1)[:, 0:64], io[:, 0:64], -64.0, op=ALU.is_equal))
        v(lambda: nc.vector.tensor_tensor(Jt[:, 0:64], Jt[:, 0:64], pl(T1)[:, 0:64], op=ALU.subtract))
        bm = []
        for z in range(2):
            m = T(kb, es, [128, 128], F32, f"s5bm{z}")
            v(lambda m=m: nc.vector.memset(m[:], 0.0))
            for j in range(0, 8, 2):
                for jj in range(2):
                    pass
            bm.append(m)
        rowb = T(kb, es, [128, 1], F32, "s5rowb")
        pcol = T(kb, es, [128, 1], F32, "s5pcol")
        kb.op("pool", lambda: nc.gpsimd.iota(pcol[:], pattern=[[0, 1]], base=0, channel_multiplier=1,
                                              allow_small_or_imprecise_dtypes=True), writes=[pd])
        v(lambda: nc.vector.memset(rowb[:], 0.0))
        for t in range(1, 8):
            v(lambda t=t: nc.vector.tensor_scalar(pl(T1)[:, 0:1], pcol[:], float(16 * t), 16.0, op0=ALU.is_ge, op1=ALU.mult))
            v(lambda: nc.vector.tensor_tensor(rowb[:], rowb[:], pl(T1)[:, 0:1], op=ALU.add))
        colf = T(kb, es, [128, 128], F32, "s5colf")
        kb.op("pool", lambda: nc.gpsimd.iota(colf[:], pattern=[[1, 128]], base=0, channel_multiplier=0,
                                              allow_small_or_imprecise_dtypes=True), writes=[pd])
        v(lambda: nc.vector.tensor_scalar(bm[0][:], colf[:], rowb[:, 0:1], 0.0, op0=ALU.subtract, op1=ALU.is_ge))
        v(lambda: nc.vector.tensor_scalar(bm[1][:], colf[:], rowb[:, 0:1], 15.0, op0=ALU.subtract, op1=ALU.is_le))
        CC = T(kb, es, [128, 64, 16], F32, "s5cc")
        CCs = T(kb, es, [128, 64, 16], F32, "s5ccs")
        BB = T(kb, es, [128, 64, 16], F32, "s5bb")
        BBs = T(kb, es, [128, 64, 16], F32, "s5bbs")
        kb.dma("sp", lambda: nc.sync.dma_start(out=BB[0:64, :, :], in_=H.s5_b_re[0].rearrange("z g p h -> p (z g) h")),
               writes=[pd])
        kb.dma("act", lambda: nc.scalar.dma_start(out=BB[64:128, :, :], in_=H.s5_b_im[0].rearrange("z g p h -> p (z g) h")),
               writes=[pd])
        kb.dma("sp", lambda: nc.sync.dma_start(out=BBs[0:64, :, :], in_=H.s5_b_im[0].rearrange("z g p h -> p (z g) h")),
               writes=[pd])
        kb.dma("act", lambda: nc.scalar.dma_start(out=BBs[64:128, :, :], in_=H.s5_b_re[0].rearrange("z g p h -> p (z g) h")),
               writes=[pd])
        with ExitStack() as esx:
            xc = [T(kb, esx, [128, 8, 128], F32, f"s5xc{i}") for i in range(2)]
            for i, (aa, bb) in enumerate(((H.s5_c_re, H.s5_c_im), (H.s5_c_im, H.s5_c_re))):
                kb.dma("sp", lambda aa=aa, i=i: nc.sync.dma_start(
                    out=xc[i][:, :, 0:64], in_=aa[0].rearrange("z g h p -> (z g h) p").rearrange("(t r) p -> r t p", r=128)),
                    writes=[pd])
                kb.dma("act", lambda bb=bb, i=i: nc.scalar.dma_start(
                    out=xc[i][:, :, 64:128], in_=bb[0].rearrange("z g h p -> (z g h) p").rearrange("(t r) p -> r t p", r=128)),
                    writes=[pd])
            for i, dst in enumerate((CC, CCs)):
                for t in range(8):
                    ps = next_ps(g)
                    kb.op("pe", lambda t=t, i=i, ps=ps: nc.tensor.transpose(ps[:, 0:128], xc[i][:, t, :], g.ident[:]),
                          reads=[pd, g.ident.d], writes=[ps.d])
                    kb.op("act", lambda t=t, dst=dst, ps=ps: nc.scalar.copy(
                        dst[:, t * 8:(t + 1) * 8, :], ps[:, 0:128].rearrange("p (a h) -> p a h", a=8)),
                        reads=[ps.d], writes=[pd])
            kb.barrier()
        with ExitStack() as esu:
            U8 = [T(kb, esu, [128, 8, 512], BF16, f"s5u8_{i}") for i in range(2)]
            U8g = [T(kb, esu, [128, 32, 128], BF16, f"s5u8g_{i}") for i in range(2)]
            utst = [T(kb, esu, [128, 4, 128], BF16, f"s5utst_{i}") for i in range(2)]
            blocks = [(0, 32)] + [(32 + 128 * b, 128) for b in range(4)]
            for bi_, (c0, ncb) in enumerate(blocks):
                kb.pump(1)
                u8, u8g = U8[bi_ % 2], U8g[bi_ % 2]
                kb.dma("sp", lambda u8=u8, c0=c0, ncb=ncb: nc.sync.dma_start(
                    out=u8[0:ncb, :, :], in_=S.u[c0 * 8:(c0 + ncb) * 8, :].rearrange("(c j) f -> c j f", j=8)),
                    reads=[S.u_d], writes=[u8.d])
                kb.op("pool", lambda u8=u8, u8g=u8g, ncb=ncb: nc.gpsimd.tensor_copy(
                    u8g[0:ncb, :, :].rearrange("c g (j h) -> c g j h", j=8),
                    u8[0:ncb, :, :].rearrange("c j (g h) -> c g j h", g=32)), reads=[u8.d], writes=[u8g.d])
                for g4 in range(0, 32, 4):
                    for gg in range(4):
                        kb.op("pe", lambda gg=gg, g4=g4, u8g=u8g, ncb=ncb: nc.tensor.transpose(
                            psb[:, gg * 128:gg * 128 + ncb], u8g[0:ncb, g4 + gg, :], g.identb[0:ncb, 0:ncb]),
                            reads=[u8g.d, g.identb.d], writes=[psb.d])
                    ust = utst[(g4 // 4) % 2]
                    kb.op("act", lambda g4=g4, c0=c0, ncb=ncb, ust=ust: nc.scalar.copy(
                        ust[:, :, 0:ncb], psb[:, 0:512].rearrange("p (a c) -> p a c", a=4)[:, :, 0:ncb]),
                        reads=[psb.d], writes=[ust.d])
                    kb.dma("act", lambda g4=g4, c0=c0, ncb=ncb, ust=ust: nc.scalar.dma_start(
                        out=S.Ut[g4:g4 + 4, :, c0:c0 + ncb].rearrange("a p c -> p a c"), in_=ust[:, :, 0:ncb]),
                        reads=[ust.d], writes=[S.Ut_d])
            kb.barrier()
        for z in range(2):
            gs = slice(z * 32, z * 32 + 32)
            with ExitStack() as ez:
                MT = T(kb, ez, [128, 32, 128], BF16, "s5mt")
                RT = T(kb, ez, [128, 32, 128], BF16, "s5rt")
                OTb = T(kb, ez, [128, 32, 128], BF16, "s5otb")
                A8T = T(kb, ez, [128, 32, 128], F32, "s5a8t")
                with ExitStack() as ep:
                    Gall = T(kb, ep, [128, 32, 9, 16], F32, "s5gall")
                    Kn = T(kb, ep, [128, 32, 8, 16], F32, "s5kn")
                    Kp = T(kb, ep, [128, 32, 8, 16], F32, "s5kp")
                    tmpk = T(kb, ep, [128, 32, 16], F32, "s5tmpk")
                    bc = lambda ap2: ap2.unsqueeze(2).to_broadcast([128, 32, 16])
                    for k in range(9):
                        i = k if z == 0 else 8 - k
                        v(lambda k=k, i=i: nc.vector.tensor_tensor(Gall[:, :, i, :], CC[:, gs, :], bc(PW[:, k, 0, gs]),
                                                                  op=ALU.mult))
                        v(lambda k=k: nc.vector.tensor_tensor(tmpk[:], CCs[:, gs, :], bc(PW[:, k, 1, gs]), op=ALU.mult))
                        v(lambda i=i: nc.vector.tensor_tensor(Gall[:, :, i, :], Gall[:, :, i, :], tmpk[:], op=ALU.subtract))
                    for k in range(8):
                        j = k if z == 0 else 7 - k
                        v(lambda k=k, j=j: nc.vector.tensor_tensor(Kn[:, :, j, :], BB[:, gs, :], bc(WN[:, k, 0, gs]),
                                                                  op=ALU.mult))
                        v(lambda k=k: nc.vector.tensor_tensor(tmpk[:], BBs[:, gs, :], bc(WN[:, k, 1, gs]), op=ALU.mult))
                        v(lambda j=j: nc.vector.tensor_tensor(Kn[:, :, j, :], Kn[:, :, j, :], tmpk[:], op=ALU.add))
                        j2 = 7 - k if z == 0 else k
                        v(lambda k=k, j2=j2: nc.vector.tensor_tensor(Kp[:, :, j2, :], BB[:, gs, :], bc(WP[:, k, 0, gs]),
                                                                    op=ALU.mult))
                        v(lambda k=k: nc.vector.tensor_tensor(tmpk[:], BBs[:, gs, :], bc(WP[:, k, 1, gs]), op=ALU.mult))
                        v(lambda j2=j2: nc.vector.tensor_tensor(Kp[:, :, j2, :], Kp[:, :, j2, :], tmpk[:], op=ALU.add))
                    q0 = 0 if z == 0 else 1
                    o0 = 1 if z == 0 else 0
                    for gi in range(32):
                        ps = next_ps(g)
                        kb.op("pe", lambda gi=gi, ps=ps: nc.tensor.matmul(
                            ps[:, 0:128], lhsT=Kn[:, gi, :, :].rearrange("p j h -> p (j h)"),
                            rhs=Gall[:, gi, q0:q0 + 8, :].rearrange("p s h -> p (s h)"), start=True, stop=True),
                            reads=D1, writes=[ps.d])
                        kb.op("dve", lambda gi=gi, ps=ps: nc.vector.tensor_tensor(MT[:, gi, :], ps[:, 0:128], bm[z][:],
                                                                                 op=ALU.mult),
                              reads=[ps.d] + D1, writes=[MT.d])
                        ps2 = next_ps(g)
                        kb.op("pe", lambda gi=gi, ps2=ps2: nc.tensor.transpose(
                            ps2[:, 0:128], Kp[:, gi, :, :].rearrange("p j h -> p (j h)"), g.ident[:]),
                            reads=D1 + [g.ident.d], writes=[ps2.d])
                        kb.op("act", lambda gi=gi, ps2=ps2: nc.scalar.copy(RT[:, gi, :], ps2[:, 0:128]),
                              reads=[ps2.d], writes=[RT.d])
                        kb.op("act", lambda gi=gi: nc.scalar.copy(
                            OTb[:, gi, :], Gall[:, gi, o0:o0 + 8, :].rearrange("p s h -> p (s h)")),
                            reads=D1, writes=[OTb.d])
                        kb.op("pool", lambda gi=gi: nc.gpsimd.tensor_scalar(
                            A8T[:, gi, :], g.ident[:], A8s[:, 0, z * 32 + gi:z * 32 + gi + 1], None, op0=ALU.mult),
                            reads=D1 + [g.ident.d], writes=[A8T.d])
                        kb.op("dve", lambda gi=gi: nc.vector.scalar_tensor_tensor(
                            A8T[:, gi, :], Jt[:], A8s[:, 1, z * 32 + gi:z * 32 + gi + 1], A8T[:, gi, :],
                            op0=ALU.mult, op1=ALU.add), reads=D1 + [A8T.d], writes=[A8T.d])
                    kb.barrier()
                if f"s5mat{z}" in g.dbg:
                    dbg_out(g, f"MT{z}", MT[:], [MT.d], [128, 32, 128], BF16)
                    dbg_out(g, f"RT{z}", RT[:], [RT.d], [128, 32, 128], BF16)
                    dbg_out(g, f"OTb{z}", OTb[:], [OTb.d], [128, 32, 128], BF16)
                    dbg_out(g, f"A8T{z}", A8T[:], [A8T.d], [128, 32, 128], F32)
                X = T(kb, ez, [128, 32, NCK], F32, "s5x", nd=32)
                utg = [T(kb, ez, [128, NCK], BF16, f"s5utg{i}") for i in range(3)]
                for gi in range(32):
                    ug = utg[gi % 3]
                    kb.dma("sp", lambda gi=gi, ug=ug: nc.sync.dma_start(out=ug[:], in_=S.Ut[gi, :, :]),
                           reads=[S.Ut_d], writes=[ug.d])
                    for (c0, n) in ((0, 512), (512, NCK - 512)):
                        ps = next_ps(g)
                        kb.op("pe", lambda gi=gi, c0=c0, n=n, ps=ps: nc.tensor.matmul(
                            ps[:, 0:n], lhsT=RT[:, gi, :], rhs=ug[:, c0:c0 + n], start=True, stop=True),
                            reads=[RT.d, ug.d], writes=[ps.d])
                        eng = "act" if gi % 2 == 0 else "dve"
                        if eng == "act":
                            kb.op("act", lambda gi=gi, c0=c0, n=n, ps=ps: nc.scalar.copy(X[:, gi, c0:c0 + n], ps[:, 0:n]),
                                  reads=[ps.d], writes=[X.ds[gi]])
                        else:
                            kb.op("dve", lambda gi=gi, c0=c0, n=n, ps=ps: nc.vector.tensor_copy(X[:, gi, c0:c0 + n], ps[:, 0:n]),
                                  reads=[ps.d], writes=[X.ds[gi]])
                cur = [T(kb, ez, [128, 32, NMAC], F32, f"s5cur{i}") for i in range(2)]
                Gm = T(kb, ez, [128, 32, NMAC], F32, "s5gm")
                gv = T(kb, ez, [128, 32], F32, "s5gv")
                tu = T(kb, ez, [128, 32], F32, "s5tu")
                tt = T(kb, ez, [128, 32], F32, "s5tt")
                xall = X.ds
                colsel = (lambda i: i) if z == 0 else (lambda i: 15 - i)
                gbanks = ((0, 15), (15, 30), (30, 32))

                def step(src, dst, i, store):
                    col = colsel(i)
                    pss = []
                    for (g0, g1) in gbanks:
                        ps = next_ps(g)
                        pss.append(ps)
                        for gi in range(g0, g1):
                            kb.op("pe", lambda gi=gi, ps=ps, g0=g0: nc.tensor.matmul(
                                ps[:, (gi - g0) * NMAC:(gi - g0 + 1) * NMAC], lhsT=A8T[:, gi, :], rhs=src[:, gi, :],
                                start=True, stop=True), reads=[A8T.d, src.d], writes=[ps.d])
                    for (g0, g1), ps in zip(gbanks, pss):
                        kb.op("dve", lambda g0=g0, g1=g1, ps=ps: nc.vector.tensor_tensor(
                            dst[:, g0:g1, :], ps[:, 0:(g1 - g0) * NMAC].rearrange("p (a m) -> p a m", m=NMAC),
                            X[:, g0:g1, col:NCK:16], op=ALU.add), reads=[ps.d] + xall, writes=[dst.d])
                    if store:
                        kb.op("pool", lambda: nc.gpsimd.tensor_copy(X[:, :, col:NCK:16], src[:]),
                              reads=[src.d] + xall, writes=xall)

                kb.op("pool", lambda: nc.gpsimd.memset(cur[0][:], 0.0), writes=[cur[0].d])
                for i in range(16):
                    if i % 4 == 0:
                        kb.pump(1)
                    step(cur[i % 2], cur[(i + 1) % 2], i, False)
                Em = cur[0]
                qorder = list(range(NMAC)) if z == 0 else [1, 0] + list(range(NMAC - 1, 1, -1))
                kb.op("pool", lambda: nc.gpsimd.memset(gv[:], 0.0), writes=[gv.d])
                for q in qorder:
                    kb.op("act", lambda q=q: nc.scalar.copy(Gm[:, :, q], gv[:]), reads=[gv.d], writes=[Gm.d])
                    ps = next_ps(g)
                    kb.op("pe", lambda ps=ps: nc.tensor.matmul(ps[:, 0:32], lhsT=Jt[:], rhs=gv[:], start=True, stop=True),
                          reads=D1 + [gv.d], writes=[ps.d])
                    kb.op("dve", lambda ps=ps: nc.vector.tensor_tensor(tt[:], ps[:, 0:32], P128[:, 1, gs], op=ALU.mult),
                          reads=[ps.d] + D1, writes=[tt.d])
                    kb.op("pool", lambda q=q: nc.gpsimd.tensor_tensor(tu[:], gv[:], P128[:, 0, gs], op=ALU.mult),
                          reads=[gv.d] + D1, writes=[tu.d])
                    kb.op("pool", lambda q=q: nc.gpsimd.tensor_tensor(tu[:], tu[:], Em[:, :, q], op=ALU.add),
                          reads=[tu.d, Em.d], writes=[tu.d])
                    kb.op("dve", lambda: nc.vector.tensor_tensor(gv[:], tu[:], tt[:], op=ALU.add),
                          reads=[tu.d, tt.d], writes=[gv.d])
                kb.op("dve", lambda: nc.vector.tensor_copy(cur[0][:], Gm[:]), reads=[Gm.d], writes=[cur[0].d])
                for i in range(16):
                    step(cur[i % 2], cur[(i + 1) % 2], i, True)
                if f"s5x{z}" in g.dbg:
                    dbg_out(g, f"X{z}", X[:], xall, [128, 32, NCK], F32)
                xb = [T(kb, ez, [128, 512], BF16, f"s5xb{i}") for i in range(2)]
                yo = [T(kb, ez, [128, 512], F32, f"s5yo{i}") for i in range(2)]
                yfl = [T(kb, ez, [128, 512], F32, f"s5yfl{i}") for i in range(2)]
                for gi in range(32):
                    xbt = xb[gi % 2]
                    ug = utg[gi % 3]
                    kb.dma("sp", lambda gi=gi, ug=ug: nc.sync.dma_start(out=ug[:], in_=S.Ut[gi, :, :]),
                           reads=[S.Ut_d], writes=[ug.d])
                    kb.op("act", lambda gi=gi, xbt=xbt: nc.scalar.copy(xbt[:], X[:, gi, 32:NCK]), reads=xall, writes=[xbt.d])
                    ps = next_ps(g)
                    kb.op("pe", lambda gi=gi, ps=ps, ug=ug: nc.tensor.matmul(ps[:, :], lhsT=MT[:, gi, :], rhs=ug[:, 32:NCK],
                                                                             start=True, stop=False),
                          reads=[MT.d, ug.d], writes=[ps.d])
                    kb.op("pe", lambda gi=gi, ps=ps, xbt=xbt: nc.tensor.matmul(ps[:, :], lhsT=OTb[:, gi, :], rhs=xbt[:],
                                                                               start=False, stop=True),
                          reads=[OTb.d, xbt.d], writes=[ps.d])
                    yt = yo[gi % 2]
                    if z == 0:
                        kb.op("dve", lambda ps=ps, yt=yt: nc.vector.tensor_copy(yt[:], ps[:, :]),
                              reads=[ps.d], writes=[yt.d])
                    else:
                        yf = yfl[gi % 2]
                        kb.dma("act", lambda gi=gi, yf=yf: nc.scalar.dma_start(out=yf[:], in_=S.y0[gi, :, :]),
                               reads=[S.y0_d], writes=[yf.d])
                        kb.op("dve", lambda ps=ps, yt=yt, yf=yf: nc.vector.tensor_tensor(yt[:], ps[:, :], yf[:], op=ALU.add),
                              reads=[ps.d, yf.d], writes=[yt.d])
                    kb.dma("sp", lambda gi=gi, yt=yt: nc.sync.dma_start(out=S.y0[gi, :, :], in_=yt[:]),
                           reads=[yt.d], writes=[S.y0_d])
                kb.barrier()


SEG = 512
NSEG = 64


def bc_tile(g, es, name, src_row_ap, n, reads=()):
    nc, kb = g.nc, g.kb
    t = T(kb, es, [128, n], F32, name)
    kb.dma("sp", lambda: nc.sync.dma_start(out=t[:], in_=src_row_ap.partition_broadcast(128)),
           reads=list(reads), writes=[t.d])
    return t


def phase_e(g):
    nc, kb, H, S = g.nc, g.kb, g.H, g.S
    R = g.R
    with ExitStack() as es:
        psb = g.psb[0]
        s5T = T(kb, es, [128, 4, NL], BF16, "s5T", nd=4)
        with ExitStack() as e1:
            glw = T(kb, e1, [128, 4, 512], BF16, "glw")
            kb.dma("pool", lambda: nc.gpsimd.dma_start(out=glw[:], in_=H.glu_w[0].rearrange("(k p) n -> p k n", p=128)),
                   writes=[glw.d])
            glb = T(kb, e1, [128, 4], F32, "glb")
            kb.dma("sp", lambda: nc.sync.dma_start(out=glb[:], in_=H.glu_b[0, :].rearrange("(k p) -> p k", p=128),
                                                   allow_slow_non_contiguous=True), writes=[glb.d])
            s5d = T(kb, e1, [128, 4], F32, "s5d")
            kb.dma("sp", lambda: nc.sync.dma_start(out=s5d[:], in_=H.s5_d[0, :].rearrange("(k p) -> p k", p=128),
                                                   allow_slow_non_contiguous=True), writes=[s5d.d])
            Yg = T(kb, e1, [128, 32, 128], F32, "Yg")
            Ytm = T(kb, e1, [128, 8, 512], F32, "Ytm")
            yT = T(kb, e1, [128, 4, 1024], F32, "yTt")
            uTt = T(kb, e1, [128, 4, 1024], BF16, "uTt")
            t1 = T(kb, e1, [128, 4, 1024], F32, "glt1")
            glg = T(kb, e1, [128, 4, 1024], BF16, "glg")
            sg = T(kb, e1, [128, 512], BF16, "glsg")
            for cb in range(4):
                kb.pump(1)
                kb.dma("sp", lambda cb=cb: nc.sync.dma_start(
                    out=Yg[:], in_=S.y0[:, :, cb * 128:(cb + 1) * 128].rearrange("g p c -> p g c")),
                    reads=[S.y0_d], writes=[Yg.d])
                kb.dma("act", lambda cb=cb: nc.scalar.dma_start(
                    out=uTt[:], in_=S.uT[:, NCX + cb * 1024:NCX + (cb + 1) * 1024].rearrange("(k p) t -> p k t", p=128)),
                    reads=[S.uT_d], writes=[uTt.d])
                for g4 in range(0, 32, 4):
                    ps = next_ps(g)
                    for gg in range(4):
                        kb.op("pe", lambda gg=gg, g4=g4, ps=ps: nc.tensor.transpose(
                            ps[:, gg * 128:(gg + 1) * 128], Yg[:, g4 + gg, :], g.ident[:]),
                            reads=[Yg.d, g.ident.d], writes=[ps.d])
                    kb.op("act", lambda g4=g4, ps=ps: nc.scalar.copy(
                        Ytm[:, :, g4 * 16:g4 * 16 + 64].rearrange("c s (a h) -> c a s h", a=4),
                        ps[:, :].rearrange("c (a s h) -> c a s h", a=4, s=8)), reads=[ps.d], writes=[Ytm.d])
                for s_ in range(8):
                    ps = next_ps(g)
                    for cc in range(4):
                        kb.op("pe", lambda cc=cc, s_=s_, ps=ps: nc.tensor.transpose(
                            ps[:, cc * 128:(cc + 1) * 128], Ytm[:, s_, cc * 128:(cc + 1) * 128], g.ident[:]),
                            reads=[Ytm.d, g.ident.d], writes=[ps.d])
                    kb.op("dve", lambda s_=s_, ps=ps: nc.vector.tensor_copy(
                        yT[:, :, s_:1024:8], ps[:, :].rearrange("p (a c) -> p a c", a=4)), reads=[ps.d], writes=[yT.d])
                for cc in range(4):
                    kb.op("dve", lambda cc=cc: nc.vector.scalar_tensor_tensor(
                        yT[:, cc, :], uTt[:, cc, :], s5d[:, cc:cc + 1], yT[:, cc, :], op0=ALU.mult, op1=ALU.add),
                        reads=[uTt.d, s5d.d, yT.d], writes=[yT.d])
                kb.op("pool", lambda: nc.gpsimd.tensor_tensor(t1[:], yT[:], yT[:], op=ALU.mult), reads=[yT.d], writes=[t1.d])
                kb.op("dve", lambda: nc.vector.tensor_scalar(t1[:], t1[:], 0.044715, 1.0, op0=ALU.mult, op1=ALU.add),
                      reads=[t1.d], writes=[t1.d])
                kb.op("pool", lambda: nc.gpsimd.tensor_tensor(t1[:], t1[:], yT[:], op=ALU.mult), reads=[t1.d, yT.d], writes=[t1.d])
                kb.op("act", lambda: nc.scalar.activation(t1[:], t1[:], ACT.Sigmoid, scale=1.5957691216057308),
                      reads=[t1.d], writes=[t1.d])
                kb.op("dve", lambda: nc.vector.tensor_tensor(glg[:], t1[:], yT[:], op=ALU.mult), reads=[t1.d, yT.d], writes=[glg.d])
                for nn in range(4):
                    for th in range(2):
                        ps = next_ps(g)
                        for kc in range(4):
                            kb.op("pe", lambda nn=nn, th=th, kc=kc, ps=ps: nc.tensor.matmul(
                                ps[:, :], lhsT=glw[:, kc, nn * 128:(nn + 1) * 128], rhs=glg[:, kc, th * 512:(th + 1) * 512],
                                start=(kc == 0), stop=(kc == 3)), reads=[glw.d, glg.d], writes=[ps.d])
                        kb.op("act", lambda nn=nn, ps=ps: nc.scalar.activation(sg[:], ps[:, :], ACT.Sigmoid,
                                                                              bias=glb[:, nn:nn + 1], scale=1.0),
                              reads=[ps.d, glb.d], writes=[sg.d])
                        kb.op("dve", lambda nn=nn, th=th, cb=cb: nc.vector.tensor_tensor(
                            s5T[:, nn, cb * 1024 + th * 512:cb * 1024 + (th + 1) * 512], sg[:],
                            glg[:, nn, th * 512:(th + 1) * 512], op=ALU.mult), reads=[sg.d, glg.d], writes=[s5T.ds[nn]])
            kb.barrier()
        if "s5T" in g.dbg:
            dbg_out(g, "s5T", s5T[:], s5T.ds, [128, 4, NL], BF16)
        glaT = T(kb, es, [128, 4, NL], BF16, "glaTt")
        kb.dma("sp", lambda: nc.sync.dma_start(out=glaT[:], in_=S.glaT.ap().rearrange("(k p) t -> p k t", p=128)),
               reads=[S.glaT_d], writes=[glaT.d])
        wo = T(kb, es, [128, 8, D], BF16, "wo")
        kb.dma("pool", lambda: nc.gpsimd.dma_start(out=wo[:], in_=H.w_out[0].rearrange("(k p) n -> p k n", p=128)),
               writes=[wo.d])
        g1b = bc_tile(g, es, "g1b", S.mod[0:1, 2 * D:3 * D], D, [S.mod_d])
        sh2b = bc_tile(g, es, "sh2b", S.mod[0:1, 3 * D:4 * D], D, [S.mod_d])
        g2e = bc_tile(g, es, "g2e", S.mod[0:1, 4 * D:5 * D], D, [S.mod_d])
        nfw = bc_tile(g, es, "nfw", H.norm_ffn_w[0:1, :], D)
        kb.op("dve", lambda: nc.vector.scalar_tensor_tensor(g2e[:], g2e[:], 1.0, nfw[:], op0=ALU.add, op1=ALU.mult),
              reads=[g2e.d, nfw.d], writes=[g2e.d])
        rw = T(kb, es, [128, 8, NE], F32, "rw")
        kb.dma("sp", lambda: nc.sync.dma_start(out=rw[:], in_=H.router_w[0].rearrange("(k p) e -> p k e", p=128)),
               writes=[rw.d])
        rbb = bc_tile(g, es, "rbb", H.router_b[0:1, :], NE)
        xt = [T(kb, es, [128, D], F32, f"ext{i}") for i in range(2)]
        x2t = [T(kb, es, [128, D], F32, f"ex2{i}") for i in range(2)]
        tmp = T(kb, es, [128, D], F32, "etmp")
        h2 = T(kb, es, [128, D], F32, "eh2")
        h2b = [T(kb, es, [128, D], BF16, f"eh2b{i}") for i in range(2)]
        hT2 = T(kb, es, [128, 8, 128], F32, "ehT2")
        junk = T(kb, es, [128, D], BF16, "ejunk")
        lg = T(kb, es, [128, NE], F32, "elg")
        mx8 = T(kb, es, [128, 8], F32, "emx8")
        ix8 = T(kb, es, [128, 8], U32, "eix8")
        sm1 = T(kb, es, [128, 4], F32, "esm")
        for tl in range(NL // 128):
            if tl % 4 == 0:
                kb.pump(1)
            t0 = tl * 128
            x_t, x2 = xt[tl % 2], x2t[tl % 2]
            kb.dma("act", lambda x_t=x_t, t0=t0: nc.scalar.dma_start(out=x_t[:], in_=H.x[t0:t0 + 128, :]), writes=[x_t.d])
            for nh in range(2):
                ps = next_ps(g)
                for r in range(2):
                    row = tl * 2 + r
                    for kc in range(8):
                        if kc < 4:
                            lh = glaT[:, kc, row * 64:(row + 1) * 64]
                            rd = [glaT.d]
                        else:
                            lh = s5T[:, kc - 4, row:NL:64]
                            rd = s5T.ds
                        kb.op("pe", lambda lh=lh, r=r, kc=kc, nh=nh, ps=ps: nc.tensor.matmul(
                            ps[r * 64:(r + 1) * 64, :], lhsT=lh, rhs=wo[:, kc, nh * 512:(nh + 1) * 512],
                            start=(kc == 0), stop=(kc == 7)), reads=rd + [wo.d], writes=[ps.d])
                kb.op("dve", lambda nh=nh, ps=ps: nc.vector.tensor_tensor(
                    tmp[:, nh * 512:(nh + 1) * 512], ps[:, :], g1b[:, nh * 512:(nh + 1) * 512], op=ALU.mult),
                    reads=[ps.d, g1b.d], writes=[tmp.d])
            kb.op("pool", lambda x2=x2, x_t=x_t: nc.gpsimd.tensor_tensor(x2[:], tmp[:], x_t[:], op=ALU.add),
                  reads=[tmp.d, x_t.d], writes=[x2.d])
            kb.dma("sp", lambda x2=x2, t0=t0: nc.sync.dma_start(out=S.x2[t0:t0 + 128, :], in_=x2[:]),
                   reads=[x2.d], writes=[S.x2_d])
            kb.op("act", lambda x2=x2: nc.scalar.activation(junk[:], x2[:], ACT.Square, accum_out=sm1[:, 0:1]),
                  reads=[x2.d], writes=[junk.d, sm1.d])
            kb.op("dve", lambda: nc.vector.tensor_scalar(sm1[:, 1:2], sm1[:, 0:1], 1.0 / D, EPS, op0=ALU.mult, op1=ALU.add),
                  reads=[sm1.d], writes=[sm1.d])
            kb.op("act", lambda: nc.scalar.activation(sm1[:, 1:2], sm1[:, 1:2], ACT.Sqrt), reads=[sm1.d], writes=[sm1.d])
            kb.op("dve", lambda: nc.vector.reciprocal(sm1[:, 2:3], sm1[:, 1:2]), reads=[sm1.d], writes=[sm1.d])
            kb.op("dve", lambda x2=x2: nc.vector.scalar_tensor_tensor(tmp[:], x2[:], sm1[:, 2:3], g2e[:],
                                                                      op0=ALU.mult, op1=ALU.mult),
                  reads=[x2.d, sm1.d, g2e.d], writes=[tmp.d])
            kb.op("pool", lambda: nc.gpsimd.tensor_tensor(h2[:], tmp[:], sh2b[:], op=ALU.add),
                  reads=[tmp.d, sh2b.d], writes=[h2.d])
            hb = h2b[tl % 2]
            kb.op("act", lambda hb=hb: nc.scalar.copy(hb[:], h2[:]), reads=[h2.d], writes=[hb.d])
            kb.dma("sp", lambda hb=hb, t0=t0: nc.sync.dma_start(out=S.h2[t0:t0 + 128, :], in_=hb[:]),
                   reads=[hb.d], writes=[S.h2_d])
            for hf in range(2):
                ps = next_ps(g)
                for kk in range(4):
                    kc = hf * 4 + kk
                    kb.op("pe", lambda kc=kc, kk=kk, ps=ps: nc.tensor.transpose(
                        ps[:, kk * 128:(kk + 1) * 128], h2[:, kc * 128:(kc + 1) * 128], g.ident[:]),
                        reads=[h2.d, g.ident.d], writes=[ps.d])
                kb.op("act", lambda hf=hf, ps=ps: nc.scalar.copy(
                    hT2[:, hf * 4:(hf + 1) * 4, :], ps[:, :].rearrange("p (a t) -> p a t", a=4)),
                    reads=[ps.d], writes=[hT2.d])
            ps = next_ps(g)
            for kc in range(8):
                kb.op("pe", lambda kc=kc, ps=ps: nc.tensor.matmul(ps[:, 0:NE], lhsT=hT2[:, kc, :], rhs=rw[:, kc, :],
                                                                  start=(kc == 0), stop=(kc == 7)),
                      reads=[hT2.d, rw.d], writes=[ps.d])
            kb.op("dve", lambda ps=ps: nc.vector.tensor_tensor(lg[:], ps[:, 0:NE], rbb[:], op=ALU.add),
                  reads=[ps.d, rbb.d], writes=[lg.d])
            kb.op("dve", lambda: nc.vector.max(mx8[:], lg[:]), reads=[lg.d], writes=[mx8.d])
            kb.op("dve", lambda: nc.vector.max_index(ix8[:], mx8[:], lg[:]), reads=[lg.d, mx8.d], writes=[ix8.d])
            kb.op("dve", lambda tl=tl: nc.vector.tensor_copy(R.idxf[:, tl, :], ix8[:, 0:4]), reads=[ix8.d], writes=[R.idxf.d])
            kb.op("dve", lambda tl=tl: nc.vector.tensor_scalar(R.mask[:, tl, :], lg[:], mx8[:, 3:4], None, op0=ALU.is_ge),
                  reads=[lg.d, mx8.d], writes=[R.mask.d])
            kb.op("dve", lambda: nc.vector.tensor_scalar(sm1[:, 3:4], mx8[:, 0:1], -1.0, None, op0=ALU.mult),
                  reads=[mx8.d, sm1.d], writes=[sm1.d])
            kb.op("act", lambda tl=tl: nc.scalar.activation(R.gate[:, tl, :], mx8[:, 0:4], ACT.Exp, bias=sm1[:, 3:4],
                                                            scale=1.0, accum_out=sm1[:, 0:1]),
                  reads=[mx8.d, sm1.d], writes=[R.gate.d, sm1.d])
            kb.op("dve", lambda: nc.vector.reciprocal(sm1[:, 1:2], sm1[:, 0:1]), reads=[sm1.d], writes=[sm1.d])
            kb.op("dve", lambda tl=tl: nc.vector.tensor_scalar(R.gate[:, tl, :], R.gate[:, tl, :], sm1[:, 1:2], None,
                                                               op0=ALU.mult), reads=[R.gate.d, sm1.d], writes=[R.gate.d])
        if "logits" in g.dbg:
            dbg_out(g, "gate", R.gate[:], [R.gate.d], [128, 32, 4])
            dbg_out(g, "idxf", R.idxf[:], [R.idxf.d], [128, 32, 4])
        onesf = T(kb, es, [128, 128], F32, "onesf")
        kb.op("pool", lambda: nc.gpsimd.memset(onesf[:], 1.0), writes=[onesf.d])
        triu = T(kb, es, [128, 128], F32, "triu")
        kb.op("dve", lambda: nc.vector.tensor_single_scalar(triu[:], g.iota[:], 0.0, op=ALU.is_gt),
              reads=[g.iota.d], writes=[triu.d])
        ps = next_ps(g)
        for tl in range(32):
            kb.op("pe", lambda tl=tl, ps=ps: nc.tensor.matmul(ps[:, 0:NE], lhsT=onesf[:], rhs=R.mask[:, tl, :],
                                                              start=(tl == 0), stop=(tl == 31)),
                  reads=[onesf.d, R.mask.d], writes=[ps.d])
        cnt = T(kb, es, [128, NE], F32, "cnt")
        nsg = T(kb, es, [128, NE], F32, "nsg")
        pend = T(kb, es, [128, NE], F32, "pend")
        pst = T(kb, es, [128, NE], F32, "pst")
        t32 = T(kb, es, [128, NE], F32, "t32")
        kb.op("dve", lambda ps=ps: nc.vector.tensor_copy(cnt[:], ps[:, 0:NE]), reads=[ps.d], writes=[cnt.d])
        kb.op("dve", lambda: nc.vector.memset(nsg[:], 0.0), writes=[nsg.d])
        for k in range(8):
            kb.op("dve", lambda k=k: nc.vector.tensor_scalar(t32[:], cnt[:], float(SEG * k) + 0.5, None, op0=ALU.is_ge),
                  reads=[cnt.d], writes=[t32.d])
            kb.op("dve", lambda: nc.vector.tensor_tensor(nsg[:], nsg[:], t32[:], op=ALU.add), reads=[nsg.d, t32.d], writes=[nsg.d])
        kb.op("dve", lambda: nc.vector.tensor_tensor_scan(pend[:], onesf[:, 0:NE], nsg[:], 0.0, ALU.mult, ALU.add),
              reads=[onesf.d, nsg.d], writes=[pend.d])
        kb.op("dve", lambda: nc.vector.tensor_tensor(pst[:], pend[:], nsg[:], op=ALU.subtract), reads=[pend.d, nsg.d], writes=[pst.d])
        kb.op("dve", lambda: nc.vector.tensor_scalar(pst[:], pst[:], float(SEG), None, op0=ALU.mult), reads=[pst.d], writes=[pst.d])
        sidx = T(kb, es, [128, NSEG], F32, "sidx")
        kb.op("pool", lambda: nc.gpsimd.iota(sidx[:], pattern=[[1, NSEG]], base=0, channel_multiplier=0,
                                              allow_small_or_imprecise_dtypes=True), writes=[sidx.d])
        cmp3 = T(kb, es, [128, NSEG, NE], F32, "cmp3")
        kb.op("dve", lambda: nc.vector.tensor_tensor(cmp3[:], pend[:].unsqueeze(1).to_broadcast([128, NSEG, NE]),
                                                     sidx[:].unsqueeze(2).to_broadcast([128, NSEG, NE]), op=ALU.is_le),
              reads=[pend.d, sidx.d], writes=[cmp3.d])
        sef = T(kb, es, [128, NSEG], F32, "sef")
        kb.op("dve", lambda: nc.vector.tensor_reduce(sef[:], cmp3[:], axis=AX.X, op=ALU.add), reads=[cmp3.d], writes=[sef.d])
        kb.op("dve", lambda: nc.vector.tensor_scalar(sef[:], sef[:], float(NE - 1), None, op0=ALU.min), reads=[sef.d], writes=[sef.d])
        kb.op("dve", lambda: nc.vector.tensor_copy(R.segexp[:], sef[:]), reads=[sef.d], writes=[R.segexp.d])
        used = T(kb, es, [128, NSEG], F32, "used")
        kb.op("dve", lambda: nc.vector.tensor_scalar(used[:], sidx[:], pend[:, NE - 1:NE], None, op0=ALU.is_lt),
              reads=[sidx.d, pend.d], writes=[used.d])
        pcf = T(kb, es, [128, 1], F32, "pcf")
        kb.op("pool", lambda: nc.gpsimd.iota(pcf[:], pattern=[[0, 1]], base=0, channel_multiplier=1,
                                              allow_small_or_imprecise_dtypes=True), writes=[pcf.d])
        OOB = 1000000.0
        sgf = T(kb, es, [128, NSEG], F32, "sgf")
        kb.op("dve", lambda: nc.vector.tensor_scalar(sgf[:], sef[:], 128.0, pcf[:, 0:1], op0=ALU.mult, op1=ALU.add),
              reads=[sef.d, pcf.d], writes=[sgf.d])
        for src, dst in ((sgf, R.segidx), (sef, R.segrow)):
            kb.op("dve", lambda src=src: nc.vector.tensor_scalar(src[:], src[:], -OOB, None, op0=ALU.add),
                  reads=[src.d], writes=[src.d])
            kb.op("dve", lambda src=src: nc.vector.tensor_tensor(src[:], src[:], used[:], op=ALU.mult),
                  reads=[src.d, used.d], writes=[src.d])
            kb.op("dve", lambda src=src: nc.vector.tensor_scalar(src[:], src[:], OOB, None, op0=ALU.add),
                  reads=[src.d], writes=[src.d])
            kb.op("dve", lambda src=src, dst=dst: nc.vector.tensor_copy(dst[:], src[:]), reads=[src.d], writes=[dst.d])
        if "logits" in g.dbg:
            dbg_out(g, "cnt", cnt[:], [cnt.d], [128, NE])
            dbg_out(g, "segexp", R.segexp[:], [R.segexp.d], [128, NSEG], I32)
        carry = T(kb, es, [128, NE], F32, "carry")
        kb.op("dve", lambda: nc.vector.tensor_copy(carry[:], pst[:]), reads=[pst.d], writes=[carry.d])
        ief = T(kb, es, [128, NE], F32, "ief")
        kb.op("pool", lambda: nc.gpsimd.iota(ief[:], pattern=[[1, NE]], base=0, channel_multiplier=0,
                                              allow_small_or_imprecise_dtypes=True), writes=[ief.d])
        slf = T(kb, es, [128, NE], F32, "slf")
        slk = T(kb, es, [128, 4], F32, "slk")
        hld = [T(kb, es, [128, D], BF16, f"hld{i}") for i in range(2)]
        for tl in range(32):
            t0 = tl * 128
            psA = next_ps(g)
            kb.op("pe", lambda tl=tl, psA=psA: nc.tensor.matmul(psA[:, 0:NE], lhsT=triu[:], rhs=R.mask[:, tl, :],
                                                                start=True, stop=True),
                  reads=[triu.d, R.mask.d], writes=[psA.d])
            kb.op("dve", lambda psA=psA: nc.vector.tensor_tensor(slf[:], psA[:, 0:NE], carry[:], op=ALU.add),
                  reads=[psA.d, carry.d], writes=[slf.d])
            psB = next_ps(g)
            kb.op("pe", lambda tl=tl, psB=psB: nc.tensor.matmul(psB[:, 0:NE], lhsT=onesf[:], rhs=R.mask[:, tl, :],
                                                                start=True, stop=True),
                  reads=[onesf.d, R.mask.d], writes=[psB.d])
            kb.op("dve", lambda psB=psB: nc.vector.tensor_tensor(carry[:], carry[:], psB[:, 0:NE], op=ALU.add),
                  reads=[psB.d, carry.d, slf.d], writes=[carry.d])
            for k in range(4):
                kb.op("dve", lambda k=k, tl=tl: nc.vector.scalar_tensor_tensor(
                    t32[:], ief[:], R.idxf[:, tl, k:k + 1], slf[:], op0=ALU.is_equal, op1=ALU.mult,
                    accum_out=slk[:, k:k + 1]), reads=[ief.d, R.idxf.d, slf.d], writes=[t32.d, slk.d])
            kb.op("dve", lambda tl=tl: nc.vector.tensor_copy(R.slot[:, tl, :], slk[:]), reads=[slk.d], writes=[R.slot.ds[tl]])
            hl = hld[tl % 2]
            kb.dma("sp", lambda hl=hl, t0=t0: nc.sync.dma_start(out=hl[:], in_=S.h2[t0:t0 + 128, :]),
                   reads=[S.h2_d], writes=[hl.d])
            for k in range(4):
                kb.dma("pool", lambda hl=hl, tl=tl, k=k: nc.gpsimd.indirect_dma_start(
                    out=S.xg[:, :], out_offset=bass.IndirectOffsetOnAxis(ap=R.slot[:, tl, k:k + 1], axis=0),
                    in_=hl[:, :], in_offset=None), reads=[hl.d, R.slot.ds[tl]], writes=[S.xg_d])
        if "logits" in g.dbg:
            dbg_out(g, "slot", R.slot[:], R.slot.ds, [128, 32, 4], I32)
        bg = T(kb, es, [NE, 2 * D], F32, "bgrow")
        kb.dma("sp", lambda: nc.sync.dma_start(out=bg[:], in_=H.exp_b_gu[0]), writes=[bg.d])
        bgt = T(kb, es, [128, 16, NE], F32, "bgt")
        for c4 in range(0, 16, 4):
            ps = next_ps(g)
            for cc in range(4):
                kb.op("pe", lambda cc=cc, c4=c4, ps=ps: nc.tensor.transpose(
                    ps[:, cc * NE:(cc + 1) * NE], bg[:, (c4 + cc) * 128:(c4 + cc + 1) * 128], g.ident[0:NE, 0:NE]),
                    reads=[bg.d, g.ident.d], writes=[ps.d])
            kb.op("act", lambda c4=c4, ps=ps: nc.scalar.copy(
                bgt[:, c4:c4 + 4, :], ps[:, 0:4 * NE].rearrange("p (a e) -> p a e", a=4)), reads=[ps.d], writes=[bgt.d])
        kb.dma("sp", lambda: nc.sync.dma_start(out=S.bguT.ap().rearrange("e p c -> p c e"), in_=bgt[:],
                                               allow_slow_non_contiguous=True), reads=[bgt.d], writes=[S.bguT_d])


def phase_f(g):
    nc, kb, H, S = g.nc, g.kb, g.H, g.S
    R = g.R
    with ExitStack() as es:
        wgu = [T(kb, es, [128, 8, 2 * D], BF16, f"wgu{i}") for i in range(2)]
        wdn = [T(kb, es, [128, 8, D], BF16, f"wdn{i}") for i in range(2)]
        bgu = [T(kb, es, [128, 16], F32, f"bgu{i}") for i in range(2)]
        bdn = [T(kb, es, [128, D], F32, f"bdn{i}") for i in range(2)]
        xrow = [T(kb, es, [128, 4, D], BF16, f"xrow{i}") for i in range(2)]
        xT = [T(kb, es, [128, 8, SEG], BF16, f"xT{i}") for i in range(2)]
        actT = [T(kb, es, [128, 8, SEG], BF16, f"actT{i}", nd=8) for i in range(2)]
        yrow = [T(kb, es, [128, 4, D], BF16, f"yrow{i}") for i in range(2)]
        L0 = [T(kb, es, [128, SEG], F32, f"fL0_{i}") for i in range(2)]
        Gc = [T(kb, es, [128, SEG], F32, f"fGc_{i}") for i in range(2)]
        sg = [T(kb, es, [128, SEG], F32, f"fsg_{i}") for i in range(2)]
        tt = [T(kb, es, [128, SEG], F32, f"ftt_{i}") for i in range(2)]
        bc_rows = nc.gpsimd.to_reg(NE * 128 - 1)
        bc_e = nc.gpsimd.to_reg(NE - 1)
        wg_rows = S.wg.ap().rearrange("e p f -> (e p) f")
        wd_rows = S.wd.ap().rearrange("e p f -> (e p) f")
        bgu_rows = S.bguT.ap().rearrange("e p c -> (e p) c")
        nseg = g.nseg_limit if getattr(g, "nseg_limit", None) else NSEG
        bench = getattr(g, "bench", "") or ""
        for s in range(nseg):
            b = s % 2
            if "nodma" in bench and s >= 2:
                KB.dead = True
            kb.dma("pool", lambda: nc.gpsimd.indirect_dma_start(
                out=wgu[b][:, :, :].rearrange("p k n -> p (k n)"), out_offset=None, in_=wg_rows,
                in_offset=bass.IndirectOffsetOnAxis(ap=R.segidx[:, s:s + 1], axis=0),
                bounds_check=bc_rows, oob_is_err=False),
                reads=[R.segidx.d, S.wg_d], writes=[wgu[b].d])
            kb.dma("pool", lambda: nc.gpsimd.indirect_dma_start(
                out=wdn[b][:, :, :].rearrange("p k n -> p (k n)"), out_offset=None, in_=wd_rows,
                in_offset=bass.IndirectOffsetOnAxis(ap=R.segidx[:, s:s + 1], axis=0),
                bounds_check=bc_rows, oob_is_err=False),
                reads=[R.segidx.d, S.wd_d], writes=[wdn[b].d])
            kb.dma("pool", lambda: nc.gpsimd.indirect_dma_start(
                out=bgu[b][:, :], out_offset=None, in_=bgu_rows,
                in_offset=bass.IndirectOffsetOnAxis(ap=R.segidx[:, s:s + 1], axis=0),
                bounds_check=bc_rows, oob_is_err=False),
                reads=[R.segidx.d, S.bguT_d], writes=[bgu[b].d])
            kb.dma("pool", lambda: nc.gpsimd.indirect_dma_start(
                out=bdn[b][:, :], out_offset=None, in_=H.exp_b_down[0],
                in_offset=bass.IndirectOffsetOnAxis(ap=R.segrow[:, s:s + 1], axis=0),
                bounds_check=bc_e, oob_is_err=False),
                reads=[R.segrow.d], writes=[bdn[b].d])
            KB.dead = False
            kb.dma("sp", lambda: nc.sync.dma_start(
                out=xrow[b][:], in_=S.xg[s * SEG:(s + 1) * SEG, :].rearrange("(j p) d -> p j d", p=128)),
                reads=[S.xg_d], writes=[xrow[b].d])
            if "nocomp" in bench:
                KB.dead = True
            for kc in range(8):
                pb = g.psb[kc % 2]
                for j in range(4):
                    kb.op("pe", lambda j=j, kc=kc, pb=pb: nc.tensor.transpose(
                        pb[:, j * 128:(j + 1) * 128], xrow[b][:, j, kc * 128:(kc + 1) * 128], g.identb[:]),
                        reads=[xrow[b].d, g.identb.d], writes=[pb.d])
                if kc % 2 == 0:
                    kb.op("act", lambda kc=kc, pb=pb: nc.scalar.copy(xT[b][:, kc, :], pb[:, 0:SEG]),
                          reads=[pb.d], writes=[xT[b].d])
                else:
                    kb.op("dve", lambda kc=kc, pb=pb: nc.vector.tensor_copy(xT[b][:, kc, :], pb[:, 0:SEG]),
                          reads=[pb.d], writes=[xT[b].d])
            for c in range(8):
                i2 = c % 2
                pg = next_ps(g)
                for kc in range(8):
                    kb.op("pe", lambda kc=kc, c=c, pg=pg: nc.tensor.matmul(
                        pg[:, :], lhsT=wgu[b][:, kc, c * 128:(c + 1) * 128], rhs=xT[b][:, kc, :],
                        start=(kc == 0), stop=(kc == 7)), reads=[wgu[b].d, xT[b].d], writes=[pg.d])
                pl_ = next_ps(g)
                for kc in range(8):
                    kb.op("pe", lambda kc=kc, c=c, pl_=pl_: nc.tensor.matmul(
                        pl_[:, :], lhsT=wgu[b][:, kc, D + c * 128:D + (c + 1) * 128], rhs=xT[b][:, kc, :],
                        start=(kc == 0), stop=(kc == 7)), reads=[wgu[b].d, xT[b].d], writes=[pl_.d])
                kb.op("dve", lambda c=c, pg=pg: nc.vector.tensor_scalar(Gc[i2][:], pg[:, :], bgu[b][:, c:c + 1], 7.0,
                                                                      op0=ALU.add, op1=ALU.min),
                      reads=[pg.d, bgu[b].d], writes=[Gc[i2].d])
                kb.op("act", lambda: nc.scalar.activation(sg[i2][:], Gc[i2][:], ACT.Sigmoid, scale=1.702),
                      reads=[Gc[i2].d], writes=[sg[i2].d])
                kb.op("act", lambda c=c, pl_=pl_: nc.scalar.activation(L0[i2][:], pl_[:, :], ACT.Identity,
                                                                      bias=bgu[b][:, 8 + c:9 + c], scale=1.0),
                      reads=[pl_.d, bgu[b].d], writes=[L0[i2].d])
                kb.op("dve", lambda: nc.vector.tensor_scalar(L0[i2][:], L0[i2][:], 7.0, -7.0, op0=ALU.min, op1=ALU.max),
                      reads=[L0[i2].d], writes=[L0[i2].d])
                kb.op("pool", lambda: nc.gpsimd.tensor_tensor(tt[i2][:], Gc[i2][:], sg[i2][:], op=ALU.mult),
                      reads=[Gc[i2].d, sg[i2].d], writes=[tt[i2].d])
                kb.op("dve", lambda c=c: nc.vector.scalar_tensor_tensor(actT[b][:, c, :], L0[i2][:], 1.0, tt[i2][:],
                                                                       op0=ALU.add, op1=ALU.mult),
                      reads=[L0[i2].d, tt[i2].d], writes=[actT[b].ds[c]])
            for j in range(4):
                for nh in range(2):
                    ps = next_ps(g)
                    for c in range(8):
                        kb.op("pe", lambda c=c, j=j, nh=nh, ps=ps: nc.tensor.matmul(
                            ps[:, :], lhsT=actT[b][:, c, j * 128:(j + 1) * 128], rhs=wdn[b][:, c, nh * 512:(nh + 1) * 512],
                            start=(c == 0), stop=(c == 7)), reads=[actT[b].ds[c], wdn[b].d], writes=[ps.d])
                    kb.op("dve", lambda j=j, nh=nh, ps=ps: nc.vector.tensor_tensor(
                        yrow[b][:, j, nh * 512:(nh + 1) * 512], ps[:, :], bdn[b][:, nh * 512:(nh + 1) * 512], op=ALU.add),
                        reads=[ps.d, bdn[b].d], writes=[yrow[b].d])
            kb.dma("act", lambda: nc.scalar.dma_start(
                out=S.yg[s * SEG:(s + 1) * SEG, :].rearrange("(j p) d -> p j d", p=128), in_=yrow[b][:]),
                reads=[yrow[b].d], writes=[S.yg_d])
            KB.dead = False


def phase_g(g):
    nc, kb, H, S = g.nc, g.kb, g.H, g.S
    R = g.R
    with ExitStack() as es:
        g2b = bc_tile(g, es, "g2b", S.mod[0:1, 5 * D:6 * D], D, [S.mod_d])
        fnw = bc_tile(g, es, "fnw", H.final_norm_w.ap().rearrange("(o d) -> o d", o=1), D)
        yk = [[T(kb, es, [128, D], BF16, f"yk{i}_{k}") for k in range(4)] for i in range(2)]
        x2t = [T(kb, es, [128, D], F32, f"gx2{i}") for i in range(2)]
        acc = T(kb, es, [128, D], F32, "gacc")
        x3 = T(kb, es, [128, D], F32, "gx3")
        ot = [T(kb, es, [128, D], F32, f"got{i}") for i in range(2)]
        junk = T(kb, es, [128, D], BF16, "gjunk")
        sm1 = T(kb, es, [128, 4], F32, "gsm")
        for tl in range(NL // 128):
            t0 = tl * 128
            b = tl % 2
            for k in range(4):
                kb.dma("pool", lambda k=k: nc.gpsimd.indirect_dma_start(
                    out=yk[b][k][:, :], out_offset=None, in_=S.yg[:, :],
                    in_offset=bass.IndirectOffsetOnAxis(ap=R.slot[:, tl, k:k + 1], axis=0)),
                    reads=[S.yg_d, R.slot.ds[tl]], writes=[yk[b][k].d])
            kb.dma("sp", lambda: nc.sync.dma_start(out=x2t[b][:], in_=S.x2[t0:t0 + 128, :]), reads=[S.x2_d], writes=[x2t[b].d])
            kb.op("dve", lambda: nc.vector.tensor_scalar(acc[:], yk[b][0][:], R.gate[:, tl, 0:1], None, op0=ALU.mult),
                  reads=[yk[b][0].d, R.gate.d], writes=[acc.d])
            for k in range(1, 4):
                kb.op("dve", lambda k=k: nc.vector.scalar_tensor_tensor(acc[:], yk[b][k][:], R.gate[:, tl, k:k + 1], acc[:],
                                                                       op0=ALU.mult, op1=ALU.add),
                      reads=[yk[b][k].d, R.gate.d, acc.d], writes=[acc.d])
            kb.op("pool", lambda: nc.gpsimd.tensor_tensor(acc[:], acc[:], g2b[:], op=ALU.mult), reads=[acc.d, g2b.d], writes=[acc.d])
            kb.op("pool", lambda: nc.gpsimd.tensor_tensor(x3[:], acc[:], x2t[b][:], op=ALU.add), reads=[acc.d, x2t[b].d], writes=[x3.d])
            kb.op("act", lambda: nc.scalar.activation(junk[:], x3[:], ACT.Square, accum_out=sm1[:, 0:1]),
                  reads=[x3.d], writes=[junk.d, sm1.d])
            kb.op("dve", lambda: nc.vector.tensor_scalar(sm1[:, 1:2], sm1[:, 0:1], 1.0 / D, EPS, op0=ALU.mult, op1=ALU.add),
                  reads=[sm1.d], writes=[sm1.d])
            kb.op("act", lambda: nc.scalar.activation(sm1[:, 1:2], sm1[:, 1:2], ACT.Sqrt), reads=[sm1.d], writes=[sm1.d])
            kb.op("dve", lambda: nc.vector.reciprocal(sm1[:, 2:3], sm1[:, 1:2]), reads=[sm1.d], writes=[sm1.d])
            kb.op("dve", lambda: nc.vector.scalar_tensor_tensor(ot[b][:], x3[:], sm1[:, 2:3], fnw[:], op0=ALU.mult, op1=ALU.mult),
                  reads=[x3.d, sm1.d, fnw.d], writes=[ot[b].d])
            kb.dma("act", lambda: nc.scalar.dma_start(out=H.out[t0:t0 + 128, :], in_=ot[b][:]), reads=[ot[b].d], writes=[g.out_d])


_SHARED = ("c_ctx", "ada_w", "ada_b", "norm_mix_w", "w_in", "gla_lr_up", "gla_lr_bias", "gla_norm_w",
           "s5_lam_re", "s5_lam_im", "s5_log_dt", "s5_b_re", "s5_b_im", "s5_c_re", "s5_c_im", "s5_d",
           "glu_w", "glu_b", "w_out", "norm_ffn_w", "router_w", "router_b", "exp_w_gu", "exp_b_gu",
           "exp_w_down", "exp_b_down", "final_norm_w")


def kernel(x, c, ctx, c_ctx, ada_w, ada_b, norm_mix_w, w_in, gla_lr_up, gla_lr_bias, gla_norm_w,
           s5_lam_re, s5_lam_im, s5_log_dt, s5_b_re, s5_b_im, s5_c_re, s5_c_im, s5_d, glu_w, glu_b,
           w_out, norm_ffn_w, router_w, router_b, exp_w_gu, exp_b_gu, exp_w_down, exp_b_down,
           final_norm_w):
    loc = locals()
    shared = {k: np.ascontiguousarray(np.asarray(loc[k], dtype=np.float32)) for k in _SHARED}
    x = np.asarray(x, dtype=np.float32)
    c = np.asarray(c, dtype=np.float32)
    ctx = np.asarray(ctx, dtype=np.float32)
    nb = x.shape[0]
    nc, _ = build()
    in_maps = []
    for b in range(nb):
        m = dict(shared)
        m["x"] = np.ascontiguousarray(x[b])
        m["ctx"] = np.ascontiguousarray(ctx[b])
        m["c"] = np.ascontiguousarray(c[b:b + 1])
        in_maps.append(m)
    res = run_bass_kernel_spmd(nc, in_maps, core_ids=list(range(nb)))
    return np.stack([np.asarray(r["out"], dtype=np.float32) for r in res.results], axis=0)
```

```python
import numpy as np
import concourse.bass as bass
import concourse.mybir as mybir
from concourse.bass_utils import run_bass_kernel_spmd
from contextlib import ExitStack

F32 = mybir.dt.float32
BF16 = mybir.dt.bfloat16
U32 = mybir.dt.uint32
I32 = mybir.dt.int32
ACT = mybir.ActivationFunctionType
ALU = mybir.AluOpType
AX = mybir.AxisListType
PoolE = mybir.EngineType.Pool

D = 1024
NL = 4096
NCX = 256
NT = NL + NCX
NE = 32
EPS = 1e-6


class Dep:
    __slots__ = ("w", "r", "name")

    def __init__(self, name=""):
        self.w = None
        self.r = []
        self.name = name


class KB:
    NSLOT = 8

    def __init__(self, nc, es):
        self.nc = nc
        self.engs = {"pe": nc.tensor, "dve": nc.vector, "act": nc.scalar,
                     "pool": nc.gpsimd, "sp": nc.sync}
        self.sem = {}
        self.cnt = {}
        for k in self.engs:
            self.sem[k] = es.enter_context(nc.semaphore("s_" + k))
            self.cnt[k] = 0
        self.slots = {}
        self.slot_rr = {}
        for q in ("sp", "act", "pool"):
            self.slots[q] = [[es.enter_context(nc.semaphore(f"d_{q}{i}")), 0]
                             for i in range(self.NSLOT)]
            self.slot_rr[q] = 0
        self.slots["conv"] = [[es.enter_context(nc.semaphore(f"d_conv{i}")), 0] for i in range(6)]
        self.slot_rr["conv"] = 0
        self.pending = []
        self.seen = {k: {} for k in self.engs}
        self.ninst = 0
        self.nwaits = 0

    def _wait(self, eng, tok):
        if tok is None:
            return
        sem, val, key = tok
        if eng == "pe" and key == "pe":
            return
        s = self.seen[eng]
        if s.get(key, 0) >= val:
            return
        self.engs[eng].wait_ge(sem, val)
        s[key] = val
        self.nwaits += 1

    def _deps(self, eng, reads, writes):
        for d in reads:
            self._wait(eng, d.w)
        for d in writes:
            self._wait(eng, d.w)
            for t in d.r:
                self._wait(eng, t)

    def _commit(self, tok, reads, writes):
        for d in reads:
            d.r.append(tok)
            if len(d.r) > 16:
                best = {}
                for t in d.r:
                    if t[2] not in best or best[t[2]][1] < t[1]:
                        best[t[2]] = t
                d.r = list(best.values())
        for d in writes:
            d.w = tok
            d.r = []

    dead = False

    def op(self, eng, fn, reads=(), writes=()):
        if KB.dead:
            return None
        self._deps(eng, reads, writes)
        inst = fn()
        self.cnt[eng] += 1
        inst.then_inc(self.sem[eng], 1)
        tok = (self.sem[eng], self.cnt[eng], eng)
        self._commit(tok, reads, writes)
        self.ninst += 1
        return tok

    def pump(self, n=1):
        for _ in range(n):
            if not self.pending:
                return
            fn, reads, writes = self.pending.pop(0)
            self.dma("pool", fn, reads=reads, writes=writes, grp="conv")

    def dma(self, q, fn, reads=(), writes=(), grp=None):
        if KB.dead:
            return None
        grp = grp or q
        i = self.slot_rr[grp]
        self.slot_rr[grp] = (i + 1) % len(self.slots[grp])
        slot = self.slots[grp][i]
        key = f"d_{grp}{i}"
        if slot[1] > 0:
            self._wait(q, (slot[0], slot[1], key))
        self._deps(q, reads, writes)
        inst = fn()
        slot[1] += 16
        inst.then_inc(slot[0], 16)
        tok = (slot[0], slot[1], key)
        self._commit(tok, reads, writes)
        self.ninst += 1
        return tok

    def wait_all(self, eng, deps):
        for d in deps:
            self._wait(eng, d.w)
            for t in d.r:
                self._wait(eng, t)

    def barrier(self, conv=False):
        for e in self.engs:
            self.finish(e, conv)

    def finish(self, eng="sp", conv=True):
        for k in self.engs:
            if self.cnt[k] > 0:
                self._wait(eng, (self.sem[k], self.cnt[k], k))
        for q in self.slots:
            if q == "conv" and not conv:
                continue
            for i, s in enumerate(self.slots[q]):
                if s[1] > 0:
                    self._wait(eng, (s[0], s[1], f"d_{q}{i}"))


class T:
    _ctr = [0]

    def __init__(self, kb, es, shape, dtype, name, psum=False, nd=1):
        nc = kb.nc
        T._ctr[0] += 1
        name = f"{name}_{T._ctr[0]}"
        if psum:
            self.t = es.enter_context(nc.psum_tensor(name, list(shape), dtype))
        else:
            self.t = es.enter_context(nc.sbuf_tensor(name, list(shape), dtype))
        self.ds = [Dep(f"{name}.{i}") for i in range(nd)]
        self.d = self.ds[0]
        self.shape = shape

    def __getitem__(self, k):
        return self.t[k]


class Ctx:
    pass


class StopBuild(Exception):
    pass


def ckpt(name):
    if STOP == name:
        KB.dead = True


STOP = None


def build(dbg=(), stop=None):
    global STOP
    STOP = stop
    KB.dead = False
    nc = bass.Bass("TRN2", target_bir_lowering=False)
    g = Ctx()
    g.nc = nc
    g.dbg = set(dbg)
    g.outs = {}
    g.out_d = Dep("out")

    def din(name, shape, dt=F32):
        return nc.dram_tensor(name, list(shape), dt, kind="ExternalInput")

    H = Ctx()
    g.H = H
    H.x = din("x", [NL, D])
    H.ctx = din("ctx", [NCX, D])
    H.c = din("c", [1, D])
    H.c_ctx = din("c_ctx", [D])
    H.ada_w = din("ada_w", [1, D, 6 * D])
    H.ada_b = din("ada_b", [1, 6 * D])
    H.norm_mix_w = din("norm_mix_w", [1, D])
    H.w_in = din("w_in", [1, D, 2080])
    H.gla_lr_up = din("gla_lr_up", [1, 2, 16, 256])
    H.gla_lr_bias = din("gla_lr_bias", [1, 2, 256])
    H.gla_norm_w = din("gla_norm_w", [1, 128])
    H.s5_lam_re = din("s5_lam_re", [1, 2, 32, 64])
    H.s5_lam_im = din("s5_lam_im", [1, 2, 32, 64])
    H.s5_log_dt = din("s5_log_dt", [1, 2, 32])
    H.s5_b_re = din("s5_b_re", [1, 2, 32, 64, 16])
    H.s5_b_im = din("s5_b_im", [1, 2, 32, 64, 16])
    H.s5_c_re = din("s5_c_re", [1, 2, 32, 16, 64])
    H.s5_c_im = din("s5_c_im", [1, 2, 32, 16, 64])
    H.s5_d = din("s5_d", [1, 512])
    H.glu_w = din("glu_w", [1, 512, 512])
    H.glu_b = din("glu_b", [1, 512])
    H.w_out = din("w_out", [1, D, D])
    H.norm_ffn_w = din("norm_ffn_w", [1, D])
    H.router_w = din("router_w", [1, D, NE])
    H.router_b = din("router_b", [1, NE])
    H.exp_w_gu = din("exp_w_gu", [1, NE, D, 2 * D])
    H.exp_b_gu = din("exp_b_gu", [1, NE, 2 * D])
    H.exp_w_down = din("exp_w_down", [1, NE, D, D])
    H.exp_b_down = din("exp_b_down", [1, NE, D])
    H.final_norm_w = din("final_norm_w", [D])
    H.out = nc.dram_tensor("out", [NL, D], F32, kind="ExternalOutput")

    S = Ctx()
    g.S = S

    def scr(name, shape, dt):
        if name in g.dbg:
            h = nc.dram_tensor(name, list(shape), dt, kind="ExternalOutput")
        else:
            h = nc.dram_tensor(name, list(shape), dt)
        return h, Dep(name)

    S.mod, S.mod_d = scr("mod_s", [2, 6 * D], F32)
    S.qT, S.qT_d = scr("qT_s", [256, NT], BF16)
    S.kT, S.kT_d = scr("kT_s", [256, NT], BF16)
    S.rT, S.rT_d = scr("rT_s", [512, NL], BF16)
    S.lrT, S.lrT_d = scr("lrT_s", [2, 16, NT], F32)
    S.v, S.v_d = scr("v_s", [NT, 512], BF16)
    S.uT, S.uT_d = scr("uT_s", [512, NT], BF16)
    S.u, S.u_d = scr("u_s", [NT, 512], BF16)
    S.glaT, S.glaT_d = scr("glaT_s", [512, NL], BF16)
    S.y0, S.y0_d = scr("y0_s", [32, 128, 512], F32)
    S.Ut, S.Ut_d = scr("Ut_s", [32, 128, NT // 8], BF16)
    S.x2, S.x2_d = scr("x2_s", [NL, D], F32)
    S.h2, S.h2_d = scr("h2_s", [NL, D], BF16)
    S.xg, S.xg_d = scr("xg_s", [NSEG * SEG, D], BF16)
    S.yg, S.yg_d = scr("yg_s", [NSEG * SEG, D], BF16)
    S.bguT, S.bguT_d = scr("bguT_s", [NE, 128, 16], F32)
    S.wg, S.wg_d = scr("wg_s", [NE, 128, 8 * 2 * D], BF16)
    S.wd, S.wd_d = scr("wd_s", [NE, 128, 8 * D], BF16)

    with ExitStack() as es:
        kb = KB(nc, es)
        g.kb = kb
        g.es = es
        g.ident = T(kb, es, [128, 128], F32, "ident")
        g.identb = T(kb, es, [128, 128], BF16, "identb")
        io = T(kb, es, [128, 128], F32, "iota0")
        kb.op("pool", lambda: nc.gpsimd.iota(io[:], pattern=[[1, 128]], base=0, channel_multiplier=-1,
                                              allow_small_or_imprecise_dtypes=True), writes=[io.d])
        kb.op("dve", lambda: nc.vector.tensor_single_scalar(g.ident[:], io[:], 0.0, op=ALU.is_equal),
              reads=[io.d], writes=[g.ident.d])
        kb.op("dve", lambda: nc.vector.tensor_copy(g.identb[:], g.ident[:]), reads=[g.ident.d], writes=[g.identb.d])
        g.iota = io
        g.ps = [T(kb, es, [128, 512], F32, f"ps{i}", psum=True) for i in range(6)]
        g.psb = [T(kb, es, [128, 1024], BF16, f"psb{i}", psum=True) for i in range(2)]
        g.ps_rr = 0
        R = Ctx()
        g.R = R
        R.mask = T(kb, es, [128, 32, NE], F32, "r_mask")
        R.idxf = T(kb, es, [128, 32, 4], F32, "r_idxf")
        R.gate = T(kb, es, [128, 32, 4], F32, "r_gate")
        R.slot = T(kb, es, [128, 32, 4], I32, "r_slot", nd=32)
        R.segexp = T(kb, es, [128, NSEG], I32, "r_segexp")
        R.segidx = T(kb, es, [128, NSEG], I32, "r_segidx")
        R.segrow = T(kb, es, [128, NSEG], I32, "r_segrow")

        if stop is not None and str(stop).startswith("benchf"):
            kb.op("pool", lambda: nc.gpsimd.iota(R.segidx[:], pattern=[[0, NSEG]], base=0, channel_multiplier=1),
                  writes=[R.segidx.d])
            kb.op("pool", lambda: nc.gpsimd.memset(R.segrow[:], 0), writes=[R.segrow.d])
            g.bench = stop
            phase_f(g)
            kb.barrier()
            kb.finish("sp")
            print("instructions", kb.ninst, "waits", kb.nwaits)
            return nc, g
        phase_a(g)
        kb.barrier()
        for e in range(NE):
            kb.pending.append((lambda e=e: nc.gpsimd.dma_start(
                out=S.wg[e, :, :].rearrange("p (k n) -> p k n", k=8),
                in_=H.exp_w_gu[0, e, :, :].rearrange("(k p) n -> p k n", p=128)), [], [S.wg_d]))
            kb.pending.append((lambda e=e: nc.gpsimd.dma_start(
                out=S.wd[e, :, :].rearrange("p (k n) -> p k n", k=8),
                in_=H.exp_w_down[0, e, :, :].rearrange("(k p) n -> p k n", p=128)), [], [S.wd_d]))
        kb.pump(6)
        if stop != 'a':
            phase_b(g)
            kb.barrier()
            if stop != 'b':
                if stop not in ('d_only', 'e_only'):
                    phase_c(g)
                    kb.barrier()
                if stop not in ('c', 'c0', 'c1', 'c2', 'c3'):
                    if stop != 'e_only':
                        phase_d(g)
                        kb.barrier()
                    if stop not in ('d', 'd_only') and not KB.dead:
                        phase_e(g)
                        kb.barrier()
                        if stop != 'e' and not KB.dead:
                            kb.pump(1000)
                            kb.barrier(conv=True)
                            phase_f(g)
                            kb.barrier()
                            phase_g(g)
                            kb.barrier()

        kb.finish("sp")
        print("instructions", kb.ninst, "waits", kb.nwaits)
    return nc, g


def next_ps(g):
    p = g.ps[g.ps_rr]
    g.ps_rr = (g.ps_rr + 1) % len(g.ps)
    return p


def dbg_out(g, name, src_ap, deps, shape, dt=F32, q="sp"):
    if name not in g.dbg:
        return
    nc, kb = g.nc, g.kb
    o = nc.dram_tensor("dbg_" + name, list(shape), dt, kind="ExternalOutput")
    g.outs[name] = o
    kb.dma(q, lambda: nc.sync.dma_start(out=o.ap(), in_=src_ap), reads=deps)


def phase_a(g):
    nc, kb, H, S = g.nc, g.kb, g.H, g.S
    with ExitStack() as es:
        cc = T(kb, es, [128, 8, 2], F32, "cc")
        kb.dma("sp", lambda: nc.sync.dma_start(out=cc[:, :, 0], in_=H.c[0, :].rearrange("(k p) -> p k", p=128),
                                               allow_slow_non_contiguous=True), writes=[cc.d])
        kb.dma("sp", lambda: nc.sync.dma_start(out=cc[:, :, 1], in_=H.c_ctx.ap().rearrange("(k p) -> p k", p=128),
                                               allow_slow_non_contiguous=True), writes=[cc.d])
        kb.op("act", lambda: nc.scalar.activation(cc[:], cc[:], ACT.Silu), reads=[cc.d], writes=[cc.d])
        ab = T(kb, es, [2, 6 * D], F32, "ab")
        kb.dma("sp", lambda: nc.sync.dma_start(out=ab[0:1, :], in_=H.ada_b[0:1, :]), writes=[ab.d])
        kb.dma("sp", lambda: nc.sync.dma_start(out=ab[1:2, :], in_=H.ada_b[0:1, :]), writes=[ab.d])
        modsb = T(kb, es, [2, 6 * D], F32, "modsb")
        aw = [T(kb, es, [128, 8, 512], F32, f"aw{i}") for i in range(2)]
        for j in range(12):
            a = aw[j % 2]
            kb.dma("sp" if j % 2 == 0 else "act",
                   (lambda a=a, j=j: (nc.sync if j % 2 == 0 else nc.scalar).dma_start(
                       out=a[:], in_=H.ada_w[0, :, j * 512:(j + 1) * 512].rearrange("(k p) n -> p k n", p=128))),
                   writes=[a.d])
            ps = next_ps(g)
            for k in range(8):
                kb.op("pe", lambda k=k: nc.tensor.matmul(ps[0:2, :], lhsT=cc[:, k, :], rhs=a[:, k, :],
                                                         start=(k == 0), stop=(k == 7)),
                      reads=[cc.d, a.d], writes=[ps.d])
            kb.op("dve", lambda j=j: nc.vector.tensor_tensor(modsb[:, j * 512:(j + 1) * 512], ps[0:2, :],
                                                            ab[:, j * 512:(j + 1) * 512], op=ALU.add),
                  reads=[ps.d, ab.d], writes=[modsb.d])
        kb.dma("sp", lambda: nc.sync.dma_start(out=S.mod.ap(), in_=modsb[:]), reads=[modsb.d], writes=[S.mod_d])


def load_fm_vec(g, es, name, src_ap_1d):
    nc, kb = g.nc, g.kb
    t = T(kb, es, [128, 8], F32, name)
    kb.dma("sp", lambda: nc.sync.dma_start(out=t[:], in_=src_ap_1d.rearrange("(k p) -> p k", p=128),
                                           allow_slow_non_contiguous=True), reads=[g.S.mod_d], writes=[t.d])
    return t


def s5_cols(hT, k, s0, n):
    if s0 < NCX:
        assert s0 + n <= NCX
        return hT[:, k, s0:s0 + n]
    c0 = (s0 - NCX) // 64
    ncol = n // 64
    v = hT[:, k, NCX:NT].rearrange("p (row col) -> p col row", col=64)
    return v[:, c0:c0 + ncol, :]


def phase_b(g):
    nc, kb, H, S = g.nc, g.kb, g.H, g.S
    with ExitStack() as es:
        sh1 = load_fm_vec(g, es, "sh1", S.mod[0, 0:D])
        sc1 = load_fm_vec(g, es, "sc1", S.mod[0, D:2 * D])
        csh1 = load_fm_vec(g, es, "csh1", S.mod[1, 0:D])
        csc1 = load_fm_vec(g, es, "csc1", S.mod[1, D:2 * D])
        nmw = load_fm_vec(g, es, "nmw", H.norm_mix_w[0, :])
        g1f = T(kb, es, [128, 8], F32, "g1f")
        cg1f = T(kb, es, [128, 8], F32, "cg1f")
        kb.op("dve", lambda: nc.vector.scalar_tensor_tensor(g1f[:], sc1[:], 1.0, nmw[:], op0=ALU.add, op1=ALU.mult),
              reads=[sc1.d, nmw.d], writes=[g1f.d])
        kb.op("dve", lambda: nc.vector.scalar_tensor_tensor(cg1f[:], csc1[:], 1.0, nmw[:], op0=ALU.add, op1=ALU.mult),
              reads=[csc1.d, nmw.d], writes=[cg1f.d])
        wi = T(kb, es, [128, 8, 2080], BF16, "wi")
        for (a, b) in ((0, 1040), (1040, 2080)):
            kb.dma("pool", lambda a=a, b=b: nc.gpsimd.dma_start(
                out=wi[:, :, a:b], in_=H.w_in[0, :, a:b].rearrange("(k p) n -> p k n", p=128)), writes=[wi.d])
        hT = T(kb, es, [128, 8, NT], BF16, "hT", nd=9)
        xg = [T(kb, es, [128, 4, D], F32, f"xg{i}") for i in range(2)]
        junk = T(kb, es, [128, D], BF16, "junkb")
        groups = [("ctx", 0, 2)] + [("lat", gi, 4) for gi in range(8)]
        for gi, (kind, idx, ntile) in enumerate(groups):
            kb.pump(1)
            xt = xg[gi % 2]
            ntok = ntile * 128
            if kind == "ctx":
                src = H.ctx[0:ntok, :]
                col0 = 0
                gsc, gsh = cg1f, csh1
            else:
                src = H.x[idx * 512:(idx + 1) * 512, :]
                col0 = NCX + idx * 512
                gsc, gsh = g1f, sh1
            q = "sp" if gi % 2 == 0 else "act"
            kb.dma(q, lambda xt=xt, src=src, ntile=ntile, q=q: (nc.sync if q == "sp" else nc.scalar).dma_start(
                out=xt[:, 0:ntile, :], in_=src.rearrange("(j p) d -> p j d", p=128)), writes=[xt.d])
            ss = T(kb, es, [128, 4], F32, f"ss{gi}")
            rs = T(kb, es, [128, 4], F32, f"rs{gi}")
            for j in range(ntile):
                kb.op("act", lambda j=j: nc.scalar.activation(junk[:], xt[:, j, :], ACT.Square,
                                                              accum_out=ss[:, j:j + 1]),
                      reads=[xt.d], writes=[junk.d, ss.d])
            kb.op("dve", lambda: nc.vector.tensor_scalar(rs[:, 0:ntile], ss[:, 0:ntile], 1.0 / D, EPS,
                                                         op0=ALU.mult, op1=ALU.add), reads=[ss.d], writes=[rs.d])
            kb.op("act", lambda: nc.scalar.activation(rs[:, 0:ntile], rs[:, 0:ntile], ACT.Sqrt),
                  reads=[rs.d], writes=[rs.d])
            kb.op("dve", lambda: nc.vector.reciprocal(rs[:, 0:ntile], rs[:, 0:ntile]), reads=[rs.d], writes=[rs.d])
            for j in range(ntile):
                kb.op("dve", lambda j=j: nc.vector.tensor_scalar(xt[:, j, :], xt[:, j, :], rs[:, j:j + 1], None,
                                                                 op0=ALU.mult), reads=[xt.d, rs.d], writes=[xt.d])
            for k in range(8):
                ps = next_ps(g)
                for j in range(ntile):
                    kb.op("pe", lambda j=j, k=k: nc.tensor.transpose(ps[:, j * 128:(j + 1) * 128],
                                                                     xt[:, j, k * 128:(k + 1) * 128], g.ident[:]),
                          reads=[xt.d, g.ident.d], writes=[ps.d])
                kb.op("act", lambda k=k, ps=ps: nc.scalar.activation(
                    hT[:, k, col0:col0 + ntok], ps[:, 0:ntok], ACT.Identity,
                    bias=gsh[:, k:k + 1], scale=gsc[:, k:k + 1]),
                    reads=[ps.d, gsh.d, gsc.d], writes=[hT.ds[gi]])
        hall = hT.ds
        if "hT" in g.dbg:
            dbg_out(g, "hT", hT[:], hall, [128, 8, NT], BF16)
        stg = [T(kb, es, [128, NT], BF16, f"stg{i}") for i in range(2)]
        stgf = T(kb, es, [16, NT], F32, "stgf")
        rr = [0]

        def fm_proj(c0, m, dst_fn, t0, t1, cols_fn, f32=False):
            kb.pump(1)
            st = stgf if f32 else stg[rr[0] % 2]
            rr[0] += 0 if f32 else 1
            t = t0
            while t < t1:
                n = min(512, t1 - t)
                if t < NCX:
                    n = min(n, NCX - t)
                ps = next_ps(g)
                for k in range(8):
                    kb.op("pe", lambda k=k, t=t, n=n: nc.tensor.matmul(
                        ps[0:m, 0:n], lhsT=wi[:, k, c0:c0 + m], rhs=cols_fn(k, t, n),
                        start=(k == 0), stop=(k == 7)), reads=[wi.d] + hall, writes=[ps.d])
                eng = "act" if (t // 512) % 2 == 0 else "dve"
                if eng == "act":
                    kb.op("act", lambda t=t, n=n, ps=ps: nc.scalar.copy(st[0:m, t:t + n], ps[0:m, 0:n]),
                          reads=[ps.d], writes=[st.d])
                else:
                    kb.op("dve", lambda t=t, n=n, ps=ps: nc.vector.tensor_copy(st[0:m, t:t + n], ps[0:m, 0:n]),
                          reads=[ps.d], writes=[st.d])
                t += n
            dst, dd = dst_fn()
            kb.dma("sp", lambda: nc.sync.dma_start(out=dst, in_=st[0:m, t0:t1]), reads=[st.d], writes=[dd])

        raster = lambda k, t, n: hT[:, k, t:t + n]
        s5t = lambda k, t, n: s5_cols(hT, k, t, n)
        for mt in range(2):
            fm_proj(mt * 128, 128, lambda mt=mt: (S.qT[mt * 128:(mt + 1) * 128, :], S.qT_d), 0, NT, raster)
        for mt in range(2):
            fm_proj(256 + mt * 128, 128, lambda mt=mt: (S.kT[mt * 128:(mt + 1) * 128, :], S.kT_d), 0, NT, raster)
        for mt in range(4):
            fm_proj(1024 + mt * 128, 128, lambda mt=mt: (S.rT[mt * 128:(mt + 1) * 128, :], S.rT_d), NCX, NT, raster)
        for z in range(2):
            fm_proj(1536 + z * 16, 16, lambda z=z: (S.lrT[z, :, :], S.lrT_d), 0, NT, raster, f32=True)
        for mt in range(4):
            fm_proj(1568 + mt * 128, 128, lambda mt=mt: (S.uT[mt * 128:(mt + 1) * 128, :], S.uT_d), 0, NT, s5t)
        st4 = [T(kb, es, [128, 4, 512], BF16, f"st4_{i}") for i in range(2)]
        ngrp = 0
        for (c0, dst, dd, s5) in ((512, S.v, S.v_d, False), (1568, S.u, S.u_d, True)):
            for t0 in list(range(0, NCX, 512)) + list(range(NCX, NT, 512)):
                nt = 2 if t0 < NCX else 4
                st = st4[ngrp % 2]
                ngrp += 1
                for j in range(nt):
                    ps = next_ps(g)
                    tt = t0 + j * 128
                    if s5 and tt >= NCX:
                        for hf in range(2):
                            col = (tt - NCX) // 64 + hf
                            for k in range(8):
                                lh = hT[:, k, NCX + col:NT:64]
                                kb.op("pe", lambda k=k, lh=lh, ps=ps, hf=hf: nc.tensor.matmul(
                                    ps[hf * 64:(hf + 1) * 64, :], lhsT=lh, rhs=wi[:, k, c0:c0 + 512],
                                    start=(k == 0), stop=(k == 7)), reads=[wi.d] + hall, writes=[ps.d])
                    else:
                        for k in range(8):
                            lh = hT[:, k, tt:tt + 128]
                            kb.op("pe", lambda k=k, lh=lh, ps=ps: nc.tensor.matmul(
                                ps[:, :], lhsT=lh, rhs=wi[:, k, c0:c0 + 512], start=(k == 0), stop=(k == 7)),
                                reads=[wi.d] + hall, writes=[ps.d])
                    if j % 2 == 0:
                        kb.op("act", lambda j=j, ps=ps, st=st: nc.scalar.copy(st[:, j, :], ps[:, :]),
                              reads=[ps.d], writes=[st.d])
                    else:
                        kb.op("dve", lambda j=j, ps=ps, st=st: nc.vector.tensor_copy(st[:, j, :], ps[:, :]),
                              reads=[ps.d], writes=[st.d])
                kb.dma("sp", lambda st=st, t0=t0, nt=nt, dst=dst: nc.sync.dma_start(
                    out=dst[t0:t0 + nt * 128, :].rearrange("(j p) e -> p j e", p=128), in_=st[:, 0:nt, :]),
                    reads=[st.d], writes=[dd])


def phase_c(g):
    nc, kb, H, S = g.nc, g.kb, g.H, g.S
    NCH = NT // 64
    with ExitStack() as es:
        psb = g.psb[0]
        lup = T(kb, es, [16, 2, 256], F32, "lup")
        kb.dma("sp", lambda: nc.sync.dma_start(out=lup[:], in_=H.gla_lr_up[0].rearrange("z r f -> r z f")),
               writes=[lup.d])
        nb = T(kb, es, [128, 2, 2], F32, "nbias")
        kb.dma("sp", lambda: nc.sync.dma_start(out=nb[:], in_=H.gla_lr_bias[0].rearrange("z (t p) -> p z t", p=128),
                                               allow_slow_non_contiguous=True), writes=[nb.d])
        kb.op("dve", lambda: nc.vector.tensor_scalar(nb[:], nb[:], -1.0, None, op0=ALU.mult), reads=[nb.d], writes=[nb.d])
        nw = T(kb, es, [128, 1], F32, "gnw")
        kb.dma("sp", lambda: nc.sync.dma_start(out=nw[:], in_=H.gla_norm_w[0, :].rearrange("(p o) -> p o", o=1)),
               writes=[nw.d])
        ones = T(kb, es, [128, 128], BF16, "onesb")
        kb.op("pool", lambda: nc.gpsimd.memset(ones[:], 1.0), writes=[ones.d])
        io = g.iota
        mk = []
        for z in range(2):
            m4 = T(kb, es, [128, 4, 64], BF16, f"tri{z}")
            op = ALU.is_ge if z == 0 else ALU.is_le
            for h in range(4):
                kb.op("dve", lambda h=h, m4=m4, op=op: nc.vector.tensor_single_scalar(
                    m4[0:64, h, :], io[0:64, 0:64], 0.0, op=op), reads=[io.d], writes=[m4.d])
                kb.op("dve", lambda h=h, m4=m4, op=op: nc.vector.tensor_single_scalar(
                    m4[64:128, h, :], io[64:128, 0:64], -64.0, op=op), reads=[io.d], writes=[m4.d])
            mk.append(m4)
        qt = [T(kb, es, [128, 2, NT], BF16, f"qtl{z}") for z in range(2)]
        kt = [T(kb, es, [128, 2, NT], BF16, f"ktl{z}") for z in range(2)]
        elast = [T(kb, es, [128, 2, NCH], F32, f"elast{z}") for z in range(2)]
        if STOP == 'c0':
            return
        with ExitStack() as es2:
            mask = T(kb, es2, [128, NT + 1], BF16, "cmask")
            kb.op("pool", lambda: nc.gpsimd.memset(mask[:], 1.0), writes=[mask.d])
            kb.op("pool", lambda: nc.gpsimd.memset(mask[:, 0:NT + 1:64], 0.0), writes=[mask.d])
            lrtz = T(kb, es2, [16, NT], F32, "lrt")
            A = T(kb, es2, [128, NT], F32, "scrA")
            B = T(kb, es2, [128, NT], F32, "scrB")
            raw = [T(kb, es2, [128, NT], BF16, f"raw{i}") for i in range(2)]
            for z in range(2):
                kb.dma("sp", lambda z=z: nc.sync.dma_start(out=lrtz[:], in_=S.lrT[z, :, :]),
                       reads=[S.lrT_d], writes=[lrtz.d])
                for pt in range(2):
                    if STOP == 'c1a' and (z, pt) != (0, 0):
                        continue
                    for t0 in range(0, NT, 512):
                        n = min(512, NT - t0)
                        ps = next_ps(g)
                        kb.op("pe", lambda t0=t0, n=n, ps=ps: nc.tensor.matmul(
                            ps[:, 0:n], lhsT=lup[:, z, pt * 128:(pt + 1) * 128], rhs=lrtz[:, t0:t0 + n],
                            start=True, stop=True), reads=[lup.d, lrtz.d], writes=[ps.d])
                        kb.op("act", lambda t0=t0, n=n, ps=ps: nc.scalar.activation(
                            A[:, t0:t0 + n], ps[:, 0:n], ACT.Exp, bias=nb[:, z, pt:pt + 1], scale=-1.0),
                            reads=[ps.d, nb.d], writes=[A.d])
                    ckpt("k1")
                    kb.op("act", lambda: nc.scalar.activation(A[:], A[:], ACT.Ln, bias=1.0, scale=1.0),
                          reads=[A.d], writes=[A.d])
                    ckpt("k2")
                    if z == 0:
                        kb.op("dve", lambda: nc.vector.tensor_tensor_scan(B[:], mask[:, 0:NT], A[:], 0.0,
                                                                          ALU.mult, ALU.add),
                              reads=[mask.d, A.d], writes=[B.d])
                    else:
                        kb.op("dve", lambda: nc.vector.tensor_tensor_scan(B[:, NT - 1::-1] if False else B[:, ::-1],
                                                                          mask[:, NT:0:-1], A[:, ::-1], 0.0,
                                                                          ALU.mult, ALU.add),
                              reads=[mask.d, A.d], writes=[B.d])
                    ckpt("k3")
                    kb.op("act", lambda: nc.scalar.activation(A[:], B[:], ACT.Exp, scale=-1.0 / 16.0),
                          reads=[B.d], writes=[A.d])
                    kb.op("act", lambda: nc.scalar.activation(B[:], B[:], ACT.Exp, scale=1.0 / 16.0),
                          reads=[B.d], writes=[B.d])
                    ckpt("k4")
                    rq, rk = raw
                    kb.dma("sp", lambda: nc.sync.dma_start(out=rq[:], in_=S.qT[pt * 128:(pt + 1) * 128, :]),
                           reads=[S.qT_d], writes=[rq.d])
                    kb.dma("sp", lambda: nc.sync.dma_start(out=rk[:], in_=S.kT[pt * 128:(pt + 1) * 128, :]),
                           reads=[S.kT_d], writes=[rk.d])
                    ckpt("k5")
                    kb.op("dve", lambda: nc.vector.scalar_tensor_tensor(qt[z][:, pt, :], rq[:], 0.125, A[:],
                                                                        op0=ALU.mult, op1=ALU.mult),
                          reads=[rq.d, A.d], writes=[qt[z].d])
                    ckpt("k6")
                    kb.op("pool", lambda: nc.gpsimd.tensor_tensor(kt[z][:, pt, :], rk[:], B[:], op=ALU.mult),
                          reads=[rk.d, B.d], writes=[kt[z].d])
                    ckpt("k7")
                    e0 = 63 if z == 0 else 0
                    kb.op("dve", lambda: nc.vector.tensor_copy(elast[z][:, pt, :], A[:, e0:NT:64]),
                          reads=[A.d], writes=[elast[z].d])
        kb.barrier()
        if STOP == 'c1':
            return
        vt = T(kb, es, [128, NT // 128, 512], BF16, "vt")
        kb.dma("act", lambda: nc.scalar.dma_start(out=vt[:], in_=S.v.ap().rearrange("(n p) e -> p n e", p=128)),
               reads=[S.v_d], writes=[vt.d])
        ktm = [T(kb, es, [128, NT // 128, 256], BF16, f"ktm{z}") for z in range(2)]
        oT = T(kb, es, [128, 4, NL], BF16, "oT", nd=NL // 64)
        for z in range(2):
            for n2 in range(0, NT // 128, 2):
                for a in range(2):
                    for pt in range(2):
                        kb.op("pe", lambda a=a, pt=pt, n2=n2: nc.tensor.transpose(
                            psb[:, (a * 2 + pt) * 128:(a * 2 + pt + 1) * 128],
                            kt[z][:, pt, (n2 + a) * 128:(n2 + a + 1) * 128], g.identb[:]),
                            reads=[kt[z].d, g.identb.d], writes=[psb.d])
                kb.op("act", lambda n2=n2: nc.scalar.copy(
                    ktm[z][:, n2:n2 + 2, :], psb[:, 0:512].rearrange("p (a f) -> p a f", a=2)),
                    reads=[psb.d], writes=[ktm[z].d])
        if "qtl" in g.dbg:
            dbg_out(g, "qtl0", qt[0][:], [qt[0].d], [128, 2, NT], BF16)
            dbg_out(g, "ktl1", kt[1][:], [kt[1].d], [128, 2, NT], BF16)
            dbg_out(g, "ktm1", ktm[1][:], [ktm[1].d], [128, NT // 128, 256], BF16)
            dbg_out(g, "elast1", elast[1][:], [elast[1].d], [128, 2, NCH])
        if STOP == 'c2':
            return
        Sst = [T(kb, es, [128, 2, 128], F32, f"Sst{z}") for z in range(2)]
        SbfZ = [T(kb, es, [128, 4, 128], BF16, f"SbfZ{z}") for z in range(2)]
        tmpS = [T(kb, es, [128, 2, 128], F32, f"tmpS{z}") for z in range(2)]
        smZ = [[T(kb, es, [128, 4, 64], BF16, f"smZ{z}_{i}") for i in range(2)] for z in range(2)]
        for z in range(2):
            kb.op("pool", lambda z=z: nc.gpsimd.memset(Sst[z][:], 0.0), writes=[Sst[z].d])
            kb.op("pool", lambda z=z: nc.gpsimd.memset(SbfZ[z][:], 0.0), writes=[SbfZ[z].d])
            for i in range(2):
                kb.op("pool", lambda z=z, i=i: nc.gpsimd.memset(smZ[z][i][:], 0.0), writes=[smZ[z][i].d])
        order = [list(range(NCH)), [3, 2, 1, 0] + list(range(NCH - 1, 3, -1))]
        written = set()
        for i in range(NCH):
            if i % 4 == 0:
                kb.pump(1)
            for z in range(2):
                n = order[z][i]
                t0 = 64 * n
                nt = n // 2
                jo = (n % 2) * 64
                if n >= 4:
                    smt = smZ[z][n % 2]
                    for par in range(2):
                        ho = par * 64
                        ps_s = next_ps(g)
                        for hh in range(2):
                            h = hh * 2 + par
                            pt = hh
                            kb.op("pe", lambda h=h, pt=pt, ho=ho, ps_s=ps_s, hh=hh: nc.tensor.matmul(
                                ps_s[jo:jo + 64, hh * 64:(hh + 1) * 64], lhsT=kt[z][ho:ho + 64, pt, t0:t0 + 64],
                                rhs=qt[z][ho:ho + 64, pt, t0:t0 + 64], start=True, stop=True),
                                reads=[kt[z].d, qt[z].d], writes=[ps_s.d])
                        kb.op("dve", lambda ps_s=ps_s, smt=smt, par=par: nc.vector.tensor_tensor(
                            smt[jo:jo + 64, par:4:2, :], ps_s[jo:jo + 64, 0:128].rearrange("p (h i) -> p h i", h=2),
                            mk[z][jo:jo + 64, 0:2, :], op=ALU.mult), reads=[ps_s.d, mk[z].d], writes=[smt.d])
                    ps_o = next_ps(g)
                    for h in range(4):
                        pt = h // 2
                        kb.op("pe", lambda h=h, ps_o=ps_o, smt=smt: nc.tensor.matmul(
                            ps_o[:, h * 64:(h + 1) * 64], lhsT=vt[:, nt, h * 128:(h + 1) * 128],
                            rhs=smt[:, h, :], start=True, stop=False),
                            reads=[vt.d, smt.d], writes=[ps_o.d])
                        kb.op("pe", lambda h=h, pt=pt, ps_o=ps_o: nc.tensor.matmul(
                            ps_o[:, h * 64:(h + 1) * 64], lhsT=SbfZ[z][:, h, :],
                            rhs=qt[z][:, pt, t0:t0 + 64], start=False, stop=True),
                            reads=[SbfZ[z].d, qt[z].d], writes=[ps_o.d])
                    tl = t0 - NCX
                    od = oT.ds[tl // 64]
                    osl = oT[:, :, tl:tl + 64]
                    pv = ps_o[:, 0:256].rearrange("p (h i) -> p h i", h=4)
                    if n not in written:
                        written.add(n)
                        kb.op("act", lambda osl=osl, pv=pv: nc.scalar.copy(osl, pv), reads=[ps_o.d], writes=[od])
                    else:
                        kb.op("dve", lambda osl=osl, pv=pv: nc.vector.tensor_tensor(osl, pv, osl, op=ALU.add),
                              reads=[ps_o.d, od], writes=[od])
                ps_kv = next_ps(g)
                for h in range(4):
                    pt, ho = h // 2, (h % 2) * 64
                    kb.op("pe", lambda h=h, pt=pt, ho=ho, ps_kv=ps_kv: nc.tensor.matmul(
                        ps_kv[ho:ho + 64, pt * 128:(pt + 1) * 128], lhsT=ktm[z][jo:jo + 64, nt, h * 64:(h + 1) * 64],
                        rhs=vt[jo:jo + 64, nt, h * 128:(h + 1) * 128], start=True, stop=True),
                        reads=[ktm[z].d, vt.d], writes=[ps_kv.d])
                kb.op("dve", lambda ps_kv=ps_kv: nc.vector.tensor_tensor(
                    tmpS[z][:], ps_kv[:, 0:256].rearrange("p (t e) -> p t e", t=2), Sst[z][:], op=ALU.add),
                    reads=[ps_kv.d, Sst[z].d], writes=[tmpS[z].d])
                kb.op("dve", lambda n=n: nc.vector.tensor_tensor(
                    Sst[z][:], tmpS[z][:], elast[z][:, :, n:n + 1].to_broadcast([128, 2, 128]), op=ALU.mult),
                    reads=[tmpS[z].d, elast[z].d], writes=[Sst[z].d])
                for par in range(2):
                    ho = par * 64
                    kb.op("act", lambda par=par, ho=ho: nc.scalar.copy(SbfZ[z][ho:ho + 64, par:4:2, :],
                                                                       Sst[z][ho:ho + 64, :, :]),
                          reads=[Sst[z].d], writes=[SbfZ[z].d])
        if "oT" in g.dbg:
            dbg_out(g, "oT", oT[:], oT.ds, [128, 4, NL], BF16)
        if STOP == 'c3':
            return
        sq = T(kb, es, [128, 512], BF16, "gsq")
        rstd = T(kb, es, [128, 512], F32, "grstd")
        rt = [T(kb, es, [128, 4, 512], BF16, f"grt{i}") for i in range(2)]
        gl = [T(kb, es, [128, 4, 512], BF16, f"ggl{i}") for i in range(1)]
        tmpb = T(kb, es, [128, 512], BF16, "gtmp")
        for sp in range(NL // 512):
            c0 = sp * 512
            r_t, g_t = rt[sp % 2], gl[0]
            kb.dma("sp", lambda r_t=r_t, c0=c0: nc.sync.dma_start(
                out=r_t[:], in_=S.rT[:, c0:c0 + 512].rearrange("(m p) t -> p m t", p=128)),
                reads=[S.rT_d], writes=[r_t.d])
            kb.op("act", lambda r_t=r_t: nc.scalar.activation(r_t[:], r_t[:], ACT.Silu), reads=[r_t.d], writes=[r_t.d])
            ods = oT.ds[c0 // 64:(c0 + 512) // 64]
            for h in range(4):
                kb.op("dve", lambda h=h: nc.vector.tensor_tensor(sq[:], oT[:, h, c0:c0 + 512], oT[:, h, c0:c0 + 512],
                                                                 op=ALU.mult), reads=ods, writes=[sq.d])
                ps = next_ps(g)
                kb.op("pe", lambda ps=ps: nc.tensor.matmul(ps[:, :], lhsT=ones[:], rhs=sq[:], start=True, stop=True),
                      reads=[ones.d, sq.d], writes=[ps.d])
                kb.op("dve", lambda ps=ps: nc.vector.tensor_scalar(rstd[:], ps[:, :], 1.0 / 128.0, EPS,
                                                                   op0=ALU.mult, op1=ALU.add),
                      reads=[ps.d], writes=[rstd.d])
                kb.op("act", lambda: nc.scalar.activation(rstd[:], rstd[:], ACT.Sqrt), reads=[rstd.d], writes=[rstd.d])
                kb.op("dve", lambda: nc.vector.reciprocal(rstd[:], rstd[:]), reads=[rstd.d], writes=[rstd.d])
                kb.op("dve", lambda h=h: nc.vector.scalar_tensor_tensor(
                    tmpb[:], oT[:, h, c0:c0 + 512], nw[:, 0:1], rstd[:], op0=ALU.mult, op1=ALU.mult),
                    reads=ods + [nw.d, rstd.d], writes=[tmpb.d])
                kb.op("pool", lambda h=h, g_t=g_t, r_t=r_t: nc.gpsimd.tensor_tensor(
                    g_t[:, h, :], tmpb[:], r_t[:, h, :], op=ALU.mult), reads=[tmpb.d, r_t.d], writes=[g_t.d])
            kb.dma("act", lambda g_t=g_t, c0=c0: nc.scalar.dma_start(
                out=S.glaT[:, c0:c0 + 512].rearrange("(m p) t -> p m t", p=128), in_=g_t[:]),
                reads=[g_t.d], writes=[S.glaT_d])


def cmul(g, out_r, out_i, ar, ai, br, bi, tmp, deps_in, dep_out, sl=None):
    nc, kb = g.nc, g.kb
    kb.op("dve", lambda: nc.vector.tensor_tensor(tmp, ai, bi, op=ALU.mult), reads=deps_in, writes=[dep_out])
    kb.op("dve", lambda: nc.vector.tensor_tensor(out_r, ar, br, op=ALU.mult), reads=deps_in, writes=[dep_out])
    kb.op("dve", lambda: nc.vector.tensor_tensor(out_r, out_r, tmp, op=ALU.subtract), reads=[dep_out], writes=[dep_out])
    kb.op("dve", lambda: nc.vector.tensor_tensor(tmp, ai, br, op=ALU.mult), reads=deps_in, writes=[dep_out])
    kb.op("dve", lambda: nc.vector.tensor_tensor(out_i, ar, bi, op=ALU.mult), reads=deps_in, writes=[dep_out])
    kb.op("dve", lambda: nc.vector.tensor_tensor(out_i, out_i, tmp, op=ALU.add), reads=[dep_out], writes=[dep_out])


def phase_d(g):
    nc, kb, H, S = g.nc, g.kb, g.H, g.S
    NCK = NT // 8
    NMAC = NCK // 16
    io = g.iota
    with ExitStack() as es:
        psb = g.psb[0]
        pd = Dep("s5par")
        P0 = T(kb, es, [128, 24, 64], F32, "s5p0")
        pl = lambda i: P0[:, i, :]
        LRE, LIM, DT, MAG, CS, SN, T1, T2, T3, LBR, LBI, CR, CI, IR, II = range(15)
        for half in range(2):
            rows = slice(half * 64, half * 64 + 64)
            kb.dma("sp", lambda rows=rows: nc.sync.dma_start(
                out=P0[rows, LRE, :], in_=H.s5_lam_re[0].rearrange("z g p -> p (z g)"),
                allow_slow_non_contiguous=True), writes=[pd])
            kb.dma("act", lambda rows=rows: nc.scalar.dma_start(
                out=P0[rows, LIM, :], in_=H.s5_lam_im[0].rearrange("z g p -> p (z g)"),
                allow_slow_non_contiguous=True), writes=[pd])
        kb.dma("sp", lambda: nc.sync.dma_start(
            out=pl(DT), in_=H.s5_log_dt[0:1, :, :].rearrange("o z g -> o (z g)").partition_broadcast(128)),
            writes=[pd])
        D1 = [pd]

        def v(fn):
            kb.op("dve", fn, reads=D1, writes=D1)

        def a(fn):
            kb.op("act", fn, reads=D1, writes=D1)

        a(lambda: nc.scalar.activation(pl(DT), pl(DT), ACT.Exp))
        v(lambda: nc.vector.tensor_tensor(pl(MAG), pl(LRE), pl(DT), op=ALU.mult))
        a(lambda: nc.scalar.activation(pl(MAG), pl(MAG), ACT.Exp))
        v(lambda: nc.vector.tensor_tensor(pl(T1), pl(LIM), pl(DT), op=ALU.mult))
        a(lambda: nc.scalar.activation(pl(SN), pl(T1), ACT.Sin, scale=1.0 / 16.0))
        a(lambda: nc.scalar.activation(pl(CS), pl(T1), ACT.Sin, bias=float(np.pi / 2), scale=1.0 / 16.0))
        for _ in range(4):
            v(lambda: nc.vector.tensor_tensor(pl(T2), pl(CS), pl(CS), op=ALU.mult))
            v(lambda: nc.vector.tensor_tensor(pl(T3), pl(SN), pl(SN), op=ALU.mult))
            v(lambda: nc.vector.scalar_tensor_tensor(pl(SN), pl(CS), 2.0, pl(SN), op0=ALU.mult, op1=ALU.mult))
            v(lambda: nc.vector.tensor_tensor(pl(CS), pl(T2), pl(T3), op=ALU.subtract))
        v(lambda: nc.vector.tensor_tensor(pl(LBR), pl(MAG), pl(CS), op=ALU.mult))
        v(lambda: nc.vector.tensor_tensor(pl(LBI), pl(MAG), pl(SN), op=ALU.mult))
        v(lambda: nc.vector.tensor_tensor(pl(T1), pl(LRE), pl(LRE), op=ALU.mult))
        v(lambda: nc.vector.tensor_tensor(pl(T2), pl(LIM), pl(LIM), op=ALU.mult))
        v(lambda: nc.vector.tensor_tensor(pl(T1), pl(T1), pl(T2), op=ALU.add))
        v(lambda: nc.vector.reciprocal(pl(T1), pl(T1)))
        v(lambda: nc.vector.tensor_scalar(pl(T2), pl(LBR), -1.0, None, op0=ALU.add))
        v(lambda: nc.vector.tensor_tensor(pl(CR), pl(T2), pl(LRE), op=ALU.mult))
        v(lambda: nc.vector.tensor_tensor(pl(T3), pl(LBI), pl(LIM), op=ALU.mult))
        v(lambda: nc.vector.tensor_tensor(pl(CR), pl(CR), pl(T3), op=ALU.add))
        v(lambda: nc.vector.tensor_tensor(pl(CR), pl(CR), pl(T1), op=ALU.mult))
        v(lambda: nc.vector.tensor_tensor(pl(CI), pl(LBI), pl(LRE), op=ALU.mult))
        v(lambda: nc.vector.tensor_tensor(pl(T3), pl(T2), pl(LIM), op=ALU.mult))
        v(lambda: nc.vector.tensor_tensor(pl(CI), pl(CI), pl(T3), op=ALU.subtract))
        v(lambda: nc.vector.tensor_tensor(pl(CI), pl(CI), pl(T1), op=ALU.mult))
        v(lambda: nc.vector.tensor_tensor(pl(T1), pl(LBR), pl(LBR), op=ALU.mult))
        v(lambda: nc.vector.tensor_tensor(pl(T2), pl(LBI), pl(LBI), op=ALU.mult))
        v(lambda: nc.vector.tensor_tensor(pl(T1), pl(T1), pl(T2), op=ALU.add))
        v(lambda: nc.vector.reciprocal(pl(T1), pl(T1)))
        v(lambda: nc.vector.tensor_tensor(pl(IR), pl(LBR), pl(T1), op=ALU.mult))
        v(lambda: nc.vector.scalar_tensor_tensor(pl(II), pl(LBI), -1.0, pl(T1), op0=ALU.mult, op1=ALU.mult))
        PW = T(kb, es, [128, 9, 2, 64], F32, "s5pw")
        NW = T(kb, es, [128, 8, 2, 64], F32, "s5nw")
        for W, br_, bi_, n in ((PW, LBR, LBI, 9), (NW, IR, II, 8)):
            v(lambda W=W: nc.vector.memset(W[:, 0, 0, :], 1.0))
            v(lambda W=W: nc.vector.memset(W[:, 0, 1, :], 0.0))
            for k in range(1, n):
                cmul(g, W[:, k, 0, :], W[:, k, 1, :], W[:, k - 1, 0, :], W[:, k - 1, 1, :], pl(br_), pl(bi_),
                     pl(T3), D1, pd)
        P128 = T(kb, es, [128, 2, 64], F32, "s5p128")
        v(lambda: nc.vector.tensor_copy(P128[:], PW[:, 8, :, :]))
        for _ in range(4):
            cmul(g, pl(T1), pl(T2), P128[:, 0, :], P128[:, 1, :], P128[:, 0, :], P128[:, 1, :], pl(T3), D1, pd)
            v(lambda: nc.vector.tensor_copy(P128[:, 0, :], pl(T1)))
            v(lambda: nc.vector.tensor_copy(P128[:, 1, :], pl(T2)))
        WN = T(kb, es, [128, 8, 2, 64], F32, "s5wn")
        WP = T(kb, es, [128, 8, 2, 64], F32, "s5wp")
        for k in range(8):
            cmul(g, WN[:, k, 0, :], WN[:, k, 1, :], NW[:, k, 0, :], NW[:, k, 1, :], pl(CR), pl(CI), pl(T3), D1, pd)
            cmul(g, WP[:, k, 0, :], WP[:, k, 1, :], PW[:, k, 0, :], PW[:, k, 1, :], pl(CR), pl(CI), pl(T3), D1, pd)
        v(lambda: nc.vector.tensor_scalar(PW[64:128, :, 0, :], PW[64:128, :, 0, :], -1.0, None, op0=ALU.mult))
        v(lambda: nc.vector.tensor_scalar(WN[0:64, :, 1, :], WN[0:64, :, 1, :], -1.0, None, op0=ALU.mult))
        v(lambda: nc.vector.tensor_scalar(WP[0:64, :, 1, :], WP[0:64, :, 1, :], -1.0, None, op0=ALU.mult))
        A8s = T(kb, es, [128, 2, 64], F32, "s5a8")
        v(lambda: nc.vector.tensor_copy(A8s[:, 1, :], PW[:, 8, 1, :]))
        v(lambda: nc.vector.tensor_copy(A8s[0:64, 0, :], PW[0:64, 8, 0, :]))
        v(lambda: nc.vector.tensor_scalar(A8s[64:128, 0, :], PW[64:128, 8, 0, :], -1.0, None, op0=ALU.mult))
        Jt = T(kb, es, [128, 128], F32, "s5jt")
        v(lambda: nc.vector.tensor_single_scalar(Jt[:], io[:], 64.0, op=ALU.is_equal))
        v(lambda: nc.vector.tensor_single_scalar(pl(T1)[:, 0:64], io[:, 0:64], -64.0, op=ALU.is_equal))
        v(lambda: nc.vector.tensor_tensor(Jt[:, 0:64], Jt[:, 0:64], pl(T1)[:, 0:64], op=ALU.subtract))
        bm = []
        for z in range(2):
            m = T(kb, es, [128, 128], F32, f"s5bm{z}")
            v(lambda m=m: nc.vector.memset(m[:], 0.0))
            for j in range(0, 8, 2):
                for jj in range(2):
                    pass
            bm.append(m)
        rowb = T(kb, es, [128, 1], F32, "s5rowb")
        pcol = T(kb, es, [128, 1], F32, "s5pcol")
        kb.op("pool", lambda: nc.gpsimd.iota(pcol[:], pattern=[[0, 1]], base=0, channel_multiplier=1,
                                              allow_small_or_imprecise_dtypes=True), writes=[pd])
        v(lambda: nc.vector.memset(rowb[:], 0.0))
        for t in range(1, 8):
            v(lambda t=t: nc.vector.tensor_scalar(pl(T1)[:, 0:1], pcol[:], float(16 * t), 16.0, op0=ALU.is_ge, op1=ALU.mult))
            v(lambda: nc.vector.tensor_tensor(rowb[:], rowb[:], pl(T1)[:, 0:1], op=ALU.add))
        colf = T(kb, es, [128, 128], F32, "s5colf")
        kb.op("pool", lambda: nc.gpsimd.iota(colf[:], pattern=[[1, 128]], base=0, channel_multiplier=0,
                                              allow_small_or_imprecise_dtypes=True), writes=[pd])
        v(lambda: nc.vector.tensor_scalar(bm[0][:], colf[:], rowb[:, 0:1], 0.0, op0=ALU.subtract, op1=ALU.is_ge))
        v(lambda: nc.vector.tensor_scalar(bm[1][:], colf[:], rowb[:, 0:1], 15.0, op0=ALU.subtract, op1=ALU.is_le))
        CC = T(kb, es, [128, 64, 16], F32, "s5cc")
        CCs = T(kb, es, [128, 64, 16], F32, "s5ccs")
        BB = T(kb, es, [128, 64, 16], F32, "s5bb")
        BBs = T(kb, es, [128, 64, 16], F32, "s5bbs")
        kb.dma("sp", lambda: nc.sync.dma_start(out=BB[0:64, :, :], in_=H.s5_b_re[0].rearrange("z g p h -> p (z g) h")),
               writes=[pd])
        kb.dma("act", lambda: nc.scalar.dma_start(out=BB[64:128, :, :], in_=H.s5_b_im[0].rearrange("z g p h -> p (z g) h")),
               writes=[pd])
        kb.dma("sp", lambda: nc.sync.dma_start(out=BBs[0:64, :, :], in_=H.s5_b_im[0].rearrange("z g p h -> p (z g) h")),
               writes=[pd])
        kb.dma("act", lambda: nc.scalar.dma_start(out=BBs[64:128, :, :], in_=H.s5_b_re[0].rearrange("z g p h -> p (z g) h")),
               writes=[pd])
        with ExitStack() as esx:
            xc = [T(kb, esx, [128, 8, 128], F32, f"s5xc{i}") for i in range(2)]
            for i, (aa, bb) in enumerate(((H.s5_c_re, H.s5_c_im), (H.s5_c_im, H.s5_c_re))):
                kb.dma("sp", lambda aa=aa, i=i: nc.sync.dma_start(
                    out=xc[i][:, :, 0:64], in_=aa[0].rearrange("z g h p -> (z g h) p").rearrange("(t r) p -> r t p", r=128)),
                    writes=[pd])
                kb.dma("act", lambda bb=bb, i=i: nc.scalar.dma_start(
                    out=xc[i][:, :, 64:128], in_=bb[0].rearrange("z g h p -> (z g h) p").rearrange("(t r) p -> r t p", r=128)),
                    writes=[pd])
            for i, dst in enumerate((CC, CCs)):
                for t in range(8):
                    ps = next_ps(g)
                    kb.op("pe", lambda t=t, i=i, ps=ps: nc.tensor.transpose(ps[:, 0:128], xc[i][:, t, :], g.ident[:]),
                          reads=[pd, g.ident.d], writes=[ps.d])
                    kb.op("act", lambda t=t, dst=dst, ps=ps: nc.scalar.copy(
                        dst[:, t * 8:(t + 1) * 8, :], ps[:, 0:128].rearrange("p (a h) -> p a h", a=8)),
                        reads=[ps.d], writes=[pd])
            kb.barrier()
        ckpt("d0")
        with ExitStack() as esu:
            U8 = [T(kb, esu, [128, 8, 512], BF16, f"s5u8_{i}") for i in range(2)]
            U8g = [T(kb, esu, [128, 32, 128], BF16, f"s5u8g_{i}") for i in range(2)]
            utst = [T(kb, esu, [128, 4, 128], BF16, f"s5utst_{i}") for i in range(2)]
            blocks = [(0, 32)] + [(32 + 128 * b, 128) for b in range(4)]
            for bi_, (c0, ncb) in enumerate(blocks):
                kb.pump(1)
                u8, u8g = U8[bi_ % 2], U8g[bi_ % 2]
                kb.dma("sp", lambda u8=u8, c0=c0, ncb=ncb: nc.sync.dma_start(
                    out=u8[0:ncb, :, :], in_=S.u[c0 * 8:(c0 + ncb) * 8, :].rearrange("(c j) f -> c j f", j=8)),
                    reads=[S.u_d], writes=[u8.d])
                kb.op("pool", lambda u8=u8, u8g=u8g, ncb=ncb: nc.gpsimd.tensor_copy(
                    u8g[0:ncb, :, :].rearrange("c g (j h) -> c g j h", j=8),
                    u8[0:ncb, :, :].rearrange("c j (g h) -> c g j h", g=32)), reads=[u8.d], writes=[u8g.d])
                for g4 in range(0, 32, 4):
                    for gg in range(4):
                        kb.op("pe", lambda gg=gg, g4=g4, u8g=u8g, ncb=ncb: nc.tensor.transpose(
                            psb[:, gg * 128:gg * 128 + ncb], u8g[0:ncb, g4 + gg, :], g.identb[0:ncb, 0:ncb]),
                            reads=[u8g.d, g.identb.d], writes=[psb.d])
                    ust = utst[(g4 // 4) % 2]
                    kb.op("act", lambda g4=g4, c0=c0, ncb=ncb, ust=ust: nc.scalar.copy(
                        ust[:, :, 0:ncb], psb[:, 0:512].rearrange("p (a c) -> p a c", a=4)[:, :, 0:ncb]),
                        reads=[psb.d], writes=[ust.d])
                    kb.dma("act", lambda g4=g4, c0=c0, ncb=ncb, ust=ust: nc.scalar.dma_start(
                        out=S.Ut[g4:g4 + 4, :, c0:c0 + ncb].rearrange("a p c -> p a c"), in_=ust[:, :, 0:ncb]),
                        reads=[ust.d], writes=[S.Ut_d])
            kb.barrier()
        ckpt("d1")
        for z in range(2):
            gs = slice(z * 32, z * 32 + 32)
            if z == 1:
                ckpt("dz0")
            with ExitStack() as ez:
                MT = T(kb, ez, [128, 32, 128], BF16, "s5mt")
                RT = T(kb, ez, [128, 32, 128], BF16, "s5rt")
                OTb = T(kb, ez, [128, 32, 128], BF16, "s5otb")
                A8T = T(kb, ez, [128, 32, 128], F32, "s5a8t")
                with ExitStack() as ep:
                    Gall = T(kb, ep, [128, 32, 9, 16], F32, "s5gall")
                    Kn = T(kb, ep, [128, 32, 8, 16], F32, "s5kn")
                    Kp = T(kb, ep, [128, 32, 8, 16], F32, "s5kp")
                    tmpk = T(kb, ep, [128, 32, 16], F32, "s5tmpk")
                    bc = lambda ap2: ap2.unsqueeze(2).to_broadcast([128, 32, 16])
                    for k in range(9):
                        i = k if z == 0 else 8 - k
                        v(lambda k=k, i=i: nc.vector.tensor_tensor(Gall[:, :, i, :], CC[:, gs, :], bc(PW[:, k, 0, gs]),
                                                                  op=ALU.mult))
                        v(lambda k=k: nc.vector.tensor_tensor(tmpk[:], CCs[:, gs, :], bc(PW[:, k, 1, gs]), op=ALU.mult))
                        v(lambda i=i: nc.vector.tensor_tensor(Gall[:, :, i, :], Gall[:, :, i, :], tmpk[:], op=ALU.subtract))
                    for k in range(8):
                        j = k if z == 0 else 7 - k
                        v(lambda k=k, j=j: nc.vector.tensor_tensor(Kn[:, :, j, :], BB[:, gs, :], bc(WN[:, k, 0, gs]),
                                                                  op=ALU.mult))
                        v(lambda k=k: nc.vector.tensor_tensor(tmpk[:], BBs[:, gs, :], bc(WN[:, k, 1, gs]), op=ALU.mult))
                        v(lambda j=j: nc.vector.tensor_tensor(Kn[:, :, j, :], Kn[:, :, j, :], tmpk[:], op=ALU.add))
                        j2 = 7 - k if z == 0 else k
                        v(lambda k=k, j2=j2: nc.vector.tensor_tensor(Kp[:, :, j2, :], BB[:, gs, :], bc(WP[:, k, 0, gs]),
                                                                    op=ALU.mult))
                        v(lambda k=k: nc.vector.tensor_tensor(tmpk[:], BBs[:, gs, :], bc(WP[:, k, 1, gs]), op=ALU.mult))
                        v(lambda j2=j2: nc.vector.tensor_tensor(Kp[:, :, j2, :], Kp[:, :, j2, :], tmpk[:], op=ALU.add))
                    q0 = 0 if z == 0 else 1
                    o0 = 1 if z == 0 else 0
                    for gi in range(32):
                        ps = next_ps(g)
                        kb.op("pe", lambda gi=gi, ps=ps: nc.tensor.matmul(
                            ps[:, 0:128], lhsT=Kn[:, gi, :, :].rearrange("p j h -> p (j h)"),
                            rhs=Gall[:, gi, q0:q0 + 8, :].rearrange("p s h -> p (s h)"), start=True, stop=True),
                            reads=D1, writes=[ps.d])
                        kb.op("dve", lambda gi=gi, ps=ps: nc.vector.tensor_tensor(MT[:, gi, :], ps[:, 0:128], bm[z][:],
                                                                                 op=ALU.mult),
                              reads=[ps.d] + D1, writes=[MT.d])
                        ps2 = next_ps(g)
                        kb.op("pe", lambda gi=gi, ps2=ps2: nc.tensor.transpose(
                            ps2[:, 0:128], Kp[:, gi, :, :].rearrange("p j h -> p (j h)"), g.ident[:]),
                            reads=D1 + [g.ident.d], writes=[ps2.d])
                        kb.op("act", lambda gi=gi, ps2=ps2: nc.scalar.copy(RT[:, gi, :], ps2[:, 0:128]),
                              reads=[ps2.d], writes=[RT.d])
                        kb.op("act", lambda gi=gi: nc.scalar.copy(
                            OTb[:, gi, :], Gall[:, gi, o0:o0 + 8, :].rearrange("p s h -> p (s h)")),
                            reads=D1, writes=[OTb.d])
                        kb.op("pool", lambda gi=gi: nc.gpsimd.tensor_scalar(
                            A8T[:, gi, :], g.ident[:], A8s[:, 0, z * 32 + gi:z * 32 + gi + 1], None, op0=ALU.mult),
                            reads=D1 + [g.ident.d], writes=[A8T.d])
                        kb.op("dve", lambda gi=gi: nc.vector.scalar_tensor_tensor(
                            A8T[:, gi, :], Jt[:], A8s[:, 1, z * 32 + gi:z * 32 + gi + 1], A8T[:, gi, :],
                            op0=ALU.mult, op1=ALU.add), reads=D1 + [A8T.d], writes=[A8T.d])
                    kb.barrier()
                if f"s5mat{z}" in g.dbg:
                    dbg_out(g, f"MT{z}", MT[:], [MT.d], [128, 32, 128], BF16)
                    dbg_out(g, f"RT{z}", RT[:], [RT.d], [128, 32, 128], BF16)
                    dbg_out(g, f"OTb{z}", OTb[:], [OTb.d], [128, 32, 128], BF16)
                    dbg_out(g, f"A8T{z}", A8T[:], [A8T.d], [128, 32, 128], F32)
                if z == 0:
                    ckpt("dm0")
                X = T(kb, ez, [128, 32, NCK], F32, "s5x", nd=32)
                utg = [T(kb, ez, [128, NCK], BF16, f"s5utg{i}") for i in range(3)]
                for gi in range(32):
                    ug = utg[gi % 3]
                    kb.dma("sp", lambda gi=gi, ug=ug: nc.sync.dma_start(out=ug[:], in_=S.Ut[gi, :, :]),
                           reads=[S.Ut_d], writes=[ug.d])
                    for (c0, n) in ((0, 512), (512, NCK - 512)):
                        ps = next_ps(g)
                        kb.op("pe", lambda gi=gi, c0=c0, n=n, ps=ps: nc.tensor.matmul(
                            ps[:, 0:n], lhsT=RT[:, gi, :], rhs=ug[:, c0:c0 + n], start=True, stop=True),
                            reads=[RT.d, ug.d], writes=[ps.d])
                        eng = "act" if gi % 2 == 0 else "dve"
                        if eng == "act":
                            kb.op("act", lambda gi=gi, c0=c0, n=n, ps=ps: nc.scalar.copy(X[:, gi, c0:c0 + n], ps[:, 0:n]),
                                  reads=[ps.d], writes=[X.ds[gi]])
                        else:
                            kb.op("dve", lambda gi=gi, c0=c0, n=n, ps=ps: nc.vector.tensor_copy(X[:, gi, c0:c0 + n], ps[:, 0:n]),
                                  reads=[ps.d], writes=[X.ds[gi]])
                if z == 0:
                    ckpt("d20")
                cur = [T(kb, ez, [128, 32, NMAC], F32, f"s5cur{i}") for i in range(2)]
                Gm = T(kb, ez, [128, 32, NMAC], F32, "s5gm")
                gv = T(kb, ez, [128, 32], F32, "s5gv")
                tu = T(kb, ez, [128, 32], F32, "s5tu")
                tt = T(kb, ez, [128, 32], F32, "s5tt")
                xall = X.ds
                colsel = (lambda i: i) if z == 0 else (lambda i: 15 - i)
                gbanks = ((0, 15), (15, 30), (30, 32))

                def step(src, dst, i, store):
                    col = colsel(i)
                    pss = []
                    for (g0, g1) in gbanks:
                        ps = next_ps(g)
                        pss.append(ps)
                        for gi in range(g0, g1):
                            kb.op("pe", lambda gi=gi, ps=ps, g0=g0: nc.tensor.matmul(
                                ps[:, (gi - g0) * NMAC:(gi - g0 + 1) * NMAC], lhsT=A8T[:, gi, :], rhs=src[:, gi, :],
                                start=True, stop=True), reads=[A8T.d, src.d], writes=[ps.d])
                    for (g0, g1), ps in zip(gbanks, pss):
                        kb.op("dve", lambda g0=g0, g1=g1, ps=ps: nc.vector.tensor_tensor(
                            dst[:, g0:g1, :], ps[:, 0:(g1 - g0) * NMAC].rearrange("p (a m) -> p a m", m=NMAC),
                            X[:, g0:g1, col:NCK:16], op=ALU.add), reads=[ps.d] + xall, writes=[dst.d])
                    if store:
                        kb.op("pool", lambda: nc.gpsimd.tensor_copy(X[:, :, col:NCK:16], src[:]),
                              reads=[src.d] + xall, writes=xall)

                kb.op("pool", lambda: nc.gpsimd.memset(cur[0][:], 0.0), writes=[cur[0].d])
                for i in range(16):
                    if i % 4 == 0:
                        kb.pump(1)
                    step(cur[i % 2], cur[(i + 1) % 2], i, False)
                Em = cur[0]
                qorder = list(range(NMAC)) if z == 0 else [1, 0] + list(range(NMAC - 1, 1, -1))
                kb.op("pool", lambda: nc.gpsimd.memset(gv[:], 0.0), writes=[gv.d])
                for q in qorder:
                    kb.op("act", lambda q=q: nc.scalar.copy(Gm[:, :, q], gv[:]), reads=[gv.d], writes=[Gm.d])
                    ps = next_ps(g)
                    kb.op("pe", lambda ps=ps: nc.tensor.matmul(ps[:, 0:32], lhsT=Jt[:], rhs=gv[:], start=True, stop=True),
                          reads=D1 + [gv.d], writes=[ps.d])
                    kb.op("dve", lambda ps=ps: nc.vector.tensor_tensor(tt[:], ps[:, 0:32], P128[:, 1, gs], op=ALU.mult),
                          reads=[ps.d] + D1, writes=[tt.d])
                    kb.op("pool", lambda q=q: nc.gpsimd.tensor_tensor(tu[:], gv[:], P128[:, 0, gs], op=ALU.mult),
                          reads=[gv.d] + D1, writes=[tu.d])
                    kb.op("pool", lambda q=q: nc.gpsimd.tensor_tensor(tu[:], tu[:], Em[:, :, q], op=ALU.add),
                          reads=[tu.d, Em.d], writes=[tu.d])
                    kb.op("dve", lambda: nc.vector.tensor_tensor(gv[:], tu[:], tt[:], op=ALU.add),
                          reads=[tu.d, tt.d], writes=[gv.d])
                kb.op("dve", lambda: nc.vector.tensor_copy(cur[0][:], Gm[:]), reads=[Gm.d], writes=[cur[0].d])
                for i in range(16):
                    step(cur[i % 2], cur[(i + 1) % 2], i, True)
                if f"s5x{z}" in g.dbg:
                    dbg_out(g, f"X{z}", X[:], xall, [128, 32, NCK], F32)
                if z == 0:
                    ckpt("d30")
                xb = [T(kb, ez, [128, 512], BF16, f"s5xb{i}") for i in range(2)]
                yo = [T(kb, ez, [128, 512], F32, f"s5yo{i}") for i in range(2)]
                yfl = [T(kb, ez, [128, 512], F32, f"s5yfl{i}") for i in range(2)]
                for gi in range(32):
                    xbt = xb[gi % 2]
                    ug = utg[gi % 3]
                    kb.dma("sp", lambda gi=gi, ug=ug: nc.sync.dma_start(out=ug[:], in_=S.Ut[gi, :, :]),
                           reads=[S.Ut_d], writes=[ug.d])
                    kb.op("act", lambda gi=gi, xbt=xbt: nc.scalar.copy(xbt[:], X[:, gi, 32:NCK]), reads=xall, writes=[xbt.d])
                    ps = next_ps(g)
                    kb.op("pe", lambda gi=gi, ps=ps, ug=ug: nc.tensor.matmul(ps[:, :], lhsT=MT[:, gi, :], rhs=ug[:, 32:NCK],
                                                                             start=True, stop=False),
                          reads=[MT.d, ug.d], writes=[ps.d])
                    kb.op("pe", lambda gi=gi, ps=ps, xbt=xbt: nc.tensor.matmul(ps[:, :], lhsT=OTb[:, gi, :], rhs=xbt[:],
                                                                               start=False, stop=True),
                          reads=[OTb.d, xbt.d], writes=[ps.d])
                    yt = yo[gi % 2]
                    if z == 0:
                        kb.op("dve", lambda ps=ps, yt=yt: nc.vector.tensor_copy(yt[:], ps[:, :]),
                              reads=[ps.d], writes=[yt.d])
                    else:
                        yf = yfl[gi % 2]
                        kb.dma("act", lambda gi=gi, yf=yf: nc.scalar.dma_start(out=yf[:], in_=S.y0[gi, :, :]),
                               reads=[S.y0_d], writes=[yf.d])
                        kb.op("dve", lambda ps=ps, yt=yt, yf=yf: nc.vector.tensor_tensor(yt[:], ps[:, :], yf[:], op=ALU.add),
                              reads=[ps.d, yf.d], writes=[yt.d])
                    kb.dma("sp", lambda gi=gi, yt=yt: nc.sync.dma_start(out=S.y0[gi, :, :], in_=yt[:]),
                           reads=[yt.d], writes=[S.y0_d])
                kb.barrier()


SEG = 256
NSEG = NL * 4 // SEG + NE
JB = SEG // 128


def bc_tile(g, es, name, src_row_ap, n, reads=()):
    nc, kb = g.nc, g.kb
    t = T(kb, es, [128, n], F32, name)
    kb.dma("sp", lambda: nc.sync.dma_start(out=t[:], in_=src_row_ap.partition_broadcast(128)),
           reads=list(reads), writes=[t.d])
    return t


def phase_e(g):
    nc, kb, H, S = g.nc, g.kb, g.H, g.S
    R = g.R
    with ExitStack() as es:
        psb = g.psb[0]
        s5T = T(kb, es, [128, 4, NL], BF16, "s5T", nd=4)
        with ExitStack() as e1:
            glw = T(kb, e1, [128, 4, 512], BF16, "glw")
            kb.dma("pool", lambda: nc.gpsimd.dma_start(out=glw[:], in_=H.glu_w[0].rearrange("(k p) n -> p k n", p=128)),
                   writes=[glw.d])
            glb = T(kb, e1, [128, 4], F32, "glb")
            kb.dma("sp", lambda: nc.sync.dma_start(out=glb[:], in_=H.glu_b[0, :].rearrange("(k p) -> p k", p=128),
                                                   allow_slow_non_contiguous=True), writes=[glb.d])
            s5d = T(kb, e1, [128, 4], F32, "s5d")
            kb.dma("sp", lambda: nc.sync.dma_start(out=s5d[:], in_=H.s5_d[0, :].rearrange("(k p) -> p k", p=128),
                                                   allow_slow_non_contiguous=True), writes=[s5d.d])
            Yg = T(kb, e1, [128, 32, 128], F32, "Yg")
            Ytm = T(kb, e1, [128, 8, 512], F32, "Ytm")
            yT = T(kb, e1, [128, 4, 1024], F32, "yTt")
            uTt = T(kb, e1, [128, 4, 1024], BF16, "uTt")
            t1 = T(kb, e1, [128, 4, 1024], F32, "glt1")
            glg = T(kb, e1, [128, 4, 1024], BF16, "glg")
            sg = T(kb, e1, [128, 512], BF16, "glsg")
            for cb in range(4):
                kb.pump(1)
                kb.dma("sp", lambda cb=cb: nc.sync.dma_start(
                    out=Yg[:], in_=S.y0[:, :, cb * 128:(cb + 1) * 128].rearrange("g p c -> p g c")),
                    reads=[S.y0_d], writes=[Yg.d])
                kb.dma("act", lambda cb=cb: nc.scalar.dma_start(
                    out=uTt[:], in_=S.uT[:, NCX + cb * 1024:NCX + (cb + 1) * 1024].rearrange("(k p) t -> p k t", p=128)),
                    reads=[S.uT_d], writes=[uTt.d])
                for g4 in range(0, 32, 4):
                    ps = next_ps(g)
                    for gg in range(4):
                        kb.op("pe", lambda gg=gg, g4=g4, ps=ps: nc.tensor.transpose(
                            ps[:, gg * 128:(gg + 1) * 128], Yg[:, g4 + gg, :], g.ident[:]),
                            reads=[Yg.d, g.ident.d], writes=[ps.d])
                    kb.op("act", lambda g4=g4, ps=ps: nc.scalar.copy(
                        Ytm[:, :, g4 * 16:g4 * 16 + 64].rearrange("c s (a h) -> c a s h", a=4),
                        ps[:, :].rearrange("c (a s h) -> c a s h", a=4, s=8)), reads=[ps.d], writes=[Ytm.d])
                for s_ in range(8):
                    ps = next_ps(g)
                    for cc in range(4):
                        kb.op("pe", lambda cc=cc, s_=s_, ps=ps: nc.tensor.transpose(
                            ps[:, cc * 128:(cc + 1) * 128], Ytm[:, s_, cc * 128:(cc + 1) * 128], g.ident[:]),
                            reads=[Ytm.d, g.ident.d], writes=[ps.d])
                    kb.op("dve", lambda s_=s_, ps=ps: nc.vector.tensor_copy(
                        yT[:, :, s_:1024:8], ps[:, :].rearrange("p (a c) -> p a c", a=4)), reads=[ps.d], writes=[yT.d])
                for cc in range(4):
                    kb.op("dve", lambda cc=cc: nc.vector.scalar_tensor_tensor(
                        yT[:, cc, :], uTt[:, cc, :], s5d[:, cc:cc + 1], yT[:, cc, :], op0=ALU.mult, op1=ALU.add),
                        reads=[uTt.d, s5d.d, yT.d], writes=[yT.d])
                kb.op("pool", lambda: nc.gpsimd.tensor_tensor(t1[:], yT[:], yT[:], op=ALU.mult), reads=[yT.d], writes=[t1.d])
                kb.op("dve", lambda: nc.vector.tensor_scalar(t1[:], t1[:], 0.044715, 1.0, op0=ALU.mult, op1=ALU.add),
                      reads=[t1.d], writes=[t1.d])
                kb.op("pool", lambda: nc.gpsimd.tensor_tensor(t1[:], t1[:], yT[:], op=ALU.mult), reads=[t1.d, yT.d], writes=[t1.d])
                kb.op("act", lambda: nc.scalar.activation(t1[:], t1[:], ACT.Sigmoid, scale=1.5957691216057308),
                      reads=[t1.d], writes=[t1.d])
                kb.op("dve", lambda: nc.vector.tensor_tensor(glg[:], t1[:], yT[:], op=ALU.mult), reads=[t1.d, yT.d], writes=[glg.d])
                for nn in range(4):
                    for th in range(2):
                        ps = next_ps(g)
                        for kc in range(4):
                            kb.op("pe", lambda nn=nn, th=th, kc=kc, ps=ps: nc.tensor.matmul(
                                ps[:, :], lhsT=glw[:, kc, nn * 128:(nn + 1) * 128], rhs=glg[:, kc, th * 512:(th + 1) * 512],
                                start=(kc == 0), stop=(kc == 3)), reads=[glw.d, glg.d], writes=[ps.d])
                        kb.op("act", lambda nn=nn, ps=ps: nc.scalar.activation(sg[:], ps[:, :], ACT.Sigmoid,
                                                                              bias=glb[:, nn:nn + 1], scale=1.0),
                              reads=[ps.d, glb.d], writes=[sg.d])
                        kb.op("dve", lambda nn=nn, th=th, cb=cb: nc.vector.tensor_tensor(
                            s5T[:, nn, cb * 1024 + th * 512:cb * 1024 + (th + 1) * 512], sg[:],
                            glg[:, nn, th * 512:(th + 1) * 512], op=ALU.mult), reads=[sg.d, glg.d], writes=[s5T.ds[nn]])
            kb.barrier()
        if "s5T" in g.dbg:
            dbg_out(g, "s5T", s5T[:], s5T.ds, [128, 4, NL], BF16)
        glaT = T(kb, es, [128, 4, NL], BF16, "glaTt")
        kb.dma("sp", lambda: nc.sync.dma_start(out=glaT[:], in_=S.glaT.ap().rearrange("(k p) t -> p k t", p=128)),
               reads=[S.glaT_d], writes=[glaT.d])
        wo = T(kb, es, [128, 8, D], BF16, "wo")
        kb.dma("pool", lambda: nc.gpsimd.dma_start(out=wo[:], in_=H.w_out[0].rearrange("(k p) n -> p k n", p=128)),
               writes=[wo.d])
        g1b = bc_tile(g, es, "g1b", S.mod[0:1, 2 * D:3 * D], D, [S.mod_d])
        sh2b = bc_tile(g, es, "sh2b", S.mod[0:1, 3 * D:4 * D], D, [S.mod_d])
        g2e = bc_tile(g, es, "g2e", S.mod[0:1, 4 * D:5 * D], D, [S.mod_d])
        nfw = bc_tile(g, es, "nfw", H.norm_ffn_w[0:1, :], D)
        kb.op("dve", lambda: nc.vector.scalar_tensor_tensor(g2e[:], g2e[:], 1.0, nfw[:], op0=ALU.add, op1=ALU.mult),
              reads=[g2e.d, nfw.d], writes=[g2e.d])
        rw = T(kb, es, [128, 8, NE], F32, "rw")
        kb.dma("sp", lambda: nc.sync.dma_start(out=rw[:], in_=H.router_w[0].rearrange("(k p) e -> p k e", p=128)),
               writes=[rw.d])
        rbb = bc_tile(g, es, "rbb", H.router_b[0:1, :], NE)
        xt = [T(kb, es, [128, D], F32, f"ext{i}") for i in range(2)]
        x2t = [T(kb, es, [128, D], F32, f"ex2{i}") for i in range(2)]
        tmp_ = [T(kb, es, [128, D], F32, f"etmp{i}") for i in range(2)]
        tmq_ = [T(kb, es, [128, D], F32, f"etmq{i}") for i in range(2)]
        h2_ = [T(kb, es, [128, D], F32, f"eh2{i}") for i in range(2)]
        h2b = [T(kb, es, [128, D], BF16, f"eh2b{i}") for i in range(2)]
        hT2_ = [T(kb, es, [128, 8, 128], F32, f"ehT2{i}") for i in range(2)]
        junk = T(kb, es, [128, D], BF16, "ejunk")
        lg_ = [T(kb, es, [128, NE], F32, f"elg{i}") for i in range(2)]
        mx8_ = [T(kb, es, [128, 8], F32, f"emx8{i}") for i in range(2)]
        ix8_ = [T(kb, es, [128, 8], U32, f"eix8{i}") for i in range(2)]
        sm1_ = [T(kb, es, [128, 4], F32, f"esm{i}") for i in range(2)]
        for tl in range(NL // 128):
            if tl % 4 == 0:
                kb.pump(1)
            t0 = tl * 128
            x_t, x2 = xt[tl % 2], x2t[tl % 2]
            tmp, tmq, h2, hT2 = tmp_[tl % 2], tmq_[tl % 2], h2_[tl % 2], hT2_[tl % 2]
            lg, mx8, ix8, sm1 = lg_[tl % 2], mx8_[tl % 2], ix8_[tl % 2], sm1_[tl % 2]
            kb.dma("act", lambda x_t=x_t, t0=t0: nc.scalar.dma_start(out=x_t[:], in_=H.x[t0:t0 + 128, :]), writes=[x_t.d])
            for nh in range(2):
                ps = next_ps(g)
                for r in range(2):
                    row = tl * 2 + r
                    for kc in range(8):
                        if kc < 4:
                            lh = glaT[:, kc, row * 64:(row + 1) * 64]
                            rd = [glaT.d]
                        else:
                            lh = s5T[:, kc - 4, row:NL:64]
                            rd = s5T.ds
                        kb.op("pe", lambda lh=lh, r=r, kc=kc, nh=nh, ps=ps: nc.tensor.matmul(
                            ps[r * 64:(r + 1) * 64, :], lhsT=lh, rhs=wo[:, kc, nh * 512:(nh + 1) * 512],
                            start=(kc == 0), stop=(kc == 7)), reads=rd + [wo.d], writes=[ps.d])
                kb.op("dve", lambda nh=nh, ps=ps: nc.vector.tensor_tensor(
                    tmp[:, nh * 512:(nh + 1) * 512], ps[:, :], g1b[:, nh * 512:(nh + 1) * 512], op=ALU.mult),
                    reads=[ps.d, g1b.d], writes=[tmp.d])
            kb.op("pool", lambda x2=x2, x_t=x_t, tmp=tmp: nc.gpsimd.tensor_tensor(x2[:], tmp[:], x_t[:], op=ALU.add),
                  reads=[tmp.d, x_t.d], writes=[x2.d])
            kb.dma("sp", lambda x2=x2, t0=t0: nc.sync.dma_start(out=S.x2[t0:t0 + 128, :], in_=x2[:]),
                   reads=[x2.d], writes=[S.x2_d])
            kb.op("act", lambda x2=x2: nc.scalar.activation(junk[:], x2[:], ACT.Square, accum_out=sm1[:, 0:1]),
                  reads=[x2.d], writes=[junk.d, sm1.d])
            kb.op("dve", lambda: nc.vector.tensor_scalar(sm1[:, 1:2], sm1[:, 0:1], 1.0 / D, EPS, op0=ALU.mult, op1=ALU.add),
                  reads=[sm1.d], writes=[sm1.d])
            kb.op("act", lambda: nc.scalar.activation(sm1[:, 1:2], sm1[:, 1:2], ACT.Sqrt), reads=[sm1.d], writes=[sm1.d])
            kb.op("dve", lambda: nc.vector.reciprocal(sm1[:, 2:3], sm1[:, 1:2]), reads=[sm1.d], writes=[sm1.d])
            kb.op("dve", lambda x2=x2: nc.vector.scalar_tensor_tensor(tmq[:], x2[:], sm1[:, 2:3], g2e[:],
                                                                      op0=ALU.mult, op1=ALU.mult),
                  reads=[x2.d, sm1.d, g2e.d], writes=[tmq.d])
            kb.op("pool", lambda: nc.gpsimd.tensor_tensor(h2[:], tmq[:], sh2b[:], op=ALU.add),
                  reads=[tmq.d, sh2b.d], writes=[h2.d])
            hb = h2b[tl % 2]
            kb.op("act", lambda hb=hb: nc.scalar.copy(hb[:], h2[:]), reads=[h2.d], writes=[hb.d])
            kb.dma("sp", lambda hb=hb, t0=t0: nc.sync.dma_start(out=S.h2[t0:t0 + 128, :], in_=hb[:]),
                   reads=[hb.d], writes=[S.h2_d])
            for hf in range(2):
                ps = next_ps(g)
                for kk in range(4):
                    kc = hf * 4 + kk
                    kb.op("pe", lambda kc=kc, kk=kk, ps=ps: nc.tensor.transpose(
                        ps[:, kk * 128:(kk + 1) * 128], h2[:, kc * 128:(kc + 1) * 128], g.ident[:]),
                        reads=[h2.d, g.ident.d], writes=[ps.d])
                kb.op("act", lambda hf=hf, ps=ps: nc.scalar.copy(
                    hT2[:, hf * 4:(hf + 1) * 4, :], ps[:, :].rearrange("p (a t) -> p a t", a=4)),
                    reads=[ps.d], writes=[hT2.d])
            ps = next_ps(g)
            for kc in range(8):
                kb.op("pe", lambda kc=kc, ps=ps: nc.tensor.matmul(ps[:, 0:NE], lhsT=hT2[:, kc, :], rhs=rw[:, kc, :],
                                                                  start=(kc == 0), stop=(kc == 7)),
                      reads=[hT2.d, rw.d], writes=[ps.d])
            kb.op("dve", lambda ps=ps: nc.vector.tensor_tensor(lg[:], ps[:, 0:NE], rbb[:], op=ALU.add),
                  reads=[ps.d, rbb.d], writes=[lg.d])
            kb.op("dve", lambda: nc.vector.max(mx8[:], lg[:]), reads=[lg.d], writes=[mx8.d])
            kb.op("dve", lambda: nc.vector.max_index(ix8[:], mx8[:], lg[:]), reads=[lg.d, mx8.d], writes=[ix8.d])
            kb.op("dve", lambda tl=tl: nc.vector.tensor_copy(R.idxf[:, tl, :], ix8[:, 0:4]), reads=[ix8.d], writes=[R.idxf.d])
            kb.op("dve", lambda tl=tl: nc.vector.tensor_scalar(R.mask[:, tl, :], lg[:], mx8[:, 3:4], None, op0=ALU.is_ge),
                  reads=[lg.d, mx8.d], writes=[R.mask.d])
            kb.op("dve", lambda: nc.vector.tensor_scalar(sm1[:, 3:4], mx8[:, 0:1], -1.0, None, op0=ALU.mult),
                  reads=[mx8.d, sm1.d], writes=[sm1.d])
            kb.op("act", lambda tl=tl: nc.scalar.activation(R.gate[:, tl, :], mx8[:, 0:4], ACT.Exp, bias=sm1[:, 3:4],
                                                            scale=1.0, accum_out=sm1[:, 0:1]),
                  reads=[mx8.d, sm1.d], writes=[R.gate.d, sm1.d])
            kb.op("dve", lambda: nc.vector.reciprocal(sm1[:, 1:2], sm1[:, 0:1]), reads=[sm1.d], writes=[sm1.d])
            kb.op("dve", lambda tl=tl: nc.vector.tensor_scalar(R.gate[:, tl, :], R.gate[:, tl, :], sm1[:, 1:2], None,
                                                               op0=ALU.mult), reads=[R.gate.d, sm1.d], writes=[R.gate.d])
        if "logits" in g.dbg:
            dbg_out(g, "gate", R.gate[:], [R.gate.d], [128, 32, 4])
            dbg_out(g, "idxf", R.idxf[:], [R.idxf.d], [128, 32, 4])
        onesf = T(kb, es, [128, 128], F32, "onesf")
        kb.op("pool", lambda: nc.gpsimd.memset(onesf[:], 1.0), writes=[onesf.d])
        triu = T(kb, es, [128, 128], F32, "triu")
        kb.op("dve", lambda: nc.vector.tensor_single_scalar(triu[:], g.iota[:], 0.0, op=ALU.is_gt),
              reads=[g.iota.d], writes=[triu.d])
        ps = next_ps(g)
        for tl in range(32):
            kb.op("pe", lambda tl=tl, ps=ps: nc.tensor.matmul(ps[:, 0:NE], lhsT=onesf[:], rhs=R.mask[:, tl, :],
                                                              start=(tl == 0), stop=(tl == 31)),
                  reads=[onesf.d, R.mask.d], writes=[ps.d])
        cnt = T(kb, es, [128, NE], F32, "cnt")
        nsg = T(kb, es, [128, NE], F32, "nsg")
        pend = T(kb, es, [128, NE], F32, "pend")
        pst = T(kb, es, [128, NE], F32, "pst")
        t32 = T(kb, es, [128, NE], F32, "t32")
        kb.op("dve", lambda ps=ps: nc.vector.tensor_copy(cnt[:], ps[:, 0:NE]), reads=[ps.d], writes=[cnt.d])
        kb.op("dve", lambda: nc.vector.memset(nsg[:], 0.0), writes=[nsg.d])
        for k in range(NL // SEG):
            kb.op("dve", lambda k=k: nc.vector.tensor_scalar(t32[:], cnt[:], float(SEG * k) + 0.5, None, op0=ALU.is_ge),
                  reads=[cnt.d], writes=[t32.d])
            kb.op("dve", lambda: nc.vector.tensor_tensor(nsg[:], nsg[:], t32[:], op=ALU.add), reads=[nsg.d, t32.d], writes=[nsg.d])
        kb.op("dve", lambda: nc.vector.tensor_tensor_scan(pend[:], onesf[:, 0:NE], nsg[:], 0.0, ALU.mult, ALU.add),
              reads=[onesf.d, nsg.d], writes=[pend.d])
        kb.op("dve", lambda: nc.vector.tensor_tensor(pst[:], pend[:], nsg[:], op=ALU.subtract), reads=[pend.d, nsg.d], writes=[pst.d])
        kb.op("dve", lambda: nc.vector.tensor_scalar(pst[:], pst[:], float(SEG), None, op0=ALU.mult), reads=[pst.d], writes=[pst.d])
        sidx = T(kb, es, [128, NSEG], F32, "sidx")
        kb.op("pool", lambda: nc.gpsimd.iota(sidx[:], pattern=[[1, NSEG]], base=0, channel_multiplier=0,
                                              allow_small_or_imprecise_dtypes=True), writes=[sidx.d])
        cmp3 = T(kb, es, [128, NSEG, NE], F32, "cmp3")
        kb.op("dve", lambda: nc.vector.tensor_tensor(cmp3[:], pend[:].unsqueeze(1).to_broadcast([128, NSEG, NE]),
                                                     sidx[:].unsqueeze(2).to_broadcast([128, NSEG, NE]), op=ALU.is_le),
              reads=[pend.d, sidx.d], writes=[cmp3.d])
        sef = T(kb, es, [128, NSEG], F32, "sef")
        kb.op("dve", lambda: nc.vector.tensor_reduce(sef[:], cmp3[:], axis=AX.X, op=ALU.add), reads=[cmp3.d], writes=[sef.d])
        kb.op("dve", lambda: nc.vector.tensor_scalar(sef[:], sef[:], float(NE - 1), None, op0=ALU.min), reads=[sef.d], writes=[sef.d])
        kb.op("dve", lambda: nc.vector.tensor_copy(R.segexp[:], sef[:]), reads=[sef.d], writes=[R.segexp.d])
        used = T(kb, es, [128, NSEG], F32, "used")
        kb.op("dve", lambda: nc.vector.tensor_scalar(used[:], sidx[:], pend[:, NE - 1:NE], None, op0=ALU.is_lt),
              reads=[sidx.d, pend.d], writes=[used.d])
        pcf = T(kb, es, [128, 1], F32, "pcf")
        kb.op("pool", lambda: nc.gpsimd.iota(pcf[:], pattern=[[0, 1]], base=0, channel_multiplier=1,
                                              allow_small_or_imprecise_dtypes=True), writes=[pcf.d])
        OOB = 1000000.0
        sgf = T(kb, es, [128, NSEG], F32, "sgf")
        kb.op("dve", lambda: nc.vector.tensor_scalar(sgf[:], sef[:], 128.0, pcf[:, 0:1], op0=ALU.mult, op1=ALU.add),
              reads=[sef.d, pcf.d], writes=[sgf.d])
        for src, dst in ((sgf, R.segidx), (sef, R.segrow)):
            kb.op("dve", lambda src=src: nc.vector.tensor_scalar(src[:], src[:], -OOB, None, op0=ALU.add),
                  reads=[src.d], writes=[src.d])
            kb.op("dve", lambda src=src: nc.vector.tensor_tensor(src[:], src[:], used[:], op=ALU.mult),
                  reads=[src.d, used.d], writes=[src.d])
            kb.op("dve", lambda src=src: nc.vector.tensor_scalar(src[:], src[:], OOB, None, op0=ALU.add),
                  reads=[src.d], writes=[src.d])
            kb.op("dve", lambda src=src, dst=dst: nc.vector.tensor_copy(dst[:], src[:]), reads=[src.d], writes=[dst.d])
        if "logits" in g.dbg:
            dbg_out(g, "cnt", cnt[:], [cnt.d], [128, NE])
            dbg_out(g, "segexp", R.segexp[:], [R.segexp.d], [128, NSEG], I32)
        carry = T(kb, es, [128, NE], F32, "carry")
        kb.op("dve", lambda: nc.vector.tensor_copy(carry[:], pst[:]), reads=[pst.d], writes=[carry.d])
        ief = T(kb, es, [128, NE], F32, "ief")
        kb.op("pool", lambda: nc.gpsimd.iota(ief[:], pattern=[[1, NE]], base=0, channel_multiplier=0,
                                              allow_small_or_imprecise_dtypes=True), writes=[ief.d])
        slf = T(kb, es, [128, NE], F32, "slf")
        slk = T(kb, es, [128, 4], F32, "slk")
        hld = [T(kb, es, [128, D], BF16, f"hld{i}") for i in range(2)]
        for tl in range(32):
            t0 = tl * 128
            psA = next_ps(g)
            kb.op("pe", lambda tl=tl, psA=psA: nc.tensor.matmul(psA[:, 0:NE], lhsT=triu[:], rhs=R.mask[:, tl, :],
                                                                start=True, stop=True),
                  reads=[triu.d, R.mask.d], writes=[psA.d])
            kb.op("dve", lambda psA=psA: nc.vector.tensor_tensor(slf[:], psA[:, 0:NE], carry[:], op=ALU.add),
                  reads=[psA.d, carry.d], writes=[slf.d])
            psB = next_ps(g)
            kb.op("pe", lambda tl=tl, psB=psB: nc.tensor.matmul(psB[:, 0:NE], lhsT=onesf[:], rhs=R.mask[:, tl, :],
                                                                start=True, stop=True),
                  reads=[onesf.d, R.mask.d], writes=[psB.d])
            kb.op("dve", lambda psB=psB: nc.vector.tensor_tensor(carry[:], carry[:], psB[:, 0:NE], op=ALU.add),
                  reads=[psB.d, carry.d, slf.d], writes=[carry.d])
            for k in range(4):
                kb.op("dve", lambda k=k, tl=tl: nc.vector.scalar_tensor_tensor(
                    t32[:], ief[:], R.idxf[:, tl, k:k + 1], slf[:], op0=ALU.is_equal, op1=ALU.mult,
                    accum_out=slk[:, k:k + 1]), reads=[ief.d, R.idxf.d, slf.d], writes=[t32.d, slk.d])
            kb.op("dve", lambda tl=tl: nc.vector.tensor_copy(R.slot[:, tl, :], slk[:]), reads=[slk.d], writes=[R.slot.ds[tl]])
            hl = hld[tl % 2]
            kb.dma("sp", lambda hl=hl, t0=t0: nc.sync.dma_start(out=hl[:], in_=S.h2[t0:t0 + 128, :]),
                   reads=[S.h2_d], writes=[hl.d])
            for k in range(4):
                kb.dma("pool", lambda hl=hl, tl=tl, k=k: nc.gpsimd.indirect_dma_start(
                    out=S.xg[:, :], out_offset=bass.IndirectOffsetOnAxis(ap=R.slot[:, tl, k:k + 1], axis=0),
                    in_=hl[:, :], in_offset=None), reads=[hl.d, R.slot.ds[tl]], writes=[S.xg_d])
        if "logits" in g.dbg:
            dbg_out(g, "slot", R.slot[:], R.slot.ds, [128, 32, 4], I32)
        bg = T(kb, es, [NE, 2 * D], F32, "bgrow")
        kb.dma("sp", lambda: nc.sync.dma_start(out=bg[:], in_=H.exp_b_gu[0]), writes=[bg.d])
        bgt = T(kb, es, [128, 16, NE], F32, "bgt")
        for c4 in range(0, 16, 4):
            ps = next_ps(g)
            for cc in range(4):
                kb.op("pe", lambda cc=cc, c4=c4, ps=ps: nc.tensor.transpose(
                    ps[:, cc * NE:(cc + 1) * NE], bg[:, (c4 + cc) * 128:(c4 + cc + 1) * 128], g.ident[0:NE, 0:NE]),
                    reads=[bg.d, g.ident.d], writes=[ps.d])
            kb.op("act", lambda c4=c4, ps=ps: nc.scalar.copy(
                bgt[:, c4:c4 + 4, :], ps[:, 0:4 * NE].rearrange("p (a e) -> p a e", a=4)), reads=[ps.d], writes=[bgt.d])
        kb.dma("sp", lambda: nc.sync.dma_start(out=S.bguT.ap().rearrange("e p c -> p c e"), in_=bgt[:],
                                               allow_slow_non_contiguous=True), reads=[bgt.d], writes=[S.bguT_d])


def phase_f(g):
    nc, kb, H, S = g.nc, g.kb, g.H, g.S
    R = g.R
    with ExitStack() as es:
        wgu = [T(kb, es, [128, 8, 2 * D], BF16, f"wgu{i}") for i in range(2)]
        wdn = [T(kb, es, [128, 8, D], BF16, f"wdn{i}") for i in range(2)]
        bgu = [T(kb, es, [128, 16], F32, f"bgu{i}") for i in range(2)]
        bdn = [T(kb, es, [128, D], F32, f"bdn{i}") for i in range(2)]
        bgs = [T(kb, es, [128, 8], F32, f"bgs{i}") for i in range(2)]
        xrow = [T(kb, es, [128, JB, D], BF16, f"xrow{i}") for i in range(2)]
        xT = [T(kb, es, [128, 8, SEG], BF16, f"xT{i}") for i in range(2)]
        actT = [T(kb, es, [128, 8, SEG], BF16, f"actT{i}", nd=8) for i in range(2)]
        yrow = [T(kb, es, [128, JB, D], BF16, f"yrow{i}") for i in range(2)]
        L0 = [T(kb, es, [128, SEG], F32, f"fL0_{i}") for i in range(2)]
        Gc = [T(kb, es, [128, SEG], F32, f"fGc_{i}") for i in range(2)]
        sg = [T(kb, es, [128, SEG], F32, f"fsg_{i}") for i in range(2)]
        tt = [T(kb, es, [128, SEG], F32, f"ftt_{i}") for i in range(2)]
        bc_rows = nc.gpsimd.to_reg(NE * 128 - 1)
        bc_e = nc.gpsimd.to_reg(NE - 1)
        wg_rows = S.wg.ap().rearrange("e p f -> (e p) f")
        wd_rows = S.wd.ap().rearrange("e p f -> (e p) f")
        bgu_rows = S.bguT.ap().rearrange("e p c -> (e p) c")
        nseg = g.nseg_limit if getattr(g, "nseg_limit", None) else NSEG
        bench = getattr(g, "bench", "") or ""
        for s in range(nseg):
            b = s % 2
            if "nodma" in bench and s >= 2:
                KB.dead = True
            kb.dma("pool", lambda: nc.gpsimd.indirect_dma_start(
                out=wgu[b][:, :, :].rearrange("p k n -> p (k n)"), out_offset=None, in_=wg_rows,
                in_offset=bass.IndirectOffsetOnAxis(ap=R.segidx[:, s:s + 1], axis=0),
                bounds_check=bc_rows, oob_is_err=False),
                reads=[R.segidx.d, S.wg_d], writes=[wgu[b].d])
            kb.dma("pool", lambda: nc.gpsimd.indirect_dma_start(
                out=wdn[b][:, :, :].rearrange("p k n -> p (k n)"), out_offset=None, in_=wd_rows,
                in_offset=bass.IndirectOffsetOnAxis(ap=R.segidx[:, s:s + 1], axis=0),
                bounds_check=bc_rows, oob_is_err=False),
                reads=[R.segidx.d, S.wd_d], writes=[wdn[b].d])
            kb.dma("pool", lambda: nc.gpsimd.indirect_dma_start(
                out=bgu[b][:, :], out_offset=None, in_=bgu_rows,
                in_offset=bass.IndirectOffsetOnAxis(ap=R.segidx[:, s:s + 1], axis=0),
                bounds_check=bc_rows, oob_is_err=False),
                reads=[R.segidx.d, S.bguT_d], writes=[bgu[b].d])
            kb.dma("pool", lambda: nc.gpsimd.indirect_dma_start(
                out=bdn[b][:, :], out_offset=None, in_=H.exp_b_down[0],
                in_offset=bass.IndirectOffsetOnAxis(ap=R.segrow[:, s:s + 1], axis=0),
                bounds_check=bc_e, oob_is_err=False),
                reads=[R.segrow.d], writes=[bdn[b].d])
            if bench:
                KB.dead = False
            kb.op("act", lambda: nc.scalar.mul(bgs[b][:], bgu[b][:, 8:16], 1.0 / 1.702), reads=[bgu[b].d], writes=[bgs[b].d])
            kb.dma("sp", lambda: nc.sync.dma_start(
                out=xrow[b][:], in_=S.xg[s * SEG:(s + 1) * SEG, :].rearrange("(j p) d -> p j d", p=128)),
                reads=[S.xg_d], writes=[xrow[b].d])
            if "nocomp" in bench:
                KB.dead = True
            for kc in range(8):
                pb = g.psb[kc % 2]
                for j in range(JB):
                    kb.op("pe", lambda j=j, kc=kc, pb=pb: nc.tensor.transpose(
                        pb[:, j * 128:(j + 1) * 128], xrow[b][:, j, kc * 128:(kc + 1) * 128], g.identb[:]),
                        reads=[xrow[b].d, g.identb.d], writes=[pb.d])
                if kc % 2 == 0:
                    kb.op("act", lambda kc=kc, pb=pb: nc.scalar.copy(xT[b][:, kc, :], pb[:, 0:SEG]),
                          reads=[pb.d], writes=[xT[b].d])
                else:
                    kb.op("dve", lambda kc=kc, pb=pb: nc.vector.tensor_copy(xT[b][:, kc, :], pb[:, 0:SEG]),
                          reads=[pb.d], writes=[xT[b].d])
            for c in range(8):
                i2 = c % 2
                pg = next_ps(g)
                for kc in range(8):
                    kb.op("pe", lambda kc=kc, c=c, pg=pg: nc.tensor.matmul(
                        pg[:, 0:SEG], lhsT=wgu[b][:, kc, c * 128:(c + 1) * 128], rhs=xT[b][:, kc, :],
                        start=(kc == 0), stop=(kc == 7)), reads=[wgu[b].d, xT[b].d], writes=[pg.d])
                pl_ = next_ps(g)
                for kc in range(8):
                    kb.op("pe", lambda kc=kc, c=c, pl_=pl_: nc.tensor.matmul(
                        pl_[:, 0:SEG], lhsT=wgu[b][:, kc, D + c * 128:D + (c + 1) * 128], rhs=xT[b][:, kc, :],
                        start=(kc == 0), stop=(kc == 7)), reads=[wgu[b].d, xT[b].d], writes=[pl_.d])
                kb.op("dve", lambda c=c, pg=pg: nc.vector.tensor_scalar(Gc[i2][:], pg[:, 0:SEG], bgu[b][:, c:c + 1], 7.0,
                                                                      op0=ALU.add, op1=ALU.min),
                      reads=[pg.d, bgu[b].d], writes=[Gc[i2].d])
                kb.op("act", lambda: nc.scalar.activation(sg[i2][:], Gc[i2][:], ACT.Silu, scale=1.702),
                      reads=[Gc[i2].d], writes=[sg[i2].d])
                kb.op("act", lambda c=c, pl_=pl_: nc.scalar.activation(L0[i2][:], pl_[:, 0:SEG], ACT.Identity,
                                                                      bias=bgs[b][:, c:c + 1], scale=1.0 / 1.702),
                      reads=[pl_.d, bgs[b].d], writes=[L0[i2].d])
                kb.op("dve", lambda: nc.vector.tensor_scalar(L0[i2][:], L0[i2][:], 7.0 / 1.702, -7.0 / 1.702,
                                                             op0=ALU.min, op1=ALU.max),
                      reads=[L0[i2].d], writes=[L0[i2].d])
                kb.op("dve", lambda c=c: nc.vector.scalar_tensor_tensor(actT[b][:, c, :], L0[i2][:], 1.0 / 1.702, sg[i2][:],
                                                                       op0=ALU.add, op1=ALU.mult),
                      reads=[L0[i2].d, sg[i2].d], writes=[actT[b].ds[c]])
            for j in range(JB):
                for nh in range(2):
                    ps = next_ps(g)
                    for c in range(8):
                        kb.op("pe", lambda c=c, j=j, nh=nh, ps=ps: nc.tensor.matmul(
                            ps[:, :], lhsT=actT[b][:, c, j * 128:(j + 1) * 128], rhs=wdn[b][:, c, nh * 512:(nh + 1) * 512],
                            start=(c == 0), stop=(c == 7)), reads=[actT[b].ds[c], wdn[b].d], writes=[ps.d])
                    kb.op("dve", lambda j=j, nh=nh, ps=ps: nc.vector.tensor_tensor(
                        yrow[b][:, j, nh * 512:(nh + 1) * 512], ps[:, :], bdn[b][:, nh * 512:(nh + 1) * 512], op=ALU.add),
                        reads=[ps.d, bdn[b].d], writes=[yrow[b].d])
            kb.dma("act", lambda: nc.scalar.dma_start(
                out=S.yg[s * SEG:(s + 1) * SEG, :].rearrange("(j p) d -> p j d", p=128), in_=yrow[b][:]),
                reads=[yrow[b].d], writes=[S.yg_d])
            if bench:
                KB.dead = False


def phase_g(g):
    nc, kb, H, S = g.nc, g.kb, g.H, g.S
    R = g.R
    with ExitStack() as es:
        g2b = bc_tile(g, es, "g2b", S.mod[0:1, 5 * D:6 * D], D, [S.mod_d])
        fnw = bc_tile(g, es, "fnw", H.final_norm_w.ap().rearrange("(o d) -> o d", o=1), D)
        yk = [[T(kb, es, [128, D], BF16, f"yk{i}_{k}") for k in range(4)] for i in range(2)]
        x2t = [T(kb, es, [128, D], F32, f"gx2{i}") for i in range(2)]
        acc_ = [T(kb, es, [128, D], F32, f"gacc{i}") for i in range(2)]
        x3_ = [T(kb, es, [128, D], F32, f"gx3{i}") for i in range(2)]
        sm_ = [T(kb, es, [128, 4], F32, f"gsm{i}") for i in range(2)]
        ot = [T(kb, es, [128, D], F32, f"got{i}") for i in range(2)]
        junk = T(kb, es, [128, D], BF16, "gjunk")
        sm1 = T(kb, es, [128, 4], F32, "gsm")
        for tl in range(NL // 128):
            t0 = tl * 128
            b = tl % 2
            acc, x3, sm1 = acc_[b], x3_[b], sm_[b]
            for k in range(4):
                kb.dma("pool", lambda k=k: nc.gpsimd.indirect_dma_start(
                    out=yk[b][k][:, :], out_offset=None, in_=S.yg[:, :],
                    in_offset=bass.IndirectOffsetOnAxis(ap=R.slot[:, tl, k:k + 1], axis=0)),
                    reads=[S.yg_d, R.slot.ds[tl]], writes=[yk[b][k].d])
            kb.dma("sp", lambda: nc.sync.dma_start(out=x2t[b][:], in_=S.x2[t0:t0 + 128, :]), reads=[S.x2_d], writes=[x2t[b].d])
            kb.op("dve", lambda: nc.vector.tensor_scalar(acc[:], yk[b][0][:], R.gate[:, tl, 0:1], None, op0=ALU.mult),
                  reads=[yk[b][0].d, R.gate.d], writes=[acc.d])
            for k in range(1, 4):
                kb.op("dve", lambda k=k: nc.vector.scalar_tensor_tensor(acc[:], yk[b][k][:], R.gate[:, tl, k:k + 1], acc[:],
                                                                       op0=ALU.mult, op1=ALU.add),
                      reads=[yk[b][k].d, R.gate.d, acc.d], writes=[acc.d])
            kb.op("pool", lambda: nc.gpsimd.tensor_tensor(acc[:], acc[:], g2b[:], op=ALU.mult), reads=[acc.d, g2b.d], writes=[acc.d])
            kb.op("dve", lambda: nc.vector.tensor_tensor(x3[:], acc[:], x2t[b][:], op=ALU.add), reads=[acc.d, x2t[b].d], writes=[x3.d])
            kb.op("act", lambda: nc.scalar.activation(junk[:], x3[:], ACT.Square, accum_out=sm1[:, 0:1]),
                  reads=[x3.d], writes=[junk.d, sm1.d])
            kb.op("dve", lambda: nc.vector.tensor_scalar(sm1[:, 1:2], sm1[:, 0:1], 1.0 / D, EPS, op0=ALU.mult, op1=ALU.add),
                  reads=[sm1.d], writes=[sm1.d])
            kb.op("act", lambda: nc.scalar.activation(sm1[:, 1:2], sm1[:, 1:2], ACT.Sqrt), reads=[sm1.d], writes=[sm1.d])
            kb.op("dve", lambda: nc.vector.reciprocal(sm1[:, 2:3], sm1[:, 1:2]), reads=[sm1.d], writes=[sm1.d])
            kb.op("dve", lambda: nc.vector.scalar_tensor_tensor(ot[b][:], x3[:], sm1[:, 2:3], fnw[:], op0=ALU.mult, op1=ALU.mult),
                  reads=[x3.d, sm1.d, fnw.d], writes=[ot[b].d])
            kb.dma("act", lambda: nc.scalar.dma_start(out=H.out[t0:t0 + 128, :], in_=ot[b][:]), reads=[ot[b].d], writes=[g.out_d])


_SHARED = ("c_ctx", "ada_w", "ada_b", "norm_mix_w", "w_in", "gla_lr_up", "gla_lr_bias", "gla_norm_w",
           "s5_lam_re", "s5_lam_im", "s5_log_dt", "s5_b_re", "s5_b_im", "s5_c_re", "s5_c_im", "s5_d",
           "glu_w", "glu_b", "w_out", "norm_ffn_w", "router_w", "router_b", "exp_w_gu", "exp_b_gu",
           "exp_w_down", "exp_b_down", "final_norm_w")


def kernel(x, c, ctx, c_ctx, ada_w, ada_b, norm_mix_w, w_in, gla_lr_up, gla_lr_bias, gla_norm_w,
           s5_lam_re, s5_lam_im, s5_log_dt, s5_b_re, s5_b_im, s5_c_re, s5_c_im, s5_d, glu_w, glu_b,
           w_out, norm_ffn_w, router_w, router_b, exp_w_gu, exp_b_gu, exp_w_down, exp_b_down,
           final_norm_w):
    loc = locals()
    shared = {k: np.ascontiguousarray(np.asarray(loc[k], dtype=np.float32)) for k in _SHARED}
    x = np.asarray(x, dtype=np.float32)
    c = np.asarray(c, dtype=np.float32)
    ctx = np.asarray(ctx, dtype=np.float32)
    nb = x.shape[0]
    nc, _ = build()
    in_maps = []
    for b in range(nb):
        m = dict(shared)
        m["x"] = np.ascontiguousarray(x[b])
        m["ctx"] = np.ascontiguousarray(ctx[b])
        m["c"] = np.ascontiguousarray(c[b:b + 1])
        in_maps.append(m)
    res = run_bass_kernel_spmd(nc, in_maps, core_ids=list(range(nb)))
    return np.stack([np.asarray(r["out"], dtype=np.float32) for r in res.results], axis=0)
```

```python
import numpy as np
import concourse.bass as bass
import concourse.mybir as mybir
from concourse.bass_utils import run_bass_kernel_spmd
from contextlib import ExitStack

F32 = mybir.dt.float32
BF16 = mybir.dt.bfloat16
U32 = mybir.dt.uint32
I32 = mybir.dt.int32
ACT = mybir.ActivationFunctionType
ALU = mybir.AluOpType
AX = mybir.AxisListType
PoolE = mybir.EngineType.Pool

D = 1024
NL = 4096
NCX = 256
NT = NL + NCX
NE = 32
EPS = 1e-6


class Dep:
    __slots__ = ("w", "r", "name")

    def __init__(self, name=""):
        self.w = None
        self.r = []
        self.name = name


class KB:
    NSLOT = 8

    def __init__(self, nc, es):
        self.nc = nc
        self.engs = {"pe": nc.tensor, "dve": nc.vector, "act": nc.scalar,
                     "pool": nc.gpsimd, "sp": nc.sync}
        self.sem = {}
        self.cnt = {}
        for k in self.engs:
            self.sem[k] = es.enter_context(nc.semaphore("s_" + k))
            self.cnt[k] = 0
        self.slots = {}
        self.slot_rr = {}
        for q in ("sp", "act", "pool"):
            self.slots[q] = [[es.enter_context(nc.semaphore(f"d_{q}{i}")), 0]
                             for i in range(self.NSLOT)]
            self.slot_rr[q] = 0
        self.slots["conv"] = [[es.enter_context(nc.semaphore(f"d_conv{i}")), 0] for i in range(6)]
        self.slot_rr["conv"] = 0
        self.pending = []
        self.seen = {k: {} for k in self.engs}
        self.ninst = 0
        self.nwaits = 0

    def _wait(self, eng, tok):
        if tok is None:
            return
        sem, val, key = tok
        if eng == "pe" and key == "pe":
            return
        s = self.seen[eng]
        if s.get(key, 0) >= val:
            return
        self.engs[eng].wait_ge(sem, val)
        s[key] = val
        self.nwaits += 1

    def _deps(self, eng, reads, writes):
        for d in reads:
            self._wait(eng, d.w)
        for d in writes:
            self._wait(eng, d.w)
            for t in d.r:
                self._wait(eng, t)

    def _commit(self, tok, reads, writes):
        for d in reads:
            d.r.append(tok)
            if len(d.r) > 16:
                best = {}
                for t in d.r:
                    if t[2] not in best or best[t[2]][1] < t[1]:
                        best[t[2]] = t
                d.r = list(best.values())
        for d in writes:
            d.w = tok
            d.r = []

    dead = False

    def op(self, eng, fn, reads=(), writes=()):
        if KB.dead:
            return None
        self._deps(eng, reads, writes)
        inst = fn()
        self.cnt[eng] += 1
        inst.then_inc(self.sem[eng], 1)
        tok = (self.sem[eng], self.cnt[eng], eng)
        self._commit(tok, reads, writes)
        self.ninst += 1
        return tok

    def pump(self, n=1):
        for _ in range(n):
            if not self.pending:
                return
            fn, reads, writes = self.pending.pop(0)
            self.dma("pool", fn, reads=reads, writes=writes, grp="conv")

    def dma(self, q, fn, reads=(), writes=(), grp=None):
        if KB.dead:
            return None
        grp = grp or q
        i = self.slot_rr[grp]
        self.slot_rr[grp] = (i + 1) % len(self.slots[grp])
        slot = self.slots[grp][i]
        key = f"d_{grp}{i}"
        if slot[1] > 0:
            self._wait(q, (slot[0], slot[1], key))
        self._deps(q, reads, writes)
        inst = fn()
        slot[1] += 16
        inst.then_inc(slot[0], 16)
        tok = (slot[0], slot[1], key)
        self._commit(tok, reads, writes)
        self.ninst += 1
        return tok

    def wait_all(self, eng, deps):
        for d in deps:
            self._wait(eng, d.w)
            for t in d.r:
                self._wait(eng, t)

    def barrier(self, conv=False):
        for e in self.engs:
            self.finish(e, conv)

    def finish(self, eng="sp", conv=True):
        for k in self.engs:
            if self.cnt[k] > 0:
                self._wait(eng, (self.sem[k], self.cnt[k], k))
        for q in self.slots:
            if q == "conv" and not conv:
                continue
            for i, s in enumerate(self.slots[q]):
                if s[1] > 0:
                    self._wait(eng, (s[0], s[1], f"d_{q}{i}"))


class T:
    _ctr = [0]

    def __init__(self, kb, es, shape, dtype, name, psum=False, nd=1):
        nc = kb.nc
        T._ctr[0] += 1
        name = f"{name}_{T._ctr[0]}"
        if psum:
            self.t = es.enter_context(nc.psum_tensor(name, list(shape), dtype))
        else:
            self.t = es.enter_context(nc.sbuf_tensor(name, list(shape), dtype))
        self.ds = [Dep(f"{name}.{i}") for i in range(nd)]
        self.d = self.ds[0]
        self.shape = shape

    def __getitem__(self, k):
        return self.t[k]


class Ctx:
    pass


class StopBuild(Exception):
    pass


def ckpt(name):
    if STOP == name:
        KB.dead = True


STOP = None


def build(dbg=(), stop=None):
    global STOP
    STOP = stop
    KB.dead = False
    nc = bass.Bass("TRN2", target_bir_lowering=False)
    g = Ctx()
    g.nc = nc
    g.dbg = set(dbg)
    g.outs = {}
    g.out_d = Dep("out")

    def din(name, shape, dt=F32):
        return nc.dram_tensor(name, list(shape), dt, kind="ExternalInput")

    H = Ctx()
    g.H = H
    H.x = din("x", [NL, D])
    H.ctx = din("ctx", [NCX, D])
    H.c = din("c", [1, D])
    H.c_ctx = din("c_ctx", [D])
    H.ada_w = din("ada_w", [1, D, 6 * D])
    H.ada_b = din("ada_b", [1, 6 * D])
    H.norm_mix_w = din("norm_mix_w", [1, D])
    H.w_in = din("w_in", [1, D, 2080])
    H.gla_lr_up = din("gla_lr_up", [1, 2, 16, 256])
    H.gla_lr_bias = din("gla_lr_bias", [1, 2, 256])
    H.gla_norm_w = din("gla_norm_w", [1, 128])
    H.s5_lam_re = din("s5_lam_re", [1, 2, 32, 64])
    H.s5_lam_im = din("s5_lam_im", [1, 2, 32, 64])
    H.s5_log_dt = din("s5_log_dt", [1, 2, 32])
    H.s5_b_re = din("s5_b_re", [1, 2, 32, 64, 16])
    H.s5_b_im = din("s5_b_im", [1, 2, 32, 64, 16])
    H.s5_c_re = din("s5_c_re", [1, 2, 32, 16, 64])
    H.s5_c_im = din("s5_c_im", [1, 2, 32, 16, 64])
    H.s5_d = din("s5_d", [1, 512])
    H.glu_w = din("glu_w", [1, 512, 512])
    H.glu_b = din("glu_b", [1, 512])
    H.w_out = din("w_out", [1, D, D])
    H.norm_ffn_w = din("norm_ffn_w", [1, D])
    H.router_w = din("router_w", [1, D, NE])
    H.router_b = din("router_b", [1, NE])
    H.exp_w_gu = din("exp_w_gu", [1, NE, D, 2 * D])
    H.exp_b_gu = din("exp_b_gu", [1, NE, 2 * D])
    H.exp_w_down = din("exp_w_down", [1, NE, D, D])
    H.exp_b_down = din("exp_b_down", [1, NE, D])
    H.final_norm_w = din("final_norm_w", [D])
    H.out = nc.dram_tensor("out", [NL, D], F32, kind="ExternalOutput")

    S = Ctx()
    g.S = S

    def scr(name, shape, dt):
        if name in g.dbg:
            h = nc.dram_tensor(name, list(shape), dt, kind="ExternalOutput")
        else:
            h = nc.dram_tensor(name, list(shape), dt)
        return h, Dep(name)

    S.mod, S.mod_d = scr("mod_s", [2, 6 * D], F32)
    S.qT, S.qT_d = scr("qT_s", [256, NT], BF16)
    S.kT, S.kT_d = scr("kT_s", [256, NT], BF16)
    S.rT, S.rT_d = scr("rT_s", [512, NL], BF16)
    S.lrT, S.lrT_d = scr("lrT_s", [2, 16, NT], F32)
    S.v, S.v_d = scr("v_s", [NT, 512], BF16)
    S.uT, S.uT_d = scr("uT_s", [512, NT], BF16)
    S.u, S.u_d = scr("u_s", [NT, 512], BF16)
    S.glaT, S.glaT_d = scr("glaT_s", [512, NL], BF16)
    S.y0, S.y0_d = scr("y0_s", [32, 128, 512], F32)
    S.Ut, S.Ut_d = scr("Ut_s", [32, 128, NT // 8], BF16)
    S.x2, S.x2_d = scr("x2_s", [NL, D], F32)
    S.h2, S.h2_d = scr("h2_s", [NL, D], BF16)
    S.xg, S.xg_d = scr("xg_s", [NSEG * SEG, D], BF16)
    S.yg, S.yg_d = scr("yg_s", [NSEG * SEG, D], BF16)
    S.bguT, S.bguT_d = scr("bguT_s", [NE, 128, 16], F32)
    S.wg, S.wg_d = scr("wg_s", [NE, 128, 8 * 2 * D], BF16)
    S.wd, S.wd_d = scr("wd_s", [NE, 128, 8 * D], BF16)

    with ExitStack() as es:
        kb = KB(nc, es)
        g.kb = kb
        g.es = es
        g.ident = T(kb, es, [128, 128], F32, "ident")
        g.identb = T(kb, es, [128, 128], BF16, "identb")
        io = T(kb, es, [128, 128], F32, "iota0")
        kb.op("pool", lambda: nc.gpsimd.iota(io[:], pattern=[[1, 128]], base=0, channel_multiplier=-1,
                                              allow_small_or_imprecise_dtypes=True), writes=[io.d])
        kb.op("dve", lambda: nc.vector.tensor_single_scalar(g.ident[:], io[:], 0.0, op=ALU.is_equal),
              reads=[io.d], writes=[g.ident.d])
        kb.op("dve", lambda: nc.vector.tensor_copy(g.identb[:], g.ident[:]), reads=[g.ident.d], writes=[g.identb.d])
        g.iota = io
        g.ps = [T(kb, es, [128, 512], F32, f"ps{i}", psum=True) for i in range(6)]
        g.psb = [T(kb, es, [128, 1024], BF16, f"psb{i}", psum=True) for i in range(2)]
        g.ps_rr = 0
        R = Ctx()
        g.R = R
        R.mask = T(kb, es, [128, 32, NE], F32, "r_mask")
        R.idxf = T(kb, es, [128, 32, 4], F32, "r_idxf")
        R.gate = T(kb, es, [128, 32, 4], F32, "r_gate")
        R.slot = T(kb, es, [128, 32, 4], I32, "r_slot", nd=32)
        R.segexp = T(kb, es, [128, NSEG], I32, "r_segexp")
        R.segidx = T(kb, es, [128, NSEG], I32, "r_segidx")
        R.segrow = T(kb, es, [128, NSEG], I32, "r_segrow")

        if stop is not None and str(stop).startswith("benchf"):
            kb.op("pool", lambda: nc.gpsimd.iota(R.segidx[:], pattern=[[0, NSEG]], base=0, channel_multiplier=1),
                  writes=[R.segidx.d])
            kb.op("pool", lambda: nc.gpsimd.memset(R.segrow[:], 0), writes=[R.segrow.d])
            g.bench = stop
            phase_f(g)
            kb.barrier()
            kb.finish("sp")
            print("instructions", kb.ninst, "waits", kb.nwaits)
            return nc, g
        phase_a(g)
        kb.barrier()
        for e in range(NE):
            kb.pending.append((lambda e=e: nc.gpsimd.dma_start(
                out=S.wg[e, :, :].rearrange("p (k n) -> p k n", k=8),
                in_=H.exp_w_gu[0, e, :, :].rearrange("(k p) n -> p k n", p=128)), [], [S.wg_d]))
            kb.pending.append((lambda e=e: nc.gpsimd.dma_start(
                out=S.wd[e, :, :].rearrange("p (k n) -> p k n", k=8),
                in_=H.exp_w_down[0, e, :, :].rearrange("(k p) n -> p k n", p=128)), [], [S.wd_d]))
        kb.pump(6)
        if stop != 'a':
            phase_b(g)
            kb.barrier()
            if stop != 'b':
                if stop not in ('d_only', 'e_only'):
                    phase_c(g)
                    kb.barrier()
                if stop not in ('c', 'c0', 'c1', 'c2', 'c3'):
                    if stop != 'e_only':
                        phase_d(g)
                        kb.barrier()
                    if stop not in ('d', 'd_only') and not KB.dead:
                        phase_e(g)
                        kb.barrier()
                        if stop != 'e' and not KB.dead:
                            kb.pump(1000)
                            kb.barrier(conv=True)
                            phase_f(g)
                            kb.barrier()
                            phase_g(g)
                            kb.barrier()

        kb.finish("sp")
        print("instructions", kb.ninst, "waits", kb.nwaits)
    return nc, g


def next_ps(g):
    p = g.ps[g.ps_rr]
    g.ps_rr = (g.ps_rr + 1) % len(g.ps)
    return p


def dbg_out(g, name, src_ap, deps, shape, dt=F32, q="sp"):
    if name not in g.dbg:
        return
    nc, kb = g.nc, g.kb
    o = nc.dram_tensor("dbg_" + name, list(shape), dt, kind="ExternalOutput")
    g.outs[name] = o
    kb.dma(q, lambda: nc.sync.dma_start(out=o.ap(), in_=src_ap), reads=deps)


def phase_a(g):
    nc, kb, H, S = g.nc, g.kb, g.H, g.S
    with ExitStack() as es:
        cc = T(kb, es, [128, 8, 2], F32, "cc")
        kb.dma("sp", lambda: nc.sync.dma_start(out=cc[:, :, 0], in_=H.c[0, :].rearrange("(k p) -> p k", p=128),
                                               allow_slow_non_contiguous=True), writes=[cc.d])
        kb.dma("sp", lambda: nc.sync.dma_start(out=cc[:, :, 1], in_=H.c_ctx.ap().rearrange("(k p) -> p k", p=128),
                                               allow_slow_non_contiguous=True), writes=[cc.d])
        kb.op("act", lambda: nc.scalar.activation(cc[:], cc[:], ACT.Silu), reads=[cc.d], writes=[cc.d])
        ab = T(kb, es, [2, 6 * D], F32, "ab")
        kb.dma("sp", lambda: nc.sync.dma_start(out=ab[0:1, :], in_=H.ada_b[0:1, :]), writes=[ab.d])
        kb.dma("sp", lambda: nc.sync.dma_start(out=ab[1:2, :], in_=H.ada_b[0:1, :]), writes=[ab.d])
        modsb = T(kb, es, [2, 6 * D], F32, "modsb")
        aw = [T(kb, es, [128, 8, 512], F32, f"aw{i}") for i in range(2)]
        for j in range(12):
            a = aw[j % 2]
            kb.dma("sp" if j % 2 == 0 else "act",
                   (lambda a=a, j=j: (nc.sync if j % 2 == 0 else nc.scalar).dma_start(
                       out=a[:], in_=H.ada_w[0, :, j * 512:(j + 1) * 512].rearrange("(k p) n -> p k n", p=128))),
                   writes=[a.d])
            ps = next_ps(g)
            for k in range(8):
                kb.op("pe", lambda k=k: nc.tensor.matmul(ps[0:2, :], lhsT=cc[:, k, :], rhs=a[:, k, :],
                                                         start=(k == 0), stop=(k == 7)),
                      reads=[cc.d, a.d], writes=[ps.d])
            kb.op("dve", lambda j=j: nc.vector.tensor_tensor(modsb[:, j * 512:(j + 1) * 512], ps[0:2, :],
                                                            ab[:, j * 512:(j + 1) * 512], op=ALU.add),
                  reads=[ps.d, ab.d], writes=[modsb.d])
        kb.dma("sp", lambda: nc.sync.dma_start(out=S.mod.ap(), in_=modsb[:]), reads=[modsb.d], writes=[S.mod_d])


def load_fm_vec(g, es, name, src_ap_1d):
    nc, kb = g.nc, g.kb
    t = T(kb, es, [128, 8], F32, name)
    kb.dma("sp", lambda: nc.sync.dma_start(out=t[:], in_=src_ap_1d.rearrange("(k p) -> p k", p=128),
                                           allow_slow_non_contiguous=True), reads=[g.S.mod_d], writes=[t.d])
    return t


def s5_cols(hT, k, s0, n):
    if s0 < NCX:
        assert s0 + n <= NCX
        return hT[:, k, s0:s0 + n]
    c0 = (s0 - NCX) // 64
    ncol = n // 64
    v = hT[:, k, NCX:NT].rearrange("p (row col) -> p col row", col=64)
    return v[:, c0:c0 + ncol, :]


def phase_b(g):
    nc, kb, H, S = g.nc, g.kb, g.H, g.S
    with ExitStack() as es:
        sh1 = load_fm_vec(g, es, "sh1", S.mod[0, 0:D])
        sc1 = load_fm_vec(g, es, "sc1", S.mod[0, D:2 * D])
        csh1 = load_fm_vec(g, es, "csh1", S.mod[1, 0:D])
        csc1 = load_fm_vec(g, es, "csc1", S.mod[1, D:2 * D])
        nmw = load_fm_vec(g, es, "nmw", H.norm_mix_w[0, :])
        g1f = T(kb, es, [128, 8], F32, "g1f")
        cg1f = T(kb, es, [128, 8], F32, "cg1f")
        kb.op("dve", lambda: nc.vector.scalar_tensor_tensor(g1f[:], sc1[:], 1.0, nmw[:], op0=ALU.add, op1=ALU.mult),
              reads=[sc1.d, nmw.d], writes=[g1f.d])
        kb.op("dve", lambda: nc.vector.scalar_tensor_tensor(cg1f[:], csc1[:], 1.0, nmw[:], op0=ALU.add, op1=ALU.mult),
              reads=[csc1.d, nmw.d], writes=[cg1f.d])
        wi = T(kb, es, [128, 8, 2080], BF16, "wi")
        for (a, b) in ((0, 1040), (1040, 2080)):
            kb.dma("pool", lambda a=a, b=b: nc.gpsimd.dma_start(
                out=wi[:, :, a:b], in_=H.w_in[0, :, a:b].rearrange("(k p) n -> p k n", p=128)), writes=[wi.d])
        hT = T(kb, es, [128, 8, NT], BF16, "hT", nd=9)
        xg = [T(kb, es, [128, 4, D], F32, f"xg{i}") for i in range(2)]
        junk = T(kb, es, [128, D], BF16, "junkb")
        groups = [("ctx", 0, 2)] + [("lat", gi, 4) for gi in range(8)]
        for gi, (kind, idx, ntile) in enumerate(groups):
            kb.pump(1)
            xt = xg[gi % 2]
            ntok = ntile * 128
            if kind == "ctx":
                src = H.ctx[0:ntok, :]
                col0 = 0
                gsc, gsh = cg1f, csh1
            else:
                src = H.x[idx * 512:(idx + 1) * 512, :]
                col0 = NCX + idx * 512
                gsc, gsh = g1f, sh1
            q = "sp"
            kb.dma(q, lambda xt=xt, src=src, ntile=ntile, q=q: (nc.sync if q == "sp" else nc.scalar).dma_start(
                out=xt[:, 0:ntile, :], in_=src.rearrange("(j p) d -> p j d", p=128)), writes=[xt.d])
            ss = T(kb, es, [128, 4], F32, f"ss{gi}")
            rs = T(kb, es, [128, 4], F32, f"rs{gi}")
            for j in range(ntile):
                kb.op("act", lambda j=j: nc.scalar.activation(junk[:], xt[:, j, :], ACT.Square,
                                                              accum_out=ss[:, j:j + 1]),
                      reads=[xt.d], writes=[junk.d, ss.d])
            kb.op("dve", lambda: nc.vector.tensor_scalar(rs[:, 0:ntile], ss[:, 0:ntile], 1.0 / D, EPS,
                                                         op0=ALU.mult, op1=ALU.add), reads=[ss.d], writes=[rs.d])
            kb.op("act", lambda: nc.scalar.activation(rs[:, 0:ntile], rs[:, 0:ntile], ACT.Sqrt),
                  reads=[rs.d], writes=[rs.d])
            kb.op("dve", lambda: nc.vector.reciprocal(rs[:, 0:ntile], rs[:, 0:ntile]), reads=[rs.d], writes=[rs.d])
            for j in range(ntile):
                kb.op("dve", lambda j=j: nc.vector.tensor_scalar(xt[:, j, :], xt[:, j, :], rs[:, j:j + 1], None,
                                                                 op0=ALU.mult), reads=[xt.d, rs.d], writes=[xt.d])
            for k in range(8):
                ps = next_ps(g)
                for j in range(ntile):
                    kb.op("pe", lambda j=j, k=k: nc.tensor.transpose(ps[:, j * 128:(j + 1) * 128],
                                                                     xt[:, j, k * 128:(k + 1) * 128], g.ident[:]),
                          reads=[xt.d, g.ident.d], writes=[ps.d])
                kb.op("act", lambda k=k, ps=ps: nc.scalar.activation(
                    hT[:, k, col0:col0 + ntok], ps[:, 0:ntok], ACT.Identity,
                    bias=gsh[:, k:k + 1], scale=gsc[:, k:k + 1]),
                    reads=[ps.d, gsh.d, gsc.d], writes=[hT.ds[gi]])
        hall = hT.ds
        if "hT" in g.dbg:
            dbg_out(g, "hT", hT[:], hall, [128, 8, NT], BF16)
        stg = [T(kb, es, [128, NT], BF16, f"stg{i}") for i in range(2)]
        stgf = T(kb, es, [16, NT], F32, "stgf")
        rr = [0]

        def fm_proj(c0, m, dst_fn, t0, t1, cols_fn, f32=False):
            kb.pump(1)
            st = stgf if f32 else stg[rr[0] % 2]
            rr[0] += 0 if f32 else 1
            t = t0
            while t < t1:
                n = min(512, t1 - t)
                if t < NCX:
                    n = min(n, NCX - t)
                ps = next_ps(g)
                for k in range(8):
                    kb.op("pe", lambda k=k, t=t, n=n: nc.tensor.matmul(
                        ps[0:m, 0:n], lhsT=wi[:, k, c0:c0 + m], rhs=cols_fn(k, t, n),
                        start=(k == 0), stop=(k == 7)), reads=[wi.d] + hall, writes=[ps.d])
                eng = "act" if (t // 512) % 2 == 0 else "dve"
                if eng == "act":
                    kb.op("act", lambda t=t, n=n, ps=ps: nc.scalar.copy(st[0:m, t:t + n], ps[0:m, 0:n]),
                          reads=[ps.d], writes=[st.d])
                else:
                    kb.op("dve", lambda t=t, n=n, ps=ps: nc.vector.tensor_copy(st[0:m, t:t + n], ps[0:m, 0:n]),
                          reads=[ps.d], writes=[st.d])
                t += n
            dst, dd = dst_fn()
            kb.dma("sp", lambda: nc.sync.dma_start(out=dst, in_=st[0:m, t0:t1]), reads=[st.d], writes=[dd])

        raster = lambda k, t, n: hT[:, k, t:t + n]
        s5t = lambda k, t, n: s5_cols(hT, k, t, n)
        for mt in range(2):
            fm_proj(mt * 128, 128, lambda mt=mt: (S.qT[mt * 128:(mt + 1) * 128, :], S.qT_d), 0, NT, raster)
        for mt in range(2):
            fm_proj(256 + mt * 128, 128, lambda mt=mt: (S.kT[mt * 128:(mt + 1) * 128, :], S.kT_d), 0, NT, raster)
        for mt in range(4):
            fm_proj(1024 + mt * 128, 128, lambda mt=mt: (S.rT[mt * 128:(mt + 1) * 128, :], S.rT_d), NCX, NT, raster)
        for z in range(2):
            fm_proj(1536 + z * 16, 16, lambda z=z: (S.lrT[z, :, :], S.lrT_d), 0, NT, raster, f32=True)
        for mt in range(4):
            fm_proj(1568 + mt * 128, 128, lambda mt=mt: (S.uT[mt * 128:(mt + 1) * 128, :], S.uT_d), 0, NT, s5t)
        st4 = [T(kb, es, [128, 4, 512], BF16, f"st4_{i}") for i in range(2)]
        ngrp = 0
        for (c0, dst, dd, s5) in ((512, S.v, S.v_d, False), (1568, S.u, S.u_d, True)):
            for t0 in list(range(0, NCX, 512)) + list(range(NCX, NT, 512)):
                nt = 2 if t0 < NCX else 4
                st = st4[ngrp % 2]
                ngrp += 1
                for j in range(nt):
                    ps = next_ps(g)
                    tt = t0 + j * 128
                    if s5 and tt >= NCX:
                        for hf in range(2):
                            col = (tt - NCX) // 64 + hf
                            for k in range(8):
                                lh = hT[:, k, NCX + col:NT:64]
                                kb.op("pe", lambda k=k, lh=lh, ps=ps, hf=hf: nc.tensor.matmul(
                                    ps[hf * 64:(hf + 1) * 64, :], lhsT=lh, rhs=wi[:, k, c0:c0 + 512],
                                    start=(k == 0), stop=(k == 7)), reads=[wi.d] + hall, writes=[ps.d])
                    else:
                        for k in range(8):
                            lh = hT[:, k, tt:tt + 128]
                            kb.op("pe", lambda k=k, lh=lh, ps=ps: nc.tensor.matmul(
                                ps[:, :], lhsT=lh, rhs=wi[:, k, c0:c0 + 512], start=(k == 0), stop=(k == 7)),
                                reads=[wi.d] + hall, writes=[ps.d])
                    if j % 2 == 0:
                        kb.op("act", lambda j=j, ps=ps, st=st: nc.scalar.copy(st[:, j, :], ps[:, :]),
                              reads=[ps.d], writes=[st.d])
                    else:
                        kb.op("dve", lambda j=j, ps=ps, st=st: nc.vector.tensor_copy(st[:, j, :], ps[:, :]),
                              reads=[ps.d], writes=[st.d])
                kb.dma("sp", lambda st=st, t0=t0, nt=nt, dst=dst: nc.sync.dma_start(
                    out=dst[t0:t0 + nt * 128, :].rearrange("(j p) e -> p j e", p=128), in_=st[:, 0:nt, :]),
                    reads=[st.d], writes=[dd])


def phase_c(g):
    nc, kb, H, S = g.nc, g.kb, g.H, g.S
    NCH = NT // 64
    with ExitStack() as es:
        psb = g.psb[0]
        lup = T(kb, es, [16, 2, 256], F32, "lup")
        kb.dma("sp", lambda: nc.sync.dma_start(out=lup[:], in_=H.gla_lr_up[0].rearrange("z r f -> r z f")),
               writes=[lup.d])
        nb = T(kb, es, [128, 2, 2], F32, "nbias")
        kb.dma("sp", lambda: nc.sync.dma_start(out=nb[:], in_=H.gla_lr_bias[0].rearrange("z (t p) -> p z t", p=128),
                                               allow_slow_non_contiguous=True), writes=[nb.d])
        kb.op("dve", lambda: nc.vector.tensor_scalar(nb[:], nb[:], -1.0, None, op0=ALU.mult), reads=[nb.d], writes=[nb.d])
        nw = T(kb, es, [128, 1], F32, "gnw")
        kb.dma("sp", lambda: nc.sync.dma_start(out=nw[:], in_=H.gla_norm_w[0, :].rearrange("(p o) -> p o", o=1)),
               writes=[nw.d])
        ones = T(kb, es, [128, 128], BF16, "onesb")
        kb.op("pool", lambda: nc.gpsimd.memset(ones[:], 1.0), writes=[ones.d])
        io = g.iota
        mk = []
        for z in range(2):
            m4 = T(kb, es, [128, 4, 64], BF16, f"tri{z}")
            op = ALU.is_ge if z == 0 else ALU.is_le
            for h in range(4):
                kb.op("dve", lambda h=h, m4=m4, op=op: nc.vector.tensor_single_scalar(
                    m4[0:64, h, :], io[0:64, 0:64], 0.0, op=op), reads=[io.d], writes=[m4.d])
                kb.op("dve", lambda h=h, m4=m4, op=op: nc.vector.tensor_single_scalar(
                    m4[64:128, h, :], io[64:128, 0:64], -64.0, op=op), reads=[io.d], writes=[m4.d])
            mk.append(m4)
        qt = [T(kb, es, [128, 2, NT], BF16, f"qtl{z}") for z in range(2)]
        kt = [T(kb, es, [128, 2, NT], BF16, f"ktl{z}") for z in range(2)]
        elast = [T(kb, es, [128, 2, NCH], F32, f"elast{z}") for z in range(2)]
        if STOP == 'c0':
            return
        with ExitStack() as es2:
            mask = T(kb, es2, [128, NT + 1], BF16, "cmask")
            kb.op("pool", lambda: nc.gpsimd.memset(mask[:], 1.0), writes=[mask.d])
            kb.op("pool", lambda: nc.gpsimd.memset(mask[:, 0:NT + 1:64], 0.0), writes=[mask.d])
            lrtz = T(kb, es2, [16, NT], F32, "lrt")
            A = T(kb, es2, [128, NT], F32, "scrA")
            B = T(kb, es2, [128, NT], F32, "scrB")
            raw = [T(kb, es2, [128, NT], BF16, f"raw{i}") for i in range(2)]
            for z in range(2):
                kb.dma("sp", lambda z=z: nc.sync.dma_start(out=lrtz[:], in_=S.lrT[z, :, :]),
                       reads=[S.lrT_d], writes=[lrtz.d])
                for pt in range(2):
                    if STOP == 'c1a' and (z, pt) != (0, 0):
                        continue
                    for t0 in range(0, NT, 512):
                        n = min(512, NT - t0)
                        ps = next_ps(g)
                        kb.op("pe", lambda t0=t0, n=n, ps=ps: nc.tensor.matmul(
                            ps[:, 0:n], lhsT=lup[:, z, pt * 128:(pt + 1) * 128], rhs=lrtz[:, t0:t0 + n],
                            start=True, stop=True), reads=[lup.d, lrtz.d], writes=[ps.d])
                        kb.op("act", lambda t0=t0, n=n, ps=ps: nc.scalar.activation(
                            A[:, t0:t0 + n], ps[:, 0:n], ACT.Exp, bias=nb[:, z, pt:pt + 1], scale=-1.0),
                            reads=[ps.d, nb.d], writes=[A.d])
                    ckpt("k1")
                    kb.op("act", lambda: nc.scalar.activation(A[:], A[:], ACT.Ln, bias=1.0, scale=1.0),
                          reads=[A.d], writes=[A.d])
                    ckpt("k2")
                    if z == 0:
                        kb.op("dve", lambda: nc.vector.tensor_tensor_scan(B[:], mask[:, 0:NT], A[:], 0.0,
                                                                          ALU.mult, ALU.add),
                              reads=[mask.d, A.d], writes=[B.d])
                    else:
                        kb.op("dve", lambda: nc.vector.tensor_tensor_scan(B[:, NT - 1::-1] if False else B[:, ::-1],
                                                                          mask[:, NT:0:-1], A[:, ::-1], 0.0,
                                                                          ALU.mult, ALU.add),
                              reads=[mask.d, A.d], writes=[B.d])
                    ckpt("k3")
                    kb.op("act", lambda: nc.scalar.activation(A[:], B[:], ACT.Exp, scale=-1.0 / 16.0),
                          reads=[B.d], writes=[A.d])
                    kb.op("act", lambda: nc.scalar.activation(B[:], B[:], ACT.Exp, scale=1.0 / 16.0),
                          reads=[B.d], writes=[B.d])
                    ckpt("k4")
                    rq, rk = raw
                    kb.dma("sp", lambda: nc.sync.dma_start(out=rq[:], in_=S.qT[pt * 128:(pt + 1) * 128, :]),
                           reads=[S.qT_d], writes=[rq.d])
                    kb.dma("sp", lambda: nc.sync.dma_start(out=rk[:], in_=S.kT[pt * 128:(pt + 1) * 128, :]),
                           reads=[S.kT_d], writes=[rk.d])
                    ckpt("k5")
                    kb.op("dve", lambda: nc.vector.scalar_tensor_tensor(qt[z][:, pt, :], rq[:], 0.125, A[:],
                                                                        op0=ALU.mult, op1=ALU.mult),
                          reads=[rq.d, A.d], writes=[qt[z].d])
                    ckpt("k6")
                    kb.op("pool", lambda: nc.gpsimd.tensor_tensor(kt[z][:, pt, :], rk[:], B[:], op=ALU.mult),
                          reads=[rk.d, B.d], writes=[kt[z].d])
                    ckpt("k7")
                    e0 = 63 if z == 0 else 0
                    kb.op("dve", lambda: nc.vector.tensor_copy(elast[z][:, pt, :], A[:, e0:NT:64]),
                          reads=[A.d], writes=[elast[z].d])
        kb.barrier()
        if STOP == 'c1':
            return
        vt = T(kb, es, [128, NT // 128, 512], BF16, "vt")
        kb.dma("act", lambda: nc.scalar.dma_start(out=vt[:], in_=S.v.ap().rearrange("(n p) e -> p n e", p=128)),
               reads=[S.v_d], writes=[vt.d])
        ktm = [T(kb, es, [128, NT // 128, 256], BF16, f"ktm{z}") for z in range(2)]
        oT = T(kb, es, [128, 4, NL], BF16, "oT", nd=NL // 64)
        for z in range(2):
            for n2 in range(0, NT // 128, 2):
                for a in range(2):
                    for pt in range(2):
                        kb.op("pe", lambda a=a, pt=pt, n2=n2: nc.tensor.transpose(
                            psb[:, (a * 2 + pt) * 128:(a * 2 + pt + 1) * 128],
                            kt[z][:, pt, (n2 + a) * 128:(n2 + a + 1) * 128], g.identb[:]),
                            reads=[kt[z].d, g.identb.d], writes=[psb.d])
                kb.op("act", lambda n2=n2: nc.scalar.copy(
                    ktm[z][:, n2:n2 + 2, :], psb[:, 0:512].rearrange("p (a f) -> p a f", a=2)),
                    reads=[psb.d], writes=[ktm[z].d])
        if "qtl" in g.dbg:
            dbg_out(g, "qtl0", qt[0][:], [qt[0].d], [128, 2, NT], BF16)
            dbg_out(g, "ktl1", kt[1][:], [kt[1].d], [128, 2, NT], BF16)
            dbg_out(g, "ktm1", ktm[1][:], [ktm[1].d], [128, NT // 128, 256], BF16)
            dbg_out(g, "elast1", elast[1][:], [elast[1].d], [128, 2, NCH])
        if STOP == 'c2':
            return
        Sst = [T(kb, es, [128, 2, 128], F32, f"Sst{z}") for z in range(2)]
        SbfZ = [T(kb, es, [128, 4, 128], BF16, f"SbfZ{z}") for z in range(2)]
        tmpS = [T(kb, es, [128, 2, 128], F32, f"tmpS{z}") for z in range(2)]
        smZ = [[T(kb, es, [128, 4, 64], BF16, f"smZ{z}_{i}") for i in range(2)] for z in range(2)]
        for z in range(2):
            kb.op("pool", lambda z=z: nc.gpsimd.memset(Sst[z][:], 0.0), writes=[Sst[z].d])
            kb.op("pool", lambda z=z: nc.gpsimd.memset(SbfZ[z][:], 0.0), writes=[SbfZ[z].d])
            for i in range(2):
                kb.op("pool", lambda z=z, i=i: nc.gpsimd.memset(smZ[z][i][:], 0.0), writes=[smZ[z][i].d])
        order = [list(range(NCH)), [3, 2, 1, 0] + list(range(NCH - 1, 3, -1))]
        written = set()
        for i in range(NCH):
            if i % 4 == 0:
                kb.pump(1)
            for z in range(2):
                n = order[z][i]
                t0 = 64 * n
                nt = n // 2
                jo = (n % 2) * 64
                if n >= 4:
                    smt = smZ[z][n % 2]
                    for par in range(2):
                        ho = par * 64
                        ps_s = next_ps(g)
                        for hh in range(2):
                            h = hh * 2 + par
                            pt = hh
                            kb.op("pe", lambda h=h, pt=pt, ho=ho, ps_s=ps_s, hh=hh: nc.tensor.matmul(
                                ps_s[jo:jo + 64, hh * 64:(hh + 1) * 64], lhsT=kt[z][ho:ho + 64, pt, t0:t0 + 64],
                                rhs=qt[z][ho:ho + 64, pt, t0:t0 + 64], start=True, stop=True),
                                reads=[kt[z].d, qt[z].d], writes=[ps_s.d])
                        kb.op("dve", lambda ps_s=ps_s, smt=smt, par=par: nc.vector.tensor_tensor(
                            smt[jo:jo + 64, par:4:2, :], ps_s[jo:jo + 64, 0:128].rearrange("p (h i) -> p h i", h=2),
                            mk[z][jo:jo + 64, 0:2, :], op=ALU.mult), reads=[ps_s.d, mk[z].d], writes=[smt.d])
                    ps_o = next_ps(g)
                    for h in range(4):
                        pt = h // 2
                        kb.op("pe", lambda h=h, ps_o=ps_o, smt=smt: nc.tensor.matmul(
                            ps_o[:, h * 64:(h + 1) * 64], lhsT=vt[:, nt, h * 128:(h + 1) * 128],
                            rhs=smt[:, h, :], start=True, stop=False),
                            reads=[vt.d, smt.d], writes=[ps_o.d])
                        kb.op("pe", lambda h=h, pt=pt, ps_o=ps_o: nc.tensor.matmul(
                            ps_o[:, h * 64:(h + 1) * 64], lhsT=SbfZ[z][:, h, :],
                            rhs=qt[z][:, pt, t0:t0 + 64], start=False, stop=True),
                            reads=[SbfZ[z].d, qt[z].d], writes=[ps_o.d])
                    tl = t0 - NCX
                    od = oT.ds[tl // 64]
                    osl = oT[:, :, tl:tl + 64]
                    pv = ps_o[:, 0:256].rearrange("p (h i) -> p h i", h=4)
                    if n not in written:
                        written.add(n)
                        kb.op("act", lambda osl=osl, pv=pv: nc.scalar.copy(osl, pv), reads=[ps_o.d], writes=[od])
                    else:
                        kb.op("dve", lambda osl=osl, pv=pv: nc.vector.tensor_tensor(osl, pv, osl, op=ALU.add),
                              reads=[ps_o.d, od], writes=[od])
                ps_kv = next_ps(g)
                for h in range(4):
                    pt, ho = h // 2, (h % 2) * 64
                    kb.op("pe", lambda h=h, pt=pt, ho=ho, ps_kv=ps_kv: nc.tensor.matmul(
                        ps_kv[ho:ho + 64, pt * 128:(pt + 1) * 128], lhsT=ktm[z][jo:jo + 64, nt, h * 64:(h + 1) * 64],
                        rhs=vt[jo:jo + 64, nt, h * 128:(h + 1) * 128], start=True, stop=True),
                        reads=[ktm[z].d, vt.d], writes=[ps_kv.d])
                kb.op("dve", lambda ps_kv=ps_kv: nc.vector.tensor_tensor(
                    tmpS[z][:], ps_kv[:, 0:256].rearrange("p (t e) -> p t e", t=2), Sst[z][:], op=ALU.add),
                    reads=[ps_kv.d, Sst[z].d], writes=[tmpS[z].d])
                kb.op("dve", lambda n=n: nc.vector.tensor_tensor(
                    Sst[z][:], tmpS[z][:], elast[z][:, :, n:n + 1].to_broadcast([128, 2, 128]), op=ALU.mult),
                    reads=[tmpS[z].d, elast[z].d], writes=[Sst[z].d])
                for par in range(2):
                    ho = par * 64
                    kb.op("act", lambda par=par, ho=ho: nc.scalar.copy(SbfZ[z][ho:ho + 64, par:4:2, :],
                                                                       Sst[z][ho:ho + 64, :, :]),
                          reads=[Sst[z].d], writes=[SbfZ[z].d])
        if "oT" in g.dbg:
            dbg_out(g, "oT", oT[:], oT.ds, [128, 4, NL], BF16)
        if STOP == 'c3':
            return
        sq = T(kb, es, [128, 512], BF16, "gsq")
        rstd = T(kb, es, [128, 512], F32, "grstd")
        rt = [T(kb, es, [128, 4, 512], BF16, f"grt{i}") for i in range(2)]
        gl = [T(kb, es, [128, 4, 512], BF16, f"ggl{i}") for i in range(1)]
        tmpb = T(kb, es, [128, 512], BF16, "gtmp")
        for sp in range(NL // 512):
            c0 = sp * 512
            r_t, g_t = rt[sp % 2], gl[0]
            kb.dma("sp", lambda r_t=r_t, c0=c0: nc.sync.dma_start(
                out=r_t[:], in_=S.rT[:, c0:c0 + 512].rearrange("(m p) t -> p m t", p=128)),
                reads=[S.rT_d], writes=[r_t.d])
            kb.op("act", lambda r_t=r_t: nc.scalar.activation(r_t[:], r_t[:], ACT.Silu), reads=[r_t.d], writes=[r_t.d])
            ods = oT.ds[c0 // 64:(c0 + 512) // 64]
            for h in range(4):
                kb.op("dve", lambda h=h: nc.vector.tensor_tensor(sq[:], oT[:, h, c0:c0 + 512], oT[:, h, c0:c0 + 512],
                                                                 op=ALU.mult), reads=ods, writes=[sq.d])
                ps = next_ps(g)
                kb.op("pe", lambda ps=ps: nc.tensor.matmul(ps[:, :], lhsT=ones[:], rhs=sq[:], start=True, stop=True),
                      reads=[ones.d, sq.d], writes=[ps.d])
                kb.op("dve", lambda ps=ps: nc.vector.tensor_scalar(rstd[:], ps[:, :], 1.0 / 128.0, EPS,
                                                                   op0=ALU.mult, op1=ALU.add),
                      reads=[ps.d], writes=[rstd.d])
                kb.op("act", lambda: nc.scalar.activation(rstd[:], rstd[:], ACT.Sqrt), reads=[rstd.d], writes=[rstd.d])
                kb.op("dve", lambda: nc.vector.reciprocal(rstd[:], rstd[:]), reads=[rstd.d], writes=[rstd.d])
                kb.op("dve", lambda h=h: nc.vector.scalar_tensor_tensor(
                    tmpb[:], oT[:, h, c0:c0 + 512], nw[:, 0:1], rstd[:], op0=ALU.mult, op1=ALU.mult),
                    reads=ods + [nw.d, rstd.d], writes=[tmpb.d])
                kb.op("pool", lambda h=h, g_t=g_t, r_t=r_t: nc.gpsimd.tensor_tensor(
                    g_t[:, h, :], tmpb[:], r_t[:, h, :], op=ALU.mult), reads=[tmpb.d, r_t.d], writes=[g_t.d])
            kb.dma("act", lambda g_t=g_t, c0=c0: nc.scalar.dma_start(
                out=S.glaT[:, c0:c0 + 512].rearrange("(m p) t -> p m t", p=128), in_=g_t[:]),
                reads=[g_t.d], writes=[S.glaT_d])


def cmul(g, out_r, out_i, ar, ai, br, bi, tmp, deps_in, dep_out, sl=None):
    nc, kb = g.nc, g.kb
    kb.op("dve", lambda: nc.vector.tensor_tensor(tmp, ai, bi, op=ALU.mult), reads=deps_in, writes=[dep_out])
    kb.op("dve", lambda: nc.vector.tensor_tensor(out_r, ar, br, op=ALU.mult), reads=deps_in, writes=[dep_out])
    kb.op("dve", lambda: nc.vector.tensor_tensor(out_r, out_r, tmp, op=ALU.subtract), reads=[dep_out], writes=[dep_out])
    kb.op("dve", lambda: nc.vector.tensor_tensor(tmp, ai, br, op=ALU.mult), reads=deps_in, writes=[dep_out])
    kb.op("dve", lambda: nc.vector.tensor_tensor(out_i, ar, bi, op=ALU.mult), reads=deps_in, writes=[dep_out])
    kb.op("dve", lambda: nc.vector.tensor_tensor(out_i, out_i, tmp, op=ALU.add), reads=[dep_out], writes=[dep_out])


def phase_d(g):
    nc, kb, H, S = g.nc, g.kb, g.H, g.S
    NCK = NT // 8
    NMAC = NCK // 16
    io = g.iota
    with ExitStack() as es:
        psb = g.psb[0]
        pd = Dep("s5par")
        P0 = T(kb, es, [128, 24, 64], F32, "s5p0")
        pl = lambda i: P0[:, i, :]
        LRE, LIM, DT, MAG, CS, SN, T1, T2, T3, LBR, LBI, CR, CI, IR, II = range(15)
        for half in range(2):
            rows = slice(half * 64, half * 64 + 64)
            kb.dma("sp", lambda rows=rows: nc.sync.dma_start(
                out=P0[rows, LRE, :], in_=H.s5_lam_re[0].rearrange("z g p -> p (z g)"),
                allow_slow_non_contiguous=True), writes=[pd])
            kb.dma("act", lambda rows=rows: nc.scalar.dma_start(
                out=P0[rows, LIM, :], in_=H.s5_lam_im[0].rearrange("z g p -> p (z g)"),
                allow_slow_non_contiguous=True), writes=[pd])
        kb.dma("sp", lambda: nc.sync.dma_start(
            out=pl(DT), in_=H.s5_log_dt[0:1, :, :].rearrange("o z g -> o (z g)").partition_broadcast(128)),
            writes=[pd])
        D1 = [pd]

        def v(fn):
            kb.op("dve", fn, reads=D1, writes=D1)

        def a(fn):
            kb.op("act", fn, reads=D1, writes=D1)

        a(lambda: nc.scalar.activation(pl(DT), pl(DT), ACT.Exp))
        v(lambda: nc.vector.tensor_tensor(pl(MAG), pl(LRE), pl(DT), op=ALU.mult))
        a(lambda: nc.scalar.activation(pl(MAG), pl(MAG), ACT.Exp))
        v(lambda: nc.vector.tensor_tensor(pl(T1), pl(LIM), pl(DT), op=ALU.mult))
        a(lambda: nc.scalar.activation(pl(SN), pl(T1), ACT.Sin, scale=1.0 / 16.0))
        a(lambda: nc.scalar.activation(pl(CS), pl(T1), ACT.Sin, bias=float(np.pi / 2), scale=1.0 / 16.0))
        for _ in range(4):
            v(lambda: nc.vector.tensor_tensor(pl(T2), pl(CS), pl(CS), op=ALU.mult))
            v(lambda: nc.vector.tensor_tensor(pl(T3), pl(SN), pl(SN), op=ALU.mult))
            v(lambda: nc.vector.scalar_tensor_tensor(pl(SN), pl(CS), 2.0, pl(SN), op0=ALU.mult, op1=ALU.mult))
            v(lambda: nc.vector.tensor_tensor(pl(CS), pl(T2), pl(T3), op=ALU.subtract))
        v(lambda: nc.vector.tensor_tensor(pl(LBR), pl(MAG), pl(CS), op=ALU.mult))
        v(lambda: nc.vector.tensor_tensor(pl(LBI), pl(MAG), pl(SN), op=ALU.mult))
        v(lambda: nc.vector.tensor_tensor(pl(T1), pl(LRE), pl(LRE), op=ALU.mult))
        v(lambda: nc.vector.tensor_tensor(pl(T2), pl(LIM), pl(LIM), op=ALU.mult))
        v(lambda: nc.vector.tensor_tensor(pl(T1), pl(T1), pl(T2), op=ALU.add))
        v(lambda: nc.vector.reciprocal(pl(T1), pl(T1)))
        v(lambda: nc.vector.tensor_scalar(pl(T2), pl(LBR), -1.0, None, op0=ALU.add))
        v(lambda: nc.vector.tensor_tensor(pl(CR), pl(T2), pl(LRE), op=ALU.mult))
        v(lambda: nc.vector.tensor_tensor(pl(T3), pl(LBI), pl(LIM), op=ALU.mult))
        v(lambda: nc.vector.tensor_tensor(pl(CR), pl(CR), pl(T3), op=ALU.add))
        v(lambda: nc.vector.tensor_tensor(pl(CR), pl(CR), pl(T1), op=ALU.mult))
        v(lambda: nc.vector.tensor_tensor(pl(CI), pl(LBI), pl(LRE), op=ALU.mult))
        v(lambda: nc.vector.tensor_tensor(pl(T3), pl(T2), pl(LIM), op=ALU.mult))
        v(lambda: nc.vector.tensor_tensor(pl(CI), pl(CI), pl(T3), op=ALU.subtract))
        v(lambda: nc.vector.tensor_tensor(pl(CI), pl(CI), pl(T1), op=ALU.mult))
        v(lambda: nc.vector.tensor_tensor(pl(T1), pl(LBR), pl(LBR), op=ALU.mult))
        v(lambda: nc.vector.tensor_tensor(pl(T2), pl(LBI), pl(LBI), op=ALU.mult))
        v(lambda: nc.vector.tensor_tensor(pl(T1), pl(T1), pl(T2), op=ALU.add))
        v(lambda: nc.vector.reciprocal(pl(T1), pl(T1)))
        v(lambda: nc.vector.tensor_tensor(pl(IR), pl(LBR), pl(T1), op=ALU.mult))
        v(lambda: nc.vector.scalar_tensor_tensor(pl(II), pl(LBI), -1.0, pl(T1), op0=ALU.mult, op1=ALU.mult))
        PW = T(kb, es, [128, 9, 2, 64], F32, "s5pw")
        NW = T(kb, es, [128, 8, 2, 64], F32, "s5nw")
        for W, br_, bi_, n in ((PW, LBR, LBI, 9), (NW, IR, II, 8)):
            v(lambda W=W: nc.vector.memset(W[:, 0, 0, :], 1.0))
            v(lambda W=W: nc.vector.memset(W[:, 0, 1, :], 0.0))
            for k in range(1, n):
                cmul(g, W[:, k, 0, :], W[:, k, 1, :], W[:, k - 1, 0, :], W[:, k - 1, 1, :], pl(br_), pl(bi_),
                     pl(T3), D1, pd)
        P128 = T(kb, es, [128, 2, 64], F32, "s5p128")
        v(lambda: nc.vector.tensor_copy(P128[:], PW[:, 8, :, :]))
        for _ in range(4):
            cmul(g, pl(T1), pl(T2), P128[:, 0, :], P128[:, 1, :], P128[:, 0, :], P128[:, 1, :], pl(T3), D1, pd)
            v(lambda: nc.vector.tensor_copy(P128[:, 0, :], pl(T1)))
            v(lambda: nc.vector.tensor_copy(P128[:, 1, :], pl(T2)))
        WN = T(kb, es, [128, 8, 2, 64], F32, "s5wn")
        WP = T(kb, es, [128, 8, 2, 64], F32, "s5wp")
        for k in range(8):
            cmul(g, WN[:, k, 0, :], WN[:, k, 1, :], NW[:, k, 0, :], NW[:, k, 1, :], pl(CR), pl(CI), pl(T3), D1, pd)
            cmul(g, WP[:, k, 0, :], WP[:, k, 1, :], PW[:, k, 0, :], PW[:, k, 1, :], pl(CR), pl(CI), pl(T3), D1, pd)
        v(lambda: nc.vector.tensor_scalar(PW[64:128, :, 0, :], PW[64:128, :, 0, :], -1.0, None, op0=ALU.mult))
        v(lambda: nc.vector.tensor_scalar(WN[0:64, :, 1, :], WN[0:64, :, 1, :], -1.0, None, op0=ALU.mult))
        v(lambda: nc.vector.tensor_scalar(WP[0:64, :, 1, :], WP[0:64, :, 1, :], -1.0, None, op0=ALU.mult))
        A8s = T(kb, es, [128, 2, 64], F32, "s5a8")
        v(lambda: nc.vector.tensor_copy(A8s[:, 1, :], PW[:, 8, 1, :]))
        v(lambda: nc.vector.tensor_copy(A8s[0:64, 0, :], PW[0:64, 8, 0, :]))
        v(lambda: nc.vector.tensor_scalar(A8s[64:128, 0, :], PW[64:128, 8, 0, :], -1.0, None, op0=ALU.mult))
        Jt = T(kb, es, [128, 128], F32, "s5jt")
        v(lambda: nc.vector.tensor_single_scalar(Jt[:], io[:], 64.0, op=ALU.is_equal))
        v(lambda: nc.vector.tensor_single_scalar(pl(T1)[:, 0:64], io[:, 0:64], -64.0, op=ALU.is_equal))
        v(lambda: nc.vector.tensor_tensor(Jt[:, 0:64], Jt[:, 0:64], pl(T1)[:, 0:64], op=ALU.subtract))
        bm = []
        for z in range(2):
            m = T(kb, es, [128, 128], F32, f"s5bm{z}")
            v(lambda m=m: nc.vector.memset(m[:], 0.0))
            for j in range(0, 8, 2):
                for jj in range(2):
                    pass
            bm.append(m)
        rowb = T(kb, es, [128, 1], F32, "s5rowb")
        pcol = T(kb, es, [128, 1], F32, "s5pcol")
        kb.op("pool", lambda: nc.gpsimd.iota(pcol[:], pattern=[[0, 1]], base=0, channel_multiplier=1,
                                              allow_small_or_imprecise_dtypes=True), writes=[pd])
        v(lambda: nc.vector.memset(rowb[:], 0.0))
        for t in range(1, 8):
            v(lambda t=t: nc.vector.tensor_scalar(pl(T1)[:, 0:1], pcol[:], float(16 * t), 16.0, op0=ALU.is_ge, op1=ALU.mult))
            v(lambda: nc.vector.tensor_tensor(rowb[:], rowb[:], pl(T1)[:, 0:1], op=ALU.add))
        colf = T(kb, es, [128, 128], F32, "s5colf")
        kb.op("pool", lambda: nc.gpsimd.iota(colf[:], pattern=[[1, 128]], base=0, channel_multiplier=0,
                                              allow_small_or_imprecise_dtypes=True), writes=[pd])
        v(lambda: nc.vector.tensor_scalar(bm[0][:], colf[:], rowb[:, 0:1], 0.0, op0=ALU.subtract, op1=ALU.is_ge))
        v(lambda: nc.vector.tensor_scalar(bm[1][:], colf[:], rowb[:, 0:1], 15.0, op0=ALU.subtract, op1=ALU.is_le))
        CC = T(kb, es, [128, 64, 16], F32, "s5cc")
        CCs = T(kb, es, [128, 64, 16], F32, "s5ccs")
        BB = T(kb, es, [128, 64, 16], F32, "s5bb")
        BBs = T(kb, es, [128, 64, 16], F32, "s5bbs")
        kb.dma("sp", lambda: nc.sync.dma_start(out=BB[0:64, :, :], in_=H.s5_b_re[0].rearrange("z g p h -> p (z g) h")),
               writes=[pd])
        kb.dma("act", lambda: nc.scalar.dma_start(out=BB[64:128, :, :], in_=H.s5_b_im[0].rearrange("z g p h -> p (z g) h")),
               writes=[pd])
        kb.dma("sp", lambda: nc.sync.dma_start(out=BBs[0:64, :, :], in_=H.s5_b_im[0].rearrange("z g p h -> p (z g) h")),
               writes=[pd])
        kb.dma("act", lambda: nc.scalar.dma_start(out=BBs[64:128, :, :], in_=H.s5_b_re[0].rearrange("z g p h -> p (z g) h")),
               writes=[pd])
        with ExitStack() as esx:
            xc = [T(kb, esx, [128, 8, 128], F32, f"s5xc{i}") for i in range(2)]
            for i, (aa, bb) in enumerate(((H.s5_c_re, H.s5_c_im), (H.s5_c_im, H.s5_c_re))):
                kb.dma("sp", lambda aa=aa, i=i: nc.sync.dma_start(
                    out=xc[i][:, :, 0:64], in_=aa[0].rearrange("z g h p -> (z g h) p").rearrange("(t r) p -> r t p", r=128)),
                    writes=[pd])
                kb.dma("act", lambda bb=bb, i=i: nc.scalar.dma_start(
                    out=xc[i][:, :, 64:128], in_=bb[0].rearrange("z g h p -> (z g h) p").rearrange("(t r) p -> r t p", r=128)),
                    writes=[pd])
            for i, dst in enumerate((CC, CCs)):
                for t in range(8):
                    ps = next_ps(g)
                    kb.op("pe", lambda t=t, i=i, ps=ps: nc.tensor.transpose(ps[:, 0:128], xc[i][:, t, :], g.ident[:]),
                          reads=[pd, g.ident.d], writes=[ps.d])
                    kb.op("act", lambda t=t, dst=dst, ps=ps: nc.scalar.copy(
                        dst[:, t * 8:(t + 1) * 8, :], ps[:, 0:128].rearrange("p (a h) -> p a h", a=8)),
                        reads=[ps.d], writes=[pd])
            kb.barrier()
        ckpt("d0")
        with ExitStack() as esu:
            U8 = [T(kb, esu, [128, 8, 512], BF16, f"s5u8_{i}") for i in range(2)]
            U8g = [T(kb, esu, [128, 32, 128], BF16, f"s5u8g_{i}") for i in range(2)]
            utst = [T(kb, esu, [128, 4, 128], BF16, f"s5utst_{i}") for i in range(2)]
            blocks = [(0, 32)] + [(32 + 128 * b, 128) for b in range(4)]
            for bi_, (c0, ncb) in enumerate(blocks):
                kb.pump(1)
                u8, u8g = U8[bi_ % 2], U8g[bi_ % 2]
                kb.dma("sp", lambda u8=u8, c0=c0, ncb=ncb: nc.sync.dma_start(
                    out=u8[0:ncb, :, :], in_=S.u[c0 * 8:(c0 + ncb) * 8, :].rearrange("(c j) f -> c j f", j=8)),
                    reads=[S.u_d], writes=[u8.d])
                kb.op("pool", lambda u8=u8, u8g=u8g, ncb=ncb: nc.gpsimd.tensor_copy(
                    u8g[0:ncb, :, :].rearrange("c g (j h) -> c g j h", j=8),
                    u8[0:ncb, :, :].rearrange("c j (g h) -> c g j h", g=32)), reads=[u8.d], writes=[u8g.d])
                for g4 in range(0, 32, 4):
                    for gg in range(4):
                        kb.op("pe", lambda gg=gg, g4=g4, u8g=u8g, ncb=ncb: nc.tensor.transpose(
                            psb[:, gg * 128:gg * 128 + ncb], u8g[0:ncb, g4 + gg, :], g.identb[0:ncb, 0:ncb]),
                            reads=[u8g.d, g.identb.d], writes=[psb.d])
                    ust = utst[(g4 // 4) % 2]
                    kb.op("act", lambda g4=g4, c0=c0, ncb=ncb, ust=ust: nc.scalar.copy(
                        ust[:, :, 0:ncb], psb[:, 0:512].rearrange("p (a c) -> p a c", a=4)[:, :, 0:ncb]),
                        reads=[psb.d], writes=[ust.d])
                    kb.dma("act", lambda g4=g4, c0=c0, ncb=ncb, ust=ust: nc.scalar.dma_start(
                        out=S.Ut[g4:g4 + 4, :, c0:c0 + ncb].rearrange("a p c -> p a c"), in_=ust[:, :, 0:ncb]),
                        reads=[ust.d], writes=[S.Ut_d])
            kb.barrier()
        ckpt("d1")
        for z in range(2):
            gs = slice(z * 32, z * 32 + 32)
            if z == 1:
                ckpt("dz0")
            with ExitStack() as ez:
                MT = T(kb, ez, [128, 32, 128], BF16, "s5mt")
                RT = T(kb, ez, [128, 32, 128], BF16, "s5rt")
                OTb = T(kb, ez, [128, 32, 128], BF16, "s5otb")
                with ExitStack() as ep:
                    Gall = T(kb, ep, [128, 32, 9, 16], F32, "s5gall")
                    Kn = T(kb, ep, [128, 32, 8, 16], F32, "s5kn")
                    Kp = T(kb, ep, [128, 32, 8, 16], F32, "s5kp")
                    tmpk = T(kb, ep, [128, 32, 16], F32, "s5tmpk")
                    bc = lambda ap2: ap2.unsqueeze(2).to_broadcast([128, 32, 16])
                    for k in range(9):
                        i = k if z == 0 else 8 - k
                        v(lambda k=k, i=i: nc.vector.tensor_tensor(Gall[:, :, i, :], CC[:, gs, :], bc(PW[:, k, 0, gs]),
                                                                  op=ALU.mult))
                        v(lambda k=k: nc.vector.tensor_tensor(tmpk[:], CCs[:, gs, :], bc(PW[:, k, 1, gs]), op=ALU.mult))
                        v(lambda i=i: nc.vector.tensor_tensor(Gall[:, :, i, :], Gall[:, :, i, :], tmpk[:], op=ALU.subtract))
                    for k in range(8):
                        j = k if z == 0 else 7 - k
                        v(lambda k=k, j=j: nc.vector.tensor_tensor(Kn[:, :, j, :], BB[:, gs, :], bc(WN[:, k, 0, gs]),
                                                                  op=ALU.mult))
                        v(lambda k=k: nc.vector.tensor_tensor(tmpk[:], BBs[:, gs, :], bc(WN[:, k, 1, gs]), op=ALU.mult))
                        v(lambda j=j: nc.vector.tensor_tensor(Kn[:, :, j, :], Kn[:, :, j, :], tmpk[:], op=ALU.add))
                        j2 = 7 - k if z == 0 else k
                        v(lambda k=k, j2=j2: nc.vector.tensor_tensor(Kp[:, :, j2, :], BB[:, gs, :], bc(WP[:, k, 0, gs]),
                                                                    op=ALU.mult))
                        v(lambda k=k: nc.vector.tensor_tensor(tmpk[:], BBs[:, gs, :], bc(WP[:, k, 1, gs]), op=ALU.mult))
                        v(lambda j2=j2: nc.vector.tensor_tensor(Kp[:, :, j2, :], Kp[:, :, j2, :], tmpk[:], op=ALU.add))
                    q0 = 0 if z == 0 else 1
                    o0 = 1 if z == 0 else 0
                    for gi in range(32):
                        ps = next_ps(g)
                        kb.op("pe", lambda gi=gi, ps=ps: nc.tensor.matmul(
                            ps[:, 0:128], lhsT=Kn[:, gi, :, :].rearrange("p j h -> p (j h)"),
                            rhs=Gall[:, gi, q0:q0 + 8, :].rearrange("p s h -> p (s h)"), start=True, stop=True),
                            reads=D1, writes=[ps.d])
                        kb.op("dve", lambda gi=gi, ps=ps: nc.vector.tensor_tensor(MT[:, gi, :], ps[:, 0:128], bm[z][:],
                                                                                 op=ALU.mult),
                              reads=[ps.d] + D1, writes=[MT.d])
                        ps2 = next_ps(g)
                        kb.op("pe", lambda gi=gi, ps2=ps2: nc.tensor.transpose(
                            ps2[:, 0:128], Kp[:, gi, :, :].rearrange("p j h -> p (j h)"), g.ident[:]),
                            reads=D1 + [g.ident.d], writes=[ps2.d])
                        kb.op("act", lambda gi=gi, ps2=ps2: nc.scalar.copy(RT[:, gi, :], ps2[:, 0:128]),
                              reads=[ps2.d], writes=[RT.d])
                        kb.op("act", lambda gi=gi: nc.scalar.copy(
                            OTb[:, gi, :], Gall[:, gi, o0:o0 + 8, :].rearrange("p s h -> p (s h)")),
                            reads=D1, writes=[OTb.d])
                    kb.barrier()
                if f"s5mat{z}" in g.dbg:
                    dbg_out(g, f"MT{z}", MT[:], [MT.d], [128, 32, 128], BF16)
                    dbg_out(g, f"RT{z}", RT[:], [RT.d], [128, 32, 128], BF16)
                    dbg_out(g, f"OTb{z}", OTb[:], [OTb.d], [128, 32, 128], BF16)
                if z == 0:
                    ckpt("dm0")
                X = T(kb, ez, [128, 32, NCK], F32, "s5x", nd=32)
                utg = [T(kb, ez, [128, NCK], BF16, f"s5utg{i}") for i in range(3)]
                for gi in range(32):
                    ug = utg[gi % 3]
                    kb.dma("sp", lambda gi=gi, ug=ug: nc.sync.dma_start(out=ug[:], in_=S.Ut[gi, :, :]),
                           reads=[S.Ut_d], writes=[ug.d])
                    for (c0, n) in ((0, 512), (512, NCK - 512)):
                        ps = next_ps(g)
                        kb.op("pe", lambda gi=gi, c0=c0, n=n, ps=ps: nc.tensor.matmul(
                            ps[:, 0:n], lhsT=RT[:, gi, :], rhs=ug[:, c0:c0 + n], start=True, stop=True),
                            reads=[RT.d, ug.d], writes=[ps.d])
                        eng = "act" if gi % 2 == 0 else "dve"
                        if eng == "act":
                            kb.op("act", lambda gi=gi, c0=c0, n=n, ps=ps: nc.scalar.copy(X[:, gi, c0:c0 + n], ps[:, 0:n]),
                                  reads=[ps.d], writes=[X.ds[gi]])
                        else:
                            kb.op("dve", lambda gi=gi, c0=c0, n=n, ps=ps: nc.vector.tensor_copy(X[:, gi, c0:c0 + n], ps[:, 0:n]),
                                  reads=[ps.d], writes=[X.ds[gi]])
                if z == 0:
                    ckpt("d20")
                cur = [T(kb, ez, [128, 32, NMAC], F32, f"s5cur{i}") for i in range(2)]
                Gm = T(kb, ez, [128, 32, NMAC], F32, "s5gm")
                uu = T(kb, ez, [128, 32, NMAC], F32, "s5uu")
                hs = [T(kb, ez, [128, 32, NMAC], F32, f"s5hs{i}") for i in range(2)]
                pw = T(kb, ez, [128, 2, 2, 32], F32, "s5pwk")
                ptmp = T(kb, ez, [128, 32], F32, "s5ptmp")
                xall = X.ds
                colsel = (lambda i: i) if z == 0 else (lambda i: 15 - i)
                gbanks = ((0, 15), (15, 30), (30, 32))

                def cplx_apply(src_ap_fn, width, ar_ap, ai_ap, dst, dst_lo, u_t, rd):
                    kb.op("pool", lambda: nc.gpsimd.tensor_tensor(
                        u_t[:, :, 0:width], src_ap_fn(0, 32), ar_ap.unsqueeze(2).to_broadcast([128, 32, width]),
                        op=ALU.mult), reads=rd + D1, writes=[u_t.d])
                    pss = []
                    for (g0, g1) in gbanks:
                        ps = next_ps(g)
                        pss.append(ps)
                        kb.op("pe", lambda ps=ps, g0=g0, g1=g1: nc.tensor.matmul(
                            ps[:, 0:(g1 - g0) * width], lhsT=Jt[:], rhs=src_ap_fn(g0, g1), start=True, stop=True),
                            reads=rd + D1, writes=[ps.d])
                    for (g0, g1), ps in zip(gbanks, pss):
                        kb.op("dve", lambda g0=g0, g1=g1, ps=ps: nc.vector.tensor_tensor(
                            dst[:, g0:g1, dst_lo:dst_lo + width],
                            ps[:, 0:(g1 - g0) * width].rearrange("p (a m) -> p a m", m=width),
                            ai_ap[:, g0:g1].unsqueeze(2).to_broadcast([128, g1 - g0, width]), op=ALU.mult),
                            reads=[ps.d] + D1, writes=[dst.d])
                    kb.op("dve", lambda: nc.vector.tensor_tensor(
                        dst[:, :, dst_lo:dst_lo + width], dst[:, :, dst_lo:dst_lo + width], u_t[:, :, 0:width], op=ALU.add),
                        reads=[dst.d, u_t.d], writes=[dst.d])

                a8r, a8i = A8s[:, 0, gs], A8s[:, 1, gs]

                def step(src, dst, i, store):
                    col = colsel(i)
                    cplx_apply(lambda g0, g1: src[:, g0:g1, :], NMAC, a8r, a8i, dst, 0, uu, [src.d])
                    kb.op("dve", lambda: nc.vector.tensor_tensor(dst[:], dst[:], X[:, :, col:NCK:16], op=ALU.add),
                          reads=[dst.d] + xall, writes=[dst.d])
                    if store:
                        kb.op("act", lambda: nc.scalar.copy(X[:, :, col:NCK:16], src[:]),
                              reads=[src.d] + xall, writes=xall)

                kb.op("pool", lambda: nc.gpsimd.memset(cur[0][:], 0.0), writes=[cur[0].d])
                for i in range(16):
                    if i % 4 == 0:
                        kb.pump(1)
                    step(cur[i % 2], cur[(i + 1) % 2], i, False)
                Em = cur[0]
                h0 = hs[0]
                if z == 0:
                    kb.op("act", lambda: nc.scalar.copy(h0[:], Em[:]), reads=[Em.d], writes=[h0.d])
                else:
                    kb.op("act", lambda: nc.scalar.copy(h0[:, :, 0:2], Em[:, :, 1::-1]), reads=[Em.d], writes=[h0.d])
                    kb.op("act", lambda: nc.scalar.copy(h0[:, :, 2:NMAC], Em[:, :, NMAC - 1:1:-1]), reads=[Em.d], writes=[h0.d])
                v(lambda: nc.vector.tensor_copy(pw[:, 0, 0, :], P128[:, 0, gs]))
                v(lambda: nc.vector.tensor_copy(pw[:, 0, 1, :], P128[:, 1, gs]))
                d_ = 1
                k_ = 0
                while d_ < NMAC:
                    src, dst = hs[k_ % 2], hs[(k_ + 1) % 2]
                    pr, pi_ = pw[:, k_ % 2, 0, :], pw[:, k_ % 2, 1, :]
                    w = NMAC - d_
                    cplx_apply(lambda g0, g1, src=src, w=w: src[:, g0:g1, 0:w], w, pr, pi_, dst, d_, uu, [src.d])
                    kb.op("dve", lambda src=src, dst=dst, d_=d_: nc.vector.tensor_tensor(
                        dst[:, :, d_:NMAC], dst[:, :, d_:NMAC], src[:, :, d_:NMAC], op=ALU.add),
                        reads=[dst.d, src.d], writes=[dst.d])
                    kb.op("act", lambda src=src, dst=dst, d_=d_: nc.scalar.copy(dst[:, :, 0:d_], src[:, :, 0:d_]),
                          reads=[src.d], writes=[dst.d])
                    nr, ni = pw[:, (k_ + 1) % 2, 0, :], pw[:, (k_ + 1) % 2, 1, :]
                    cmul(g, nr, ni, pr, pi_, pr, pi_, ptmp[:], D1, pd)
                    d_ *= 2
                    k_ += 1
                Iinc = hs[k_ % 2]
                kb.op("pool", lambda: nc.gpsimd.memset(Gm[:], 0.0), writes=[Gm.d])
                if z == 0:
                    kb.op("dve", lambda: nc.vector.tensor_copy(Gm[:, :, 1:NMAC], Iinc[:, :, 0:NMAC - 1]),
                          reads=[Iinc.d], writes=[Gm.d])
                else:
                    kb.op("dve", lambda: nc.vector.tensor_copy(Gm[:, :, 0:1], Iinc[:, :, 0:1]), reads=[Iinc.d], writes=[Gm.d])
                    kb.op("dve", lambda: nc.vector.tensor_copy(Gm[:, :, NMAC - 1:1:-1], Iinc[:, :, 1:NMAC - 1]),
                          reads=[Iinc.d], writes=[Gm.d])
                kb.op("dve", lambda: nc.vector.tensor_copy(cur[0][:], Gm[:]), reads=[Gm.d], writes=[cur[0].d])
                for i in range(16):
                    step(cur[i % 2], cur[(i + 1) % 2], i, True)
                if f"s5x{z}" in g.dbg:
                    dbg_out(g, f"X{z}", X[:], xall, [128, 32, NCK], F32)
                if z == 0:
                    ckpt("d30")
                xb = [T(kb, ez, [128, 512], BF16, f"s5xb{i}") for i in range(2)]
                yo = [T(kb, ez, [128, 512], F32, f"s5yo{i}") for i in range(2)]
                yfl = [T(kb, ez, [128, 512], F32, f"s5yfl{i}") for i in range(2)]
                for gi in range(32):
                    xbt = xb[gi % 2]
                    ug = utg[gi % 3]
                    kb.dma("sp", lambda gi=gi, ug=ug: nc.sync.dma_start(out=ug[:], in_=S.Ut[gi, :, :]),
                           reads=[S.Ut_d], writes=[ug.d])
                    kb.op("act", lambda gi=gi, xbt=xbt: nc.scalar.copy(xbt[:], X[:, gi, 32:NCK]), reads=xall, writes=[xbt.d])
                    ps = next_ps(g)
                    kb.op("pe", lambda gi=gi, ps=ps, ug=ug: nc.tensor.matmul(ps[:, :], lhsT=MT[:, gi, :], rhs=ug[:, 32:NCK],
                                                                             start=True, stop=False),
                          reads=[MT.d, ug.d], writes=[ps.d])
                    kb.op("pe", lambda gi=gi, ps=ps, xbt=xbt: nc.tensor.matmul(ps[:, :], lhsT=OTb[:, gi, :], rhs=xbt[:],
                                                                               start=False, stop=True),
                          reads=[OTb.d, xbt.d], writes=[ps.d])
                    yt = yo[gi % 2]
                    if z == 0:
                        kb.op("dve", lambda ps=ps, yt=yt: nc.vector.tensor_copy(yt[:], ps[:, :]),
                              reads=[ps.d], writes=[yt.d])
                    else:
                        yf = yfl[gi % 2]
                        kb.dma("act", lambda gi=gi, yf=yf: nc.scalar.dma_start(out=yf[:], in_=S.y0[gi, :, :]),
                               reads=[S.y0_d], writes=[yf.d])
                        kb.op("dve", lambda ps=ps, yt=yt, yf=yf: nc.vector.tensor_tensor(yt[:], ps[:, :], yf[:], op=ALU.add),
                              reads=[ps.d, yf.d], writes=[yt.d])
                    kb.dma("sp", lambda gi=gi, yt=yt: nc.sync.dma_start(out=S.y0[gi, :, :], in_=yt[:]),
                           reads=[yt.d], writes=[S.y0_d])
                kb.barrier()


SEG = 256
NSEG = NL * 4 // SEG + NE
JB = SEG // 128


def bc_tile(g, es, name, src_row_ap, n, reads=()):
    nc, kb = g.nc, g.kb
    t = T(kb, es, [128, n], F32, name)
    kb.dma("sp", lambda: nc.sync.dma_start(out=t[:], in_=src_row_ap.partition_broadcast(128)),
           reads=list(reads), writes=[t.d])
    return t


def phase_e(g):
    nc, kb, H, S = g.nc, g.kb, g.H, g.S
    R = g.R
    with ExitStack() as es:
        psb = g.psb[0]
        s5T = T(kb, es, [128, 4, NL], BF16, "s5T", nd=4)
        with ExitStack() as e1:
            glw = T(kb, e1, [128, 4, 512], BF16, "glw")
            kb.dma("pool", lambda: nc.gpsimd.dma_start(out=glw[:], in_=H.glu_w[0].rearrange("(k p) n -> p k n", p=128)),
                   writes=[glw.d])
            glb = T(kb, e1, [128, 4], F32, "glb")
            kb.dma("sp", lambda: nc.sync.dma_start(out=glb[:], in_=H.glu_b[0, :].rearrange("(k p) -> p k", p=128),
                                                   allow_slow_non_contiguous=True), writes=[glb.d])
            s5d = T(kb, e1, [128, 4], F32, "s5d")
            kb.dma("sp", lambda: nc.sync.dma_start(out=s5d[:], in_=H.s5_d[0, :].rearrange("(k p) -> p k", p=128),
                                                   allow_slow_non_contiguous=True), writes=[s5d.d])
            Yg = T(kb, e1, [128, 32, 128], F32, "Yg")
            Ytm = T(kb, e1, [128, 8, 512], F32, "Ytm")
            yT = T(kb, e1, [128, 4, 1024], F32, "yTt")
            uTt = T(kb, e1, [128, 4, 1024], BF16, "uTt")
            t1 = T(kb, e1, [128, 4, 1024], F32, "glt1")
            glg = T(kb, e1, [128, 4, 1024], BF16, "glg")
            sg = T(kb, e1, [128, 512], BF16, "glsg")
            for cb in range(4):
                kb.pump(1)
                kb.dma("sp", lambda cb=cb: nc.sync.dma_start(
                    out=Yg[:], in_=S.y0[:, :, cb * 128:(cb + 1) * 128].rearrange("g p c -> p g c")),
                    reads=[S.y0_d], writes=[Yg.d])
                kb.dma("act", lambda cb=cb: nc.scalar.dma_start(
                    out=uTt[:], in_=S.uT[:, NCX + cb * 1024:NCX + (cb + 1) * 1024].rearrange("(k p) t -> p k t", p=128)),
                    reads=[S.uT_d], writes=[uTt.d])
                for g4 in range(0, 32, 4):
                    ps = next_ps(g)
                    for gg in range(4):
                        kb.op("pe", lambda gg=gg, g4=g4, ps=ps: nc.tensor.transpose(
                            ps[:, gg * 128:(gg + 1) * 128], Yg[:, g4 + gg, :], g.ident[:]),
                            reads=[Yg.d, g.ident.d], writes=[ps.d])
                    kb.op("act", lambda g4=g4, ps=ps: nc.scalar.copy(
                        Ytm[:, :, g4 * 16:g4 * 16 + 64].rearrange("c s (a h) -> c a s h", a=4),
                        ps[:, :].rearrange("c (a s h) -> c a s h", a=4, s=8)), reads=[ps.d], writes=[Ytm.d])
                for s_ in range(8):
                    ps = next_ps(g)
                    for cc in range(4):
                        kb.op("pe", lambda cc=cc, s_=s_, ps=ps: nc.tensor.transpose(
                            ps[:, cc * 128:(cc + 1) * 128], Ytm[:, s_, cc * 128:(cc + 1) * 128], g.ident[:]),
                            reads=[Ytm.d, g.ident.d], writes=[ps.d])
                    kb.op("dve", lambda s_=s_, ps=ps: nc.vector.tensor_copy(
                        yT[:, :, s_:1024:8], ps[:, :].rearrange("p (a c) -> p a c", a=4)), reads=[ps.d], writes=[yT.d])
                for cc in range(4):
                    kb.op("dve", lambda cc=cc: nc.vector.scalar_tensor_tensor(
                        yT[:, cc, :], uTt[:, cc, :], s5d[:, cc:cc + 1], yT[:, cc, :], op0=ALU.mult, op1=ALU.add),
                        reads=[uTt.d, s5d.d, yT.d], writes=[yT.d])
                kb.op("pool", lambda: nc.gpsimd.tensor_tensor(t1[:], yT[:], yT[:], op=ALU.mult), reads=[yT.d], writes=[t1.d])
                kb.op("dve", lambda: nc.vector.tensor_scalar(t1[:], t1[:], 0.044715, 1.0, op0=ALU.mult, op1=ALU.add),
                      reads=[t1.d], writes=[t1.d])
                kb.op("pool", lambda: nc.gpsimd.tensor_tensor(t1[:], t1[:], yT[:], op=ALU.mult), reads=[t1.d, yT.d], writes=[t1.d])
                kb.op("act", lambda: nc.scalar.activation(t1[:], t1[:], ACT.Sigmoid, scale=1.5957691216057308),
                      reads=[t1.d], writes=[t1.d])
                kb.op("dve", lambda: nc.vector.tensor_tensor(glg[:], t1[:], yT[:], op=ALU.mult), reads=[t1.d, yT.d], writes=[glg.d])
                for nn in range(4):
                    for th in range(2):
                        ps = next_ps(g)
                        for kc in range(4):
                            kb.op("pe", lambda nn=nn, th=th, kc=kc, ps=ps: nc.tensor.matmul(
                                ps[:, :], lhsT=glw[:, kc, nn * 128:(nn + 1) * 128], rhs=glg[:, kc, th * 512:(th + 1) * 512],
                                start=(kc == 0), stop=(kc == 3)), reads=[glw.d, glg.d], writes=[ps.d])
                        kb.op("act", lambda nn=nn, ps=ps: nc.scalar.activation(sg[:], ps[:, :], ACT.Sigmoid,
                                                                              bias=glb[:, nn:nn + 1], scale=1.0),
                              reads=[ps.d, glb.d], writes=[sg.d])
                        kb.op("dve", lambda nn=nn, th=th, cb=cb: nc.vector.tensor_tensor(
                            s5T[:, nn, cb * 1024 + th * 512:cb * 1024 + (th + 1) * 512], sg[:],
                            glg[:, nn, th * 512:(th + 1) * 512], op=ALU.mult), reads=[sg.d, glg.d], writes=[s5T.ds[nn]])
            kb.barrier()
        if "s5T" in g.dbg:
            dbg_out(g, "s5T", s5T[:], s5T.ds, [128, 4, NL], BF16)
        ckpt("e1")
        glaT = T(kb, es, [128, 4, NL], BF16, "glaTt")
        kb.dma("sp", lambda: nc.sync.dma_start(out=glaT[:], in_=S.glaT.ap().rearrange("(k p) t -> p k t", p=128)),
               reads=[S.glaT_d], writes=[glaT.d])
        wo = T(kb, es, [128, 8, D], BF16, "wo")
        kb.dma("pool", lambda: nc.gpsimd.dma_start(out=wo[:], in_=H.w_out[0].rearrange("(k p) n -> p k n", p=128)),
               writes=[wo.d])
        g1b = bc_tile(g, es, "g1b", S.mod[0:1, 2 * D:3 * D], D, [S.mod_d])
        sh2b = bc_tile(g, es, "sh2b", S.mod[0:1, 3 * D:4 * D], D, [S.mod_d])
        g2e = bc_tile(g, es, "g2e", S.mod[0:1, 4 * D:5 * D], D, [S.mod_d])
        nfw = bc_tile(g, es, "nfw", H.norm_ffn_w[0:1, :], D)
        kb.op("dve", lambda: nc.vector.scalar_tensor_tensor(g2e[:], g2e[:], 1.0, nfw[:], op0=ALU.add, op1=ALU.mult),
              reads=[g2e.d, nfw.d], writes=[g2e.d])
        rw = T(kb, es, [128, 8, NE], F32, "rw")
        kb.dma("sp", lambda: nc.sync.dma_start(out=rw[:], in_=H.router_w[0].rearrange("(k p) e -> p k e", p=128)),
               writes=[rw.d])
        rbb = bc_tile(g, es, "rbb", H.router_b[0:1, :], NE)
        xt = [T(kb, es, [128, D], F32, f"ext{i}") for i in range(3)]
        x2t = [T(kb, es, [128, D], F32, f"ex2{i}") for i in range(2)]
        tmp_ = [T(kb, es, [128, D], F32, f"etmp{i}") for i in range(2)]
        tmq_ = [T(kb, es, [128, D], F32, f"etmq{i}") for i in range(2)]
        h2_ = [T(kb, es, [128, D], F32, f"eh2{i}") for i in range(2)]
        h2b = [T(kb, es, [128, D], BF16, f"eh2b{i}") for i in range(2)]
        hT2_ = [T(kb, es, [128, 8, 128], F32, f"ehT2{i}") for i in range(2)]
        junk = T(kb, es, [128, D], BF16, "ejunk")
        lg_ = [T(kb, es, [128, NE], F32, f"elg{i}") for i in range(2)]
        mx8_ = [T(kb, es, [128, 8], F32, f"emx8{i}") for i in range(2)]
        ix8_ = [T(kb, es, [128, 8], U32, f"eix8{i}") for i in range(2)]
        sm1_ = [T(kb, es, [128, 4], F32, f"esm{i}") for i in range(2)]
        def partA(tl):
            if tl % 4 == 0:
                kb.pump(1)
            t0 = tl * 128
            x_t, x2 = xt[tl % 3], x2t[tl % 2]
            tmp, tmq, h2, hT2 = tmp_[tl % 2], tmq_[tl % 2], h2_[tl % 2], hT2_[tl % 2]
            lg, mx8, ix8, sm1 = lg_[tl % 2], mx8_[tl % 2], ix8_[tl % 2], sm1_[tl % 2]
            for pf in ([0, 1, 2] if tl == 0 else [tl + 2]):
                if pf < NL // 128:
                    xp = xt[pf % 3]
                    kb.dma("sp", lambda xp=xp, pf=pf: nc.sync.dma_start(out=xp[:], in_=H.x[pf * 128:(pf + 1) * 128, :]),
                           writes=[xp.d])
            for nh in range(2):
                ps = next_ps(g)
                for r in range(2):
                    row = tl * 2 + r
                    for kc in range(8):
                        if kc < 4:
                            lh = glaT[:, kc, row * 64:(row + 1) * 64]
                            rd = [glaT.d]
                        else:
                            lh = s5T[:, kc - 4, row:NL:64]
                            rd = s5T.ds
                        kb.op("pe", lambda lh=lh, r=r, kc=kc, nh=nh, ps=ps: nc.tensor.matmul(
                            ps[r * 64:(r + 1) * 64, :], lhsT=lh, rhs=wo[:, kc, nh * 512:(nh + 1) * 512],
                            start=(kc == 0), stop=(kc == 7)), reads=rd + [wo.d], writes=[ps.d])
                kb.op("dve", lambda nh=nh, ps=ps: nc.vector.tensor_tensor(
                    tmp[:, nh * 512:(nh + 1) * 512], ps[:, :], g1b[:, nh * 512:(nh + 1) * 512], op=ALU.mult),
                    reads=[ps.d, g1b.d], writes=[tmp.d])
            kb.op("pool", lambda x2=x2, x_t=x_t, tmp=tmp: nc.gpsimd.tensor_tensor(x2[:], tmp[:], x_t[:], op=ALU.add),
                  reads=[tmp.d, x_t.d], writes=[x2.d])
            kb.dma("sp", lambda x2=x2, t0=t0: nc.sync.dma_start(out=S.x2[t0:t0 + 128, :], in_=x2[:]),
                   reads=[x2.d], writes=[S.x2_d])
            kb.op("act", lambda x2=x2: nc.scalar.activation(junk[:], x2[:], ACT.Square, accum_out=sm1[:, 0:1]),
                  reads=[x2.d], writes=[junk.d, sm1.d])
            kb.op("dve", lambda: nc.vector.tensor_scalar(sm1[:, 1:2], sm1[:, 0:1], 1.0 / D, EPS, op0=ALU.mult, op1=ALU.add),
                  reads=[sm1.d], writes=[sm1.d])
            kb.op("act", lambda: nc.scalar.activation(sm1[:, 1:2], sm1[:, 1:2], ACT.Sqrt), reads=[sm1.d], writes=[sm1.d])
            kb.op("dve", lambda: nc.vector.reciprocal(sm1[:, 2:3], sm1[:, 1:2]), reads=[sm1.d], writes=[sm1.d])
            kb.op("dve", lambda x2=x2: nc.vector.scalar_tensor_tensor(tmq[:], x2[:], sm1[:, 2:3], g2e[:],
                                                                      op0=ALU.mult, op1=ALU.mult),
                  reads=[x2.d, sm1.d, g2e.d], writes=[tmq.d])
            kb.op("pool", lambda: nc.gpsimd.tensor_tensor(h2[:], tmq[:], sh2b[:], op=ALU.add),
                  reads=[tmq.d, sh2b.d], writes=[h2.d])
            hb = h2b[tl % 2]
            kb.op("act", lambda hb=hb: nc.scalar.copy(hb[:], h2[:]), reads=[h2.d], writes=[hb.d])
            kb.dma("sp", lambda hb=hb, t0=t0: nc.sync.dma_start(out=S.h2[t0:t0 + 128, :], in_=hb[:]),
                   reads=[hb.d], writes=[S.h2_d])

        def partB(tl):
            tmp, tmq, h2, hT2 = tmp_[tl % 2], tmq_[tl % 2], h2_[tl % 2], hT2_[tl % 2]
            lg, mx8, ix8, sm1 = lg_[tl % 2], mx8_[tl % 2], ix8_[tl % 2], sm1_[tl % 2]
            for hf in range(2):
                ps = next_ps(g)
                for kk in range(4):
                    kc = hf * 4 + kk
                    kb.op("pe", lambda kc=kc, kk=kk, ps=ps: nc.tensor.transpose(
                        ps[:, kk * 128:(kk + 1) * 128], h2[:, kc * 128:(kc + 1) * 128], g.ident[:]),
                        reads=[h2.d, g.ident.d], writes=[ps.d])
                kb.op("act", lambda hf=hf, ps=ps: nc.scalar.copy(
                    hT2[:, hf * 4:(hf + 1) * 4, :], ps[:, :].rearrange("p (a t) -> p a t", a=4)),
                    reads=[ps.d], writes=[hT2.d])
            ps = next_ps(g)
            for kc in range(8):
                kb.op("pe", lambda kc=kc, ps=ps: nc.tensor.matmul(ps[:, 0:NE], lhsT=hT2[:, kc, :], rhs=rw[:, kc, :],
                                                                  start=(kc == 0), stop=(kc == 7)),
                      reads=[hT2.d, rw.d], writes=[ps.d])
            kb.op("dve", lambda ps=ps: nc.vector.tensor_tensor(lg[:], ps[:, 0:NE], rbb[:], op=ALU.add),
                  reads=[ps.d, rbb.d], writes=[lg.d])
            kb.op("dve", lambda: nc.vector.max(mx8[:], lg[:]), reads=[lg.d], writes=[mx8.d])
            kb.op("dve", lambda: nc.vector.max_index(ix8[:], mx8[:], lg[:]), reads=[lg.d, mx8.d], writes=[ix8.d])
            kb.op("dve", lambda tl=tl: nc.vector.tensor_copy(R.idxf[:, tl, :], ix8[:, 0:4]), reads=[ix8.d], writes=[R.idxf.d])
            kb.op("dve", lambda tl=tl: nc.vector.tensor_scalar(R.mask[:, tl, :], lg[:], mx8[:, 3:4], None, op0=ALU.is_ge),
                  reads=[lg.d, mx8.d], writes=[R.mask.d])
            kb.op("dve", lambda: nc.vector.tensor_scalar(sm1[:, 3:4], mx8[:, 0:1], -1.0, None, op0=ALU.mult),
                  reads=[mx8.d, sm1.d], writes=[sm1.d])
            kb.op("act", lambda tl=tl: nc.scalar.activation(R.gate[:, tl, :], mx8[:, 0:4], ACT.Exp, bias=sm1[:, 3:4],
                                                            scale=1.0, accum_out=sm1[:, 0:1]),
                  reads=[mx8.d, sm1.d], writes=[R.gate.d, sm1.d])
            kb.op("dve", lambda: nc.vector.reciprocal(sm1[:, 1:2], sm1[:, 0:1]), reads=[sm1.d], writes=[sm1.d])
            kb.op("dve", lambda tl=tl: nc.vector.tensor_scalar(R.gate[:, tl, :], R.gate[:, tl, :], sm1[:, 1:2], None,
                                                               op0=ALU.mult), reads=[R.gate.d, sm1.d], writes=[R.gate.d])

        NTL = NL // 128
        partA(0)
        for tl in range(1, NTL):
            partA(tl)
            partB(tl - 1)
        partB(NTL - 1)
        if "logits" in g.dbg:
            dbg_out(g, "gate", R.gate[:], [R.gate.d], [128, 32, 4])
            dbg_out(g, "idxf", R.idxf[:], [R.idxf.d], [128, 32, 4])
        ckpt("e2")
        onesf = T(kb, es, [128, 128], F32, "onesf")
        kb.op("pool", lambda: nc.gpsimd.memset(onesf[:], 1.0), writes=[onesf.d])
        triu = T(kb, es, [128, 128], F32, "triu")
        kb.op("dve", lambda: nc.vector.tensor_single_scalar(triu[:], g.iota[:], 0.0, op=ALU.is_gt),
              reads=[g.iota.d], writes=[triu.d])
        ps = next_ps(g)
        for tl in range(32):
            kb.op("pe", lambda tl=tl, ps=ps: nc.tensor.matmul(ps[:, 0:NE], lhsT=onesf[:], rhs=R.mask[:, tl, :],
                                                              start=(tl == 0), stop=(tl == 31)),
                  reads=[onesf.d, R.mask.d], writes=[ps.d])
        cnt = T(kb, es, [128, NE], F32, "cnt")
        nsg = T(kb, es, [128, NE], F32, "nsg")
        pend = T(kb, es, [128, NE], F32, "pend")
        pst = T(kb, es, [128, NE], F32, "pst")
        t32 = T(kb, es, [128, NE], F32, "t32")
        kb.op("dve", lambda ps=ps: nc.vector.tensor_copy(cnt[:], ps[:, 0:NE]), reads=[ps.d], writes=[cnt.d])
        kb.op("dve", lambda: nc.vector.memset(nsg[:], 0.0), writes=[nsg.d])
        for k in range(NL // SEG):
            kb.op("dve", lambda k=k: nc.vector.tensor_scalar(t32[:], cnt[:], float(SEG * k) + 0.5, None, op0=ALU.is_ge),
                  reads=[cnt.d], writes=[t32.d])
            kb.op("dve", lambda: nc.vector.tensor_tensor(nsg[:], nsg[:], t32[:], op=ALU.add), reads=[nsg.d, t32.d], writes=[nsg.d])
        kb.op("dve", lambda: nc.vector.tensor_tensor_scan(pend[:], onesf[:, 0:NE], nsg[:], 0.0, ALU.mult, ALU.add),
              reads=[onesf.d, nsg.d], writes=[pend.d])
        kb.op("dve", lambda: nc.vector.tensor_tensor(pst[:], pend[:], nsg[:], op=ALU.subtract), reads=[pend.d, nsg.d], writes=[pst.d])
        kb.op("dve", lambda: nc.vector.tensor_scalar(pst[:], pst[:], float(SEG), None, op0=ALU.mult), reads=[pst.d], writes=[pst.d])
        sidx = T(kb, es, [128, NSEG], F32, "sidx")
        kb.op("pool", lambda: nc.gpsimd.iota(sidx[:], pattern=[[1, NSEG]], base=0, channel_multiplier=0,
                                              allow_small_or_imprecise_dtypes=True), writes=[sidx.d])
        cmp3 = T(kb, es, [128, NSEG, NE], F32, "cmp3")
        kb.op("dve", lambda: nc.vector.tensor_tensor(cmp3[:], pend[:].unsqueeze(1).to_broadcast([128, NSEG, NE]),
                                                     sidx[:].unsqueeze(2).to_broadcast([128, NSEG, NE]), op=ALU.is_le),
              reads=[pend.d, sidx.d], writes=[cmp3.d])
        sef = T(kb, es, [128, NSEG], F32, "sef")
        kb.op("dve", lambda: nc.vector.tensor_reduce(sef[:], cmp3[:], axis=AX.X, op=ALU.add), reads=[cmp3.d], writes=[sef.d])
        kb.op("dve", lambda: nc.vector.tensor_scalar(sef[:], sef[:], float(NE - 1), None, op0=ALU.min), reads=[sef.d], writes=[sef.d])
        kb.op("dve", lambda: nc.vector.tensor_copy(R.segexp[:], sef[:]), reads=[sef.d], writes=[R.segexp.d])
        used = T(kb, es, [128, NSEG], F32, "used")
        kb.op("dve", lambda: nc.vector.tensor_scalar(used[:], sidx[:], pend[:, NE - 1:NE], None, op0=ALU.is_lt),
              reads=[sidx.d, pend.d], writes=[used.d])
        pcf = T(kb, es, [128, 1], F32, "pcf")
        kb.op("pool", lambda: nc.gpsimd.iota(pcf[:], pattern=[[0, 1]], base=0, channel_multiplier=1,
                                              allow_small_or_imprecise_dtypes=True), writes=[pcf.d])
        OOB = 1000000.0
        sgf = T(kb, es, [128, NSEG], F32, "sgf")
        kb.op("dve", lambda: nc.vector.tensor_scalar(sgf[:], sef[:], 128.0, pcf[:, 0:1], op0=ALU.mult, op1=ALU.add),
              reads=[sef.d, pcf.d], writes=[sgf.d])
        for src, dst in ((sgf, R.segidx), (sef, R.segrow)):
            kb.op("dve", lambda src=src: nc.vector.tensor_scalar(src[:], src[:], -OOB, None, op0=ALU.add),
                  reads=[src.d], writes=[src.d])
            kb.op("dve", lambda src=src: nc.vector.tensor_tensor(src[:], src[:], used[:], op=ALU.mult),
                  reads=[src.d, used.d], writes=[src.d])
            kb.op("dve", lambda src=src: nc.vector.tensor_scalar(src[:], src[:], OOB, None, op0=ALU.add),
                  reads=[src.d], writes=[src.d])
            kb.op("dve", lambda src=src, dst=dst: nc.vector.tensor_copy(dst[:], src[:]), reads=[src.d], writes=[dst.d])
        if "logits" in g.dbg:
            dbg_out(g, "cnt", cnt[:], [cnt.d], [128, NE])
            dbg_out(g, "segexp", R.segexp[:], [R.segexp.d], [128, NSEG], I32)
        carry = T(kb, es, [128, NE], F32, "carry")
        kb.op("dve", lambda: nc.vector.tensor_copy(carry[:], pst[:]), reads=[pst.d], writes=[carry.d])
        ief = T(kb, es, [128, NE], F32, "ief")
        kb.op("pool", lambda: nc.gpsimd.iota(ief[:], pattern=[[1, NE]], base=0, channel_multiplier=0,
                                              allow_small_or_imprecise_dtypes=True), writes=[ief.d])
        slf = T(kb, es, [128, NE], F32, "slf")
        slk = T(kb, es, [128, 4], F32, "slk")
        hld = [T(kb, es, [128, D], BF16, f"hld{i}") for i in range(2)]
        for tl in range(32):
            t0 = tl * 128
            psA = next_ps(g)
            kb.op("pe", lambda tl=tl, psA=psA: nc.tensor.matmul(psA[:, 0:NE], lhsT=triu[:], rhs=R.mask[:, tl, :],
                                                                start=True, stop=True),
                  reads=[triu.d, R.mask.d], writes=[psA.d])
            kb.op("dve", lambda psA=psA: nc.vector.tensor_tensor(slf[:], psA[:, 0:NE], carry[:], op=ALU.add),
                  reads=[psA.d, carry.d], writes=[slf.d])
            psB = next_ps(g)
            kb.op("pe", lambda tl=tl, psB=psB: nc.tensor.matmul(psB[:, 0:NE], lhsT=onesf[:], rhs=R.mask[:, tl, :],
                                                                start=True, stop=True),
                  reads=[onesf.d, R.mask.d], writes=[psB.d])
            kb.op("dve", lambda psB=psB: nc.vector.tensor_tensor(carry[:], carry[:], psB[:, 0:NE], op=ALU.add),
                  reads=[psB.d, carry.d, slf.d], writes=[carry.d])
            for k in range(4):
                kb.op("dve", lambda k=k, tl=tl: nc.vector.scalar_tensor_tensor(
                    t32[:], ief[:], R.idxf[:, tl, k:k + 1], slf[:], op0=ALU.is_equal, op1=ALU.mult,
                    accum_out=slk[:, k:k + 1]), reads=[ief.d, R.idxf.d, slf.d], writes=[t32.d, slk.d])
            kb.op("dve", lambda tl=tl: nc.vector.tensor_copy(R.slot[:, tl, :], slk[:]), reads=[slk.d], writes=[R.slot.ds[tl]])
            hl = hld[tl % 2]
            kb.dma("sp", lambda hl=hl, t0=t0: nc.sync.dma_start(out=hl[:], in_=S.h2[t0:t0 + 128, :]),
                   reads=[S.h2_d], writes=[hl.d])
            for k in range(4):
                kb.dma("pool", lambda hl=hl, tl=tl, k=k: nc.gpsimd.indirect_dma_start(
                    out=S.xg[:, :], out_offset=bass.IndirectOffsetOnAxis(ap=R.slot[:, tl, k:k + 1], axis=0),
                    in_=hl[:, :], in_offset=None), reads=[hl.d, R.slot.ds[tl]], writes=[S.xg_d])
        if "logits" in g.dbg:
            dbg_out(g, "slot", R.slot[:], R.slot.ds, [128, 32, 4], I32)
        bg = T(kb, es, [NE, 2 * D], F32, "bgrow")
        kb.dma("sp", lambda: nc.sync.dma_start(out=bg[:], in_=H.exp_b_gu[0]), writes=[bg.d])
        bgt = T(kb, es, [128, 16, NE], F32, "bgt")
        for c4 in range(0, 16, 4):
            ps = next_ps(g)
            for cc in range(4):
                kb.op("pe", lambda cc=cc, c4=c4, ps=ps: nc.tensor.transpose(
                    ps[:, cc * NE:(cc + 1) * NE], bg[:, (c4 + cc) * 128:(c4 + cc + 1) * 128], g.ident[0:NE, 0:NE]),
                    reads=[bg.d, g.ident.d], writes=[ps.d])
            kb.op("act", lambda c4=c4, ps=ps: nc.scalar.copy(
                bgt[:, c4:c4 + 4, :], ps[:, 0:4 * NE].rearrange("p (a e) -> p a e", a=4)), reads=[ps.d], writes=[bgt.d])
        kb.dma("sp", lambda: nc.sync.dma_start(out=S.bguT.ap().rearrange("e p c -> p c e"), in_=bgt[:],
                                               allow_slow_non_contiguous=True), reads=[bgt.d], writes=[S.bguT_d])


def phase_f(g):
    nc, kb, H, S = g.nc, g.kb, g.H, g.S
    R = g.R
    with ExitStack() as es:
        wgu = [T(kb, es, [128, 8, 2 * D], BF16, f"wgu{i}") for i in range(2)]
        wdn = [T(kb, es, [128, 8, D], BF16, f"wdn{i}") for i in range(2)]
        bgu = [T(kb, es, [128, 16], F32, f"bgu{i}") for i in range(2)]
        bdn = [T(kb, es, [128, D], F32, f"bdn{i}") for i in range(2)]
        bgs = [T(kb, es, [128, 8], F32, f"bgs{i}") for i in range(2)]
        xrow = [T(kb, es, [128, JB, D], BF16, f"xrow{i}") for i in range(2)]
        xT = [T(kb, es, [128, 8, SEG], BF16, f"xT{i}") for i in range(2)]
        actT = [T(kb, es, [128, 8, SEG], BF16, f"actT{i}", nd=8) for i in range(2)]
        yrow = [T(kb, es, [128, JB, D], BF16, f"yrow{i}") for i in range(2)]
        L0 = [T(kb, es, [128, SEG], F32, f"fL0_{i}") for i in range(2)]
        Gc = [T(kb, es, [128, SEG], F32, f"fGc_{i}") for i in range(2)]
        sg = [T(kb, es, [128, SEG], F32, f"fsg_{i}") for i in range(2)]
        tt = [T(kb, es, [128, SEG], F32, f"ftt_{i}") for i in range(2)]
        bc_rows = nc.gpsimd.to_reg(NE * 128 - 1)
        bc_e = nc.gpsimd.to_reg(NE - 1)
        wg_rows = S.wg.ap().rearrange("e p f -> (e p) f")
        wd_rows = S.wd.ap().rearrange("e p f -> (e p) f")
        bgu_rows = S.bguT.ap().rearrange("e p c -> (e p) c")
        nseg = g.nseg_limit if getattr(g, "nseg_limit", None) else NSEG
        bench = getattr(g, "bench", "") or ""
        for s in range(nseg):
            b = s % 2
            if "nodma" in bench and s >= 2:
                KB.dead = True
            kb.dma("pool", lambda: nc.gpsimd.indirect_dma_start(
                out=wgu[b][:, :, :].rearrange("p k n -> p (k n)"), out_offset=None, in_=wg_rows,
                in_offset=bass.IndirectOffsetOnAxis(ap=R.segidx[:, s:s + 1], axis=0),
                bounds_check=bc_rows, oob_is_err=False),
                reads=[R.segidx.d, S.wg_d], writes=[wgu[b].d])
            kb.dma("pool", lambda: nc.gpsimd.indirect_dma_start(
                out=wdn[b][:, :, :].rearrange("p k n -> p (k n)"), out_offset=None, in_=wd_rows,
                in_offset=bass.IndirectOffsetOnAxis(ap=R.segidx[:, s:s + 1], axis=0),
                bounds_check=bc_rows, oob_is_err=False),
                reads=[R.segidx.d, S.wd_d], writes=[wdn[b].d])
            kb.dma("pool", lambda: nc.gpsimd.indirect_dma_start(
                out=bgu[b][:, :], out_offset=None, in_=bgu_rows,
                in_offset=bass.IndirectOffsetOnAxis(ap=R.segidx[:, s:s + 1], axis=0),
                bounds_check=bc_rows, oob_is_err=False),
                reads=[R.segidx.d, S.bguT_d], writes=[bgu[b].d])
            kb.dma("pool", lambda: nc.gpsimd.indirect_dma_start(
                out=bdn[b][:, :], out_offset=None, in_=H.exp_b_down[0],
                in_offset=bass.IndirectOffsetOnAxis(ap=R.segrow[:, s:s + 1], axis=0),
                bounds_check=bc_e, oob_is_err=False),
                reads=[R.segrow.d], writes=[bdn[b].d])
            if bench:
                KB.dead = False
            kb.op("act", lambda: nc.scalar.mul(bgs[b][:], bgu[b][:, 8:16], 1.0 / 1.702), reads=[bgu[b].d], writes=[bgs[b].d])
            kb.dma("sp", lambda: nc.sync.dma_start(
                out=xrow[b][:], in_=S.xg[s * SEG:(s + 1) * SEG, :].rearrange("(j p) d -> p j d", p=128)),
                reads=[S.xg_d], writes=[xrow[b].d])
            if "nocomp" in bench:
                KB.dead = True
            for kc in range(8):
                pb = g.psb[kc % 2]
                for j in range(JB):
                    kb.op("pe", lambda j=j, kc=kc, pb=pb: nc.tensor.transpose(
                        pb[:, j * 128:(j + 1) * 128], xrow[b][:, j, kc * 128:(kc + 1) * 128], g.identb[:]),
                        reads=[xrow[b].d, g.identb.d], writes=[pb.d])
                if kc % 2 == 0:
                    kb.op("act", lambda kc=kc, pb=pb: nc.scalar.copy(xT[b][:, kc, :], pb[:, 0:SEG]),
                          reads=[pb.d], writes=[xT[b].d])
                else:
                    kb.op("dve", lambda kc=kc, pb=pb: nc.vector.tensor_copy(xT[b][:, kc, :], pb[:, 0:SEG]),
                          reads=[pb.d], writes=[xT[b].d])
            for c in range(8):
                i2 = c % 2
                pg = next_ps(g)
                for kc in range(8):
                    kb.op("pe", lambda kc=kc, c=c, pg=pg: nc.tensor.matmul(
                        pg[:, 0:SEG], lhsT=wgu[b][:, kc, c * 128:(c + 1) * 128], rhs=xT[b][:, kc, :],
                        start=(kc == 0), stop=(kc == 7)), reads=[wgu[b].d, xT[b].d], writes=[pg.d])
                pl_ = next_ps(g)
                for kc in range(8):
                    kb.op("pe", lambda kc=kc, c=c, pl_=pl_: nc.tensor.matmul(
                        pl_[:, 0:SEG], lhsT=wgu[b][:, kc, D + c * 128:D + (c + 1) * 128], rhs=xT[b][:, kc, :],
                        start=(kc == 0), stop=(kc == 7)), reads=[wgu[b].d, xT[b].d], writes=[pl_.d])
                kb.op("dve", lambda c=c, pg=pg: nc.vector.tensor_scalar(Gc[i2][:], pg[:, 0:SEG], bgu[b][:, c:c + 1], 7.0,
                                                                      op0=ALU.add, op1=ALU.min),
                      reads=[pg.d, bgu[b].d], writes=[Gc[i2].d])
                kb.op("act", lambda: nc.scalar.activation(sg[i2][:], Gc[i2][:], ACT.Silu, scale=1.702),
                      reads=[Gc[i2].d], writes=[sg[i2].d])
                kb.op("act", lambda c=c, pl_=pl_: nc.scalar.activation(L0[i2][:], pl_[:, 0:SEG], ACT.Identity,
                                                                      bias=bgs[b][:, c:c + 1], scale=1.0 / 1.702),
                      reads=[pl_.d, bgs[b].d], writes=[L0[i2].d])
                kb.op("dve", lambda: nc.vector.tensor_scalar(L0[i2][:], L0[i2][:], 7.0 / 1.702, -7.0 / 1.702,
                                                             op0=ALU.min, op1=ALU.max),
                      reads=[L0[i2].d], writes=[L0[i2].d])
                kb.op("dve", lambda c=c: nc.vector.scalar_tensor_tensor(actT[b][:, c, :], L0[i2][:], 1.0 / 1.702, sg[i2][:],
                                                                       op0=ALU.add, op1=ALU.mult),
                      reads=[L0[i2].d, sg[i2].d], writes=[actT[b].ds[c]])
            for j in range(JB):
                for nh in range(2):
                    ps = next_ps(g)
                    for c in range(8):
                        kb.op("pe", lambda c=c, j=j, nh=nh, ps=ps: nc.tensor.matmul(
                            ps[:, :], lhsT=actT[b][:, c, j * 128:(j + 1) * 128], rhs=wdn[b][:, c, nh * 512:(nh + 1) * 512],
                            start=(c == 0), stop=(c == 7)), reads=[actT[b].ds[c], wdn[b].d], writes=[ps.d])
                    kb.op("dve", lambda j=j, nh=nh, ps=ps: nc.vector.tensor_tensor(
                        yrow[b][:, j, nh * 512:(nh + 1) * 512], ps[:, :], bdn[b][:, nh * 512:(nh + 1) * 512], op=ALU.add),
                        reads=[ps.d, bdn[b].d], writes=[yrow[b].d])
            kb.dma("act", lambda: nc.scalar.dma_start(
                out=S.yg[s * SEG:(s + 1) * SEG, :].rearrange("(j p) d -> p j d", p=128), in_=yrow[b][:]),
                reads=[yrow[b].d], writes=[S.yg_d])
            if bench:
                KB.dead = False


def phase_g(g):
    nc, kb, H, S = g.nc, g.kb, g.H, g.S
    R = g.R
    with ExitStack() as es:
        g2b = bc_tile(g, es, "g2b", S.mod[0:1, 5 * D:6 * D], D, [S.mod_d])
        fnw = bc_tile(g, es, "fnw", H.final_norm_w.ap().rearrange("(o d) -> o d", o=1), D)
        yk = [[T(kb, es, [128, D], BF16, f"yk{i}_{k}") for k in range(4)] for i in range(2)]
        x2t = [T(kb, es, [128, D], F32, f"gx2{i}") for i in range(2)]
        acc_ = [T(kb, es, [128, D], F32, f"gacc{i}") for i in range(2)]
        x3_ = [T(kb, es, [128, D], F32, f"gx3{i}") for i in range(2)]
        sm_ = [T(kb, es, [128, 4], F32, f"gsm{i}") for i in range(2)]
        ot = [T(kb, es, [128, D], F32, f"got{i}") for i in range(2)]
        junk = T(kb, es, [128, D], BF16, "gjunk")
        sm1 = T(kb, es, [128, 4], F32, "gsm")
        for tl in range(NL // 128):
            t0 = tl * 128
            b = tl % 2
            acc, x3, sm1 = acc_[b], x3_[b], sm_[b]
            for k in range(4):
                kb.dma("pool", lambda k=k: nc.gpsimd.indirect_dma_start(
                    out=yk[b][k][:, :], out_offset=None, in_=S.yg[:, :],
                    in_offset=bass.IndirectOffsetOnAxis(ap=R.slot[:, tl, k:k + 1], axis=0)),
                    reads=[S.yg_d, R.slot.ds[tl]], writes=[yk[b][k].d])
            kb.dma("sp", lambda: nc.sync.dma_start(out=x2t[b][:], in_=S.x2[t0:t0 + 128, :]), reads=[S.x2_d], writes=[x2t[b].d])
            kb.op("dve", lambda: nc.vector.tensor_scalar(acc[:], yk[b][0][:], R.gate[:, tl, 0:1], None, op0=ALU.mult),
                  reads=[yk[b][0].d, R.gate.d], writes=[acc.d])
            for k in range(1, 4):
                kb.op("dve", lambda k=k: nc.vector.scalar_tensor_tensor(acc[:], yk[b][k][:], R.gate[:, tl, k:k + 1], acc[:],
                                                                       op0=ALU.mult, op1=ALU.add),
                      reads=[yk[b][k].d, R.gate.d, acc.d], writes=[acc.d])
            kb.op("dve", lambda: nc.vector.tensor_tensor(acc[:], acc[:], g2b[:], op=ALU.mult), reads=[acc.d, g2b.d], writes=[acc.d])
            kb.op("dve", lambda: nc.vector.tensor_tensor(x3[:], acc[:], x2t[b][:], op=ALU.add), reads=[acc.d, x2t[b].d], writes=[x3.d])
            kb.op("act", lambda: nc.scalar.activation(junk[:], x3[:], ACT.Square, accum_out=sm1[:, 0:1]),
                  reads=[x3.d], writes=[junk.d, sm1.d])
            kb.op("dve", lambda: nc.vector.tensor_scalar(sm1[:, 1:2], sm1[:, 0:1], 1.0 / D, EPS, op0=ALU.mult, op1=ALU.add),
                  reads=[sm1.d], writes=[sm1.d])
            kb.op("act", lambda: nc.scalar.activation(sm1[:, 1:2], sm1[:, 1:2], ACT.Sqrt), reads=[sm1.d], writes=[sm1.d])
            kb.op("dve", lambda: nc.vector.reciprocal(sm1[:, 2:3], sm1[:, 1:2]), reads=[sm1.d], writes=[sm1.d])
            kb.op("dve", lambda: nc.vector.scalar_tensor_tensor(ot[b][:], x3[:], sm1[:, 2:3], fnw[:], op0=ALU.mult, op1=ALU.mult),
                  reads=[x3.d, sm1.d, fnw.d], writes=[ot[b].d])
            kb.dma("act", lambda: nc.scalar.dma_start(out=H.out[t0:t0 + 128, :], in_=ot[b][:]), reads=[ot[b].d], writes=[g.out_d])


_SHARED = ("c_ctx", "ada_w", "ada_b", "norm_mix_w", "w_in", "gla_lr_up", "gla_lr_bias", "gla_norm_w",
           "s5_lam_re", "s5_lam_im", "s5_log_dt", "s5_b_re", "s5_b_im", "s5_c_re", "s5_c_im", "s5_d",
           "glu_w", "glu_b", "w_out", "norm_ffn_w", "router_w", "router_b", "exp_w_gu", "exp_b_gu",
           "exp_w_down", "exp_b_down", "final_norm_w")


def kernel(x, c, ctx, c_ctx, ada_w, ada_b, norm_mix_w, w_in, gla_lr_up, gla_lr_bias, gla_norm_w,
           s5_lam_re, s5_lam_im, s5_log_dt, s5_b_re, s5_b_im, s5_c_re, s5_c_im, s5_d, glu_w, glu_b,
           w_out, norm_ffn_w, router_w, router_b, exp_w_gu, exp_b_gu, exp_w_down, exp_b_down,
           final_norm_w):
    loc = locals()
    shared = {k: np.ascontiguousarray(np.asarray(loc[k], dtype=np.float32)) for k in _SHARED}
    x = np.asarray(x, dtype=np.float32)
    c = np.asarray(c, dtype=np.float32)
    ctx = np.asarray(ctx, dtype=np.float32)
    nb = x.shape[0]
    nc, _ = build()
    in_maps = []
    for b in range(nb):
        m = dict(shared)
        m["x"] = np.ascontiguousarray(x[b])
        m["ctx"] = np.ascontiguousarray(ctx[b])
        m["c"] = np.ascontiguousarray(c[b:b + 1])
        in_maps.append(m)
    res = run_bass_kernel_spmd(nc, in_maps, core_ids=list(range(nb)))
    return np.stack([np.asarray(r["out"], dtype=np.float32) for r in res.results], axis=0)
```

```python
import numpy as np
import concourse.bass as bass
import concourse.mybir as mybir
from concourse.bass_utils import run_bass_kernel_spmd
from contextlib import ExitStack

F32 = mybir.dt.float32
BF16 = mybir.dt.bfloat16
U32 = mybir.dt.uint32
I32 = mybir.dt.int32
ACT = mybir.ActivationFunctionType
ALU = mybir.AluOpType
AX = mybir.AxisListType
PoolE = mybir.EngineType.Pool

D = 1024
NL = 4096
NCX = 256
NT = NL + NCX
NE = 32
EPS = 1e-6


class Dep:
    __slots__ = ("w", "r", "name")

    def __init__(self, name=""):
        self.w = None
        self.r = []
        self.name = name


class KB:
    NSLOT = 8

    def __init__(self, nc, es):
        self.nc = nc
        self.engs = {"pe": nc.tensor, "dve": nc.vector, "act": nc.scalar,
                     "pool": nc.gpsimd, "sp": nc.sync}
        self.sem = {}
        self.cnt = {}
        for k in self.engs:
            self.sem[k] = es.enter_context(nc.semaphore("s_" + k))
            self.cnt[k] = 0
        self.slots = {}
        self.slot_rr = {}
        for q in ("sp", "act", "pool"):
            self.slots[q] = [[es.enter_context(nc.semaphore(f"d_{q}{i}")), 0]
                             for i in range(self.NSLOT)]
            self.slot_rr[q] = 0
        self.slots["conv"] = [[es.enter_context(nc.semaphore(f"d_conv{i}")), 0] for i in range(6)]
        self.slot_rr["conv"] = 0
        self.pending = []
        self.seen = {k: {} for k in self.engs}
        self.ninst = 0
        self.nwaits = 0

    def _wait(self, eng, tok):
        if tok is None:
            return
        sem, val, key = tok
        if eng == "pe" and key == "pe":
            return
        s = self.seen[eng]
        if s.get(key, 0) >= val:
            return
        self.engs[eng].wait_ge(sem, val)
        s[key] = val
        self.nwaits += 1

    def _deps(self, eng, reads, writes):
        for d in reads:
            self._wait(eng, d.w)
        for d in writes:
            self._wait(eng, d.w)
            for t in d.r:
                self._wait(eng, t)

    def _commit(self, tok, reads, writes):
        for d in reads:
            d.r.append(tok)
            if len(d.r) > 16:
                best = {}
                for t in d.r:
                    if t[2] not in best or best[t[2]][1] < t[1]:
                        best[t[2]] = t
                d.r = list(best.values())
        for d in writes:
            d.w = tok
            d.r = []

    dead = False

    def op(self, eng, fn, reads=(), writes=()):
        if KB.dead:
            return None
        self._deps(eng, reads, writes)
        inst = fn()
        self.cnt[eng] += 1
        inst.then_inc(self.sem[eng], 1)
        tok = (self.sem[eng], self.cnt[eng], eng)
        self._commit(tok, reads, writes)
        self.ninst += 1
        return tok

    def pump(self, n=1):
        for _ in range(n):
            if not self.pending:
                return
            fn, reads, writes = self.pending.pop(0)
            self.dma("pool", fn, reads=reads, writes=writes, grp="conv")

    def dma(self, q, fn, reads=(), writes=(), grp=None):
        if KB.dead:
            return None
        grp = grp or q
        i = self.slot_rr[grp]
        self.slot_rr[grp] = (i + 1) % len(self.slots[grp])
        slot = self.slots[grp][i]
        key = f"d_{grp}{i}"
        if slot[1] > 0:
            self._wait(q, (slot[0], slot[1], key))
        self._deps(q, reads, writes)
        inst = fn()
        slot[1] += 16
        inst.then_inc(slot[0], 16)
        tok = (slot[0], slot[1], key)
        self._commit(tok, reads, writes)
        self.ninst += 1
        return tok

    def wait_all(self, eng, deps):
        for d in deps:
            self._wait(eng, d.w)
            for t in d.r:
                self._wait(eng, t)

    def barrier(self, conv=False):
        for e in self.engs:
            self.finish(e, conv)

    def finish(self, eng="sp", conv=True):
        for k in self.engs:
            if self.cnt[k] > 0:
                self._wait(eng, (self.sem[k], self.cnt[k], k))
        for q in self.slots:
            if q == "conv" and not conv:
                continue
            for i, s in enumerate(self.slots[q]):
                if s[1] > 0:
                    self._wait(eng, (s[0], s[1], f"d_{q}{i}"))


class T:
    _ctr = [0]

    def __init__(self, kb, es, shape, dtype, name, psum=False, nd=1):
        nc = kb.nc
        T._ctr[0] += 1
        name = f"{name}_{T._ctr[0]}"
        if psum:
            self.t = es.enter_context(nc.psum_tensor(name, list(shape), dtype))
        else:
            self.t = es.enter_context(nc.sbuf_tensor(name, list(shape), dtype))
        self.ds = [Dep(f"{name}.{i}") for i in range(nd)]
        self.d = self.ds[0]
        self.shape = shape

    def __getitem__(self, k):
        return self.t[k]


class Ctx:
    pass


class StopBuild(Exception):
    pass


def ckpt(name):
    if STOP == name:
        KB.dead = True


STOP = None


def build(dbg=(), stop=None):
    global STOP
    STOP = stop
    KB.dead = False
    nc = bass.Bass("TRN2", target_bir_lowering=False)
    g = Ctx()
    g.nc = nc
    g.dbg = set(dbg)
    g.outs = {}
    g.out_d = Dep("out")

    def din(name, shape, dt=F32):
        return nc.dram_tensor(name, list(shape), dt, kind="ExternalInput")

    H = Ctx()
    g.H = H
    H.x = din("x", [NL, D])
    H.ctx = din("ctx", [NCX, D])
    H.c = din("c", [1, D])
    H.c_ctx = din("c_ctx", [D])
    H.ada_w = din("ada_w", [1, D, 6 * D])
    H.ada_b = din("ada_b", [1, 6 * D])
    H.norm_mix_w = din("norm_mix_w", [1, D])
    H.w_in = din("w_in", [1, D, 2080])
    H.gla_lr_up = din("gla_lr_up", [1, 2, 16, 256])
    H.gla_lr_bias = din("gla_lr_bias", [1, 2, 256])
    H.gla_norm_w = din("gla_norm_w", [1, 128])
    H.s5_lam_re = din("s5_lam_re", [1, 2, 32, 64])
    H.s5_lam_im = din("s5_lam_im", [1, 2, 32, 64])
    H.s5_log_dt = din("s5_log_dt", [1, 2, 32])
    H.s5_b_re = din("s5_b_re", [1, 2, 32, 64, 16])
    H.s5_b_im = din("s5_b_im", [1, 2, 32, 64, 16])
    H.s5_c_re = din("s5_c_re", [1, 2, 32, 16, 64])
    H.s5_c_im = din("s5_c_im", [1, 2, 32, 16, 64])
    H.s5_d = din("s5_d", [1, 512])
    H.glu_w = din("glu_w", [1, 512, 512])
    H.glu_b = din("glu_b", [1, 512])
    H.w_out = din("w_out", [1, D, D])
    H.norm_ffn_w = din("norm_ffn_w", [1, D])
    H.router_w = din("router_w", [1, D, NE])
    H.router_b = din("router_b", [1, NE])
    H.exp_w_gu = din("exp_w_gu", [1, NE, D, 2 * D])
    H.exp_b_gu = din("exp_b_gu", [1, NE, 2 * D])
    H.exp_w_down = din("exp_w_down", [1, NE, D, D])
    H.exp_b_down = din("exp_b_down", [1, NE, D])
    H.final_norm_w = din("final_norm_w", [D])
    H.out = nc.dram_tensor("out", [NL, D], F32, kind="ExternalOutput")

    S = Ctx()
    g.S = S

    def scr(name, shape, dt):
        if name in g.dbg:
            h = nc.dram_tensor(name, list(shape), dt, kind="ExternalOutput")
        else:
            h = nc.dram_tensor(name, list(shape), dt)
        return h, Dep(name)

    S.mod, S.mod_d = scr("mod_s", [2, 6 * D], F32)
    S.qT, S.qT_d = scr("qT_s", [256, NT], BF16)
    S.kT, S.kT_d = scr("kT_s", [256, NT], BF16)
    S.rT, S.rT_d = scr("rT_s", [512, NL], BF16)
    S.lrT, S.lrT_d = scr("lrT_s", [2, 16, NT], F32)
    S.v, S.v_d = scr("v_s", [NT, 512], BF16)
    S.uT, S.uT_d = scr("uT_s", [512, NT], BF16)
    S.u, S.u_d = scr("u_s", [NT, 512], BF16)
    S.glaT, S.glaT_d = scr("glaT_s", [512, NL], BF16)
    S.y0, S.y0_d = scr("y0_s", [32, 128, 512], F32)
    S.Ut, S.Ut_d = scr("Ut_s", [32, 128, NT // 8], BF16)
    S.x2, S.x2_d = scr("x2_s", [NL, D], F32)
    S.h2, S.h2_d = scr("h2_s", [NL, D], BF16)
    S.xg, S.xg_d = scr("xg_s", [NSEG * SEG, D], BF16)
    S.yg, S.yg_d = scr("yg_s", [NSEG * SEG, D], BF16)
    S.bguT, S.bguT_d = scr("bguT_s", [NE, 128, 16], F32)
    S.wg, S.wg_d = scr("wg_s", [NE, 128, 8 * 2 * D], BF16)
    S.wd, S.wd_d = scr("wd_s", [NE, 128, 8 * D], BF16)

    with ExitStack() as es:
        kb = KB(nc, es)
        g.kb = kb
        g.es = es
        g.ident = T(kb, es, [128, 128], F32, "ident")
        g.identb = T(kb, es, [128, 128], BF16, "identb")
        io = T(kb, es, [128, 128], F32, "iota0")
        kb.op("pool", lambda: nc.gpsimd.iota(io[:], pattern=[[1, 128]], base=0, channel_multiplier=-1,
                                              allow_small_or_imprecise_dtypes=True), writes=[io.d])
        kb.op("dve", lambda: nc.vector.tensor_single_scalar(g.ident[:], io[:], 0.0, op=ALU.is_equal),
              reads=[io.d], writes=[g.ident.d])
        kb.op("dve", lambda: nc.vector.tensor_copy(g.identb[:], g.ident[:]), reads=[g.ident.d], writes=[g.identb.d])
        g.iota = io
        g.ps = [T(kb, es, [128, 512], F32, f"ps{i}", psum=True) for i in range(6)]
        g.psb = [T(kb, es, [128, 1024], BF16, f"psb{i}", psum=True) for i in range(2)]
        g.ps_rr = 0
        R = Ctx()
        g.R = R
        R.mask = T(kb, es, [128, 32, NE], F32, "r_mask")
        R.idxf = T(kb, es, [128, 32, 4], F32, "r_idxf")
        R.gate = T(kb, es, [128, 32, 4], F32, "r_gate")
        R.slot = T(kb, es, [128, 32, 4], I32, "r_slot", nd=32)
        R.segexp = T(kb, es, [128, NSEG], I32, "r_segexp")
        R.segidx = T(kb, es, [128, NSEG], I32, "r_segidx")
        R.segrow = T(kb, es, [128, NSEG], I32, "r_segrow")

        if stop is not None and str(stop).startswith("benchf"):
            kb.op("pool", lambda: nc.gpsimd.iota(R.segidx[:], pattern=[[0, NSEG]], base=0, channel_multiplier=1),
                  writes=[R.segidx.d])
            kb.op("pool", lambda: nc.gpsimd.memset(R.segrow[:], 0), writes=[R.segrow.d])
            g.bench = stop
            phase_f(g)
            kb.barrier()
            kb.finish("sp")
            print("instructions", kb.ninst, "waits", kb.nwaits)
            return nc, g
        phase_a(g)
        kb.barrier()
        for e in range(NE):
            kb.pending.append((lambda e=e: nc.gpsimd.dma_start(
                out=S.wg[e, :, :].rearrange("p (k n) -> p k n", k=8),
                in_=H.exp_w_gu[0, e, :, :].rearrange("(k p) n -> p k n", p=128)), [], [S.wg_d]))
            kb.pending.append((lambda e=e: nc.gpsimd.dma_start(
                out=S.wd[e, :, :].rearrange("p (k n) -> p k n", k=8),
                in_=H.exp_w_down[0, e, :, :].rearrange("(k p) n -> p k n", p=128)), [], [S.wd_d]))
        kb.pump(6)
        if stop != 'a':
            phase_b(g)
            kb.barrier()
            if stop != 'b':
                if stop not in ('d_only', 'e_only'):
                    phase_c(g)
                    kb.barrier()
                if stop not in ('c', 'c0', 'c1', 'c2', 'c3'):
                    if stop != 'e_only':
                        phase_d(g)
                        kb.barrier()
                    if stop not in ('d', 'd_only') and not KB.dead:
                        phase_e(g)
                        kb.barrier()
                        if stop != 'e' and not KB.dead:
                            kb.pump(1000)
                            kb.barrier(conv=True)
                            phase_f(g)
                            kb.barrier()
                            phase_g(g)
                            kb.barrier()

        kb.finish("sp")
        print("instructions", kb.ninst, "waits", kb.nwaits)
    return nc, g


def next_ps(g):
    p = g.ps[g.ps_rr]
    g.ps_rr = (g.ps_rr + 1) % len(g.ps)
    return p


def dbg_out(g, name, src_ap, deps, shape, dt=F32, q="sp"):
    if name not in g.dbg:
        return
    nc, kb = g.nc, g.kb
    o = nc.dram_tensor("dbg_" + name, list(shape), dt, kind="ExternalOutput")
    g.outs[name] = o
    kb.dma(q, lambda: nc.sync.dma_start(out=o.ap(), in_=src_ap), reads=deps)


def phase_a(g):
    nc, kb, H, S = g.nc, g.kb, g.H, g.S
    with ExitStack() as es:
        cc = T(kb, es, [128, 8, 2], F32, "cc")
        kb.dma("sp", lambda: nc.sync.dma_start(out=cc[:, :, 0], in_=H.c[0, :].rearrange("(k p) -> p k", p=128),
                                               allow_slow_non_contiguous=True), writes=[cc.d])
        kb.dma("sp", lambda: nc.sync.dma_start(out=cc[:, :, 1], in_=H.c_ctx.ap().rearrange("(k p) -> p k", p=128),
                                               allow_slow_non_contiguous=True), writes=[cc.d])
        kb.op("act", lambda: nc.scalar.activation(cc[:], cc[:], ACT.Silu), reads=[cc.d], writes=[cc.d])
        ab = T(kb, es, [2, 6 * D], F32, "ab")
        kb.dma("sp", lambda: nc.sync.dma_start(out=ab[0:1, :], in_=H.ada_b[0:1, :]), writes=[ab.d])
        kb.dma("sp", lambda: nc.sync.dma_start(out=ab[1:2, :], in_=H.ada_b[0:1, :]), writes=[ab.d])
        modsb = T(kb, es, [2, 6 * D], F32, "modsb")
        aw = [T(kb, es, [128, 8, 512], F32, f"aw{i}") for i in range(2)]
        for j in range(12):
            a = aw[j % 2]
            kb.dma("sp" if j % 2 == 0 else "act",
                   (lambda a=a, j=j: (nc.sync if j % 2 == 0 else nc.scalar).dma_start(
                       out=a[:], in_=H.ada_w[0, :, j * 512:(j + 1) * 512].rearrange("(k p) n -> p k n", p=128))),
                   writes=[a.d])
            ps = next_ps(g)
            for k in range(8):
                kb.op("pe", lambda k=k: nc.tensor.matmul(ps[0:2, :], lhsT=cc[:, k, :], rhs=a[:, k, :],
                                                         start=(k == 0), stop=(k == 7)),
                      reads=[cc.d, a.d], writes=[ps.d])
            kb.op("dve", lambda j=j: nc.vector.tensor_tensor(modsb[:, j * 512:(j + 1) * 512], ps[0:2, :],
                                                            ab[:, j * 512:(j + 1) * 512], op=ALU.add),
                  reads=[ps.d, ab.d], writes=[modsb.d])
        kb.dma("sp", lambda: nc.sync.dma_start(out=S.mod.ap(), in_=modsb[:]), reads=[modsb.d], writes=[S.mod_d])


def load_fm_vec(g, es, name, src_ap_1d):
    nc, kb = g.nc, g.kb
    t = T(kb, es, [128, 8], F32, name)
    kb.dma("sp", lambda: nc.sync.dma_start(out=t[:], in_=src_ap_1d.rearrange("(k p) -> p k", p=128),
                                           allow_slow_non_contiguous=True), reads=[g.S.mod_d], writes=[t.d])
    return t


def s5_cols(hT, k, s0, n):
    if s0 < NCX:
        assert s0 + n <= NCX
        return hT[:, k, s0:s0 + n]
    c0 = (s0 - NCX) // 64
    ncol = n // 64
    v = hT[:, k, NCX:NT].rearrange("p (row col) -> p col row", col=64)
    return v[:, c0:c0 + ncol, :]


def phase_b(g):
    nc, kb, H, S = g.nc, g.kb, g.H, g.S
    with ExitStack() as es:
        sh1 = load_fm_vec(g, es, "sh1", S.mod[0, 0:D])
        sc1 = load_fm_vec(g, es, "sc1", S.mod[0, D:2 * D])
        csh1 = load_fm_vec(g, es, "csh1", S.mod[1, 0:D])
        csc1 = load_fm_vec(g, es, "csc1", S.mod[1, D:2 * D])
        nmw = load_fm_vec(g, es, "nmw", H.norm_mix_w[0, :])
        g1f = T(kb, es, [128, 8], F32, "g1f")
        cg1f = T(kb, es, [128, 8], F32, "cg1f")
        kb.op("dve", lambda: nc.vector.scalar_tensor_tensor(g1f[:], sc1[:], 1.0, nmw[:], op0=ALU.add, op1=ALU.mult),
              reads=[sc1.d, nmw.d], writes=[g1f.d])
        kb.op("dve", lambda: nc.vector.scalar_tensor_tensor(cg1f[:], csc1[:], 1.0, nmw[:], op0=ALU.add, op1=ALU.mult),
              reads=[csc1.d, nmw.d], writes=[cg1f.d])
        wi = T(kb, es, [128, 8, 2080], BF16, "wi")
        for (a, b) in ((0, 1040), (1040, 2080)):
            kb.dma("pool", lambda a=a, b=b: nc.gpsimd.dma_start(
                out=wi[:, :, a:b], in_=H.w_in[0, :, a:b].rearrange("(k p) n -> p k n", p=128)), writes=[wi.d])
        hT = T(kb, es, [128, 8, NT], BF16, "hT", nd=9)
        xg = [T(kb, es, [128, 4, D], F32, f"xg{i}") for i in range(2)]
        junk = T(kb, es, [128, D], BF16, "junkb")
        groups = [("ctx", 0, 2)] + [("lat", gi, 4) for gi in range(8)]
        for gi, (kind, idx, ntile) in enumerate(groups):
            kb.pump(1)
            xt = xg[gi % 2]
            ntok = ntile * 128
            if kind == "ctx":
                src = H.ctx[0:ntok, :]
                col0 = 0
                gsc, gsh = cg1f, csh1
            else:
                src = H.x[idx * 512:(idx + 1) * 512, :]
                col0 = NCX + idx * 512
                gsc, gsh = g1f, sh1
            q = "sp"
            kb.dma(q, lambda xt=xt, src=src, ntile=ntile, q=q: (nc.sync if q == "sp" else nc.scalar).dma_start(
                out=xt[:, 0:ntile, :], in_=src.rearrange("(j p) d -> p j d", p=128)), writes=[xt.d])
            ss = T(kb, es, [128, 4], F32, f"ss{gi}")
            rs = T(kb, es, [128, 4], F32, f"rs{gi}")
            for j in range(ntile):
                kb.op("act", lambda j=j: nc.scalar.activation(junk[:], xt[:, j, :], ACT.Square,
                                                              accum_out=ss[:, j:j + 1]),
                      reads=[xt.d], writes=[junk.d, ss.d])
            kb.op("dve", lambda: nc.vector.tensor_scalar(rs[:, 0:ntile], ss[:, 0:ntile], 1.0 / D, EPS,
                                                         op0=ALU.mult, op1=ALU.add), reads=[ss.d], writes=[rs.d])
            kb.op("act", lambda: nc.scalar.activation(rs[:, 0:ntile], rs[:, 0:ntile], ACT.Sqrt),
                  reads=[rs.d], writes=[rs.d])
            kb.op("dve", lambda: nc.vector.reciprocal(rs[:, 0:ntile], rs[:, 0:ntile]), reads=[rs.d], writes=[rs.d])
            for j in range(ntile):
                kb.op("dve", lambda j=j: nc.vector.tensor_scalar(xt[:, j, :], xt[:, j, :], rs[:, j:j + 1], None,
                                                                 op0=ALU.mult), reads=[xt.d, rs.d], writes=[xt.d])
            for k in range(8):
                ps = next_ps(g)
                for j in range(ntile):
                    kb.op("pe", lambda j=j, k=k: nc.tensor.transpose(ps[:, j * 128:(j + 1) * 128],
                                                                     xt[:, j, k * 128:(k + 1) * 128], g.ident[:]),
                          reads=[xt.d, g.ident.d], writes=[ps.d])
                kb.op("act", lambda k=k, ps=ps: nc.scalar.activation(
                    hT[:, k, col0:col0 + ntok], ps[:, 0:ntok], ACT.Identity,
                    bias=gsh[:, k:k + 1], scale=gsc[:, k:k + 1]),
                    reads=[ps.d, gsh.d, gsc.d], writes=[hT.ds[gi]])
        hall = hT.ds
        if "hT" in g.dbg:
            dbg_out(g, "hT", hT[:], hall, [128, 8, NT], BF16)
        stg = [T(kb, es, [128, NT], BF16, f"stg{i}") for i in range(2)]
        stgf = T(kb, es, [16, NT], F32, "stgf")
        rr = [0]

        def fm_proj(c0, m, dst_fn, t0, t1, cols_fn, f32=False):
            kb.pump(1)
            st = stgf if f32 else stg[rr[0] % 2]
            rr[0] += 0 if f32 else 1
            t = t0
            while t < t1:
                n = min(512, t1 - t)
                if t < NCX:
                    n = min(n, NCX - t)
                ps = next_ps(g)
                for k in range(8):
                    kb.op("pe", lambda k=k, t=t, n=n: nc.tensor.matmul(
                        ps[0:m, 0:n], lhsT=wi[:, k, c0:c0 + m], rhs=cols_fn(k, t, n),
                        start=(k == 0), stop=(k == 7)), reads=[wi.d] + hall, writes=[ps.d])
                eng = "act" if (t // 512) % 2 == 0 else "dve"
                if eng == "act":
                    kb.op("act", lambda t=t, n=n, ps=ps: nc.scalar.copy(st[0:m, t:t + n], ps[0:m, 0:n]),
                          reads=[ps.d], writes=[st.d])
                else:
                    kb.op("dve", lambda t=t, n=n, ps=ps: nc.vector.tensor_copy(st[0:m, t:t + n], ps[0:m, 0:n]),
                          reads=[ps.d], writes=[st.d])
                t += n
            dst, dd = dst_fn()
            kb.dma("sp", lambda: nc.sync.dma_start(out=dst, in_=st[0:m, t0:t1]), reads=[st.d], writes=[dd])

        raster = lambda k, t, n: hT[:, k, t:t + n]
        s5t = lambda k, t, n: s5_cols(hT, k, t, n)
        for mt in range(2):
            fm_proj(mt * 128, 128, lambda mt=mt: (S.qT[mt * 128:(mt + 1) * 128, :], S.qT_d), 0, NT, raster)
        for mt in range(2):
            fm_proj(256 + mt * 128, 128, lambda mt=mt: (S.kT[mt * 128:(mt + 1) * 128, :], S.kT_d), 0, NT, raster)
        for mt in range(4):
            fm_proj(1024 + mt * 128, 128, lambda mt=mt: (S.rT[mt * 128:(mt + 1) * 128, :], S.rT_d), NCX, NT, raster)
        for z in range(2):
            fm_proj(1536 + z * 16, 16, lambda z=z: (S.lrT[z, :, :], S.lrT_d), 0, NT, raster, f32=True)
        for mt in range(4):
            fm_proj(1568 + mt * 128, 128, lambda mt=mt: (S.uT[mt * 128:(mt + 1) * 128, :], S.uT_d), 0, NT, s5t)
        st4 = [T(kb, es, [128, 4, 512], BF16, f"st4_{i}") for i in range(2)]
        ngrp = 0
        for (c0, dst, dd, s5) in ((512, S.v, S.v_d, False), (1568, S.u, S.u_d, True)):
            for t0 in list(range(0, NCX, 512)) + list(range(NCX, NT, 512)):
                nt = 2 if t0 < NCX else 4
                st = st4[ngrp % 2]
                ngrp += 1
                for j in range(nt):
                    ps = next_ps(g)
                    tt = t0 + j * 128
                    if s5 and tt >= NCX:
                        for hf in range(2):
                            col = (tt - NCX) // 64 + hf
                            for k in range(8):
                                lh = hT[:, k, NCX + col:NT:64]
                                kb.op("pe", lambda k=k, lh=lh, ps=ps, hf=hf: nc.tensor.matmul(
                                    ps[hf * 64:(hf + 1) * 64, :], lhsT=lh, rhs=wi[:, k, c0:c0 + 512],
                                    start=(k == 0), stop=(k == 7)), reads=[wi.d] + hall, writes=[ps.d])
                    else:
                        for k in range(8):
                            lh = hT[:, k, tt:tt + 128]
                            kb.op("pe", lambda k=k, lh=lh, ps=ps: nc.tensor.matmul(
                                ps[:, :], lhsT=lh, rhs=wi[:, k, c0:c0 + 512], start=(k == 0), stop=(k == 7)),
                                reads=[wi.d] + hall, writes=[ps.d])
                    if j % 2 == 0:
                        kb.op("act", lambda j=j, ps=ps, st=st: nc.scalar.copy(st[:, j, :], ps[:, :]),
                              reads=[ps.d], writes=[st.d])
                    else:
                        kb.op("dve", lambda j=j, ps=ps, st=st: nc.vector.tensor_copy(st[:, j, :], ps[:, :]),
                              reads=[ps.d], writes=[st.d])
                kb.dma("sp", lambda st=st, t0=t0, nt=nt, dst=dst: nc.sync.dma_start(
                    out=dst[t0:t0 + nt * 128, :].rearrange("(j p) e -> p j e", p=128), in_=st[:, 0:nt, :]),
                    reads=[st.d], writes=[dd])


def phase_c(g):
    nc, kb, H, S = g.nc, g.kb, g.H, g.S
    NCH = NT // 64
    with ExitStack() as es:
        psb = g.psb[0]
        lup = T(kb, es, [16, 2, 256], F32, "lup")
        kb.dma("sp", lambda: nc.sync.dma_start(out=lup[:], in_=H.gla_lr_up[0].rearrange("z r f -> r z f")),
               writes=[lup.d])
        nb = T(kb, es, [128, 2, 2], F32, "nbias")
        kb.dma("sp", lambda: nc.sync.dma_start(out=nb[:], in_=H.gla_lr_bias[0].rearrange("z (t p) -> p z t", p=128),
                                               allow_slow_non_contiguous=True), writes=[nb.d])
        kb.op("dve", lambda: nc.vector.tensor_scalar(nb[:], nb[:], -1.0, None, op0=ALU.mult), reads=[nb.d], writes=[nb.d])
        nw = T(kb, es, [128, 1], F32, "gnw")
        kb.dma("sp", lambda: nc.sync.dma_start(out=nw[:], in_=H.gla_norm_w[0, :].rearrange("(p o) -> p o", o=1)),
               writes=[nw.d])
        ones = T(kb, es, [128, 128], BF16, "onesb")
        kb.op("pool", lambda: nc.gpsimd.memset(ones[:], 1.0), writes=[ones.d])
        io = g.iota
        mk = []
        for z in range(2):
            m4 = T(kb, es, [128, 4, 64], BF16, f"tri{z}")
            op = ALU.is_ge if z == 0 else ALU.is_le
            for h in range(4):
                kb.op("dve", lambda h=h, m4=m4, op=op: nc.vector.tensor_single_scalar(
                    m4[0:64, h, :], io[0:64, 0:64], 0.0, op=op), reads=[io.d], writes=[m4.d])
                kb.op("dve", lambda h=h, m4=m4, op=op: nc.vector.tensor_single_scalar(
                    m4[64:128, h, :], io[64:128, 0:64], -64.0, op=op), reads=[io.d], writes=[m4.d])
            mk.append(m4)
        qt = [T(kb, es, [128, 2, NT], BF16, f"qtl{z}") for z in range(2)]
        kt = [T(kb, es, [128, 2, NT], BF16, f"ktl{z}") for z in range(2)]
        elast = [T(kb, es, [128, 2, NCH], F32, f"elast{z}") for z in range(2)]
        if STOP == 'c0':
            return
        with ExitStack() as es2:
            mask = T(kb, es2, [128, NT + 1], BF16, "cmask")
            kb.op("pool", lambda: nc.gpsimd.memset(mask[:], 1.0), writes=[mask.d])
            kb.op("pool", lambda: nc.gpsimd.memset(mask[:, 0:NT + 1:64], 0.0), writes=[mask.d])
            lrtz = T(kb, es2, [16, NT], F32, "lrt")
            A = T(kb, es2, [128, NT], F32, "scrA")
            B = T(kb, es2, [128, NT], F32, "scrB")
            raw = [T(kb, es2, [128, NT], BF16, f"raw{i}") for i in range(2)]
            for z in range(2):
                kb.dma("sp", lambda z=z: nc.sync.dma_start(out=lrtz[:], in_=S.lrT[z, :, :]),
                       reads=[S.lrT_d], writes=[lrtz.d])
                for pt in range(2):
                    if STOP == 'c1a' and (z, pt) != (0, 0):
                        continue
                    for t0 in range(0, NT, 512):
                        n = min(512, NT - t0)
                        ps = next_ps(g)
                        kb.op("pe", lambda t0=t0, n=n, ps=ps: nc.tensor.matmul(
                            ps[:, 0:n], lhsT=lup[:, z, pt * 128:(pt + 1) * 128], rhs=lrtz[:, t0:t0 + n],
                            start=True, stop=True), reads=[lup.d, lrtz.d], writes=[ps.d])
                        kb.op("act", lambda t0=t0, n=n, ps=ps: nc.scalar.activation(
                            A[:, t0:t0 + n], ps[:, 0:n], ACT.Exp, bias=nb[:, z, pt:pt + 1], scale=-1.0),
                            reads=[ps.d, nb.d], writes=[A.d])
                    ckpt("k1")
                    kb.op("act", lambda: nc.scalar.activation(A[:], A[:], ACT.Ln, bias=1.0, scale=1.0),
                          reads=[A.d], writes=[A.d])
                    ckpt("k2")
                    if z == 0:
                        kb.op("dve", lambda: nc.vector.tensor_tensor_scan(B[:], mask[:, 0:NT], A[:], 0.0,
                                                                          ALU.mult, ALU.add),
                              reads=[mask.d, A.d], writes=[B.d])
                    else:
                        kb.op("dve", lambda: nc.vector.tensor_tensor_scan(B[:, NT - 1::-1] if False else B[:, ::-1],
                                                                          mask[:, NT:0:-1], A[:, ::-1], 0.0,
                                                                          ALU.mult, ALU.add),
                              reads=[mask.d, A.d], writes=[B.d])
                    ckpt("k3")
                    kb.op("act", lambda: nc.scalar.activation(A[:], B[:], ACT.Exp, scale=-1.0 / 16.0),
                          reads=[B.d], writes=[A.d])
                    kb.op("act", lambda: nc.scalar.activation(B[:], B[:], ACT.Exp, scale=1.0 / 16.0),
                          reads=[B.d], writes=[B.d])
                    ckpt("k4")
                    rq, rk = raw
                    kb.dma("sp", lambda: nc.sync.dma_start(out=rq[:], in_=S.qT[pt * 128:(pt + 1) * 128, :]),
                           reads=[S.qT_d], writes=[rq.d])
                    kb.dma("sp", lambda: nc.sync.dma_start(out=rk[:], in_=S.kT[pt * 128:(pt + 1) * 128, :]),
                           reads=[S.kT_d], writes=[rk.d])
                    ckpt("k5")
                    kb.op("dve", lambda: nc.vector.scalar_tensor_tensor(qt[z][:, pt, :], rq[:], 0.125, A[:],
                                                                        op0=ALU.mult, op1=ALU.mult),
                          reads=[rq.d, A.d], writes=[qt[z].d])
                    ckpt("k6")
                    kb.op("pool", lambda: nc.gpsimd.tensor_tensor(kt[z][:, pt, :], rk[:], B[:], op=ALU.mult),
                          reads=[rk.d, B.d], writes=[kt[z].d])
                    ckpt("k7")
                    e0 = 63 if z == 0 else 0
                    kb.op("dve", lambda: nc.vector.tensor_copy(elast[z][:, pt, :], A[:, e0:NT:64]),
                          reads=[A.d], writes=[elast[z].d])
        kb.barrier()
        if STOP == 'c1':
            return
        vt = T(kb, es, [128, NT // 128, 512], BF16, "vt")
        kb.dma("act", lambda: nc.scalar.dma_start(out=vt[:], in_=S.v.ap().rearrange("(n p) e -> p n e", p=128)),
               reads=[S.v_d], writes=[vt.d])
        ktm = [T(kb, es, [128, NT // 128, 256], BF16, f"ktm{z}") for z in range(2)]
        oT = T(kb, es, [128, 4, NL], BF16, "oT", nd=NL // 64)
        for z in range(2):
            for n2 in range(0, NT // 128, 2):
                for a in range(2):
                    for pt in range(2):
                        kb.op("pe", lambda a=a, pt=pt, n2=n2: nc.tensor.transpose(
                            psb[:, (a * 2 + pt) * 128:(a * 2 + pt + 1) * 128],
                            kt[z][:, pt, (n2 + a) * 128:(n2 + a + 1) * 128], g.identb[:]),
                            reads=[kt[z].d, g.identb.d], writes=[psb.d])
                kb.op("act", lambda n2=n2: nc.scalar.copy(
                    ktm[z][:, n2:n2 + 2, :], psb[:, 0:512].rearrange("p (a f) -> p a f", a=2)),
                    reads=[psb.d], writes=[ktm[z].d])
        if "qtl" in g.dbg:
            dbg_out(g, "qtl0", qt[0][:], [qt[0].d], [128, 2, NT], BF16)
            dbg_out(g, "ktl1", kt[1][:], [kt[1].d], [128, 2, NT], BF16)
            dbg_out(g, "ktm1", ktm[1][:], [ktm[1].d], [128, NT // 128, 256], BF16)
            dbg_out(g, "elast1", elast[1][:], [elast[1].d], [128, 2, NCH])
        if STOP == 'c2':
            return
        Sst = [T(kb, es, [128, 2, 128], F32, f"Sst{z}") for z in range(2)]
        SbfZ = [T(kb, es, [128, 4, 128], BF16, f"SbfZ{z}") for z in range(2)]
        tmpS = [T(kb, es, [128, 2, 128], F32, f"tmpS{z}") for z in range(2)]
        smZ = [[T(kb, es, [128, 4, 64], BF16, f"smZ{z}_{i}") for i in range(2)] for z in range(2)]
        for z in range(2):
            kb.op("pool", lambda z=z: nc.gpsimd.memset(Sst[z][:], 0.0), writes=[Sst[z].d])
            kb.op("pool", lambda z=z: nc.gpsimd.memset(SbfZ[z][:], 0.0), writes=[SbfZ[z].d])
            for i in range(2):
                kb.op("pool", lambda z=z, i=i: nc.gpsimd.memset(smZ[z][i][:], 0.0), writes=[smZ[z][i].d])
        order = [list(range(NCH)), [3, 2, 1, 0] + list(range(NCH - 1, 3, -1))]
        written = set()
        for i in range(NCH):
            if i % 4 == 0:
                kb.pump(1)
            for z in range(2):
                n = order[z][i]
                t0 = 64 * n
                nt = n // 2
                jo = (n % 2) * 64
                if n >= 4:
                    smt = smZ[z][n % 2]
                    for par in range(2):
                        ho = par * 64
                        ps_s = next_ps(g)
                        for hh in range(2):
                            h = hh * 2 + par
                            pt = hh
                            kb.op("pe", lambda h=h, pt=pt, ho=ho, ps_s=ps_s, hh=hh: nc.tensor.matmul(
                                ps_s[jo:jo + 64, hh * 64:(hh + 1) * 64], lhsT=kt[z][ho:ho + 64, pt, t0:t0 + 64],
                                rhs=qt[z][ho:ho + 64, pt, t0:t0 + 64], start=True, stop=True),
                                reads=[kt[z].d, qt[z].d], writes=[ps_s.d])
                        kb.op("dve", lambda ps_s=ps_s, smt=smt, par=par: nc.vector.tensor_tensor(
                            smt[jo:jo + 64, par:4:2, :], ps_s[jo:jo + 64, 0:128].rearrange("p (h i) -> p h i", h=2),
                            mk[z][jo:jo + 64, 0:2, :], op=ALU.mult), reads=[ps_s.d, mk[z].d], writes=[smt.d])
                    ps_o = next_ps(g)
                    for h in range(4):
                        pt = h // 2
                        kb.op("pe", lambda h=h, ps_o=ps_o, smt=smt: nc.tensor.matmul(
                            ps_o[:, h * 64:(h + 1) * 64], lhsT=vt[:, nt, h * 128:(h + 1) * 128],
                            rhs=smt[:, h, :], start=True, stop=False),
                            reads=[vt.d, smt.d], writes=[ps_o.d])
                        kb.op("pe", lambda h=h, pt=pt, ps_o=ps_o: nc.tensor.matmul(
                            ps_o[:, h * 64:(h + 1) * 64], lhsT=SbfZ[z][:, h, :],
                            rhs=qt[z][:, pt, t0:t0 + 64], start=False, stop=True),
                            reads=[SbfZ[z].d, qt[z].d], writes=[ps_o.d])
                    tl = t0 - NCX
                    od = oT.ds[tl // 64]
                    osl = oT[:, :, tl:tl + 64]
                    pv = ps_o[:, 0:256].rearrange("p (h i) -> p h i", h=4)
                    if n not in written:
                        written.add(n)
                        kb.op("act", lambda osl=osl, pv=pv: nc.scalar.copy(osl, pv), reads=[ps_o.d], writes=[od])
                    else:
                        kb.op("dve", lambda osl=osl, pv=pv: nc.vector.tensor_tensor(osl, pv, osl, op=ALU.add),
                              reads=[ps_o.d, od], writes=[od])
                ps_kv = next_ps(g)
                for h in range(4):
                    pt, ho = h // 2, (h % 2) * 64
                    kb.op("pe", lambda h=h, pt=pt, ho=ho, ps_kv=ps_kv: nc.tensor.matmul(
                        ps_kv[ho:ho + 64, pt * 128:(pt + 1) * 128], lhsT=ktm[z][jo:jo + 64, nt, h * 64:(h + 1) * 64],
                        rhs=vt[jo:jo + 64, nt, h * 128:(h + 1) * 128], start=True, stop=True),
                        reads=[ktm[z].d, vt.d], writes=[ps_kv.d])
                kb.op("dve", lambda ps_kv=ps_kv: nc.vector.tensor_tensor(
                    tmpS[z][:], ps_kv[:, 0:256].rearrange("p (t e) -> p t e", t=2), Sst[z][:], op=ALU.add),
                    reads=[ps_kv.d, Sst[z].d], writes=[tmpS[z].d])
                kb.op("dve", lambda n=n: nc.vector.tensor_tensor(
                    Sst[z][:], tmpS[z][:], elast[z][:, :, n:n + 1].to_broadcast([128, 2, 128]), op=ALU.mult),
                    reads=[tmpS[z].d, elast[z].d], writes=[Sst[z].d])
                for par in range(2):
                    ho = par * 64
                    kb.op("act", lambda par=par, ho=ho: nc.scalar.copy(SbfZ[z][ho:ho + 64, par:4:2, :],
                                                                       Sst[z][ho:ho + 64, :, :]),
                          reads=[Sst[z].d], writes=[SbfZ[z].d])
        if "oT" in g.dbg:
            dbg_out(g, "oT", oT[:], oT.ds, [128, 4, NL], BF16)
        if STOP == 'c3':
            return
        sq = T(kb, es, [128, 512], BF16, "gsq")
        rstd = T(kb, es, [128, 512], F32, "grstd")
        rt = [T(kb, es, [128, 4, 512], BF16, f"grt{i}") for i in range(2)]
        gl = [T(kb, es, [128, 4, 512], BF16, f"ggl{i}") for i in range(1)]
        tmpb = T(kb, es, [128, 512], BF16, "gtmp")
        for sp in range(NL // 512):
            c0 = sp * 512
            r_t, g_t = rt[sp % 2], gl[0]
            kb.dma("sp", lambda r_t=r_t, c0=c0: nc.sync.dma_start(
                out=r_t[:], in_=S.rT[:, c0:c0 + 512].rearrange("(m p) t -> p m t", p=128)),
                reads=[S.rT_d], writes=[r_t.d])
            kb.op("act", lambda r_t=r_t: nc.scalar.activation(r_t[:], r_t[:], ACT.Silu), reads=[r_t.d], writes=[r_t.d])
            ods = oT.ds[c0 // 64:(c0 + 512) // 64]
            for h in range(4):
                kb.op("dve", lambda h=h: nc.vector.tensor_tensor(sq[:], oT[:, h, c0:c0 + 512], oT[:, h, c0:c0 + 512],
                                                                 op=ALU.mult), reads=ods, writes=[sq.d])
                ps = next_ps(g)
                kb.op("pe", lambda ps=ps: nc.tensor.matmul(ps[:, :], lhsT=ones[:], rhs=sq[:], start=True, stop=True),
                      reads=[ones.d, sq.d], writes=[ps.d])
                kb.op("dve", lambda ps=ps: nc.vector.tensor_scalar(rstd[:], ps[:, :], 1.0 / 128.0, EPS,
                                                                   op0=ALU.mult, op1=ALU.add),
                      reads=[ps.d], writes=[rstd.d])
                kb.op("act", lambda: nc.scalar.activation(rstd[:], rstd[:], ACT.Sqrt), reads=[rstd.d], writes=[rstd.d])
                kb.op("dve", lambda: nc.vector.reciprocal(rstd[:], rstd[:]), reads=[rstd.d], writes=[rstd.d])
                kb.op("dve", lambda h=h: nc.vector.scalar_tensor_tensor(
                    tmpb[:], oT[:, h, c0:c0 + 512], nw[:, 0:1], rstd[:], op0=ALU.mult, op1=ALU.mult),
                    reads=ods + [nw.d, rstd.d], writes=[tmpb.d])
                kb.op("pool", lambda h=h, g_t=g_t, r_t=r_t: nc.gpsimd.tensor_tensor(
                    g_t[:, h, :], tmpb[:], r_t[:, h, :], op=ALU.mult), reads=[tmpb.d, r_t.d], writes=[g_t.d])
            kb.dma("act", lambda g_t=g_t, c0=c0: nc.scalar.dma_start(
                out=S.glaT[:, c0:c0 + 512].rearrange("(m p) t -> p m t", p=128), in_=g_t[:]),
                reads=[g_t.d], writes=[S.glaT_d])


def cmul(g, out_r, out_i, ar, ai, br, bi, tmp, deps_in, dep_out, sl=None):
    nc, kb = g.nc, g.kb
    kb.op("dve", lambda: nc.vector.tensor_tensor(tmp, ai, bi, op=ALU.mult), reads=deps_in, writes=[dep_out])
    kb.op("dve", lambda: nc.vector.tensor_tensor(out_r, ar, br, op=ALU.mult), reads=deps_in, writes=[dep_out])
    kb.op("dve", lambda: nc.vector.tensor_tensor(out_r, out_r, tmp, op=ALU.subtract), reads=[dep_out], writes=[dep_out])
    kb.op("dve", lambda: nc.vector.tensor_tensor(tmp, ai, br, op=ALU.mult), reads=deps_in, writes=[dep_out])
    kb.op("dve", lambda: nc.vector.tensor_tensor(out_i, ar, bi, op=ALU.mult), reads=deps_in, writes=[dep_out])
    kb.op("dve", lambda: nc.vector.tensor_tensor(out_i, out_i, tmp, op=ALU.add), reads=[dep_out], writes=[dep_out])


def phase_d(g):
    nc, kb, H, S = g.nc, g.kb, g.H, g.S
    NCK = NT // 8
    NMAC = NCK // 16
    io = g.iota
    with ExitStack() as es:
        psb = g.psb[0]
        pd = Dep("s5par")
        P0 = T(kb, es, [128, 24, 64], F32, "s5p0")
        pl = lambda i: P0[:, i, :]
        LRE, LIM, DT, MAG, CS, SN, T1, T2, T3, LBR, LBI, CR, CI, IR, II = range(15)
        for half in range(2):
            rows = slice(half * 64, half * 64 + 64)
            kb.dma("sp", lambda rows=rows: nc.sync.dma_start(
                out=P0[rows, LRE, :], in_=H.s5_lam_re[0].rearrange("z g p -> p (z g)"),
                allow_slow_non_contiguous=True), writes=[pd])
            kb.dma("act", lambda rows=rows: nc.scalar.dma_start(
                out=P0[rows, LIM, :], in_=H.s5_lam_im[0].rearrange("z g p -> p (z g)"),
                allow_slow_non_contiguous=True), writes=[pd])
        kb.dma("sp", lambda: nc.sync.dma_start(
            out=pl(DT), in_=H.s5_log_dt[0:1, :, :].rearrange("o z g -> o (z g)").partition_broadcast(128)),
            writes=[pd])
        D1 = [pd]

        def v(fn):
            kb.op("dve", fn, reads=D1, writes=D1)

        def a(fn):
            kb.op("act", fn, reads=D1, writes=D1)

        a(lambda: nc.scalar.activation(pl(DT), pl(DT), ACT.Exp))
        v(lambda: nc.vector.tensor_tensor(pl(MAG), pl(LRE), pl(DT), op=ALU.mult))
        a(lambda: nc.scalar.activation(pl(MAG), pl(MAG), ACT.Exp))
        v(lambda: nc.vector.tensor_tensor(pl(T1), pl(LIM), pl(DT), op=ALU.mult))
        a(lambda: nc.scalar.activation(pl(SN), pl(T1), ACT.Sin, scale=1.0 / 16.0))
        a(lambda: nc.scalar.activation(pl(CS), pl(T1), ACT.Sin, bias=float(np.pi / 2), scale=1.0 / 16.0))
        for _ in range(4):
            v(lambda: nc.vector.tensor_tensor(pl(T2), pl(CS), pl(CS), op=ALU.mult))
            v(lambda: nc.vector.tensor_tensor(pl(T3), pl(SN), pl(SN), op=ALU.mult))
            v(lambda: nc.vector.scalar_tensor_tensor(pl(SN), pl(CS), 2.0, pl(SN), op0=ALU.mult, op1=ALU.mult))
            v(lambda: nc.vector.tensor_tensor(pl(CS), pl(T2), pl(T3), op=ALU.subtract))
        v(lambda: nc.vector.tensor_tensor(pl(LBR), pl(MAG), pl(CS), op=ALU.mult))
        v(lambda: nc.vector.tensor_tensor(pl(LBI), pl(MAG), pl(SN), op=ALU.mult))
        v(lambda: nc.vector.tensor_tensor(pl(T1), pl(LRE), pl(LRE), op=ALU.mult))
        v(lambda: nc.vector.tensor_tensor(pl(T2), pl(LIM), pl(LIM), op=ALU.mult))
        v(lambda: nc.vector.tensor_tensor(pl(T1), pl(T1), pl(T2), op=ALU.add))
        v(lambda: nc.vector.reciprocal(pl(T1), pl(T1)))
        v(lambda: nc.vector.tensor_scalar(pl(T2), pl(LBR), -1.0, None, op0=ALU.add))
        v(lambda: nc.vector.tensor_tensor(pl(CR), pl(T2), pl(LRE), op=ALU.mult))
        v(lambda: nc.vector.tensor_tensor(pl(T3), pl(LBI), pl(LIM), op=ALU.mult))
        v(lambda: nc.vector.tensor_tensor(pl(CR), pl(CR), pl(T3), op=ALU.add))
        v(lambda: nc.vector.tensor_tensor(pl(CR), pl(CR), pl(T1), op=ALU.mult))
        v(lambda: nc.vector.tensor_tensor(pl(CI), pl(LBI), pl(LRE), op=ALU.mult))
        v(lambda: nc.vector.tensor_tensor(pl(T3), pl(T2), pl(LIM), op=ALU.mult))
        v(lambda: nc.vector.tensor_tensor(pl(CI), pl(CI), pl(T3), op=ALU.subtract))
        v(lambda: nc.vector.tensor_tensor(pl(CI), pl(CI), pl(T1), op=ALU.mult))
        v(lambda: nc.vector.tensor_tensor(pl(T1), pl(LBR), pl(LBR), op=ALU.mult))
        v(lambda: nc.vector.tensor_tensor(pl(T2), pl(LBI), pl(LBI), op=ALU.mult))
        v(lambda: nc.vector.tensor_tensor(pl(T1), pl(T1), pl(T2), op=ALU.add))
        v(lambda: nc.vector.reciprocal(pl(T1), pl(T1)))
        v(lambda: nc.vector.tensor_tensor(pl(IR), pl(LBR), pl(T1), op=ALU.mult))
        v(lambda: nc.vector.scalar_tensor_tensor(pl(II), pl(LBI), -1.0, pl(T1), op0=ALU.mult, op1=ALU.mult))
        PW = T(kb, es, [128, 9, 2, 64], F32, "s5pw")
        NW = T(kb, es, [128, 8, 2, 64], F32, "s5nw")
        for W, br_, bi_, n in ((PW, LBR, LBI, 9), (NW, IR, II, 8)):
            v(lambda W=W: nc.vector.memset(W[:, 0, 0, :], 1.0))
            v(lambda W=W: nc.vector.memset(W[:, 0, 1, :], 0.0))
            for k in range(1, n):
                cmul(g, W[:, k, 0, :], W[:, k, 1, :], W[:, k - 1, 0, :], W[:, k - 1, 1, :], pl(br_), pl(bi_),
                     pl(T3), D1, pd)
        P128 = T(kb, es, [128, 2, 64], F32, "s5p128")
        v(lambda: nc.vector.tensor_copy(P128[:], PW[:, 8, :, :]))
        for _ in range(4):
            cmul(g, pl(T1), pl(T2), P128[:, 0, :], P128[:, 1, :], P128[:, 0, :], P128[:, 1, :], pl(T3), D1, pd)
            v(lambda: nc.vector.tensor_copy(P128[:, 0, :], pl(T1)))
            v(lambda: nc.vector.tensor_copy(P128[:, 1, :], pl(T2)))
        WN = T(kb, es, [128, 8, 2, 64], F32, "s5wn")
        WP = T(kb, es, [128, 8, 2, 64], F32, "s5wp")
        for k in range(8):
            cmul(g, WN[:, k, 0, :], WN[:, k, 1, :], NW[:, k, 0, :], NW[:, k, 1, :], pl(CR), pl(CI), pl(T3), D1, pd)
            cmul(g, WP[:, k, 0, :], WP[:, k, 1, :], PW[:, k, 0, :], PW[:, k, 1, :], pl(CR), pl(CI), pl(T3), D1, pd)
        v(lambda: nc.vector.tensor_scalar(PW[64:128, :, 0, :], PW[64:128, :, 0, :], -1.0, None, op0=ALU.mult))
        v(lambda: nc.vector.tensor_scalar(WN[0:64, :, 1, :], WN[0:64, :, 1, :], -1.0, None, op0=ALU.mult))
        v(lambda: nc.vector.tensor_scalar(WP[0:64, :, 1, :], WP[0:64, :, 1, :], -1.0, None, op0=ALU.mult))
        A8s = T(kb, es, [128, 2, 64], F32, "s5a8")
        v(lambda: nc.vector.tensor_copy(A8s[:, 1, :], PW[:, 8, 1, :]))
        v(lambda: nc.vector.tensor_copy(A8s[0:64, 0, :], PW[0:64, 8, 0, :]))
        v(lambda: nc.vector.tensor_scalar(A8s[64:128, 0, :], PW[64:128, 8, 0, :], -1.0, None, op0=ALU.mult))
        Jt = T(kb, es, [128, 128], F32, "s5jt")
        v(lambda: nc.vector.tensor_single_scalar(Jt[:], io[:], 64.0, op=ALU.is_equal))
        v(lambda: nc.vector.tensor_single_scalar(pl(T1)[:, 0:64], io[:, 0:64], -64.0, op=ALU.is_equal))
        v(lambda: nc.vector.tensor_tensor(Jt[:, 0:64], Jt[:, 0:64], pl(T1)[:, 0:64], op=ALU.subtract))
        bm = []
        for z in range(2):
            m = T(kb, es, [128, 128], F32, f"s5bm{z}")
            v(lambda m=m: nc.vector.memset(m[:], 0.0))
            for j in range(0, 8, 2):
                for jj in range(2):
                    pass
            bm.append(m)
        rowb = T(kb, es, [128, 1], F32, "s5rowb")
        pcol = T(kb, es, [128, 1], F32, "s5pcol")
        kb.op("pool", lambda: nc.gpsimd.iota(pcol[:], pattern=[[0, 1]], base=0, channel_multiplier=1,
                                              allow_small_or_imprecise_dtypes=True), writes=[pd])
        v(lambda: nc.vector.memset(rowb[:], 0.0))
        for t in range(1, 8):
            v(lambda t=t: nc.vector.tensor_scalar(pl(T1)[:, 0:1], pcol[:], float(16 * t), 16.0, op0=ALU.is_ge, op1=ALU.mult))
            v(lambda: nc.vector.tensor_tensor(rowb[:], rowb[:], pl(T1)[:, 0:1], op=ALU.add))
        colf = T(kb, es, [128, 128], F32, "s5colf")
        kb.op("pool", lambda: nc.gpsimd.iota(colf[:], pattern=[[1, 128]], base=0, channel_multiplier=0,
                                              allow_small_or_imprecise_dtypes=True), writes=[pd])
        v(lambda: nc.vector.tensor_scalar(bm[0][:], colf[:], rowb[:, 0:1], 0.0, op0=ALU.subtract, op1=ALU.is_ge))
        v(lambda: nc.vector.tensor_scalar(bm[1][:], colf[:], rowb[:, 0:1], 15.0, op0=ALU.subtract, op1=ALU.is_le))
        CC = T(kb, es, [128, 64, 16], F32, "s5cc")
        CCs = T(kb, es, [128, 64, 16], F32, "s5ccs")
        BB = T(kb, es, [128, 64, 16], F32, "s5bb")
        BBs = T(kb, es, [128, 64, 16], F32, "s5bbs")
        kb.dma("sp", lambda: nc.sync.dma_start(out=BB[0:64, :, :], in_=H.s5_b_re[0].rearrange("z g p h -> p (z g) h")),
               writes=[pd])
        kb.dma("act", lambda: nc.scalar.dma_start(out=BB[64:128, :, :], in_=H.s5_b_im[0].rearrange("z g p h -> p (z g) h")),
               writes=[pd])
        kb.dma("sp", lambda: nc.sync.dma_start(out=BBs[0:64, :, :], in_=H.s5_b_im[0].rearrange("z g p h -> p (z g) h")),
               writes=[pd])
        kb.dma("act", lambda: nc.scalar.dma_start(out=BBs[64:128, :, :], in_=H.s5_b_re[0].rearrange("z g p h -> p (z g) h")),
               writes=[pd])
        with ExitStack() as esx:
            xc = [T(kb, esx, [128, 8, 128], F32, f"s5xc{i}") for i in range(2)]
            for i, (aa, bb) in enumerate(((H.s5_c_re, H.s5_c_im), (H.s5_c_im, H.s5_c_re))):
                kb.dma("sp", lambda aa=aa, i=i: nc.sync.dma_start(
                    out=xc[i][:, :, 0:64], in_=aa[0].rearrange("z g h p -> (z g h) p").rearrange("(t r) p -> r t p", r=128)),
                    writes=[pd])
                kb.dma("act", lambda bb=bb, i=i: nc.scalar.dma_start(
                    out=xc[i][:, :, 64:128], in_=bb[0].rearrange("z g h p -> (z g h) p").rearrange("(t r) p -> r t p", r=128)),
                    writes=[pd])
            for i, dst in enumerate((CC, CCs)):
                for t in range(8):
                    ps = next_ps(g)
                    kb.op("pe", lambda t=t, i=i, ps=ps: nc.tensor.transpose(ps[:, 0:128], xc[i][:, t, :], g.ident[:]),
                          reads=[pd, g.ident.d], writes=[ps.d])
                    kb.op("act", lambda t=t, dst=dst, ps=ps: nc.scalar.copy(
                        dst[:, t * 8:(t + 1) * 8, :], ps[:, 0:128].rearrange("p (a h) -> p a h", a=8)),
                        reads=[ps.d], writes=[pd])
            kb.barrier()
        ckpt("d0")
        with ExitStack() as esu:
            U8 = [T(kb, esu, [128, 8, 512], BF16, f"s5u8_{i}") for i in range(2)]
            U8g = [T(kb, esu, [128, 32, 128], BF16, f"s5u8g_{i}") for i in range(2)]
            utst = [T(kb, esu, [128, 4, 128], BF16, f"s5utst_{i}") for i in range(2)]
            blocks = [(0, 32)] + [(32 + 128 * b, 128) for b in range(4)]
            for bi_, (c0, ncb) in enumerate(blocks):
                kb.pump(1)
                u8, u8g = U8[bi_ % 2], U8g[bi_ % 2]
                kb.dma("sp", lambda u8=u8, c0=c0, ncb=ncb: nc.sync.dma_start(
                    out=u8[0:ncb, :, :], in_=S.u[c0 * 8:(c0 + ncb) * 8, :].rearrange("(c j) f -> c j f", j=8)),
                    reads=[S.u_d], writes=[u8.d])
                kb.op("pool", lambda u8=u8, u8g=u8g, ncb=ncb: nc.gpsimd.tensor_copy(
                    u8g[0:ncb, :, :].rearrange("c g (j h) -> c g j h", j=8),
                    u8[0:ncb, :, :].rearrange("c j (g h) -> c g j h", g=32)), reads=[u8.d], writes=[u8g.d])
                for g4 in range(0, 32, 4):
                    for gg in range(4):
                        kb.op("pe", lambda gg=gg, g4=g4, u8g=u8g, ncb=ncb: nc.tensor.transpose(
                            psb[:, gg * 128:gg * 128 + ncb], u8g[0:ncb, g4 + gg, :], g.identb[0:ncb, 0:ncb]),
                            reads=[u8g.d, g.identb.d], writes=[psb.d])
                    ust = utst[(g4 // 4) % 2]
                    kb.op("act", lambda g4=g4, c0=c0, ncb=ncb, ust=ust: nc.scalar.copy(
                        ust[:, :, 0:ncb], psb[:, 0:512].rearrange("p (a c) -> p a c", a=4)[:, :, 0:ncb]),
                        reads=[psb.d], writes=[ust.d])
                    kb.dma("act", lambda g4=g4, c0=c0, ncb=ncb, ust=ust: nc.scalar.dma_start(
                        out=S.Ut[g4:g4 + 4, :, c0:c0 + ncb].rearrange("a p c -> p a c"), in_=ust[:, :, 0:ncb]),
                        reads=[ust.d], writes=[S.Ut_d])
            kb.barrier()
        ckpt("d1")
        for z in range(2):
            gs = slice(z * 32, z * 32 + 32)
            if z == 1:
                ckpt("dz0")
            with ExitStack() as ez:
                MT = T(kb, ez, [128, 32, 128], BF16, "s5mt")
                RT = T(kb, ez, [128, 32, 128], BF16, "s5rt")
                OTb = T(kb, ez, [128, 32, 128], BF16, "s5otb")
                with ExitStack() as ep:
                    Gall = T(kb, ep, [128, 32, 9, 16], F32, "s5gall")
                    Kn = T(kb, ep, [128, 32, 8, 16], F32, "s5kn")
                    Kp = T(kb, ep, [128, 32, 8, 16], F32, "s5kp")
                    tmpk = T(kb, ep, [128, 32, 16], F32, "s5tmpk")
                    bc = lambda ap2: ap2.unsqueeze(2).to_broadcast([128, 32, 16])
                    for k in range(9):
                        i = k if z == 0 else 8 - k
                        v(lambda k=k, i=i: nc.vector.tensor_tensor(Gall[:, :, i, :], CC[:, gs, :], bc(PW[:, k, 0, gs]),
                                                                  op=ALU.mult))
                        v(lambda k=k: nc.vector.tensor_tensor(tmpk[:], CCs[:, gs, :], bc(PW[:, k, 1, gs]), op=ALU.mult))
                        v(lambda i=i: nc.vector.tensor_tensor(Gall[:, :, i, :], Gall[:, :, i, :], tmpk[:], op=ALU.subtract))
                    for k in range(8):
                        j = k if z == 0 else 7 - k
                        v(lambda k=k, j=j: nc.vector.tensor_tensor(Kn[:, :, j, :], BB[:, gs, :], bc(WN[:, k, 0, gs]),
                                                                  op=ALU.mult))
                        v(lambda k=k: nc.vector.tensor_tensor(tmpk[:], BBs[:, gs, :], bc(WN[:, k, 1, gs]), op=ALU.mult))
                        v(lambda j=j: nc.vector.tensor_tensor(Kn[:, :, j, :], Kn[:, :, j, :], tmpk[:], op=ALU.add))
                        j2 = 7 - k if z == 0 else k
                        v(lambda k=k, j2=j2: nc.vector.tensor_tensor(Kp[:, :, j2, :], BB[:, gs, :], bc(WP[:, k, 0, gs]),
                                                                    op=ALU.mult))
                        v(lambda k=k: nc.vector.tensor_tensor(tmpk[:], BBs[:, gs, :], bc(WP[:, k, 1, gs]), op=ALU.mult))
                        v(lambda j2=j2: nc.vector.tensor_tensor(Kp[:, :, j2, :], Kp[:, :, j2, :], tmpk[:], op=ALU.add))
                    q0 = 0 if z == 0 else 1
                    o0 = 1 if z == 0 else 0
                    for gi in range(32):
                        ps = next_ps(g)
                        kb.op("pe", lambda gi=gi, ps=ps: nc.tensor.matmul(
                            ps[:, 0:128], lhsT=Kn[:, gi, :, :].rearrange("p j h -> p (j h)"),
                            rhs=Gall[:, gi, q0:q0 + 8, :].rearrange("p s h -> p (s h)"), start=True, stop=True),
                            reads=D1, writes=[ps.d])
                        kb.op("dve", lambda gi=gi, ps=ps: nc.vector.tensor_tensor(MT[:, gi, :], ps[:, 0:128], bm[z][:],
                                                                                 op=ALU.mult),
                              reads=[ps.d] + D1, writes=[MT.d])
                        ps2 = next_ps(g)
                        kb.op("pe", lambda gi=gi, ps2=ps2: nc.tensor.transpose(
                            ps2[:, 0:128], Kp[:, gi, :, :].rearrange("p j h -> p (j h)"), g.ident[:]),
                            reads=D1 + [g.ident.d], writes=[ps2.d])
                        kb.op("act", lambda gi=gi, ps2=ps2: nc.scalar.copy(RT[:, gi, :], ps2[:, 0:128]),
                              reads=[ps2.d], writes=[RT.d])
                        kb.op("act", lambda gi=gi: nc.scalar.copy(
                            OTb[:, gi, :], Gall[:, gi, o0:o0 + 8, :].rearrange("p s h -> p (s h)")),
                            reads=D1, writes=[OTb.d])
                    kb.barrier()
                if f"s5mat{z}" in g.dbg:
                    dbg_out(g, f"MT{z}", MT[:], [MT.d], [128, 32, 128], BF16)
                    dbg_out(g, f"RT{z}", RT[:], [RT.d], [128, 32, 128], BF16)
                    dbg_out(g, f"OTb{z}", OTb[:], [OTb.d], [128, 32, 128], BF16)
                if z == 0:
                    ckpt("dm0")
                X = T(kb, ez, [128, 32, NCK], F32, "s5x", nd=32)
                utg = [T(kb, ez, [128, NCK], BF16, f"s5utg{i}") for i in range(3)]
                for gi in range(32):
                    ug = utg[gi % 3]
                    kb.dma("sp", lambda gi=gi, ug=ug: nc.sync.dma_start(out=ug[:], in_=S.Ut[gi, :, :]),
                           reads=[S.Ut_d], writes=[ug.d])
                    for (c0, n) in ((0, 512), (512, NCK - 512)):
                        ps = next_ps(g)
                        kb.op("pe", lambda gi=gi, c0=c0, n=n, ps=ps: nc.tensor.matmul(
                            ps[:, 0:n], lhsT=RT[:, gi, :], rhs=ug[:, c0:c0 + n], start=True, stop=True),
                            reads=[RT.d, ug.d], writes=[ps.d])
                        eng = "act" if gi % 2 == 0 else "dve"
                        if eng == "act":
                            kb.op("act", lambda gi=gi, c0=c0, n=n, ps=ps: nc.scalar.copy(X[:, gi, c0:c0 + n], ps[:, 0:n]),
                                  reads=[ps.d], writes=[X.ds[gi]])
                        else:
                            kb.op("dve", lambda gi=gi, c0=c0, n=n, ps=ps: nc.vector.tensor_copy(X[:, gi, c0:c0 + n], ps[:, 0:n]),
                                  reads=[ps.d], writes=[X.ds[gi]])
                if z == 0:
                    ckpt("d20")
                cur = [T(kb, ez, [128, 32, NMAC], F32, f"s5cur{i}") for i in range(2)]
                Gm = T(kb, ez, [128, 32, NMAC], F32, "s5gm")
                uu = T(kb, ez, [128, 32, NMAC], F32, "s5uu")
                hs = [T(kb, ez, [128, 32, NMAC], F32, f"s5hs{i}") for i in range(2)]
                pw = T(kb, ez, [128, 2, 2, 32], F32, "s5pwk")
                ptmp = T(kb, ez, [128, 32], F32, "s5ptmp")
                xall = X.ds
                colsel = (lambda i: i) if z == 0 else (lambda i: 15 - i)
                gbanks = ((0, 15), (15, 30), (30, 32))

                def cplx_apply(src_ap_fn, width, ar_ap, ai_ap, dst, dst_lo, u_t, rd):
                    kb.op("pool", lambda: nc.gpsimd.tensor_tensor(
                        u_t[:, :, 0:width], src_ap_fn(0, 32), ar_ap.unsqueeze(2).to_broadcast([128, 32, width]),
                        op=ALU.mult), reads=rd + D1, writes=[u_t.d])
                    pss = []
                    for (g0, g1) in gbanks:
                        ps = next_ps(g)
                        pss.append(ps)
                        kb.op("pe", lambda ps=ps, g0=g0, g1=g1: nc.tensor.matmul(
                            ps[:, 0:(g1 - g0) * width], lhsT=Jt[:], rhs=src_ap_fn(g0, g1), start=True, stop=True),
                            reads=rd + D1, writes=[ps.d])
                    for (g0, g1), ps in zip(gbanks, pss):
                        kb.op("dve", lambda g0=g0, g1=g1, ps=ps: nc.vector.tensor_tensor(
                            dst[:, g0:g1, dst_lo:dst_lo + width],
                            ps[:, 0:(g1 - g0) * width].rearrange("p (a m) -> p a m", m=width),
                            ai_ap[:, g0:g1].unsqueeze(2).to_broadcast([128, g1 - g0, width]), op=ALU.mult),
                            reads=[ps.d] + D1, writes=[dst.d])
                    kb.op("dve", lambda: nc.vector.tensor_tensor(
                        dst[:, :, dst_lo:dst_lo + width], dst[:, :, dst_lo:dst_lo + width], u_t[:, :, 0:width], op=ALU.add),
                        reads=[dst.d, u_t.d], writes=[dst.d])

                a8r, a8i = A8s[:, 0, gs], A8s[:, 1, gs]

                def step(src, dst, i, store):
                    col = colsel(i)
                    cplx_apply(lambda g0, g1: src[:, g0:g1, :], NMAC, a8r, a8i, dst, 0, uu, [src.d])
                    kb.op("dve", lambda: nc.vector.tensor_tensor(dst[:], dst[:], X[:, :, col:NCK:16], op=ALU.add),
                          reads=[dst.d] + xall, writes=[dst.d])
                    if store:
                        kb.op("act", lambda: nc.scalar.copy(X[:, :, col:NCK:16], src[:]),
                              reads=[src.d] + xall, writes=xall)

                kb.op("pool", lambda: nc.gpsimd.memset(cur[0][:], 0.0), writes=[cur[0].d])
                for i in range(16):
                    if i % 4 == 0:
                        kb.pump(1)
                    step(cur[i % 2], cur[(i + 1) % 2], i, False)
                Em = cur[0]
                h0 = hs[0]
                if z == 0:
                    kb.op("act", lambda: nc.scalar.copy(h0[:], Em[:]), reads=[Em.d], writes=[h0.d])
                else:
                    kb.op("act", lambda: nc.scalar.copy(h0[:, :, 0:2], Em[:, :, 1::-1]), reads=[Em.d], writes=[h0.d])
                    kb.op("act", lambda: nc.scalar.copy(h0[:, :, 2:NMAC], Em[:, :, NMAC - 1:1:-1]), reads=[Em.d], writes=[h0.d])
                v(lambda: nc.vector.tensor_copy(pw[:, 0, 0, :], P128[:, 0, gs]))
                v(lambda: nc.vector.tensor_copy(pw[:, 0, 1, :], P128[:, 1, gs]))
                d_ = 1
                k_ = 0
                while d_ < NMAC:
                    src, dst = hs[k_ % 2], hs[(k_ + 1) % 2]
                    pr, pi_ = pw[:, k_ % 2, 0, :], pw[:, k_ % 2, 1, :]
                    w = NMAC - d_
                    cplx_apply(lambda g0, g1, src=src, w=w: src[:, g0:g1, 0:w], w, pr, pi_, dst, d_, uu, [src.d])
                    kb.op("dve", lambda src=src, dst=dst, d_=d_: nc.vector.tensor_tensor(
                        dst[:, :, d_:NMAC], dst[:, :, d_:NMAC], src[:, :, d_:NMAC], op=ALU.add),
                        reads=[dst.d, src.d], writes=[dst.d])
                    kb.op("act", lambda src=src, dst=dst, d_=d_: nc.scalar.copy(dst[:, :, 0:d_], src[:, :, 0:d_]),
                          reads=[src.d], writes=[dst.d])
                    nr, ni = pw[:, (k_ + 1) % 2, 0, :], pw[:, (k_ + 1) % 2, 1, :]
                    cmul(g, nr, ni, pr, pi_, pr, pi_, ptmp[:], D1, pd)
                    d_ *= 2
                    k_ += 1
                Iinc = hs[k_ % 2]
                kb.op("pool", lambda: nc.gpsimd.memset(Gm[:], 0.0), writes=[Gm.d])
                if z == 0:
                    kb.op("dve", lambda: nc.vector.tensor_copy(Gm[:, :, 1:NMAC], Iinc[:, :, 0:NMAC - 1]),
                          reads=[Iinc.d], writes=[Gm.d])
                else:
                    kb.op("dve", lambda: nc.vector.tensor_copy(Gm[:, :, 0:1], Iinc[:, :, 0:1]), reads=[Iinc.d], writes=[Gm.d])
                    kb.op("dve", lambda: nc.vector.tensor_copy(Gm[:, :, NMAC - 1:1:-1], Iinc[:, :, 1:NMAC - 1]),
                          reads=[Iinc.d], writes=[Gm.d])
                kb.op("dve", lambda: nc.vector.tensor_copy(cur[0][:], Gm[:]), reads=[Gm.d], writes=[cur[0].d])
                for i in range(16):
                    step(cur[i % 2], cur[(i + 1) % 2], i, True)
                if f"s5x{z}" in g.dbg:
                    dbg_out(g, f"X{z}", X[:], xall, [128, 32, NCK], F32)
                if z == 0:
                    ckpt("d30")
                xb = [T(kb, ez, [128, 512], BF16, f"s5xb{i}") for i in range(2)]
                yo = [T(kb, ez, [128, 512], F32, f"s5yo{i}") for i in range(2)]
                yfl = [T(kb, ez, [128, 512], F32, f"s5yfl{i}") for i in range(2)]
                for gi in range(32):
                    xbt = xb[gi % 2]
                    ug = utg[gi % 3]
                    kb.dma("sp", lambda gi=gi, ug=ug: nc.sync.dma_start(out=ug[:], in_=S.Ut[gi, :, :]),
                           reads=[S.Ut_d], writes=[ug.d])
                    kb.op("act", lambda gi=gi, xbt=xbt: nc.scalar.copy(xbt[:], X[:, gi, 32:NCK]), reads=xall, writes=[xbt.d])
                    ps = next_ps(g)
                    kb.op("pe", lambda gi=gi, ps=ps, ug=ug: nc.tensor.matmul(ps[:, :], lhsT=MT[:, gi, :], rhs=ug[:, 32:NCK],
                                                                             start=True, stop=False),
                          reads=[MT.d, ug.d], writes=[ps.d])
                    kb.op("pe", lambda gi=gi, ps=ps, xbt=xbt: nc.tensor.matmul(ps[:, :], lhsT=OTb[:, gi, :], rhs=xbt[:],
                                                                               start=False, stop=True),
                          reads=[OTb.d, xbt.d], writes=[ps.d])
                    yt = yo[gi % 2]
                    if z == 0:
                        kb.op("dve", lambda ps=ps, yt=yt: nc.vector.tensor_copy(yt[:], ps[:, :]),
                              reads=[ps.d], writes=[yt.d])
                    else:
                        yf = yfl[gi % 2]
                        kb.dma("act", lambda gi=gi, yf=yf: nc.scalar.dma_start(out=yf[:], in_=S.y0[gi, :, :]),
                               reads=[S.y0_d], writes=[yf.d])
                        kb.op("dve", lambda ps=ps, yt=yt, yf=yf: nc.vector.tensor_tensor(yt[:], ps[:, :], yf[:], op=ALU.add),
                              reads=[ps.d, yf.d], writes=[yt.d])
                    kb.dma("sp", lambda gi=gi, yt=yt: nc.sync.dma_start(out=S.y0[gi, :, :], in_=yt[:]),
                           reads=[yt.d], writes=[S.y0_d])
                kb.barrier()


SEG = 256
NSEG = NL * 4 // SEG + NE
JB = SEG // 128


def bc_tile(g, es, name, src_row_ap, n, reads=()):
    nc, kb = g.nc, g.kb
    t = T(kb, es, [128, n], F32, name)
    kb.dma("sp", lambda: nc.sync.dma_start(out=t[:], in_=src_row_ap.partition_broadcast(128)),
           reads=list(reads), writes=[t.d])
    return t


def phase_e(g):
    nc, kb, H, S = g.nc, g.kb, g.H, g.S
    R = g.R
    with ExitStack() as es:
        psb = g.psb[0]
        s5T = T(kb, es, [128, 4, NL], BF16, "s5T", nd=4)
        with ExitStack() as e1:
            glw = T(kb, e1, [128, 4, 512], BF16, "glw")
            kb.dma("pool", lambda: nc.gpsimd.dma_start(out=glw[:], in_=H.glu_w[0].rearrange("(k p) n -> p k n", p=128)),
                   writes=[glw.d])
            glb = T(kb, e1, [128, 4], F32, "glb")
            kb.dma("sp", lambda: nc.sync.dma_start(out=glb[:], in_=H.glu_b[0, :].rearrange("(k p) -> p k", p=128),
                                                   allow_slow_non_contiguous=True), writes=[glb.d])
            s5d = T(kb, e1, [128, 4], F32, "s5d")
            kb.dma("sp", lambda: nc.sync.dma_start(out=s5d[:], in_=H.s5_d[0, :].rearrange("(k p) -> p k", p=128),
                                                   allow_slow_non_contiguous=True), writes=[s5d.d])
            Yg = T(kb, e1, [128, 32, 128], F32, "Yg")
            Ytm = T(kb, e1, [128, 8, 512], F32, "Ytm")
            yT = T(kb, e1, [128, 4, 1024], F32, "yTt")
            uTt = T(kb, e1, [128, 4, 1024], BF16, "uTt")
            t1 = T(kb, e1, [128, 4, 1024], F32, "glt1")
            glg = T(kb, e1, [128, 4, 1024], BF16, "glg")
            sg = T(kb, e1, [128, 512], BF16, "glsg")
            for cb in range(4):
                kb.pump(1)
                kb.dma("sp", lambda cb=cb: nc.sync.dma_start(
                    out=Yg[:], in_=S.y0[:, :, cb * 128:(cb + 1) * 128].rearrange("g p c -> p g c")),
                    reads=[S.y0_d], writes=[Yg.d])
                kb.dma("act", lambda cb=cb: nc.scalar.dma_start(
                    out=uTt[:], in_=S.uT[:, NCX + cb * 1024:NCX + (cb + 1) * 1024].rearrange("(k p) t -> p k t", p=128)),
                    reads=[S.uT_d], writes=[uTt.d])
                for g4 in range(0, 32, 4):
                    ps = next_ps(g)
                    for gg in range(4):
                        kb.op("pe", lambda gg=gg, g4=g4, ps=ps: nc.tensor.transpose(
                            ps[:, gg * 128:(gg + 1) * 128], Yg[:, g4 + gg, :], g.ident[:]),
                            reads=[Yg.d, g.ident.d], writes=[ps.d])
                    kb.op("act", lambda g4=g4, ps=ps: nc.scalar.copy(
                        Ytm[:, :, g4 * 16:g4 * 16 + 64].rearrange("c s (a h) -> c a s h", a=4),
                        ps[:, :].rearrange("c (a s h) -> c a s h", a=4, s=8)), reads=[ps.d], writes=[Ytm.d])
                for s_ in range(8):
                    ps = next_ps(g)
                    for cc in range(4):
                        kb.op("pe", lambda cc=cc, s_=s_, ps=ps: nc.tensor.transpose(
                            ps[:, cc * 128:(cc + 1) * 128], Ytm[:, s_, cc * 128:(cc + 1) * 128], g.ident[:]),
                            reads=[Ytm.d, g.ident.d], writes=[ps.d])
                    kb.op("dve", lambda s_=s_, ps=ps: nc.vector.tensor_copy(
                        yT[:, :, s_:1024:8], ps[:, :].rearrange("p (a c) -> p a c", a=4)), reads=[ps.d], writes=[yT.d])
                for cc in range(4):
                    kb.op("dve", lambda cc=cc: nc.vector.scalar_tensor_tensor(
                        yT[:, cc, :], uTt[:, cc, :], s5d[:, cc:cc + 1], yT[:, cc, :], op0=ALU.mult, op1=ALU.add),
                        reads=[uTt.d, s5d.d, yT.d], writes=[yT.d])
                kb.op("pool", lambda: nc.gpsimd.tensor_tensor(t1[:], yT[:], yT[:], op=ALU.mult), reads=[yT.d], writes=[t1.d])
                kb.op("dve", lambda: nc.vector.tensor_scalar(t1[:], t1[:], 0.044715, 1.0, op0=ALU.mult, op1=ALU.add),
                      reads=[t1.d], writes=[t1.d])
                kb.op("pool", lambda: nc.gpsimd.tensor_tensor(t1[:], t1[:], yT[:], op=ALU.mult), reads=[t1.d, yT.d], writes=[t1.d])
                kb.op("act", lambda: nc.scalar.activation(t1[:], t1[:], ACT.Sigmoid, scale=1.5957691216057308),
                      reads=[t1.d], writes=[t1.d])
                kb.op("dve", lambda: nc.vector.tensor_tensor(glg[:], t1[:], yT[:], op=ALU.mult), reads=[t1.d, yT.d], writes=[glg.d])
                for nn in range(4):
                    for th in range(2):
                        ps = next_ps(g)
                        for kc in range(4):
                            kb.op("pe", lambda nn=nn, th=th, kc=kc, ps=ps: nc.tensor.matmul(
                                ps[:, :], lhsT=glw[:, kc, nn * 128:(nn + 1) * 128], rhs=glg[:, kc, th * 512:(th + 1) * 512],
                                start=(kc == 0), stop=(kc == 3)), reads=[glw.d, glg.d], writes=[ps.d])
                        kb.op("act", lambda nn=nn, ps=ps: nc.scalar.activation(sg[:], ps[:, :], ACT.Sigmoid,
                                                                              bias=glb[:, nn:nn + 1], scale=1.0),
                              reads=[ps.d, glb.d], writes=[sg.d])
                        kb.op("dve", lambda nn=nn, th=th, cb=cb: nc.vector.tensor_tensor(
                            s5T[:, nn, cb * 1024 + th * 512:cb * 1024 + (th + 1) * 512], sg[:],
                            glg[:, nn, th * 512:(th + 1) * 512], op=ALU.mult), reads=[sg.d, glg.d], writes=[s5T.ds[nn]])
            kb.barrier()
        if "s5T" in g.dbg:
            dbg_out(g, "s5T", s5T[:], s5T.ds, [128, 4, NL], BF16)
        ckpt("e1")
        glaT = T(kb, es, [128, 4, NL], BF16, "glaTt")
        kb.dma("sp", lambda: nc.sync.dma_start(out=glaT[:], in_=S.glaT.ap().rearrange("(k p) t -> p k t", p=128)),
               reads=[S.glaT_d], writes=[glaT.d])
        wo = T(kb, es, [128, 8, D], BF16, "wo")
        kb.dma("pool", lambda: nc.gpsimd.dma_start(out=wo[:], in_=H.w_out[0].rearrange("(k p) n -> p k n", p=128)),
               writes=[wo.d])
        g1b = bc_tile(g, es, "g1b", S.mod[0:1, 2 * D:3 * D], D, [S.mod_d])
        sh2b = bc_tile(g, es, "sh2b", S.mod[0:1, 3 * D:4 * D], D, [S.mod_d])
        g2e = bc_tile(g, es, "g2e", S.mod[0:1, 4 * D:5 * D], D, [S.mod_d])
        nfw = bc_tile(g, es, "nfw", H.norm_ffn_w[0:1, :], D)
        kb.op("dve", lambda: nc.vector.scalar_tensor_tensor(g2e[:], g2e[:], 1.0, nfw[:], op0=ALU.add, op1=ALU.mult),
              reads=[g2e.d, nfw.d], writes=[g2e.d])
        rw = T(kb, es, [128, 8, NE], F32, "rw")
        kb.dma("sp", lambda: nc.sync.dma_start(out=rw[:], in_=H.router_w[0].rearrange("(k p) e -> p k e", p=128)),
               writes=[rw.d])
        rbb = bc_tile(g, es, "rbb", H.router_b[0:1, :], NE)
        xt = [T(kb, es, [128, D], F32, f"ext{i}") for i in range(3)]
        x2t = [T(kb, es, [128, D], F32, f"ex2{i}") for i in range(2)]
        tmp_ = [T(kb, es, [128, D], F32, f"etmp{i}") for i in range(2)]
        tmq_ = [T(kb, es, [128, D], F32, f"etmq{i}") for i in range(2)]
        h2_ = [T(kb, es, [128, D], F32, f"eh2{i}") for i in range(2)]
        h2b = [T(kb, es, [128, D], BF16, f"eh2b{i}") for i in range(2)]
        hT2_ = [T(kb, es, [128, 8, 128], F32, f"ehT2{i}") for i in range(2)]
        junk = T(kb, es, [128, D], BF16, "ejunk")
        lg_ = [T(kb, es, [128, NE], F32, f"elg{i}") for i in range(2)]
        mx8_ = [T(kb, es, [128, 8], F32, f"emx8{i}") for i in range(2)]
        ix8_ = [T(kb, es, [128, 8], U32, f"eix8{i}") for i in range(2)]
        sm1_ = [T(kb, es, [128, 4], F32, f"esm{i}") for i in range(2)]
        def partA(tl):
            if tl % 4 == 0:
                kb.pump(1)
            t0 = tl * 128
            x_t, x2 = xt[tl % 3], x2t[tl % 2]
            tmp, tmq, h2, hT2 = tmp_[tl % 2], tmq_[tl % 2], h2_[tl % 2], hT2_[tl % 2]
            lg, mx8, ix8, sm1 = lg_[tl % 2], mx8_[tl % 2], ix8_[tl % 2], sm1_[tl % 2]
            for pf in ([0, 1, 2] if tl == 0 else [tl + 2]):
                if pf < NL // 128:
                    xp = xt[pf % 3]
                    kb.dma("sp", lambda xp=xp, pf=pf: nc.sync.dma_start(out=xp[:], in_=H.x[pf * 128:(pf + 1) * 128, :]),
                           writes=[xp.d])
            for nh in range(2):
                ps = next_ps(g)
                for r in range(2):
                    row = tl * 2 + r
                    for kc in range(8):
                        if kc < 4:
                            lh = glaT[:, kc, row * 64:(row + 1) * 64]
                            rd = [glaT.d]
                        else:
                            lh = s5T[:, kc - 4, row:NL:64]
                            rd = s5T.ds
                        kb.op("pe", lambda lh=lh, r=r, kc=kc, nh=nh, ps=ps: nc.tensor.matmul(
                            ps[r * 64:(r + 1) * 64, :], lhsT=lh, rhs=wo[:, kc, nh * 512:(nh + 1) * 512],
                            start=(kc == 0), stop=(kc == 7)), reads=rd + [wo.d], writes=[ps.d])
                kb.op("dve", lambda nh=nh, ps=ps: nc.vector.tensor_tensor(
                    tmp[:, nh * 512:(nh + 1) * 512], ps[:, :], g1b[:, nh * 512:(nh + 1) * 512], op=ALU.mult),
                    reads=[ps.d, g1b.d], writes=[tmp.d])
            kb.op("dve", lambda x2=x2, x_t=x_t, tmp=tmp: nc.vector.tensor_tensor(x2[:], tmp[:], x_t[:], op=ALU.add),
                  reads=[tmp.d, x_t.d], writes=[x2.d])
            kb.dma("sp", lambda x2=x2, t0=t0: nc.sync.dma_start(out=S.x2[t0:t0 + 128, :], in_=x2[:]),
                   reads=[x2.d], writes=[S.x2_d])
            kb.op("act", lambda x2=x2: nc.scalar.activation(junk[:], x2[:], ACT.Square, accum_out=sm1[:, 0:1]),
                  reads=[x2.d], writes=[junk.d, sm1.d])
            kb.op("dve", lambda: nc.vector.tensor_scalar(sm1[:, 1:2], sm1[:, 0:1], 1.0 / D, EPS, op0=ALU.mult, op1=ALU.add),
                  reads=[sm1.d], writes=[sm1.d])
            kb.op("act", lambda: nc.scalar.activation(sm1[:, 1:2], sm1[:, 1:2], ACT.Sqrt), reads=[sm1.d], writes=[sm1.d])
            kb.op("dve", lambda: nc.vector.reciprocal(sm1[:, 2:3], sm1[:, 1:2]), reads=[sm1.d], writes=[sm1.d])
            kb.op("dve", lambda x2=x2: nc.vector.scalar_tensor_tensor(tmq[:], x2[:], sm1[:, 2:3], g2e[:],
                                                                      op0=ALU.mult, op1=ALU.mult),
                  reads=[x2.d, sm1.d, g2e.d], writes=[tmq.d])
            kb.op("dve", lambda: nc.vector.tensor_tensor(h2[:], tmq[:], sh2b[:], op=ALU.add),
                  reads=[tmq.d, sh2b.d], writes=[h2.d])
            hb = h2b[tl % 2]
            kb.op("act", lambda hb=hb: nc.scalar.copy(hb[:], h2[:]), reads=[h2.d], writes=[hb.d])
            kb.dma("sp", lambda hb=hb, t0=t0: nc.sync.dma_start(out=S.h2[t0:t0 + 128, :], in_=hb[:]),
                   reads=[hb.d], writes=[S.h2_d])

        def partB(tl):
            tmp, tmq, h2, hT2 = tmp_[tl % 2], tmq_[tl % 2], h2_[tl % 2], hT2_[tl % 2]
            lg, mx8, ix8, sm1 = lg_[tl % 2], mx8_[tl % 2], ix8_[tl % 2], sm1_[tl % 2]
            for hf in range(2):
                ps = next_ps(g)
                for kk in range(4):
                    kc = hf * 4 + kk
                    kb.op("pe", lambda kc=kc, kk=kk, ps=ps: nc.tensor.transpose(
                        ps[:, kk * 128:(kk + 1) * 128], h2[:, kc * 128:(kc + 1) * 128], g.ident[:]),
                        reads=[h2.d, g.ident.d], writes=[ps.d])
                kb.op("act", lambda hf=hf, ps=ps: nc.scalar.copy(
                    hT2[:, hf * 4:(hf + 1) * 4, :], ps[:, :].rearrange("p (a t) -> p a t", a=4)),
                    reads=[ps.d], writes=[hT2.d])
            ps = next_ps(g)
            for kc in range(8):
                kb.op("pe", lambda kc=kc, ps=ps: nc.tensor.matmul(ps[:, 0:NE], lhsT=hT2[:, kc, :], rhs=rw[:, kc, :],
                                                                  start=(kc == 0), stop=(kc == 7)),
                      reads=[hT2.d, rw.d], writes=[ps.d])
            kb.op("dve", lambda ps=ps: nc.vector.tensor_tensor(lg[:], ps[:, 0:NE], rbb[:], op=ALU.add),
                  reads=[ps.d, rbb.d], writes=[lg.d])
            kb.op("dve", lambda: nc.vector.max(mx8[:], lg[:]), reads=[lg.d], writes=[mx8.d])
            kb.op("dve", lambda: nc.vector.max_index(ix8[:], mx8[:], lg[:]), reads=[lg.d, mx8.d], writes=[ix8.d])
            kb.op("dve", lambda tl=tl: nc.vector.tensor_copy(R.idxf[:, tl, :], ix8[:, 0:4]), reads=[ix8.d], writes=[R.idxf.d])
            kb.op("dve", lambda tl=tl: nc.vector.tensor_scalar(R.mask[:, tl, :], lg[:], mx8[:, 3:4], None, op0=ALU.is_ge),
                  reads=[lg.d, mx8.d], writes=[R.mask.d])
            kb.op("dve", lambda: nc.vector.tensor_scalar(sm1[:, 3:4], mx8[:, 0:1], -1.0, None, op0=ALU.mult),
                  reads=[mx8.d, sm1.d], writes=[sm1.d])
            kb.op("act", lambda tl=tl: nc.scalar.activation(R.gate[:, tl, :], mx8[:, 0:4], ACT.Exp, bias=sm1[:, 3:4],
                                                            scale=1.0, accum_out=sm1[:, 0:1]),
                  reads=[mx8.d, sm1.d], writes=[R.gate.d, sm1.d])
            kb.op("dve", lambda: nc.vector.reciprocal(sm1[:, 1:2], sm1[:, 0:1]), reads=[sm1.d], writes=[sm1.d])
            kb.op("dve", lambda tl=tl: nc.vector.tensor_scalar(R.gate[:, tl, :], R.gate[:, tl, :], sm1[:, 1:2], None,
                                                               op0=ALU.mult), reads=[R.gate.d, sm1.d], writes=[R.gate.d])

        NTL = NL // 128
        partA(0)
        for tl in range(1, NTL):
            partA(tl)
            partB(tl - 1)
        partB(NTL - 1)
        if "logits" in g.dbg:
            dbg_out(g, "gate", R.gate[:], [R.gate.d], [128, 32, 4])
            dbg_out(g, "idxf", R.idxf[:], [R.idxf.d], [128, 32, 4])
        ckpt("e2")
        onesf = T(kb, es, [128, 128], F32, "onesf")
        kb.op("pool", lambda: nc.gpsimd.memset(onesf[:], 1.0), writes=[onesf.d])
        triu = T(kb, es, [128, 128], F32, "triu")
        kb.op("dve", lambda: nc.vector.tensor_single_scalar(triu[:], g.iota[:], 0.0, op=ALU.is_gt),
              reads=[g.iota.d], writes=[triu.d])
        ps = next_ps(g)
        for tl in range(32):
            kb.op("pe", lambda tl=tl, ps=ps: nc.tensor.matmul(ps[:, 0:NE], lhsT=onesf[:], rhs=R.mask[:, tl, :],
                                                              start=(tl == 0), stop=(tl == 31)),
                  reads=[onesf.d, R.mask.d], writes=[ps.d])
        cnt = T(kb, es, [128, NE], F32, "cnt")
        nsg = T(kb, es, [128, NE], F32, "nsg")
        pend = T(kb, es, [128, NE], F32, "pend")
        pst = T(kb, es, [128, NE], F32, "pst")
        t32 = T(kb, es, [128, NE], F32, "t32")
        kb.op("dve", lambda ps=ps: nc.vector.tensor_copy(cnt[:], ps[:, 0:NE]), reads=[ps.d], writes=[cnt.d])
        kb.op("dve", lambda: nc.vector.memset(nsg[:], 0.0), writes=[nsg.d])
        for k in range(NL // SEG):
            kb.op("dve", lambda k=k: nc.vector.tensor_scalar(t32[:], cnt[:], float(SEG * k) + 0.5, None, op0=ALU.is_ge),
                  reads=[cnt.d], writes=[t32.d])
            kb.op("dve", lambda: nc.vector.tensor_tensor(nsg[:], nsg[:], t32[:], op=ALU.add), reads=[nsg.d, t32.d], writes=[nsg.d])
        kb.op("dve", lambda: nc.vector.tensor_tensor_scan(pend[:], onesf[:, 0:NE], nsg[:], 0.0, ALU.mult, ALU.add),
              reads=[onesf.d, nsg.d], writes=[pend.d])
        kb.op("dve", lambda: nc.vector.tensor_tensor(pst[:], pend[:], nsg[:], op=ALU.subtract), reads=[pend.d, nsg.d], writes=[pst.d])
        kb.op("dve", lambda: nc.vector.tensor_scalar(pst[:], pst[:], float(SEG), None, op0=ALU.mult), reads=[pst.d], writes=[pst.d])
        sidx = T(kb, es, [128, NSEG], F32, "sidx")
        kb.op("pool", lambda: nc.gpsimd.iota(sidx[:], pattern=[[1, NSEG]], base=0, channel_multiplier=0,
                                              allow_small_or_imprecise_dtypes=True), writes=[sidx.d])
        cmp3 = T(kb, es, [128, NSEG, NE], F32, "cmp3")
        kb.op("dve", lambda: nc.vector.tensor_tensor(cmp3[:], pend[:].unsqueeze(1).to_broadcast([128, NSEG, NE]),
                                                     sidx[:].unsqueeze(2).to_broadcast([128, NSEG, NE]), op=ALU.is_le),
              reads=[pend.d, sidx.d], writes=[cmp3.d])
        sef = T(kb, es, [128, NSEG], F32, "sef")
        kb.op("dve", lambda: nc.vector.tensor_reduce(sef[:], cmp3[:], axis=AX.X, op=ALU.add), reads=[cmp3.d], writes=[sef.d])
        kb.op("dve", lambda: nc.vector.tensor_scalar(sef[:], sef[:], float(NE - 1), None, op0=ALU.min), reads=[sef.d], writes=[sef.d])
        kb.op("dve", lambda: nc.vector.tensor_copy(R.segexp[:], sef[:]), reads=[sef.d], writes=[R.segexp.d])
        used = T(kb, es, [128, NSEG], F32, "used")
        kb.op("dve", lambda: nc.vector.tensor_scalar(used[:], sidx[:], pend[:, NE - 1:NE], None, op0=ALU.is_lt),
              reads=[sidx.d, pend.d], writes=[used.d])
        pcf = T(kb, es, [128, 1], F32, "pcf")
        kb.op("pool", lambda: nc.gpsimd.iota(pcf[:], pattern=[[0, 1]], base=0, channel_multiplier=1,
                                              allow_small_or_imprecise_dtypes=True), writes=[pcf.d])
        OOB = 1000000.0
        sgf = T(kb, es, [128, NSEG], F32, "sgf")
        kb.op("dve", lambda: nc.vector.tensor_scalar(sgf[:], sef[:], 128.0, pcf[:, 0:1], op0=ALU.mult, op1=ALU.add),
              reads=[sef.d, pcf.d], writes=[sgf.d])
        for src, dst in ((sgf, R.segidx), (sef, R.segrow)):
            kb.op("dve", lambda src=src: nc.vector.tensor_scalar(src[:], src[:], -OOB, None, op0=ALU.add),
                  reads=[src.d], writes=[src.d])
            kb.op("dve", lambda src=src: nc.vector.tensor_tensor(src[:], src[:], used[:], op=ALU.mult),
                  reads=[src.d, used.d], writes=[src.d])
            kb.op("dve", lambda src=src: nc.vector.tensor_scalar(src[:], src[:], OOB, None, op0=ALU.add),
                  reads=[src.d], writes=[src.d])
            kb.op("dve", lambda src=src, dst=dst: nc.vector.tensor_copy(dst[:], src[:]), reads=[src.d], writes=[dst.d])
        if "logits" in g.dbg:
            dbg_out(g, "cnt", cnt[:], [cnt.d], [128, NE])
            dbg_out(g, "segexp", R.segexp[:], [R.segexp.d], [128, NSEG], I32)
        carry = T(kb, es, [128, NE], F32, "carry")
        kb.op("dve", lambda: nc.vector.tensor_copy(carry[:], pst[:]), reads=[pst.d], writes=[carry.d])
        ief = T(kb, es, [128, NE], F32, "ief")
        kb.op("pool", lambda: nc.gpsimd.iota(ief[:], pattern=[[1, NE]], base=0, channel_multiplier=0,
                                              allow_small_or_imprecise_dtypes=True), writes=[ief.d])
        slf = T(kb, es, [128, NE], F32, "slf")
        slk = T(kb, es, [128, 4], F32, "slk")
        hld = [T(kb, es, [128, D], BF16, f"hld{i}") for i in range(2)]
        for tl in range(32):
            t0 = tl * 128
            psA = next_ps(g)
            kb.op("pe", lambda tl=tl, psA=psA: nc.tensor.matmul(psA[:, 0:NE], lhsT=triu[:], rhs=R.mask[:, tl, :],
                                                                start=True, stop=True),
                  reads=[triu.d, R.mask.d], writes=[psA.d])
            kb.op("dve", lambda psA=psA: nc.vector.tensor_tensor(slf[:], psA[:, 0:NE], carry[:], op=ALU.add),
                  reads=[psA.d, carry.d], writes=[slf.d])
            psB = next_ps(g)
            kb.op("pe", lambda tl=tl, psB=psB: nc.tensor.matmul(psB[:, 0:NE], lhsT=onesf[:], rhs=R.mask[:, tl, :],
                                                                start=True, stop=True),
                  reads=[onesf.d, R.mask.d], writes=[psB.d])
            kb.op("dve", lambda psB=psB: nc.vector.tensor_tensor(carry[:], carry[:], psB[:, 0:NE], op=ALU.add),
                  reads=[psB.d, carry.d, slf.d], writes=[carry.d])
            for k in range(4):
                kb.op("dve", lambda k=k, tl=tl: nc.vector.scalar_tensor_tensor(
                    t32[:], ief[:], R.idxf[:, tl, k:k + 1], slf[:], op0=ALU.is_equal, op1=ALU.mult,
                    accum_out=slk[:, k:k + 1]), reads=[ief.d, R.idxf.d, slf.d], writes=[t32.d, slk.d])
            kb.op("dve", lambda tl=tl: nc.vector.tensor_copy(R.slot[:, tl, :], slk[:]), reads=[slk.d], writes=[R.slot.ds[tl]])
            hl = hld[tl % 2]
            kb.dma("sp", lambda hl=hl, t0=t0: nc.sync.dma_start(out=hl[:], in_=S.h2[t0:t0 + 128, :]),
                   reads=[S.h2_d], writes=[hl.d])
            for k in range(4):
                kb.dma("pool", lambda hl=hl, tl=tl, k=k: nc.gpsimd.indirect_dma_start(
                    out=S.xg[:, :], out_offset=bass.IndirectOffsetOnAxis(ap=R.slot[:, tl, k:k + 1], axis=0),
                    in_=hl[:, :], in_offset=None), reads=[hl.d, R.slot.ds[tl]], writes=[S.xg_d])
        if "logits" in g.dbg:
            dbg_out(g, "slot", R.slot[:], R.slot.ds, [128, 32, 4], I32)
        bg = T(kb, es, [NE, 2 * D], F32, "bgrow")
        kb.dma("sp", lambda: nc.sync.dma_start(out=bg[:], in_=H.exp_b_gu[0]), writes=[bg.d])
        bgt = T(kb, es, [128, 16, NE], F32, "bgt")
        for c4 in range(0, 16, 4):
            ps = next_ps(g)
            for cc in range(4):
                kb.op("pe", lambda cc=cc, c4=c4, ps=ps: nc.tensor.transpose(
                    ps[:, cc * NE:(cc + 1) * NE], bg[:, (c4 + cc) * 128:(c4 + cc + 1) * 128], g.ident[0:NE, 0:NE]),
                    reads=[bg.d, g.ident.d], writes=[ps.d])
            kb.op("act", lambda c4=c4, ps=ps: nc.scalar.copy(
                bgt[:, c4:c4 + 4, :], ps[:, 0:4 * NE].rearrange("p (a e) -> p a e", a=4)), reads=[ps.d], writes=[bgt.d])
        kb.dma("sp", lambda: nc.sync.dma_start(out=S.bguT.ap().rearrange("e p c -> p c e"), in_=bgt[:],
                                               allow_slow_non_contiguous=True), reads=[bgt.d], writes=[S.bguT_d])


def phase_f(g):
    nc, kb, H, S = g.nc, g.kb, g.H, g.S
    R = g.R
    with ExitStack() as es:
        wgu = [T(kb, es, [128, 8, 2 * D], BF16, f"wgu{i}") for i in range(2)]
        wdn = [T(kb, es, [128, 8, D], BF16, f"wdn{i}") for i in range(2)]
        bgu = [T(kb, es, [128, 16], F32, f"bgu{i}") for i in range(2)]
        bdn = [T(kb, es, [128, D], F32, f"bdn{i}") for i in range(2)]
        bgs = [T(kb, es, [128, 8], F32, f"bgs{i}") for i in range(2)]
        xrow = [T(kb, es, [128, JB, D], BF16, f"xrow{i}") for i in range(2)]
        xT = [T(kb, es, [128, 8, SEG], BF16, f"xT{i}") for i in range(2)]
        actT = [T(kb, es, [128, 8, SEG], BF16, f"actT{i}", nd=8) for i in range(2)]
        yrow = [T(kb, es, [128, JB, D], BF16, f"yrow{i}") for i in range(2)]
        L0 = [T(kb, es, [128, SEG], F32, f"fL0_{i}") for i in range(2)]
        Gc = [T(kb, es, [128, SEG], F32, f"fGc_{i}") for i in range(2)]
        sg = [T(kb, es, [128, SEG], F32, f"fsg_{i}") for i in range(2)]
        tt = [T(kb, es, [128, SEG], F32, f"ftt_{i}") for i in range(2)]
        bc_rows = nc.gpsimd.to_reg(NE * 128 - 1)
        bc_e = nc.gpsimd.to_reg(NE - 1)
        wg_rows = S.wg.ap().rearrange("e p f -> (e p) f")
        wd_rows = S.wd.ap().rearrange("e p f -> (e p) f")
        bgu_rows = S.bguT.ap().rearrange("e p c -> (e p) c")
        nseg = g.nseg_limit if getattr(g, "nseg_limit", None) else NSEG
        bench = getattr(g, "bench", "") or ""
        for s in range(nseg):
            b = s % 2
            if "nodma" in bench and s >= 2:
                KB.dead = True
            kb.dma("pool", lambda: nc.gpsimd.indirect_dma_start(
                out=wgu[b][:, :, :].rearrange("p k n -> p (k n)"), out_offset=None, in_=wg_rows,
                in_offset=bass.IndirectOffsetOnAxis(ap=R.segidx[:, s:s + 1], axis=0),
                bounds_check=bc_rows, oob_is_err=False),
                reads=[R.segidx.d, S.wg_d], writes=[wgu[b].d])
            kb.dma("pool", lambda: nc.gpsimd.indirect_dma_start(
                out=wdn[b][:, :, :].rearrange("p k n -> p (k n)"), out_offset=None, in_=wd_rows,
                in_offset=bass.IndirectOffsetOnAxis(ap=R.segidx[:, s:s + 1], axis=0),
                bounds_check=bc_rows, oob_is_err=False),
                reads=[R.segidx.d, S.wd_d], writes=[wdn[b].d])
            kb.dma("pool", lambda: nc.gpsimd.indirect_dma_start(
                out=bgu[b][:, :], out_offset=None, in_=bgu_rows,
                in_offset=bass.IndirectOffsetOnAxis(ap=R.segidx[:, s:s + 1], axis=0),
                bounds_check=bc_rows, oob_is_err=False),
                reads=[R.segidx.d, S.bguT_d], writes=[bgu[b].d])
            kb.dma("pool", lambda: nc.gpsimd.indirect_dma_start(
                out=bdn[b][:, :], out_offset=None, in_=H.exp_b_down[0],
                in_offset=bass.IndirectOffsetOnAxis(ap=R.segrow[:, s:s + 1], axis=0),
                bounds_check=bc_e, oob_is_err=False),
                reads=[R.segrow.d], writes=[bdn[b].d])
            if bench:
                KB.dead = False
            kb.op("act", lambda: nc.scalar.mul(bgs[b][:], bgu[b][:, 8:16], 1.0 / 1.702), reads=[bgu[b].d], writes=[bgs[b].d])
            kb.dma("sp", lambda: nc.sync.dma_start(
                out=xrow[b][:], in_=S.xg[s * SEG:(s + 1) * SEG, :].rearrange("(j p) d -> p j d", p=128)),
                reads=[S.xg_d], writes=[xrow[b].d])
            if "nocomp" in bench:
                KB.dead = True
            for kc in range(8):
                pb = g.psb[kc % 2]
                for j in range(JB):
                    kb.op("pe", lambda j=j, kc=kc, pb=pb: nc.tensor.transpose(
                        pb[:, j * 128:(j + 1) * 128], xrow[b][:, j, kc * 128:(kc + 1) * 128], g.identb[:]),
                        reads=[xrow[b].d, g.identb.d], writes=[pb.d])
                if kc % 2 == 0:
                    kb.op("act", lambda kc=kc, pb=pb: nc.scalar.copy(xT[b][:, kc, :], pb[:, 0:SEG]),
                          reads=[pb.d], writes=[xT[b].d])
                else:
                    kb.op("dve", lambda kc=kc, pb=pb: nc.vector.tensor_copy(xT[b][:, kc, :], pb[:, 0:SEG]),
                          reads=[pb.d], writes=[xT[b].d])
            for c in range(8):
                i2 = c % 2
                pg = next_ps(g)
                for kc in range(8):
                    kb.op("pe", lambda kc=kc, c=c, pg=pg: nc.tensor.matmul(
                        pg[:, 0:SEG], lhsT=wgu[b][:, kc, c * 128:(c + 1) * 128], rhs=xT[b][:, kc, :],
                        start=(kc == 0), stop=(kc == 7)), reads=[wgu[b].d, xT[b].d], writes=[pg.d])
                pl_ = next_ps(g)
                for kc in range(8):
                    kb.op("pe", lambda kc=kc, c=c, pl_=pl_: nc.tensor.matmul(
                        pl_[:, 0:SEG], lhsT=wgu[b][:, kc, D + c * 128:D + (c + 1) * 128], rhs=xT[b][:, kc, :],
                        start=(kc == 0), stop=(kc == 7)), reads=[wgu[b].d, xT[b].d], writes=[pl_.d])
                kb.op("dve", lambda c=c, pg=pg: nc.vector.tensor_scalar(Gc[i2][:], pg[:, 0:SEG], bgu[b][:, c:c + 1], 7.0,
                                                                      op0=ALU.add, op1=ALU.min),
                      reads=[pg.d, bgu[b].d], writes=[Gc[i2].d])
                kb.op("act", lambda: nc.scalar.activation(sg[i2][:], Gc[i2][:], ACT.Silu, scale=1.702),
                      reads=[Gc[i2].d], writes=[sg[i2].d])
                kb.op("act", lambda c=c, pl_=pl_: nc.scalar.activation(L0[i2][:], pl_[:, 0:SEG], ACT.Identity,
                                                                      bias=bgs[b][:, c:c + 1], scale=1.0 / 1.702),
                      reads=[pl_.d, bgs[b].d], writes=[L0[i2].d])
                kb.op("dve", lambda: nc.vector.tensor_scalar(L0[i2][:], L0[i2][:], 7.0 / 1.702, -7.0 / 1.702,
                                                             op0=ALU.min, op1=ALU.max),
                      reads=[L0[i2].d], writes=[L0[i2].d])
                kb.op("dve", lambda c=c: nc.vector.scalar_tensor_tensor(actT[b][:, c, :], L0[i2][:], 1.0 / 1.702, sg[i2][:],
                                                                       op0=ALU.add, op1=ALU.mult),
                      reads=[L0[i2].d, sg[i2].d], writes=[actT[b].ds[c]])
            for j in range(JB):
                for nh in range(2):
                    ps = next_ps(g)
                    for c in range(8):
                        kb.op("pe", lambda c=c, j=j, nh=nh, ps=ps: nc.tensor.matmul(
                            ps[:, :], lhsT=actT[b][:, c, j * 128:(j + 1) * 128], rhs=wdn[b][:, c, nh * 512:(nh + 1) * 512],
                            start=(c == 0), stop=(c == 7)), reads=[actT[b].ds[c], wdn[b].d], writes=[ps.d])
                    kb.op("dve", lambda j=j, nh=nh, ps=ps: nc.vector.tensor_tensor(
                        yrow[b][:, j, nh * 512:(nh + 1) * 512], ps[:, :], bdn[b][:, nh * 512:(nh + 1) * 512], op=ALU.add),
                        reads=[ps.d, bdn[b].d], writes=[yrow[b].d])
            kb.dma("act", lambda: nc.scalar.dma_start(
                out=S.yg[s * SEG:(s + 1) * SEG, :].rearrange("(j p) d -> p j d", p=128), in_=yrow[b][:]),
                reads=[yrow[b].d], writes=[S.yg_d])
            if bench:
                KB.dead = False


def phase_g(g):
    nc, kb, H, S = g.nc, g.kb, g.H, g.S
    R = g.R
    with ExitStack() as es:
        g2b = bc_tile(g, es, "g2b", S.mod[0:1, 5 * D:6 * D], D, [S.mod_d])
        fnw = bc_tile(g, es, "fnw", H.final_norm_w.ap().rearrange("(o d) -> o d", o=1), D)
        yk = [[T(kb, es, [128, D], BF16, f"yk{i}_{k}") for k in range(4)] for i in range(2)]
        x2t = [T(kb, es, [128, D], F32, f"gx2{i}") for i in range(2)]
        acc_ = [T(kb, es, [128, D], F32, f"gacc{i}") for i in range(2)]
        x3_ = [T(kb, es, [128, D], F32, f"gx3{i}") for i in range(2)]
        sm_ = [T(kb, es, [128, 4], F32, f"gsm{i}") for i in range(2)]
        ot = [T(kb, es, [128, D], F32, f"got{i}") for i in range(2)]
        junk = T(kb, es, [128, D], BF16, "gjunk")
        sm1 = T(kb, es, [128, 4], F32, "gsm")
        for tl in range(NL // 128):
            t0 = tl * 128
            b = tl % 2
            acc, x3, sm1 = acc_[b], x3_[b], sm_[b]
            for k in range(4):
                kb.dma("pool", lambda k=k: nc.gpsimd.indirect_dma_start(
                    out=yk[b][k][:, :], out_offset=None, in_=S.yg[:, :],
                    in_offset=bass.IndirectOffsetOnAxis(ap=R.slot[:, tl, k:k + 1], axis=0)),
                    reads=[S.yg_d, R.slot.ds[tl]], writes=[yk[b][k].d])
            kb.dma("sp", lambda: nc.sync.dma_start(out=x2t[b][:], in_=S.x2[t0:t0 + 128, :]), reads=[S.x2_d], writes=[x2t[b].d])
            kb.op("act", lambda: nc.scalar.activation(acc[:], yk[b][0][:], ACT.Identity, scale=R.gate[:, tl, 0:1]),
                  reads=[yk[b][0].d, R.gate.d], writes=[acc.d])
            for k in range(1, 4):
                kb.op("dve", lambda k=k: nc.vector.scalar_tensor_tensor(acc[:], yk[b][k][:], R.gate[:, tl, k:k + 1], acc[:],
                                                                       op0=ALU.mult, op1=ALU.add),
                      reads=[yk[b][k].d, R.gate.d, acc.d], writes=[acc.d])
            kb.op("dve", lambda: nc.vector.tensor_tensor(acc[:], acc[:], g2b[:], op=ALU.mult), reads=[acc.d, g2b.d], writes=[acc.d])
            kb.op("dve", lambda: nc.vector.tensor_tensor(x3[:], acc[:], x2t[b][:], op=ALU.add), reads=[acc.d, x2t[b].d], writes=[x3.d])
            kb.op("act", lambda: nc.scalar.activation(junk[:], x3[:], ACT.Square, accum_out=sm1[:, 0:1]),
                  reads=[x3.d], writes=[junk.d, sm1.d])
            kb.op("dve", lambda: nc.vector.tensor_scalar(sm1[:, 1:2], sm1[:, 0:1], 1.0 / D, EPS, op0=ALU.mult, op1=ALU.add),
                  reads=[sm1.d], writes=[sm1.d])
            kb.op("act", lambda: nc.scalar.activation(sm1[:, 1:2], sm1[:, 1:2], ACT.Sqrt), reads=[sm1.d], writes=[sm1.d])
            kb.op("dve", lambda: nc.vector.reciprocal(sm1[:, 2:3], sm1[:, 1:2]), reads=[sm1.d], writes=[sm1.d])
            kb.op("dve", lambda: nc.vector.scalar_tensor_tensor(ot[b][:], x3[:], sm1[:, 2:3], fnw[:], op0=ALU.mult, op1=ALU.mult),
                  reads=[x3.d, sm1.d, fnw.d], writes=[ot[b].d])
            kb.dma("act", lambda: nc.scalar.dma_start(out=H.out[t0:t0 + 128, :], in_=ot[b][:]), reads=[ot[b].d], writes=[g.out_d])


_SHARED = ("c_ctx", "ada_w", "ada_b", "norm_mix_w", "w_in", "gla_lr_up", "gla_lr_bias", "gla_norm_w",
           "s5_lam_re", "s5_lam_im", "s5_log_dt", "s5_b_re", "s5_b_im", "s5_c_re", "s5_c_im", "s5_d",
           "glu_w", "glu_b", "w_out", "norm_ffn_w", "router_w", "router_b", "exp_w_gu", "exp_b_gu",
           "exp_w_down", "exp_b_down", "final_norm_w")


def kernel(x, c, ctx, c_ctx, ada_w, ada_b, norm_mix_w, w_in, gla_lr_up, gla_lr_bias, gla_norm_w,
           s5_lam_re, s5_lam_im, s5_log_dt, s5_b_re, s5_b_im, s5_c_re, s5_c_im, s5_d, glu_w, glu_b,
           w_out, norm_ffn_w, router_w, router_b, exp_w_gu, exp_b_gu, exp_w_down, exp_b_down,
           final_norm_w):
    loc = locals()
    shared = {k: np.ascontiguousarray(np.asarray(loc[k], dtype=np.float32)) for k in _SHARED}
    x = np.asarray(x, dtype=np.float32)
    c = np.asarray(c, dtype=np.float32)
    ctx = np.asarray(ctx, dtype=np.float32)
    nb = x.shape[0]
    nc, _ = build()
    in_maps = []
    for b in range(nb):
        m = dict(shared)
        m["x"] = np.ascontiguousarray(x[b])
        m["ctx"] = np.ascontiguousarray(ctx[b])
        m["c"] = np.ascontiguousarray(c[b:b + 1])
        in_maps.append(m)
    res = run_bass_kernel_spmd(nc, in_maps, core_ids=list(range(nb)))
    return np.stack([np.asarray(r["out"], dtype=np.float32) for r in res.results], axis=0)
```

```python
import numpy as np
import concourse.bass as bass
import concourse.mybir as mybir
from concourse.bass_utils import run_bass_kernel_spmd
from contextlib import ExitStack

F32 = mybir.dt.float32
BF16 = mybir.dt.bfloat16
U32 = mybir.dt.uint32
I32 = mybir.dt.int32
ACT = mybir.ActivationFunctionType
ALU = mybir.AluOpType
AX = mybir.AxisListType
PoolE = mybir.EngineType.Pool

D = 1024
NL = 4096
NCX = 256
NT = NL + NCX
NE = 32
EPS = 1e-6


class Dep:
    __slots__ = ("w", "r", "name")

    def __init__(self, name=""):
        self.w = None
        self.r = []
        self.name = name


class KB:
    NSLOT = 8

    def __init__(self, nc, es):
        self.nc = nc
        self.engs = {"pe": nc.tensor, "dve": nc.vector, "act": nc.scalar,
                     "pool": nc.gpsimd, "sp": nc.sync}
        self.sem = {}
        self.cnt = {}
        for k in self.engs:
            self.sem[k] = es.enter_context(nc.semaphore("s_" + k))
            self.cnt[k] = 0
        self.slots = {}
        self.slot_rr = {}
        for q in ("sp", "act", "pool"):
            self.slots[q] = [[es.enter_context(nc.semaphore(f"d_{q}{i}")), 0]
                             for i in range(self.NSLOT)]
            self.slot_rr[q] = 0
        self.slots["conv"] = [[es.enter_context(nc.semaphore(f"d_conv{i}")), 0] for i in range(6)]
        self.slot_rr["conv"] = 0
        self.pending = []
        self.seen = {k: {} for k in self.engs}
        self.ninst = 0
        self.nwaits = 0

    def _wait(self, eng, tok):
        if tok is None:
            return
        sem, val, key = tok
        if eng == "pe" and key == "pe":
            return
        s = self.seen[eng]
        if s.get(key, 0) >= val:
            return
        self.engs[eng].wait_ge(sem, val)
        s[key] = val
        self.nwaits += 1

    def _deps(self, eng, reads, writes):
        for d in reads:
            self._wait(eng, d.w)
        for d in writes:
            self._wait(eng, d.w)
            for t in d.r:
                self._wait(eng, t)

    def _commit(self, tok, reads, writes):
        for d in reads:
            d.r.append(tok)
            if len(d.r) > 16:
                best = {}
                for t in d.r:
                    if t[2] not in best or best[t[2]][1] < t[1]:
                        best[t[2]] = t
                d.r = list(best.values())
        for d in writes:
            d.w = tok
            d.r = []

    dead = False

    def op(self, eng, fn, reads=(), writes=()):
        if KB.dead:
            return None
        self._deps(eng, reads, writes)
        inst = fn()
        self.cnt[eng] += 1
        inst.then_inc(self.sem[eng], 1)
        tok = (self.sem[eng], self.cnt[eng], eng)
        self._commit(tok, reads, writes)
        self.ninst += 1
        return tok

    def pump(self, n=1):
        for _ in range(n):
            if not self.pending:
                return
            fn, reads, writes = self.pending.pop(0)
            self.dma("pool", fn, reads=reads, writes=writes, grp="conv")

    def dma(self, q, fn, reads=(), writes=(), grp=None):
        if KB.dead:
            return None
        grp = grp or q
        i = self.slot_rr[grp]
        self.slot_rr[grp] = (i + 1) % len(self.slots[grp])
        slot = self.slots[grp][i]
        key = f"d_{grp}{i}"
        if slot[1] > 0:
            self._wait(q, (slot[0], slot[1], key))
        self._deps(q, reads, writes)
        inst = fn()
        slot[1] += 16
        inst.then_inc(slot[0], 16)
        tok = (slot[0], slot[1], key)
        self._commit(tok, reads, writes)
        self.ninst += 1
        return tok

    def wait_all(self, eng, deps):
        for d in deps:
            self._wait(eng, d.w)
            for t in d.r:
                self._wait(eng, t)

    def barrier(self, conv=False):
        for e in self.engs:
            self.finish(e, conv)

    def finish(self, eng="sp", conv=True):
        for k in self.engs:
            if self.cnt[k] > 0:
                self._wait(eng, (self.sem[k], self.cnt[k], k))
        for q in self.slots:
            if q == "conv" and not conv:
                continue
            for i, s in enumerate(self.slots[q]):
                if s[1] > 0:
                    self._wait(eng, (s[0], s[1], f"d_{q}{i}"))


class T:
    _ctr = [0]

    def __init__(self, kb, es, shape, dtype, name, psum=False, nd=1):
        nc = kb.nc
        T._ctr[0] += 1
        name = f"{name}_{T._ctr[0]}"
        if psum:
            self.t = es.enter_context(nc.psum_tensor(name, list(shape), dtype))
        else:
            self.t = es.enter_context(nc.sbuf_tensor(name, list(shape), dtype))
        self.ds = [Dep(f"{name}.{i}") for i in range(nd)]
        self.d = self.ds[0]
        self.shape = shape

    def __getitem__(self, k):
        return self.t[k]


class Ctx:
    pass


class StopBuild(Exception):
    pass


def ckpt(name):
    if STOP == name:
        KB.dead = True


STOP = None


def build(dbg=(), stop=None):
    global STOP
    STOP = stop
    KB.dead = False
    nc = bass.Bass("TRN2", target_bir_lowering=False)
    g = Ctx()
    g.nc = nc
    g.dbg = set(dbg)
    g.outs = {}
    g.out_d = Dep("out")

    def din(name, shape, dt=F32):
        return nc.dram_tensor(name, list(shape), dt, kind="ExternalInput")

    H = Ctx()
    g.H = H
    H.x = din("x", [NL, D])
    H.ctx = din("ctx", [NCX, D])
    H.c = din("c", [1, D])
    H.c_ctx = din("c_ctx", [D])
    H.ada_w = din("ada_w", [1, D, 6 * D])
    H.ada_b = din("ada_b", [1, 6 * D])
    H.norm_mix_w = din("norm_mix_w", [1, D])
    H.w_in = din("w_in", [1, D, 2080])
    H.gla_lr_up = din("gla_lr_up", [1, 2, 16, 256])
    H.gla_lr_bias = din("gla_lr_bias", [1, 2, 256])
    H.gla_norm_w = din("gla_norm_w", [1, 128])
    H.s5_lam_re = din("s5_lam_re", [1, 2, 32, 64])
    H.s5_lam_im = din("s5_lam_im", [1, 2, 32, 64])
    H.s5_log_dt = din("s5_log_dt", [1, 2, 32])
    H.s5_b_re = din("s5_b_re", [1, 2, 32, 64, 16])
    H.s5_b_im = din("s5_b_im", [1, 2, 32, 64, 16])
    H.s5_c_re = din("s5_c_re", [1, 2, 32, 16, 64])
    H.s5_c_im = din("s5_c_im", [1, 2, 32, 16, 64])
    H.s5_d = din("s5_d", [1, 512])
    H.glu_w = din("glu_w", [1, 512, 512])
    H.glu_b = din("glu_b", [1, 512])
    H.w_out = din("w_out", [1, D, D])
    H.norm_ffn_w = din("norm_ffn_w", [1, D])
    H.router_w = din("router_w", [1, D, NE])
    H.router_b = din("router_b", [1, NE])
    H.exp_w_gu = din("exp_w_gu", [1, NE, D, 2 * D])
    H.exp_b_gu = din("exp_b_gu", [1, NE, 2 * D])
    H.exp_w_down = din("exp_w_down", [1, NE, D, D])
    H.exp_b_down = din("exp_b_down", [1, NE, D])
    H.final_norm_w = din("final_norm_w", [D])
    H.out = nc.dram_tensor("out", [NL, D], F32, kind="ExternalOutput")

    S = Ctx()
    g.S = S

    def scr(name, shape, dt):
        if name in g.dbg:
            h = nc.dram_tensor(name, list(shape), dt, kind="ExternalOutput")
        else:
            h = nc.dram_tensor(name, list(shape), dt)
        return h, Dep(name)

    S.mod, S.mod_d = scr("mod_s", [2, 6 * D], F32)
    S.qT, S.qT_d = scr("qT_s", [256, NT], BF16)
    S.kT, S.kT_d = scr("kT_s", [256, NT], BF16)
    S.rT, S.rT_d = scr("rT_s", [512, NL], BF16)
    S.lrT, S.lrT_d = scr("lrT_s", [2, 16, NT], F32)
    S.v, S.v_d = scr("v_s", [NT, 512], BF16)
    S.uT, S.uT_d = scr("uT_s", [512, NT], BF16)
    S.u, S.u_d = scr("u_s", [NT, 512], BF16)
    S.glaT, S.glaT_d = scr("glaT_s", [512, NL], BF16)
    S.y0, S.y0_d = scr("y0_s", [32, 128, 512], F32)
    S.Ut, S.Ut_d = scr("Ut_s", [32, 128, NT // 8], BF16)
    S.x2, S.x2_d = scr("x2_s", [NL, D], F32)
    S.h2, S.h2_d = scr("h2_s", [NL, D], BF16)
    S.xg, S.xg_d = scr("xg_s", [NSEG * SEG, D], BF16)
    S.yg, S.yg_d = scr("yg_s", [NSEG * SEG, D], BF16)
    S.bguT, S.bguT_d = scr("bguT_s", [NE, 128, 16], F32)
    S.wg, S.wg_d = scr("wg_s", [NE, 128, 8 * 2 * D], BF16)
    S.wd, S.wd_d = scr("wd_s", [NE, 128, 8 * D], BF16)

    with ExitStack() as es:
        kb = KB(nc, es)
        g.kb = kb
        g.es = es
        g.ident = T(kb, es, [128, 128], F32, "ident")
        g.identb = T(kb, es, [128, 128], BF16, "identb")
        io = T(kb, es, [128, 128], F32, "iota0")
        kb.op("pool", lambda: nc.gpsimd.iota(io[:], pattern=[[1, 128]], base=0, channel_multiplier=-1,
                                              allow_small_or_imprecise_dtypes=True), writes=[io.d])
        kb.op("dve", lambda: nc.vector.tensor_single_scalar(g.ident[:], io[:], 0.0, op=ALU.is_equal),
              reads=[io.d], writes=[g.ident.d])
        kb.op("dve", lambda: nc.vector.tensor_copy(g.identb[:], g.ident[:]), reads=[g.ident.d], writes=[g.identb.d])
        g.iota = io
        g.ps = [T(kb, es, [128, 512], F32, f"ps{i}", psum=True) for i in range(6)]
        g.psb = [T(kb, es, [128, 1024], BF16, f"psb{i}", psum=True) for i in range(2)]
        g.ps_rr = 0
        R = Ctx()
        g.R = R
        R.mask = T(kb, es, [128, 32, NE], F32, "r_mask")
        R.idxf = T(kb, es, [128, 32, 4], F32, "r_idxf")
        R.gate = T(kb, es, [128, 32, 4], F32, "r_gate")
        R.slot = T(kb, es, [128, 32, 4], I32, "r_slot", nd=32)
        R.segexp = T(kb, es, [128, NSEG], I32, "r_segexp")
        R.segidx = T(kb, es, [128, NSEG], I32, "r_segidx")
        R.segrow = T(kb, es, [128, NSEG], I32, "r_segrow")

        if stop is not None and str(stop).startswith("benchf"):
            kb.op("pool", lambda: nc.gpsimd.iota(R.segidx[:], pattern=[[0, NSEG]], base=0, channel_multiplier=1),
                  writes=[R.segidx.d])
            kb.op("pool", lambda: nc.gpsimd.memset(R.segrow[:], 0), writes=[R.segrow.d])
            g.bench = stop
            phase_f(g)
            kb.barrier()
            kb.finish("sp")
            print("instructions", kb.ninst, "waits", kb.nwaits)
            return nc, g
        phase_a(g)
        kb.barrier()
        for e in range(NE):
            kb.pending.append((lambda e=e: nc.gpsimd.dma_start(
                out=S.wg[e, :, :].rearrange("p (k n) -> p k n", k=8),
                in_=H.exp_w_gu[0, e, :, :].rearrange("(k p) n -> p k n", p=128)), [], [S.wg_d]))
            kb.pending.append((lambda e=e: nc.gpsimd.dma_start(
                out=S.wd[e, :, :].rearrange("p (k n) -> p k n", k=8),
                in_=H.exp_w_down[0, e, :, :].rearrange("(k p) n -> p k n", p=128)), [], [S.wd_d]))
        kb.pump(6)
        if stop != 'a':
            phase_b(g)
            kb.barrier()
            if stop != 'b':
                if stop not in ('d_only', 'e_only'):
                    phase_c(g)
                    kb.barrier()
                if stop not in ('c', 'c0', 'c1', 'c2', 'c3'):
                    if stop != 'e_only':
                        phase_d(g)
                        kb.barrier()
                    if stop not in ('d', 'd_only') and not KB.dead:
                        phase_e(g)
                        kb.barrier()
                        if stop != 'e' and not KB.dead:
                            kb.pump(1000)
                            kb.barrier(conv=True)
                            phase_f(g)
                            kb.barrier()
                            phase_g(g)
                            kb.barrier()

        kb.finish("sp")
        print("instructions", kb.ninst, "waits", kb.nwaits)
    return nc, g


def next_ps(g):
    p = g.ps[g.ps_rr]
    g.ps_rr = (g.ps_rr + 1) % len(g.ps)
    return p


def dbg_out(g, name, src_ap, deps, shape, dt=F32, q="sp"):
    if name not in g.dbg:
        return
    nc, kb = g.nc, g.kb
    o = nc.dram_tensor("dbg_" + name, list(shape), dt, kind="ExternalOutput")
    g.outs[name] = o
    kb.dma(q, lambda: nc.sync.dma_start(out=o.ap(), in_=src_ap), reads=deps)


def phase_a(g):
    nc, kb, H, S = g.nc, g.kb, g.H, g.S
    with ExitStack() as es:
        cc = T(kb, es, [128, 8, 2], F32, "cc")
        kb.dma("sp", lambda: nc.sync.dma_start(out=cc[:, :, 0], in_=H.c[0, :].rearrange("(k p) -> p k", p=128),
                                               allow_slow_non_contiguous=True), writes=[cc.d])
        kb.dma("sp", lambda: nc.sync.dma_start(out=cc[:, :, 1], in_=H.c_ctx.ap().rearrange("(k p) -> p k", p=128),
                                               allow_slow_non_contiguous=True), writes=[cc.d])
        kb.op("act", lambda: nc.scalar.activation(cc[:], cc[:], ACT.Silu), reads=[cc.d], writes=[cc.d])
        ab = T(kb, es, [2, 6 * D], F32, "ab")
        kb.dma("sp", lambda: nc.sync.dma_start(out=ab[0:1, :], in_=H.ada_b[0:1, :]), writes=[ab.d])
        kb.dma("sp", lambda: nc.sync.dma_start(out=ab[1:2, :], in_=H.ada_b[0:1, :]), writes=[ab.d])
        modsb = T(kb, es, [2, 6 * D], F32, "modsb")
        aw = [T(kb, es, [128, 8, 512], F32, f"aw{i}") for i in range(2)]
        for j in range(12):
            a = aw[j % 2]
            kb.dma("sp" if j % 2 == 0 else "act",
                   (lambda a=a, j=j: (nc.sync if j % 2 == 0 else nc.scalar).dma_start(
                       out=a[:], in_=H.ada_w[0, :, j * 512:(j + 1) * 512].rearrange("(k p) n -> p k n", p=128))),
                   writes=[a.d])
            ps = next_ps(g)
            for k in range(8):
                kb.op("pe", lambda k=k: nc.tensor.matmul(ps[0:2, :], lhsT=cc[:, k, :], rhs=a[:, k, :],
                                                         start=(k == 0), stop=(k == 7)),
                      reads=[cc.d, a.d], writes=[ps.d])
            kb.op("dve", lambda j=j: nc.vector.tensor_tensor(modsb[:, j * 512:(j + 1) * 512], ps[0:2, :],
                                                            ab[:, j * 512:(j + 1) * 512], op=ALU.add),
                  reads=[ps.d, ab.d], writes=[modsb.d])
        kb.dma("sp", lambda: nc.sync.dma_start(out=S.mod.ap(), in_=modsb[:]), reads=[modsb.d], writes=[S.mod_d])


def load_fm_vec(g, es, name, src_ap_1d):
    nc, kb = g.nc, g.kb
    t = T(kb, es, [128, 8], F32, name)
    kb.dma("sp", lambda: nc.sync.dma_start(out=t[:], in_=src_ap_1d.rearrange("(k p) -> p k", p=128),
                                           allow_slow_non_contiguous=True), reads=[g.S.mod_d], writes=[t.d])
    return t


def s5_cols(hT, k, s0, n):
    if s0 < NCX:
        assert s0 + n <= NCX
        return hT[:, k, s0:s0 + n]
    c0 = (s0 - NCX) // 64
    ncol = n // 64
    v = hT[:, k, NCX:NT].rearrange("p (row col) -> p col row", col=64)
    return v[:, c0:c0 + ncol, :]


def phase_b(g):
    nc, kb, H, S = g.nc, g.kb, g.H, g.S
    with ExitStack() as es:
        sh1 = load_fm_vec(g, es, "sh1", S.mod[0, 0:D])
        sc1 = load_fm_vec(g, es, "sc1", S.mod[0, D:2 * D])
        csh1 = load_fm_vec(g, es, "csh1", S.mod[1, 0:D])
        csc1 = load_fm_vec(g, es, "csc1", S.mod[1, D:2 * D])
        nmw = load_fm_vec(g, es, "nmw", H.norm_mix_w[0, :])
        g1f = T(kb, es, [128, 8], F32, "g1f")
        cg1f = T(kb, es, [128, 8], F32, "cg1f")
        kb.op("dve", lambda: nc.vector.scalar_tensor_tensor(g1f[:], sc1[:], 1.0, nmw[:], op0=ALU.add, op1=ALU.mult),
              reads=[sc1.d, nmw.d], writes=[g1f.d])
        kb.op("dve", lambda: nc.vector.scalar_tensor_tensor(cg1f[:], csc1[:], 1.0, nmw[:], op0=ALU.add, op1=ALU.mult),
              reads=[csc1.d, nmw.d], writes=[cg1f.d])
        wi = T(kb, es, [128, 8, 2080], BF16, "wi")
        for (a, b) in ((0, 1040), (1040, 2080)):
            kb.dma("pool", lambda a=a, b=b: nc.gpsimd.dma_start(
                out=wi[:, :, a:b], in_=H.w_in[0, :, a:b].rearrange("(k p) n -> p k n", p=128)), writes=[wi.d])
        hT = T(kb, es, [128, 8, NT], BF16, "hT", nd=9)
        xg = [T(kb, es, [128, 4, D], F32, f"xg{i}") for i in range(2)]
        junk = T(kb, es, [128, D], BF16, "junkb")
        groups = [("ctx", 0, 2)] + [("lat", gi, 4) for gi in range(8)]
        for gi, (kind, idx, ntile) in enumerate(groups):
            kb.pump(1)
            xt = xg[gi % 2]
            ntok = ntile * 128
            if kind == "ctx":
                src = H.ctx[0:ntok, :]
                col0 = 0
                gsc, gsh = cg1f, csh1
            else:
                src = H.x[idx * 512:(idx + 1) * 512, :]
                col0 = NCX + idx * 512
                gsc, gsh = g1f, sh1
            q = "sp"
            kb.dma(q, lambda xt=xt, src=src, ntile=ntile, q=q: (nc.sync if q == "sp" else nc.scalar).dma_start(
                out=xt[:, 0:ntile, :], in_=src.rearrange("(j p) d -> p j d", p=128)), writes=[xt.d])
            ss = T(kb, es, [128, 4], F32, f"ss{gi}")
            rs = T(kb, es, [128, 4], F32, f"rs{gi}")
            for j in range(ntile):
                kb.op("act", lambda j=j: nc.scalar.activation(junk[:], xt[:, j, :], ACT.Square,
                                                              accum_out=ss[:, j:j + 1]),
                      reads=[xt.d], writes=[junk.d, ss.d])
            kb.op("dve", lambda: nc.vector.tensor_scalar(rs[:, 0:ntile], ss[:, 0:ntile], 1.0 / D, EPS,
                                                         op0=ALU.mult, op1=ALU.add), reads=[ss.d], writes=[rs.d])
            kb.op("act", lambda: nc.scalar.activation(rs[:, 0:ntile], rs[:, 0:ntile], ACT.Sqrt),
                  reads=[rs.d], writes=[rs.d])
            kb.op("dve", lambda: nc.vector.reciprocal(rs[:, 0:ntile], rs[:, 0:ntile]), reads=[rs.d], writes=[rs.d])
            for j in range(ntile):
                kb.op("dve", lambda j=j: nc.vector.tensor_scalar(xt[:, j, :], xt[:, j, :], rs[:, j:j + 1], None,
                                                                 op0=ALU.mult), reads=[xt.d, rs.d], writes=[xt.d])
            for k in range(8):
                ps = next_ps(g)
                for j in range(ntile):
                    kb.op("pe", lambda j=j, k=k: nc.tensor.transpose(ps[:, j * 128:(j + 1) * 128],
                                                                     xt[:, j, k * 128:(k + 1) * 128], g.ident[:]),
                          reads=[xt.d, g.ident.d], writes=[ps.d])
                kb.op("act", lambda k=k, ps=ps: nc.scalar.activation(
                    hT[:, k, col0:col0 + ntok], ps[:, 0:ntok], ACT.Identity,
                    bias=gsh[:, k:k + 1], scale=gsc[:, k:k + 1]),
                    reads=[ps.d, gsh.d, gsc.d], writes=[hT.ds[gi]])
        hall = hT.ds
        if "hT" in g.dbg:
            dbg_out(g, "hT", hT[:], hall, [128, 8, NT], BF16)
        stg = [T(kb, es, [128, NT], BF16, f"stg{i}") for i in range(2)]
        stgf = T(kb, es, [16, NT], F32, "stgf")
        rr = [0]

        def fm_proj(c0, m, dst_fn, t0, t1, cols_fn, f32=False):
            kb.pump(1)
            st = stgf if f32 else stg[rr[0] % 2]
            rr[0] += 0 if f32 else 1
            t = t0
            while t < t1:
                n = min(512, t1 - t)
                if t < NCX:
                    n = min(n, NCX - t)
                ps = next_ps(g)
                for k in range(8):
                    kb.op("pe", lambda k=k, t=t, n=n: nc.tensor.matmul(
                        ps[0:m, 0:n], lhsT=wi[:, k, c0:c0 + m], rhs=cols_fn(k, t, n),
                        start=(k == 0), stop=(k == 7)), reads=[wi.d] + hall, writes=[ps.d])
                eng = "act" if (t // 512) % 2 == 0 else "dve"
                if eng == "act":
                    kb.op("act", lambda t=t, n=n, ps=ps: nc.scalar.copy(st[0:m, t:t + n], ps[0:m, 0:n]),
                          reads=[ps.d], writes=[st.d])
                else:
                    kb.op("dve", lambda t=t, n=n, ps=ps: nc.vector.tensor_copy(st[0:m, t:t + n], ps[0:m, 0:n]),
                          reads=[ps.d], writes=[st.d])
                t += n
            dst, dd = dst_fn()
            kb.dma("sp", lambda: nc.sync.dma_start(out=dst, in_=st[0:m, t0:t1]), reads=[st.d], writes=[dd])

        raster = lambda k, t, n: hT[:, k, t:t + n]
        s5t = lambda k, t, n: s5_cols(hT, k, t, n)
        for mt in range(2):
            fm_proj(mt * 128, 128, lambda mt=mt: (S.qT[mt * 128:(mt + 1) * 128, :], S.qT_d), 0, NT, raster)
        for mt in range(2):
            fm_proj(256 + mt * 128, 128, lambda mt=mt: (S.kT[mt * 128:(mt + 1) * 128, :], S.kT_d), 0, NT, raster)
        for mt in range(4):
            fm_proj(1024 + mt * 128, 128, lambda mt=mt: (S.rT[mt * 128:(mt + 1) * 128, :], S.rT_d), NCX, NT, raster)
        for z in range(2):
            fm_proj(1536 + z * 16, 16, lambda z=z: (S.lrT[z, :, :], S.lrT_d), 0, NT, raster, f32=True)
        for mt in range(4):
            fm_proj(1568 + mt * 128, 128, lambda mt=mt: (S.uT[mt * 128:(mt + 1) * 128, :], S.uT_d), 0, NT, s5t)
        st4 = [T(kb, es, [128, 4, 512], BF16, f"st4_{i}") for i in range(2)]
        ngrp = 0
        for (c0, dst, dd, s5) in ((512, S.v, S.v_d, False), (1568, S.u, S.u_d, True)):
            for t0 in list(range(0, NCX, 512)) + list(range(NCX, NT, 512)):
                nt = 2 if t0 < NCX else 4
                st = st4[ngrp % 2]
                ngrp += 1
                for j in range(nt):
                    ps = next_ps(g)
                    tt = t0 + j * 128
                    if s5 and tt >= NCX:
                        for hf in range(2):
                            col = (tt - NCX) // 64 + hf
                            for k in range(8):
                                lh = hT[:, k, NCX + col:NT:64]
                                kb.op("pe", lambda k=k, lh=lh, ps=ps, hf=hf: nc.tensor.matmul(
                                    ps[hf * 64:(hf + 1) * 64, :], lhsT=lh, rhs=wi[:, k, c0:c0 + 512],
                                    start=(k == 0), stop=(k == 7)), reads=[wi.d] + hall, writes=[ps.d])
                    else:
                        for k in range(8):
                            lh = hT[:, k, tt:tt + 128]
                            kb.op("pe", lambda k=k, lh=lh, ps=ps: nc.tensor.matmul(
                                ps[:, :], lhsT=lh, rhs=wi[:, k, c0:c0 + 512], start=(k == 0), stop=(k == 7)),
                                reads=[wi.d] + hall, writes=[ps.d])
                    if j % 2 == 0:
                        kb.op("act", lambda j=j, ps=ps, st=st: nc.scalar.copy(st[:, j, :], ps[:, :]),
                              reads=[ps.d], writes=[st.d])
                    else:
                        kb.op("dve", lambda j=j, ps=ps, st=st: nc.vector.tensor_copy(st[:, j, :], ps[:, :]),
                              reads=[ps.d], writes=[st.d])
                kb.dma("sp", lambda st=st, t0=t0, nt=nt, dst=dst: nc.sync.dma_start(
                    out=dst[t0:t0 + nt * 128, :].rearrange("(j p) e -> p j e", p=128), in_=st[:, 0:nt, :]),
                    reads=[st.d], writes=[dd])


def phase_c(g):
    nc, kb, H, S = g.nc, g.kb, g.H, g.S
    NCH = NT // 64
    with ExitStack() as es:
        psb = g.psb[0]
        lup = T(kb, es, [16, 2, 256], F32, "lup")
        kb.dma("sp", lambda: nc.sync.dma_start(out=lup[:], in_=H.gla_lr_up[0].rearrange("z r f -> r z f")),
               writes=[lup.d])
        nb = T(kb, es, [128, 2, 2], F32, "nbias")
        kb.dma("sp", lambda: nc.sync.dma_start(out=nb[:], in_=H.gla_lr_bias[0].rearrange("z (t p) -> p z t", p=128),
                                               allow_slow_non_contiguous=True), writes=[nb.d])
        kb.op("dve", lambda: nc.vector.tensor_scalar(nb[:], nb[:], -1.0, None, op0=ALU.mult), reads=[nb.d], writes=[nb.d])
        nw = T(kb, es, [128, 1], F32, "gnw")
        kb.dma("sp", lambda: nc.sync.dma_start(out=nw[:], in_=H.gla_norm_w[0, :].rearrange("(p o) -> p o", o=1)),
               writes=[nw.d])
        ones = T(kb, es, [128, 128], BF16, "onesb")
        kb.op("pool", lambda: nc.gpsimd.memset(ones[:], 1.0), writes=[ones.d])
        io = g.iota
        mk = []
        for z in range(2):
            m4 = T(kb, es, [128, 4, 64], BF16, f"tri{z}")
            op = ALU.is_ge if z == 0 else ALU.is_le
            for h in range(4):
                kb.op("dve", lambda h=h, m4=m4, op=op: nc.vector.tensor_single_scalar(
                    m4[0:64, h, :], io[0:64, 0:64], 0.0, op=op), reads=[io.d], writes=[m4.d])
                kb.op("dve", lambda h=h, m4=m4, op=op: nc.vector.tensor_single_scalar(
                    m4[64:128, h, :], io[64:128, 0:64], -64.0, op=op), reads=[io.d], writes=[m4.d])
            mk.append(m4)
        qt = [T(kb, es, [128, 2, NT], BF16, f"qtl{z}") for z in range(2)]
        kt = [T(kb, es, [128, 2, NT], BF16, f"ktl{z}") for z in range(2)]
        elast = [T(kb, es, [128, 2, NCH], F32, f"elast{z}") for z in range(2)]
        if STOP == 'c0':
            return
        with ExitStack() as es2:
            mask = T(kb, es2, [128, NT + 1], BF16, "cmask")
            kb.op("pool", lambda: nc.gpsimd.memset(mask[:], 1.0), writes=[mask.d])
            kb.op("pool", lambda: nc.gpsimd.memset(mask[:, 0:NT + 1:64], 0.0), writes=[mask.d])
            lrtz = T(kb, es2, [16, NT], F32, "lrt")
            A = T(kb, es2, [128, NT], F32, "scrA")
            B = T(kb, es2, [128, NT], F32, "scrB")
            raw = [T(kb, es2, [128, NT], BF16, f"raw{i}") for i in range(2)]
            for z in range(2):
                kb.dma("sp", lambda z=z: nc.sync.dma_start(out=lrtz[:], in_=S.lrT[z, :, :]),
                       reads=[S.lrT_d], writes=[lrtz.d])
                for pt in range(2):
                    if STOP == 'c1a' and (z, pt) != (0, 0):
                        continue
                    for t0 in range(0, NT, 512):
                        n = min(512, NT - t0)
                        ps = next_ps(g)
                        kb.op("pe", lambda t0=t0, n=n, ps=ps: nc.tensor.matmul(
                            ps[:, 0:n], lhsT=lup[:, z, pt * 128:(pt + 1) * 128], rhs=lrtz[:, t0:t0 + n],
                            start=True, stop=True), reads=[lup.d, lrtz.d], writes=[ps.d])
                        kb.op("act", lambda t0=t0, n=n, ps=ps: nc.scalar.activation(
                            A[:, t0:t0 + n], ps[:, 0:n], ACT.Exp, bias=nb[:, z, pt:pt + 1], scale=-1.0),
                            reads=[ps.d, nb.d], writes=[A.d])
                    ckpt("k1")
                    kb.op("act", lambda: nc.scalar.activation(A[:], A[:], ACT.Ln, bias=1.0, scale=1.0),
                          reads=[A.d], writes=[A.d])
                    ckpt("k2")
                    if z == 0:
                        kb.op("dve", lambda: nc.vector.tensor_tensor_scan(B[:], mask[:, 0:NT], A[:], 0.0,
                                                                          ALU.mult, ALU.add),
                              reads=[mask.d, A.d], writes=[B.d])
                    else:
                        kb.op("dve", lambda: nc.vector.tensor_tensor_scan(B[:, NT - 1::-1] if False else B[:, ::-1],
                                                                          mask[:, NT:0:-1], A[:, ::-1], 0.0,
                                                                          ALU.mult, ALU.add),
                              reads=[mask.d, A.d], writes=[B.d])
                    ckpt("k3")
                    kb.op("act", lambda: nc.scalar.activation(A[:], B[:], ACT.Exp, scale=-1.0 / 16.0),
                          reads=[B.d], writes=[A.d])
                    kb.op("act", lambda: nc.scalar.activation(B[:], B[:], ACT.Exp, scale=1.0 / 16.0),
                          reads=[B.d], writes=[B.d])
                    ckpt("k4")
                    rq, rk = raw
                    kb.dma("sp", lambda: nc.sync.dma_start(out=rq[:], in_=S.qT[pt * 128:(pt + 1) * 128, :]),
                           reads=[S.qT_d], writes=[rq.d])
                    kb.dma("sp", lambda: nc.sync.dma_start(out=rk[:], in_=S.kT[pt * 128:(pt + 1) * 128, :]),
                           reads=[S.kT_d], writes=[rk.d])
                    ckpt("k5")
                    kb.op("dve", lambda: nc.vector.scalar_tensor_tensor(qt[z][:, pt, :], rq[:], 0.125, A[:],
                                                                        op0=ALU.mult, op1=ALU.mult),
                          reads=[rq.d, A.d], writes=[qt[z].d])
                    ckpt("k6")
                    kb.op("pool", lambda: nc.gpsimd.tensor_tensor(kt[z][:, pt, :], rk[:], B[:], op=ALU.mult),
                          reads=[rk.d, B.d], writes=[kt[z].d])
                    ckpt("k7")
                    e0 = 63 if z == 0 else 0
                    kb.op("dve", lambda: nc.vector.tensor_copy(elast[z][:, pt, :], A[:, e0:NT:64]),
                          reads=[A.d], writes=[elast[z].d])
        kb.barrier()
        if STOP == 'c1':
            return
        vt = T(kb, es, [128, NT // 128, 512], BF16, "vt")
        kb.dma("act", lambda: nc.scalar.dma_start(out=vt[:], in_=S.v.ap().rearrange("(n p) e -> p n e", p=128)),
               reads=[S.v_d], writes=[vt.d])
        ktm = [T(kb, es, [128, NT // 128, 256], BF16, f"ktm{z}") for z in range(2)]
        oT = T(kb, es, [128, 4, NL], BF16, "oT", nd=NL // 64)
        for z in range(2):
            for n2 in range(0, NT // 128, 2):
                for a in range(2):
                    for pt in range(2):
                        kb.op("pe", lambda a=a, pt=pt, n2=n2: nc.tensor.transpose(
                            psb[:, (a * 2 + pt) * 128:(a * 2 + pt + 1) * 128],
                            kt[z][:, pt, (n2 + a) * 128:(n2 + a + 1) * 128], g.identb[:]),
                            reads=[kt[z].d, g.identb.d], writes=[psb.d])
                kb.op("act", lambda n2=n2: nc.scalar.copy(
                    ktm[z][:, n2:n2 + 2, :], psb[:, 0:512].rearrange("p (a f) -> p a f", a=2)),
                    reads=[psb.d], writes=[ktm[z].d])
        if "qtl" in g.dbg:
            dbg_out(g, "qtl0", qt[0][:], [qt[0].d], [128, 2, NT], BF16)
            dbg_out(g, "ktl1", kt[1][:], [kt[1].d], [128, 2, NT], BF16)
            dbg_out(g, "ktm1", ktm[1][:], [ktm[1].d], [128, NT // 128, 256], BF16)
            dbg_out(g, "elast1", elast[1][:], [elast[1].d], [128, 2, NCH])
        if STOP == 'c2':
            return
        Sst = [T(kb, es, [128, 2, 128], F32, f"Sst{z}") for z in range(2)]
        SbfZ = [T(kb, es, [128, 4, 128], BF16, f"SbfZ{z}") for z in range(2)]
        tmpS = [T(kb, es, [128, 2, 128], F32, f"tmpS{z}") for z in range(2)]
        smZ = [[T(kb, es, [128, 4, 64], BF16, f"smZ{z}_{i}") for i in range(2)] for z in range(2)]
        for z in range(2):
            kb.op("pool", lambda z=z: nc.gpsimd.memset(Sst[z][:], 0.0), writes=[Sst[z].d])
            kb.op("pool", lambda z=z: nc.gpsimd.memset(SbfZ[z][:], 0.0), writes=[SbfZ[z].d])
            for i in range(2):
                kb.op("pool", lambda z=z, i=i: nc.gpsimd.memset(smZ[z][i][:], 0.0), writes=[smZ[z][i].d])
        order = [list(range(NCH)), [3, 2, 1, 0] + list(range(NCH - 1, 3, -1))]
        written = set()
        for i in range(NCH):
            if i % 4 == 0:
                kb.pump(1)
            for z in range(2):
                n = order[z][i]
                t0 = 64 * n
                nt = n // 2
                jo = (n % 2) * 64
                if n >= 4:
                    smt = smZ[z][n % 2]
                    for par in range(2):
                        ho = par * 64
                        ps_s = next_ps(g)
                        for hh in range(2):
                            h = hh * 2 + par
                            pt = hh
                            kb.op("pe", lambda h=h, pt=pt, ho=ho, ps_s=ps_s, hh=hh: nc.tensor.matmul(
                                ps_s[jo:jo + 64, hh * 64:(hh + 1) * 64], lhsT=kt[z][ho:ho + 64, pt, t0:t0 + 64],
                                rhs=qt[z][ho:ho + 64, pt, t0:t0 + 64], start=True, stop=True),
                                reads=[kt[z].d, qt[z].d], writes=[ps_s.d])
                        kb.op("dve", lambda ps_s=ps_s, smt=smt, par=par: nc.vector.tensor_tensor(
                            smt[jo:jo + 64, par:4:2, :], ps_s[jo:jo + 64, 0:128].rearrange("p (h i) -> p h i", h=2),
                            mk[z][jo:jo + 64, 0:2, :], op=ALU.mult), reads=[ps_s.d, mk[z].d], writes=[smt.d])
                    ps_o = next_ps(g)
                    for h in range(4):
                        pt = h // 2
                        kb.op("pe", lambda h=h, ps_o=ps_o, smt=smt: nc.tensor.matmul(
                            ps_o[:, h * 64:(h + 1) * 64], lhsT=vt[:, nt, h * 128:(h + 1) * 128],
                            rhs=smt[:, h, :], start=True, stop=False),
                            reads=[vt.d, smt.d], writes=[ps_o.d])
                        kb.op("pe", lambda h=h, pt=pt, ps_o=ps_o: nc.tensor.matmul(
                            ps_o[:, h * 64:(h + 1) * 64], lhsT=SbfZ[z][:, h, :],
                            rhs=qt[z][:, pt, t0:t0 + 64], start=False, stop=True),
                            reads=[SbfZ[z].d, qt[z].d], writes=[ps_o.d])
                    tl = t0 - NCX
                    od = oT.ds[tl // 64]
                    osl = oT[:, :, tl:tl + 64]
                    pv = ps_o[:, 0:256].rearrange("p (h i) -> p h i", h=4)
                    if n not in written:
                        written.add(n)
                        kb.op("act", lambda osl=osl, pv=pv: nc.scalar.copy(osl, pv), reads=[ps_o.d], writes=[od])
                    else:
                        kb.op("dve", lambda osl=osl, pv=pv: nc.vector.tensor_tensor(osl, pv, osl, op=ALU.add),
                              reads=[ps_o.d, od], writes=[od])
                ps_kv = next_ps(g)
                for h in range(4):
                    pt, ho = h // 2, (h % 2) * 64
                    kb.op("pe", lambda h=h, pt=pt, ho=ho, ps_kv=ps_kv: nc.tensor.matmul(
                        ps_kv[ho:ho + 64, pt * 128:(pt + 1) * 128], lhsT=ktm[z][jo:jo + 64, nt, h * 64:(h + 1) * 64],
                        rhs=vt[jo:jo + 64, nt, h * 128:(h + 1) * 128], start=True, stop=True),
                        reads=[ktm[z].d, vt.d], writes=[ps_kv.d])
                kb.op("dve", lambda ps_kv=ps_kv: nc.vector.tensor_tensor(
                    tmpS[z][:], ps_kv[:, 0:256].rearrange("p (t e) -> p t e", t=2), Sst[z][:], op=ALU.add),
                    reads=[ps_kv.d, Sst[z].d], writes=[tmpS[z].d])
                kb.op("dve", lambda n=n: nc.vector.tensor_tensor(
                    Sst[z][:], tmpS[z][:], elast[z][:, :, n:n + 1].to_broadcast([128, 2, 128]), op=ALU.mult),
                    reads=[tmpS[z].d, elast[z].d], writes=[Sst[z].d])
                for par in range(2):
                    ho = par * 64
                    kb.op("act", lambda par=par, ho=ho: nc.scalar.copy(SbfZ[z][ho:ho + 64, par:4:2, :],
                                                                       Sst[z][ho:ho + 64, :, :]),
                          reads=[Sst[z].d], writes=[SbfZ[z].d])
        if "oT" in g.dbg:
            dbg_out(g, "oT", oT[:], oT.ds, [128, 4, NL], BF16)
        if STOP == 'c3':
            return
        sq = T(kb, es, [128, 512], BF16, "gsq")
        rstd = T(kb, es, [128, 512], F32, "grstd")
        rt = [T(kb, es, [128, 4, 512], BF16, f"grt{i}") for i in range(2)]
        gl = [T(kb, es, [128, 4, 512], BF16, f"ggl{i}") for i in range(1)]
        tmpb = T(kb, es, [128, 512], BF16, "gtmp")
        for sp in range(NL // 512):
            c0 = sp * 512
            r_t, g_t = rt[sp % 2], gl[0]
            kb.dma("sp", lambda r_t=r_t, c0=c0: nc.sync.dma_start(
                out=r_t[:], in_=S.rT[:, c0:c0 + 512].rearrange("(m p) t -> p m t", p=128)),
                reads=[S.rT_d], writes=[r_t.d])
            kb.op("act", lambda r_t=r_t: nc.scalar.activation(r_t[:], r_t[:], ACT.Silu), reads=[r_t.d], writes=[r_t.d])
            ods = oT.ds[c0 // 64:(c0 + 512) // 64]
            for h in range(4):
                kb.op("dve", lambda h=h: nc.vector.tensor_tensor(sq[:], oT[:, h, c0:c0 + 512], oT[:, h, c0:c0 + 512],
                                                                 op=ALU.mult), reads=ods, writes=[sq.d])
                ps = next_ps(g)
                kb.op("pe", lambda ps=ps: nc.tensor.matmul(ps[:, :], lhsT=ones[:], rhs=sq[:], start=True, stop=True),
                      reads=[ones.d, sq.d], writes=[ps.d])
                kb.op("dve", lambda ps=ps: nc.vector.tensor_scalar(rstd[:], ps[:, :], 1.0 / 128.0, EPS,
                                                                   op0=ALU.mult, op1=ALU.add),
                      reads=[ps.d], writes=[rstd.d])
                kb.op("act", lambda: nc.scalar.activation(rstd[:], rstd[:], ACT.Sqrt), reads=[rstd.d], writes=[rstd.d])
                kb.op("dve", lambda: nc.vector.reciprocal(rstd[:], rstd[:]), reads=[rstd.d], writes=[rstd.d])
                kb.op("dve", lambda h=h: nc.vector.scalar_tensor_tensor(
                    tmpb[:], oT[:, h, c0:c0 + 512], nw[:, 0:1], rstd[:], op0=ALU.mult, op1=ALU.mult),
                    reads=ods + [nw.d, rstd.d], writes=[tmpb.d])
                kb.op("dve", lambda h=h, g_t=g_t, r_t=r_t: nc.vector.tensor_tensor(
                    g_t[:, h, :], tmpb[:], r_t[:, h, :], op=ALU.mult), reads=[tmpb.d, r_t.d], writes=[g_t.d])
            kb.dma("act", lambda g_t=g_t, c0=c0: nc.scalar.dma_start(
                out=S.glaT[:, c0:c0 + 512].rearrange("(m p) t -> p m t", p=128), in_=g_t[:]),
                reads=[g_t.d], writes=[S.glaT_d])


def cmul(g, out_r, out_i, ar, ai, br, bi, tmp, deps_in, dep_out, sl=None):
    nc, kb = g.nc, g.kb
    kb.op("dve", lambda: nc.vector.tensor_tensor(tmp, ai, bi, op=ALU.mult), reads=deps_in, writes=[dep_out])
    kb.op("dve", lambda: nc.vector.tensor_tensor(out_r, ar, br, op=ALU.mult), reads=deps_in, writes=[dep_out])
    kb.op("dve", lambda: nc.vector.tensor_tensor(out_r, out_r, tmp, op=ALU.subtract), reads=[dep_out], writes=[dep_out])
    kb.op("dve", lambda: nc.vector.tensor_tensor(tmp, ai, br, op=ALU.mult), reads=deps_in, writes=[dep_out])
    kb.op("dve", lambda: nc.vector.tensor_tensor(out_i, ar, bi, op=ALU.mult), reads=deps_in, writes=[dep_out])
    kb.op("dve", lambda: nc.vector.tensor_tensor(out_i, out_i, tmp, op=ALU.add), reads=[dep_out], writes=[dep_out])


def phase_d(g):
    nc, kb, H, S = g.nc, g.kb, g.H, g.S
    NCK = NT // 8
    NMAC = NCK // 16
    io = g.iota
    with ExitStack() as es:
        psb = g.psb[0]
        pd = Dep("s5par")
        P0 = T(kb, es, [128, 24, 64], F32, "s5p0")
        pl = lambda i: P0[:, i, :]
        LRE, LIM, DT, MAG, CS, SN, T1, T2, T3, LBR, LBI, CR, CI, IR, II = range(15)
        for half in range(2):
            rows = slice(half * 64, half * 64 + 64)
            kb.dma("sp", lambda rows=rows: nc.sync.dma_start(
                out=P0[rows, LRE, :], in_=H.s5_lam_re[0].rearrange("z g p -> p (z g)"),
                allow_slow_non_contiguous=True), writes=[pd])
            kb.dma("act", lambda rows=rows: nc.scalar.dma_start(
                out=P0[rows, LIM, :], in_=H.s5_lam_im[0].rearrange("z g p -> p (z g)"),
                allow_slow_non_contiguous=True), writes=[pd])
        kb.dma("sp", lambda: nc.sync.dma_start(
            out=pl(DT), in_=H.s5_log_dt[0:1, :, :].rearrange("o z g -> o (z g)").partition_broadcast(128)),
            writes=[pd])
        D1 = [pd]

        def v(fn):
            kb.op("dve", fn, reads=D1, writes=D1)

        def a(fn):
            kb.op("act", fn, reads=D1, writes=D1)

        a(lambda: nc.scalar.activation(pl(DT), pl(DT), ACT.Exp))
        v(lambda: nc.vector.tensor_tensor(pl(MAG), pl(LRE), pl(DT), op=ALU.mult))
        a(lambda: nc.scalar.activation(pl(MAG), pl(MAG), ACT.Exp))
        v(lambda: nc.vector.tensor_tensor(pl(T1), pl(LIM), pl(DT), op=ALU.mult))
        a(lambda: nc.scalar.activation(pl(SN), pl(T1), ACT.Sin, scale=1.0 / 16.0))
        a(lambda: nc.scalar.activation(pl(CS), pl(T1), ACT.Sin, bias=float(np.pi / 2), scale=1.0 / 16.0))
        for _ in range(4):
            v(lambda: nc.vector.tensor_tensor(pl(T2), pl(CS), pl(CS), op=ALU.mult))
            v(lambda: nc.vector.tensor_tensor(pl(T3), pl(SN), pl(SN), op=ALU.mult))
            v(lambda: nc.vector.scalar_tensor_tensor(pl(SN), pl(CS), 2.0, pl(SN), op0=ALU.mult, op1=ALU.mult))
            v(lambda: nc.vector.tensor_tensor(pl(CS), pl(T2), pl(T3), op=ALU.subtract))
        v(lambda: nc.vector.tensor_tensor(pl(LBR), pl(MAG), pl(CS), op=ALU.mult))
        v(lambda: nc.vector.tensor_tensor(pl(LBI), pl(MAG), pl(SN), op=ALU.mult))
        v(lambda: nc.vector.tensor_tensor(pl(T1), pl(LRE), pl(LRE), op=ALU.mult))
        v(lambda: nc.vector.tensor_tensor(pl(T2), pl(LIM), pl(LIM), op=ALU.mult))
        v(lambda: nc.vector.tensor_tensor(pl(T1), pl(T1), pl(T2), op=ALU.add))
        v(lambda: nc.vector.reciprocal(pl(T1), pl(T1)))
        v(lambda: nc.vector.tensor_scalar(pl(T2), pl(LBR), -1.0, None, op0=ALU.add))
        v(lambda: nc.vector.tensor_tensor(pl(CR), pl(T2), pl(LRE), op=ALU.mult))
        v(lambda: nc.vector.tensor_tensor(pl(T3), pl(LBI), pl(LIM), op=ALU.mult))
        v(lambda: nc.vector.tensor_tensor(pl(CR), pl(CR), pl(T3), op=ALU.add))
        v(lambda: nc.vector.tensor_tensor(pl(CR), pl(CR), pl(T1), op=ALU.mult))
        v(lambda: nc.vector.tensor_tensor(pl(CI), pl(LBI), pl(LRE), op=ALU.mult))
        v(lambda: nc.vector.tensor_tensor(pl(T3), pl(T2), pl(LIM), op=ALU.mult))
        v(lambda: nc.vector.tensor_tensor(pl(CI), pl(CI), pl(T3), op=ALU.subtract))
        v(lambda: nc.vector.tensor_tensor(pl(CI), pl(CI), pl(T1), op=ALU.mult))
        v(lambda: nc.vector.tensor_tensor(pl(T1), pl(LBR), pl(LBR), op=ALU.mult))
        v(lambda: nc.vector.tensor_tensor(pl(T2), pl(LBI), pl(LBI), op=ALU.mult))
        v(lambda: nc.vector.tensor_tensor(pl(T1), pl(T1), pl(T2), op=ALU.add))
        v(lambda: nc.vector.reciprocal(pl(T1), pl(T1)))
        v(lambda: nc.vector.tensor_tensor(pl(IR), pl(LBR), pl(T1), op=ALU.mult))
        v(lambda: nc.vector.scalar_tensor_tensor(pl(II), pl(LBI), -1.0, pl(T1), op0=ALU.mult, op1=ALU.mult))
        PW = T(kb, es, [128, 9, 2, 64], F32, "s5pw")
        NW = T(kb, es, [128, 8, 2, 64], F32, "s5nw")
        for W, br_, bi_, n in ((PW, LBR, LBI, 9), (NW, IR, II, 8)):
            v(lambda W=W: nc.vector.memset(W[:, 0, 0, :], 1.0))
            v(lambda W=W: nc.vector.memset(W[:, 0, 1, :], 0.0))
            for k in range(1, n):
                cmul(g, W[:, k, 0, :], W[:, k, 1, :], W[:, k - 1, 0, :], W[:, k - 1, 1, :], pl(br_), pl(bi_),
                     pl(T3), D1, pd)
        P128 = T(kb, es, [128, 2, 64], F32, "s5p128")
        v(lambda: nc.vector.tensor_copy(P128[:], PW[:, 8, :, :]))
        for _ in range(4):
            cmul(g, pl(T1), pl(T2), P128[:, 0, :], P128[:, 1, :], P128[:, 0, :], P128[:, 1, :], pl(T3), D1, pd)
            v(lambda: nc.vector.tensor_copy(P128[:, 0, :], pl(T1)))
            v(lambda: nc.vector.tensor_copy(P128[:, 1, :], pl(T2)))
        WN = T(kb, es, [128, 8, 2, 64], F32, "s5wn")
        WP = T(kb, es, [128, 8, 2, 64], F32, "s5wp")
        for k in range(8):
            cmul(g, WN[:, k, 0, :], WN[:, k, 1, :], NW[:, k, 0, :], NW[:, k, 1, :], pl(CR), pl(CI), pl(T3), D1, pd)
            cmul(g, WP[:, k, 0, :], WP[:, k, 1, :], PW[:, k, 0, :], PW[:, k, 1, :], pl(CR), pl(CI), pl(T3), D1, pd)
        v(lambda: nc.vector.tensor_scalar(PW[64:128, :, 0, :], PW[64:128, :, 0, :], -1.0, None, op0=ALU.mult))
        v(lambda: nc.vector.tensor_scalar(WN[0:64, :, 1, :], WN[0:64, :, 1, :], -1.0, None, op0=ALU.mult))
        v(lambda: nc.vector.tensor_scalar(WP[0:64, :, 1, :], WP[0:64, :, 1, :], -1.0, None, op0=ALU.mult))
        A8s = T(kb, es, [128, 2, 64], F32, "s5a8")
        v(lambda: nc.vector.tensor_copy(A8s[:, 1, :], PW[:, 8, 1, :]))
        v(lambda: nc.vector.tensor_copy(A8s[0:64, 0, :], PW[0:64, 8, 0, :]))
        v(lambda: nc.vector.tensor_scalar(A8s[64:128, 0, :], PW[64:128, 8, 0, :], -1.0, None, op0=ALU.mult))
        Jt = T(kb, es, [128, 128], F32, "s5jt")
        v(lambda: nc.vector.tensor_single_scalar(Jt[:], io[:], 64.0, op=ALU.is_equal))
        v(lambda: nc.vector.tensor_single_scalar(pl(T1)[:, 0:64], io[:, 0:64], -64.0, op=ALU.is_equal))
        v(lambda: nc.vector.tensor_tensor(Jt[:, 0:64], Jt[:, 0:64], pl(T1)[:, 0:64], op=ALU.subtract))
        bm = []
        for z in range(2):
            m = T(kb, es, [128, 128], F32, f"s5bm{z}")
            v(lambda m=m: nc.vector.memset(m[:], 0.0))
            for j in range(0, 8, 2):
                for jj in range(2):
                    pass
            bm.append(m)
        rowb = T(kb, es, [128, 1], F32, "s5rowb")
        pcol = T(kb, es, [128, 1], F32, "s5pcol")
        kb.op("pool", lambda: nc.gpsimd.iota(pcol[:], pattern=[[0, 1]], base=0, channel_multiplier=1,
                                              allow_small_or_imprecise_dtypes=True), writes=[pd])
        v(lambda: nc.vector.memset(rowb[:], 0.0))
        for t in range(1, 8):
            v(lambda t=t: nc.vector.tensor_scalar(pl(T1)[:, 0:1], pcol[:], float(16 * t), 16.0, op0=ALU.is_ge, op1=ALU.mult))
            v(lambda: nc.vector.tensor_tensor(rowb[:], rowb[:], pl(T1)[:, 0:1], op=ALU.add))
        colf = T(kb, es, [128, 128], F32, "s5colf")
        kb.op("pool", lambda: nc.gpsimd.iota(colf[:], pattern=[[1, 128]], base=0, channel_multiplier=0,
                                              allow_small_or_imprecise_dtypes=True), writes=[pd])
        v(lambda: nc.vector.tensor_scalar(bm[0][:], colf[:], rowb[:, 0:1], 0.0, op0=ALU.subtract, op1=ALU.is_ge))
        v(lambda: nc.vector.tensor_scalar(bm[1][:], colf[:], rowb[:, 0:1], 15.0, op0=ALU.subtract, op1=ALU.is_le))
        CC = T(kb, es, [128, 64, 16], F32, "s5cc")
        CCs = T(kb, es, [128, 64, 16], F32, "s5ccs")
        BB = T(kb, es, [128, 64, 16], F32, "s5bb")
        BBs = T(kb, es, [128, 64, 16], F32, "s5bbs")
        kb.dma("sp", lambda: nc.sync.dma_start(out=BB[0:64, :, :], in_=H.s5_b_re[0].rearrange("z g p h -> p (z g) h")),
               writes=[pd])
        kb.dma("act", lambda: nc.scalar.dma_start(out=BB[64:128, :, :], in_=H.s5_b_im[0].rearrange("z g p h -> p (z g) h")),
               writes=[pd])
        kb.dma("sp", lambda: nc.sync.dma_start(out=BBs[0:64, :, :], in_=H.s5_b_im[0].rearrange("z g p h -> p (z g) h")),
               writes=[pd])
        kb.dma("act", lambda: nc.scalar.dma_start(out=BBs[64:128, :, :], in_=H.s5_b_re[0].rearrange("z g p h -> p (z g) h")),
               writes=[pd])
        with ExitStack() as esx:
            xc = [T(kb, esx, [128, 8, 128], F32, f"s5xc{i}") for i in range(2)]
            for i, (aa, bb) in enumerate(((H.s5_c_re, H.s5_c_im), (H.s5_c_im, H.s5_c_re))):
                kb.dma("sp", lambda aa=aa, i=i: nc.sync.dma_start(
                    out=xc[i][:, :, 0:64], in_=aa[0].rearrange("z g h p -> (z g h) p").rearrange("(t r) p -> r t p", r=128)),
                    writes=[pd])
                kb.dma("act", lambda bb=bb, i=i: nc.scalar.dma_start(
                    out=xc[i][:, :, 64:128], in_=bb[0].rearrange("z g h p -> (z g h) p").rearrange("(t r) p -> r t p", r=128)),
                    writes=[pd])
            for i, dst in enumerate((CC, CCs)):
                for t in range(8):
                    ps = next_ps(g)
                    kb.op("pe", lambda t=t, i=i, ps=ps: nc.tensor.transpose(ps[:, 0:128], xc[i][:, t, :], g.ident[:]),
                          reads=[pd, g.ident.d], writes=[ps.d])
                    kb.op("act", lambda t=t, dst=dst, ps=ps: nc.scalar.copy(
                        dst[:, t * 8:(t + 1) * 8, :], ps[:, 0:128].rearrange("p (a h) -> p a h", a=8)),
                        reads=[ps.d], writes=[pd])
            kb.barrier()
        ckpt("d0")
        with ExitStack() as esu:
            U8 = [T(kb, esu, [128, 8, 512], BF16, f"s5u8_{i}") for i in range(2)]
            U8g = [T(kb, esu, [128, 32, 128], BF16, f"s5u8g_{i}") for i in range(2)]
            utst = [T(kb, esu, [128, 4, 128], BF16, f"s5utst_{i}") for i in range(2)]
            blocks = [(0, 32)] + [(32 + 128 * b, 128) for b in range(4)]
            for bi_, (c0, ncb) in enumerate(blocks):
                kb.pump(1)
                u8, u8g = U8[bi_ % 2], U8g[bi_ % 2]
                kb.dma("sp", lambda u8=u8, c0=c0, ncb=ncb: nc.sync.dma_start(
                    out=u8[0:ncb, :, :], in_=S.u[c0 * 8:(c0 + ncb) * 8, :].rearrange("(c j) f -> c j f", j=8)),
                    reads=[S.u_d], writes=[u8.d])
                kb.op("pool", lambda u8=u8, u8g=u8g, ncb=ncb: nc.gpsimd.tensor_copy(
                    u8g[0:ncb, :, :].rearrange("c g (j h) -> c g j h", j=8),
                    u8[0:ncb, :, :].rearrange("c j (g h) -> c g j h", g=32)), reads=[u8.d], writes=[u8g.d])
                for g4 in range(0, 32, 4):
                    for gg in range(4):
                        kb.op("pe", lambda gg=gg, g4=g4, u8g=u8g, ncb=ncb: nc.tensor.transpose(
                            psb[:, gg * 128:gg * 128 + ncb], u8g[0:ncb, g4 + gg, :], g.identb[0:ncb, 0:ncb]),
                            reads=[u8g.d, g.identb.d], writes=[psb.d])
                    ust = utst[(g4 // 4) % 2]
                    kb.op("act", lambda g4=g4, c0=c0, ncb=ncb, ust=ust: nc.scalar.copy(
                        ust[:, :, 0:ncb], psb[:, 0:512].rearrange("p (a c) -> p a c", a=4)[:, :, 0:ncb]),
                        reads=[psb.d], writes=[ust.d])
                    kb.dma("act", lambda g4=g4, c0=c0, ncb=ncb, ust=ust: nc.scalar.dma_start(
                        out=S.Ut[g4:g4 + 4, :, c0:c0 + ncb].rearrange("a p c -> p a c"), in_=ust[:, :, 0:ncb]),
                        reads=[ust.d], writes=[S.Ut_d])
            kb.barrier()
        ckpt("d1")
        for z in range(2):
            gs = slice(z * 32, z * 32 + 32)
            if z == 1:
                ckpt("dz0")
            with ExitStack() as ez:
                MT = T(kb, ez, [128, 32, 128], BF16, "s5mt")
                RT = T(kb, ez, [128, 32, 128], BF16, "s5rt")
                OTb = T(kb, ez, [128, 32, 128], BF16, "s5otb")
                with ExitStack() as ep:
                    Gall = T(kb, ep, [128, 32, 9, 16], F32, "s5gall")
                    Kn = T(kb, ep, [128, 32, 8, 16], F32, "s5kn")
                    Kp = T(kb, ep, [128, 32, 8, 16], F32, "s5kp")
                    tmpk = T(kb, ep, [128, 32, 16], F32, "s5tmpk")
                    bc = lambda ap2: ap2.unsqueeze(2).to_broadcast([128, 32, 16])
                    for k in range(9):
                        i = k if z == 0 else 8 - k
                        v(lambda k=k, i=i: nc.vector.tensor_tensor(Gall[:, :, i, :], CC[:, gs, :], bc(PW[:, k, 0, gs]),
                                                                  op=ALU.mult))
                        v(lambda k=k: nc.vector.tensor_tensor(tmpk[:], CCs[:, gs, :], bc(PW[:, k, 1, gs]), op=ALU.mult))
                        v(lambda i=i: nc.vector.tensor_tensor(Gall[:, :, i, :], Gall[:, :, i, :], tmpk[:], op=ALU.subtract))
                    for k in range(8):
                        j = k if z == 0 else 7 - k
                        v(lambda k=k, j=j: nc.vector.tensor_tensor(Kn[:, :, j, :], BB[:, gs, :], bc(WN[:, k, 0, gs]),
                                                                  op=ALU.mult))
                        v(lambda k=k: nc.vector.tensor_tensor(tmpk[:], BBs[:, gs, :], bc(WN[:, k, 1, gs]), op=ALU.mult))
                        v(lambda j=j: nc.vector.tensor_tensor(Kn[:, :, j, :], Kn[:, :, j, :], tmpk[:], op=ALU.add))
                        j2 = 7 - k if z == 0 else k
                        v(lambda k=k, j2=j2: nc.vector.tensor_tensor(Kp[:, :, j2, :], BB[:, gs, :], bc(WP[:, k, 0, gs]),
                                                                    op=ALU.mult))
                        v(lambda k=k: nc.vector.tensor_tensor(tmpk[:], BBs[:, gs, :], bc(WP[:, k, 1, gs]), op=ALU.mult))
                        v(lambda j2=j2: nc.vector.tensor_tensor(Kp[:, :, j2, :], Kp[:, :, j2, :], tmpk[:], op=ALU.add))
                    q0 = 0 if z == 0 else 1
                    o0 = 1 if z == 0 else 0
                    for gi in range(32):
                        ps = next_ps(g)
                        kb.op("pe", lambda gi=gi, ps=ps: nc.tensor.matmul(
                            ps[:, 0:128], lhsT=Kn[:, gi, :, :].rearrange("p j h -> p (j h)"),
                            rhs=Gall[:, gi, q0:q0 + 8, :].rearrange("p s h -> p (s h)"), start=True, stop=True),
                            reads=D1, writes=[ps.d])
                        kb.op("dve", lambda gi=gi, ps=ps: nc.vector.tensor_tensor(MT[:, gi, :], ps[:, 0:128], bm[z][:],
                                                                                 op=ALU.mult),
                              reads=[ps.d] + D1, writes=[MT.d])
                        ps2 = next_ps(g)
                        kb.op("pe", lambda gi=gi, ps2=ps2: nc.tensor.transpose(
                            ps2[:, 0:128], Kp[:, gi, :, :].rearrange("p j h -> p (j h)"), g.ident[:]),
                            reads=D1 + [g.ident.d], writes=[ps2.d])
                        kb.op("act", lambda gi=gi, ps2=ps2: nc.scalar.copy(RT[:, gi, :], ps2[:, 0:128]),
                              reads=[ps2.d], writes=[RT.d])
                        kb.op("act", lambda gi=gi: nc.scalar.copy(
                            OTb[:, gi, :], Gall[:, gi, o0:o0 + 8, :].rearrange("p s h -> p (s h)")),
                            reads=D1, writes=[OTb.d])
                    kb.barrier()
                if f"s5mat{z}" in g.dbg:
                    dbg_out(g, f"MT{z}", MT[:], [MT.d], [128, 32, 128], BF16)
                    dbg_out(g, f"RT{z}", RT[:], [RT.d], [128, 32, 128], BF16)
                    dbg_out(g, f"OTb{z}", OTb[:], [OTb.d], [128, 32, 128], BF16)
                if z == 0:
                    ckpt("dm0")
                X = T(kb, ez, [128, 32, NCK], F32, "s5x", nd=32)
                utg = [T(kb, ez, [128, NCK], BF16, f"s5utg{i}") for i in range(3)]
                for gi in range(32):
                    ug = utg[gi % 3]
                    kb.dma("sp", lambda gi=gi, ug=ug: nc.sync.dma_start(out=ug[:], in_=S.Ut[gi, :, :]),
                           reads=[S.Ut_d], writes=[ug.d])
                    for (c0, n) in ((0, 512), (512, NCK - 512)):
                        ps = next_ps(g)
                        kb.op("pe", lambda gi=gi, c0=c0, n=n, ps=ps: nc.tensor.matmul(
                            ps[:, 0:n], lhsT=RT[:, gi, :], rhs=ug[:, c0:c0 + n], start=True, stop=True),
                            reads=[RT.d, ug.d], writes=[ps.d])
                        eng = "act" if gi % 2 == 0 else "dve"
                        if eng == "act":
                            kb.op("act", lambda gi=gi, c0=c0, n=n, ps=ps: nc.scalar.copy(X[:, gi, c0:c0 + n], ps[:, 0:n]),
                                  reads=[ps.d], writes=[X.ds[gi]])
                        else:
                            kb.op("dve", lambda gi=gi, c0=c0, n=n, ps=ps: nc.vector.tensor_copy(X[:, gi, c0:c0 + n], ps[:, 0:n]),
                                  reads=[ps.d], writes=[X.ds[gi]])
                if z == 0:
                    ckpt("d20")
                cur = [T(kb, ez, [128, 32, NMAC], F32, f"s5cur{i}") for i in range(2)]
                Gm = T(kb, ez, [128, 32, NMAC], F32, "s5gm")
                uu = T(kb, ez, [128, 32, NMAC], F32, "s5uu")
                hs = [T(kb, ez, [128, 32, NMAC], F32, f"s5hs{i}") for i in range(2)]
                pw = T(kb, ez, [128, 2, 2, 32], F32, "s5pwk")
                ptmp = T(kb, ez, [128, 32], F32, "s5ptmp")
                xall = X.ds
                colsel = (lambda i: i) if z == 0 else (lambda i: 15 - i)
                gbanks = ((0, 15), (15, 30), (30, 32))

                def cplx_apply(src_ap_fn, width, ar_ap, ai_ap, dst, dst_lo, u_t, rd):
                    kb.op("pool", lambda: nc.gpsimd.tensor_tensor(
                        u_t[:, :, 0:width], src_ap_fn(0, 32), ar_ap.unsqueeze(2).to_broadcast([128, 32, width]),
                        op=ALU.mult), reads=rd + D1, writes=[u_t.d])
                    pss = []
                    for (g0, g1) in gbanks:
                        ps = next_ps(g)
                        pss.append(ps)
                        kb.op("pe", lambda ps=ps, g0=g0, g1=g1: nc.tensor.matmul(
                            ps[:, 0:(g1 - g0) * width], lhsT=Jt[:], rhs=src_ap_fn(g0, g1), start=True, stop=True),
                            reads=rd + D1, writes=[ps.d])
                    for (g0, g1), ps in zip(gbanks, pss):
                        kb.op("dve", lambda g0=g0, g1=g1, ps=ps: nc.vector.tensor_tensor(
                            dst[:, g0:g1, dst_lo:dst_lo + width],
                            ps[:, 0:(g1 - g0) * width].rearrange("p (a m) -> p a m", m=width),
                            ai_ap[:, g0:g1].unsqueeze(2).to_broadcast([128, g1 - g0, width]), op=ALU.mult),
                            reads=[ps.d] + D1, writes=[dst.d])
                    kb.op("dve", lambda: nc.vector.tensor_tensor(
                        dst[:, :, dst_lo:dst_lo + width], dst[:, :, dst_lo:dst_lo + width], u_t[:, :, 0:width], op=ALU.add),
                        reads=[dst.d, u_t.d], writes=[dst.d])

                a8r, a8i = A8s[:, 0, gs], A8s[:, 1, gs]

                def step(src, dst, i, store):
                    col = colsel(i)
                    cplx_apply(lambda g0, g1: src[:, g0:g1, :], NMAC, a8r, a8i, dst, 0, uu, [src.d])
                    kb.op("dve", lambda: nc.vector.tensor_tensor(dst[:], dst[:], X[:, :, col:NCK:16], op=ALU.add),
                          reads=[dst.d] + xall, writes=[dst.d])
                    if store:
                        kb.op("act", lambda: nc.scalar.copy(X[:, :, col:NCK:16], src[:]),
                              reads=[src.d] + xall, writes=xall)

                kb.op("pool", lambda: nc.gpsimd.memset(cur[0][:], 0.0), writes=[cur[0].d])
                for i in range(16):
                    if i % 4 == 0:
                        kb.pump(1)
                    step(cur[i % 2], cur[(i + 1) % 2], i, False)
                Em = cur[0]
                h0 = hs[0]
                if z == 0:
                    kb.op("act", lambda: nc.scalar.copy(h0[:], Em[:]), reads=[Em.d], writes=[h0.d])
                else:
                    kb.op("act", lambda: nc.scalar.copy(h0[:, :, 0:2], Em[:, :, 1::-1]), reads=[Em.d], writes=[h0.d])
                    kb.op("act", lambda: nc.scalar.copy(h0[:, :, 2:NMAC], Em[:, :, NMAC - 1:1:-1]), reads=[Em.d], writes=[h0.d])
                v(lambda: nc.vector.tensor_copy(pw[:, 0, 0, :], P128[:, 0, gs]))
                v(lambda: nc.vector.tensor_copy(pw[:, 0, 1, :], P128[:, 1, gs]))
                d_ = 1
                k_ = 0
                while d_ < NMAC:
                    src, dst = hs[k_ % 2], hs[(k_ + 1) % 2]
                    pr, pi_ = pw[:, k_ % 2, 0, :], pw[:, k_ % 2, 1, :]
                    w = NMAC - d_
                    cplx_apply(lambda g0, g1, src=src, w=w: src[:, g0:g1, 0:w], w, pr, pi_, dst, d_, uu, [src.d])
                    kb.op("dve", lambda src=src, dst=dst, d_=d_: nc.vector.tensor_tensor(
                        dst[:, :, d_:NMAC], dst[:, :, d_:NMAC], src[:, :, d_:NMAC], op=ALU.add),
                        reads=[dst.d, src.d], writes=[dst.d])
                    kb.op("act", lambda src=src, dst=dst, d_=d_: nc.scalar.copy(dst[:, :, 0:d_], src[:, :, 0:d_]),
                          reads=[src.d], writes=[dst.d])
                    nr, ni = pw[:, (k_ + 1) % 2, 0, :], pw[:, (k_ + 1) % 2, 1, :]
                    cmul(g, nr, ni, pr, pi_, pr, pi_, ptmp[:], D1, pd)
                    d_ *= 2
                    k_ += 1
                Iinc = hs[k_ % 2]
                kb.op("pool", lambda: nc.gpsimd.memset(Gm[:], 0.0), writes=[Gm.d])
                if z == 0:
                    kb.op("dve", lambda: nc.vector.tensor_copy(Gm[:, :, 1:NMAC], Iinc[:, :, 0:NMAC - 1]),
                          reads=[Iinc.d], writes=[Gm.d])
                else:
                    kb.op("dve", lambda: nc.vector.tensor_copy(Gm[:, :, 0:1], Iinc[:, :, 0:1]), reads=[Iinc.d], writes=[Gm.d])
                    kb.op("dve", lambda: nc.vector.tensor_copy(Gm[:, :, NMAC - 1:1:-1], Iinc[:, :, 1:NMAC - 1]),
                          reads=[Iinc.d], writes=[Gm.d])
                kb.op("dve", lambda: nc.vector.tensor_copy(cur[0][:], Gm[:]), reads=[Gm.d], writes=[cur[0].d])
                for i in range(16):
                    step(cur[i % 2], cur[(i + 1) % 2], i, True)
                if f"s5x{z}" in g.dbg:
                    dbg_out(g, f"X{z}", X[:], xall, [128, 32, NCK], F32)
                if z == 0:
                    ckpt("d30")
                xb = [T(kb, ez, [128, 512], BF16, f"s5xb{i}") for i in range(2)]
                yo = [T(kb, ez, [128, 512], F32, f"s5yo{i}") for i in range(2)]
                yfl = [T(kb, ez, [128, 512], F32, f"s5yfl{i}") for i in range(2)]
                for gi in range(32):
                    xbt = xb[gi % 2]
                    ug = utg[gi % 3]
                    kb.dma("sp", lambda gi=gi, ug=ug: nc.sync.dma_start(out=ug[:], in_=S.Ut[gi, :, :]),
                           reads=[S.Ut_d], writes=[ug.d])
                    kb.op("act", lambda gi=gi, xbt=xbt: nc.scalar.copy(xbt[:], X[:, gi, 32:NCK]), reads=xall, writes=[xbt.d])
                    ps = next_ps(g)
                    kb.op("pe", lambda gi=gi, ps=ps, ug=ug: nc.tensor.matmul(ps[:, :], lhsT=MT[:, gi, :], rhs=ug[:, 32:NCK],
                                                                             start=True, stop=False),
                          reads=[MT.d, ug.d], writes=[ps.d])
                    kb.op("pe", lambda gi=gi, ps=ps, xbt=xbt: nc.tensor.matmul(ps[:, :], lhsT=OTb[:, gi, :], rhs=xbt[:],
                                                                               start=False, stop=True),
                          reads=[OTb.d, xbt.d], writes=[ps.d])
                    yt = yo[gi % 2]
                    if z == 0:
                        kb.op("dve", lambda ps=ps, yt=yt: nc.vector.tensor_copy(yt[:], ps[:, :]),
                              reads=[ps.d], writes=[yt.d])
                    else:
                        yf = yfl[gi % 2]
                        kb.dma("act", lambda gi=gi, yf=yf: nc.scalar.dma_start(out=yf[:], in_=S.y0[gi, :, :]),
                               reads=[S.y0_d], writes=[yf.d])
                        kb.op("dve", lambda ps=ps, yt=yt, yf=yf: nc.vector.tensor_tensor(yt[:], ps[:, :], yf[:], op=ALU.add),
                              reads=[ps.d, yf.d], writes=[yt.d])
                    kb.dma("sp", lambda gi=gi, yt=yt: nc.sync.dma_start(out=S.y0[gi, :, :], in_=yt[:]),
                           reads=[yt.d], writes=[S.y0_d])
                kb.barrier()


SEG = 256
NSEG = NL * 4 // SEG + NE
JB = SEG // 128


def bc_tile(g, es, name, src_row_ap, n, reads=()):
    nc, kb = g.nc, g.kb
    t = T(kb, es, [128, n], F32, name)
    kb.dma("sp", lambda: nc.sync.dma_start(out=t[:], in_=src_row_ap.partition_broadcast(128)),
           reads=list(reads), writes=[t.d])
    return t


def phase_e(g):
    nc, kb, H, S = g.nc, g.kb, g.H, g.S
    R = g.R
    with ExitStack() as es:
        psb = g.psb[0]
        s5T = T(kb, es, [128, 4, NL], BF16, "s5T", nd=4)
        with ExitStack() as e1:
            glw = T(kb, e1, [128, 4, 512], BF16, "glw")
            kb.dma("pool", lambda: nc.gpsimd.dma_start(out=glw[:], in_=H.glu_w[0].rearrange("(k p) n -> p k n", p=128)),
                   writes=[glw.d])
            glb = T(kb, e1, [128, 4], F32, "glb")
            kb.dma("sp", lambda: nc.sync.dma_start(out=glb[:], in_=H.glu_b[0, :].rearrange("(k p) -> p k", p=128),
                                                   allow_slow_non_contiguous=True), writes=[glb.d])
            s5d = T(kb, e1, [128, 4], F32, "s5d")
            kb.dma("sp", lambda: nc.sync.dma_start(out=s5d[:], in_=H.s5_d[0, :].rearrange("(k p) -> p k", p=128),
                                                   allow_slow_non_contiguous=True), writes=[s5d.d])
            Yg = T(kb, e1, [128, 32, 128], F32, "Yg")
            Ytm = T(kb, e1, [128, 8, 512], F32, "Ytm")
            yT = T(kb, e1, [128, 4, 1024], F32, "yTt")
            uTt = T(kb, e1, [128, 4, 1024], BF16, "uTt")
            t1 = T(kb, e1, [128, 4, 1024], F32, "glt1")
            glg = T(kb, e1, [128, 4, 1024], BF16, "glg")
            sg = T(kb, e1, [128, 512], BF16, "glsg")
            for cb in range(4):
                kb.pump(1)
                kb.dma("sp", lambda cb=cb: nc.sync.dma_start(
                    out=Yg[:], in_=S.y0[:, :, cb * 128:(cb + 1) * 128].rearrange("g p c -> p g c")),
                    reads=[S.y0_d], writes=[Yg.d])
                kb.dma("act", lambda cb=cb: nc.scalar.dma_start(
                    out=uTt[:], in_=S.uT[:, NCX + cb * 1024:NCX + (cb + 1) * 1024].rearrange("(k p) t -> p k t", p=128)),
                    reads=[S.uT_d], writes=[uTt.d])
                for g4 in range(0, 32, 4):
                    ps = next_ps(g)
                    for gg in range(4):
                        kb.op("pe", lambda gg=gg, g4=g4, ps=ps: nc.tensor.transpose(
                            ps[:, gg * 128:(gg + 1) * 128], Yg[:, g4 + gg, :], g.ident[:]),
                            reads=[Yg.d, g.ident.d], writes=[ps.d])
                    kb.op("act", lambda g4=g4, ps=ps: nc.scalar.copy(
                        Ytm[:, :, g4 * 16:g4 * 16 + 64].rearrange("c s (a h) -> c a s h", a=4),
                        ps[:, :].rearrange("c (a s h) -> c a s h", a=4, s=8)), reads=[ps.d], writes=[Ytm.d])
                for s_ in range(8):
                    ps = next_ps(g)
                    for cc in range(4):
                        kb.op("pe", lambda cc=cc, s_=s_, ps=ps: nc.tensor.transpose(
                            ps[:, cc * 128:(cc + 1) * 128], Ytm[:, s_, cc * 128:(cc + 1) * 128], g.ident[:]),
                            reads=[Ytm.d, g.ident.d], writes=[ps.d])
                    kb.op("dve", lambda s_=s_, ps=ps: nc.vector.tensor_copy(
                        yT[:, :, s_:1024:8], ps[:, :].rearrange("p (a c) -> p a c", a=4)), reads=[ps.d], writes=[yT.d])
                for cc in range(4):
                    kb.op("dve", lambda cc=cc: nc.vector.scalar_tensor_tensor(
                        yT[:, cc, :], uTt[:, cc, :], s5d[:, cc:cc + 1], yT[:, cc, :], op0=ALU.mult, op1=ALU.add),
                        reads=[uTt.d, s5d.d, yT.d], writes=[yT.d])
                kb.op("dve", lambda: nc.vector.tensor_tensor(t1[:], yT[:], yT[:], op=ALU.mult), reads=[yT.d], writes=[t1.d])
                kb.op("dve", lambda: nc.vector.tensor_scalar(t1[:], t1[:], 0.044715, 1.0, op0=ALU.mult, op1=ALU.add),
                      reads=[t1.d], writes=[t1.d])
                kb.op("dve", lambda: nc.vector.tensor_tensor(t1[:], t1[:], yT[:], op=ALU.mult), reads=[t1.d, yT.d], writes=[t1.d])
                kb.op("act", lambda: nc.scalar.activation(t1[:], t1[:], ACT.Sigmoid, scale=1.5957691216057308),
                      reads=[t1.d], writes=[t1.d])
                kb.op("dve", lambda: nc.vector.tensor_tensor(glg[:], t1[:], yT[:], op=ALU.mult), reads=[t1.d, yT.d], writes=[glg.d])
                for nn in range(4):
                    for th in range(2):
                        ps = next_ps(g)
                        for kc in range(4):
                            kb.op("pe", lambda nn=nn, th=th, kc=kc, ps=ps: nc.tensor.matmul(
                                ps[:, :], lhsT=glw[:, kc, nn * 128:(nn + 1) * 128], rhs=glg[:, kc, th * 512:(th + 1) * 512],
                                start=(kc == 0), stop=(kc == 3)), reads=[glw.d, glg.d], writes=[ps.d])
                        kb.op("act", lambda nn=nn, ps=ps: nc.scalar.activation(sg[:], ps[:, :], ACT.Sigmoid,
                                                                              bias=glb[:, nn:nn + 1], scale=1.0),
                              reads=[ps.d, glb.d], writes=[sg.d])
                        kb.op("dve", lambda nn=nn, th=th, cb=cb: nc.vector.tensor_tensor(
                            s5T[:, nn, cb * 1024 + th * 512:cb * 1024 + (th + 1) * 512], sg[:],
                            glg[:, nn, th * 512:(th + 1) * 512], op=ALU.mult), reads=[sg.d, glg.d], writes=[s5T.ds[nn]])
            kb.barrier()
        if "s5T" in g.dbg:
            dbg_out(g, "s5T", s5T[:], s5T.ds, [128, 4, NL], BF16)
        ckpt("e1")
        glaT = T(kb, es, [128, 4, NL], BF16, "glaTt")
        kb.dma("sp", lambda: nc.sync.dma_start(out=glaT[:], in_=S.glaT.ap().rearrange("(k p) t -> p k t", p=128)),
               reads=[S.glaT_d], writes=[glaT.d])
        wo = T(kb, es, [128, 8, D], BF16, "wo")
        kb.dma("pool", lambda: nc.gpsimd.dma_start(out=wo[:], in_=H.w_out[0].rearrange("(k p) n -> p k n", p=128)),
               writes=[wo.d])
        g1b = bc_tile(g, es, "g1b", S.mod[0:1, 2 * D:3 * D], D, [S.mod_d])
        sh2b = bc_tile(g, es, "sh2b", S.mod[0:1, 3 * D:4 * D], D, [S.mod_d])
        g2e = bc_tile(g, es, "g2e", S.mod[0:1, 4 * D:5 * D], D, [S.mod_d])
        nfw = bc_tile(g, es, "nfw", H.norm_ffn_w[0:1, :], D)
        kb.op("dve", lambda: nc.vector.scalar_tensor_tensor(g2e[:], g2e[:], 1.0, nfw[:], op0=ALU.add, op1=ALU.mult),
              reads=[g2e.d, nfw.d], writes=[g2e.d])
        rw = T(kb, es, [128, 8, NE], F32, "rw")
        kb.dma("sp", lambda: nc.sync.dma_start(out=rw[:], in_=H.router_w[0].rearrange("(k p) e -> p k e", p=128)),
               writes=[rw.d])
        rbb = bc_tile(g, es, "rbb", H.router_b[0:1, :], NE)
        xt = [T(kb, es, [128, D], F32, f"ext{i}") for i in range(3)]
        x2t = [T(kb, es, [128, D], F32, f"ex2{i}") for i in range(2)]
        tmp_ = [T(kb, es, [128, D], F32, f"etmp{i}") for i in range(2)]
        tmq_ = [T(kb, es, [128, D], F32, f"etmq{i}") for i in range(2)]
        h2_ = [T(kb, es, [128, D], F32, f"eh2{i}") for i in range(2)]
        h2b = [T(kb, es, [128, D], BF16, f"eh2b{i}") for i in range(2)]
        hT2_ = [T(kb, es, [128, 8, 128], F32, f"ehT2{i}") for i in range(2)]
        junk = T(kb, es, [128, D], BF16, "ejunk")
        lg_ = [T(kb, es, [128, NE], F32, f"elg{i}") for i in range(2)]
        mx8_ = [T(kb, es, [128, 8], F32, f"emx8{i}") for i in range(2)]
        ix8_ = [T(kb, es, [128, 8], U32, f"eix8{i}") for i in range(2)]
        sm1_ = [T(kb, es, [128, 4], F32, f"esm{i}") for i in range(2)]
        def partA(tl):
            if tl % 4 == 0:
                kb.pump(1)
            t0 = tl * 128
            x_t, x2 = xt[tl % 3], x2t[tl % 2]
            tmp, tmq, h2, hT2 = tmp_[tl % 2], tmq_[tl % 2], h2_[tl % 2], hT2_[tl % 2]
            lg, mx8, ix8, sm1 = lg_[tl % 2], mx8_[tl % 2], ix8_[tl % 2], sm1_[tl % 2]
            for pf in ([0, 1, 2] if tl == 0 else [tl + 2]):
                if pf < NL // 128:
                    xp = xt[pf % 3]
                    kb.dma("sp", lambda xp=xp, pf=pf: nc.sync.dma_start(out=xp[:], in_=H.x[pf * 128:(pf + 1) * 128, :]),
                           writes=[xp.d])
            for nh in range(2):
                ps = next_ps(g)
                for r in range(2):
                    row = tl * 2 + r
                    for kc in range(8):
                        if kc < 4:
                            lh = glaT[:, kc, row * 64:(row + 1) * 64]
                            rd = [glaT.d]
                        else:
                            lh = s5T[:, kc - 4, row:NL:64]
                            rd = s5T.ds
                        kb.op("pe", lambda lh=lh, r=r, kc=kc, nh=nh, ps=ps: nc.tensor.matmul(
                            ps[r * 64:(r + 1) * 64, :], lhsT=lh, rhs=wo[:, kc, nh * 512:(nh + 1) * 512],
                            start=(kc == 0), stop=(kc == 7)), reads=rd + [wo.d], writes=[ps.d])
                kb.op("dve", lambda nh=nh, ps=ps: nc.vector.tensor_tensor(
                    tmp[:, nh * 512:(nh + 1) * 512], ps[:, :], g1b[:, nh * 512:(nh + 1) * 512], op=ALU.mult),
                    reads=[ps.d, g1b.d], writes=[tmp.d])
            kb.op("dve", lambda x2=x2, x_t=x_t, tmp=tmp: nc.vector.tensor_tensor(x2[:], tmp[:], x_t[:], op=ALU.add),
                  reads=[tmp.d, x_t.d], writes=[x2.d])
            kb.dma("sp", lambda x2=x2, t0=t0: nc.sync.dma_start(out=S.x2[t0:t0 + 128, :], in_=x2[:]),
                   reads=[x2.d], writes=[S.x2_d])
            kb.op("act", lambda x2=x2: nc.scalar.activation(junk[:], x2[:], ACT.Square, accum_out=sm1[:, 0:1]),
                  reads=[x2.d], writes=[junk.d, sm1.d])
            kb.op("dve", lambda: nc.vector.tensor_scalar(sm1[:, 1:2], sm1[:, 0:1], 1.0 / D, EPS, op0=ALU.mult, op1=ALU.add),
                  reads=[sm1.d], writes=[sm1.d])
            kb.op("act", lambda: nc.scalar.activation(sm1[:, 1:2], sm1[:, 1:2], ACT.Sqrt), reads=[sm1.d], writes=[sm1.d])
            kb.op("dve", lambda: nc.vector.reciprocal(sm1[:, 2:3], sm1[:, 1:2]), reads=[sm1.d], writes=[sm1.d])
            kb.op("dve", lambda x2=x2: nc.vector.scalar_tensor_tensor(tmq[:], x2[:], sm1[:, 2:3], g2e[:],
                                                                      op0=ALU.mult, op1=ALU.mult),
                  reads=[x2.d, sm1.d, g2e.d], writes=[tmq.d])
            kb.op("dve", lambda: nc.vector.tensor_tensor(h2[:], tmq[:], sh2b[:], op=ALU.add),
                  reads=[tmq.d, sh2b.d], writes=[h2.d])
            hb = h2b[tl % 2]
            kb.op("act", lambda hb=hb: nc.scalar.copy(hb[:], h2[:]), reads=[h2.d], writes=[hb.d])
            kb.dma("sp", lambda hb=hb, t0=t0: nc.sync.dma_start(out=S.h2[t0:t0 + 128, :], in_=hb[:]),
                   reads=[hb.d], writes=[S.h2_d])

        def partB(tl):
            tmp, tmq, h2, hT2 = tmp_[tl % 2], tmq_[tl % 2], h2_[tl % 2], hT2_[tl % 2]
            lg, mx8, ix8, sm1 = lg_[tl % 2], mx8_[tl % 2], ix8_[tl % 2], sm1_[tl % 2]
            for hf in range(2):
                ps = next_ps(g)
                for kk in range(4):
                    kc = hf * 4 + kk
                    kb.op("pe", lambda kc=kc, kk=kk, ps=ps: nc.tensor.transpose(
                        ps[:, kk * 128:(kk + 1) * 128], h2[:, kc * 128:(kc + 1) * 128], g.ident[:]),
                        reads=[h2.d, g.ident.d], writes=[ps.d])
                kb.op("act", lambda hf=hf, ps=ps: nc.scalar.copy(
                    hT2[:, hf * 4:(hf + 1) * 4, :], ps[:, :].rearrange("p (a t) -> p a t", a=4)),
                    reads=[ps.d], writes=[hT2.d])
            ps = next_ps(g)
            for kc in range(8):
                kb.op("pe", lambda kc=kc, ps=ps: nc.tensor.matmul(ps[:, 0:NE], lhsT=hT2[:, kc, :], rhs=rw[:, kc, :],
                                                                  start=(kc == 0), stop=(kc == 7)),
                      reads=[hT2.d, rw.d], writes=[ps.d])
            kb.op("dve", lambda ps=ps: nc.vector.tensor_tensor(lg[:], ps[:, 0:NE], rbb[:], op=ALU.add),
                  reads=[ps.d, rbb.d], writes=[lg.d])
            kb.op("dve", lambda: nc.vector.max(mx8[:], lg[:]), reads=[lg.d], writes=[mx8.d])
            kb.op("dve", lambda: nc.vector.max_index(ix8[:], mx8[:], lg[:]), reads=[lg.d, mx8.d], writes=[ix8.d])
            kb.op("dve", lambda tl=tl: nc.vector.tensor_copy(R.idxf[:, tl, :], ix8[:, 0:4]), reads=[ix8.d], writes=[R.idxf.d])
            kb.op("dve", lambda tl=tl: nc.vector.tensor_scalar(R.mask[:, tl, :], lg[:], mx8[:, 3:4], None, op0=ALU.is_ge),
                  reads=[lg.d, mx8.d], writes=[R.mask.d])
            kb.op("dve", lambda: nc.vector.tensor_scalar(sm1[:, 3:4], mx8[:, 0:1], -1.0, None, op0=ALU.mult),
                  reads=[mx8.d, sm1.d], writes=[sm1.d])
            kb.op("act", lambda tl=tl: nc.scalar.activation(R.gate[:, tl, :], mx8[:, 0:4], ACT.Exp, bias=sm1[:, 3:4],
                                                            scale=1.0, accum_out=sm1[:, 0:1]),
                  reads=[mx8.d, sm1.d], writes=[R.gate.d, sm1.d])
            kb.op("dve", lambda: nc.vector.reciprocal(sm1[:, 1:2], sm1[:, 0:1]), reads=[sm1.d], writes=[sm1.d])
            kb.op("dve", lambda tl=tl: nc.vector.tensor_scalar(R.gate[:, tl, :], R.gate[:, tl, :], sm1[:, 1:2], None,
                                                               op0=ALU.mult), reads=[R.gate.d, sm1.d], writes=[R.gate.d])

        NTL = NL // 128
        partA(0)
        for tl in range(1, NTL):
            partA(tl)
            partB(tl - 1)
        partB(NTL - 1)
        if "logits" in g.dbg:
            dbg_out(g, "gate", R.gate[:], [R.gate.d], [128, 32, 4])
            dbg_out(g, "idxf", R.idxf[:], [R.idxf.d], [128, 32, 4])
        ckpt("e2")
        onesf = T(kb, es, [128, 128], F32, "onesf")
        kb.op("pool", lambda: nc.gpsimd.memset(onesf[:], 1.0), writes=[onesf.d])
        triu = T(kb, es, [128, 128], F32, "triu")
        kb.op("dve", lambda: nc.vector.tensor_single_scalar(triu[:], g.iota[:], 0.0, op=ALU.is_gt),
              reads=[g.iota.d], writes=[triu.d])
        ps = next_ps(g)
        for tl in range(32):
            kb.op("pe", lambda tl=tl, ps=ps: nc.tensor.matmul(ps[:, 0:NE], lhsT=onesf[:], rhs=R.mask[:, tl, :],
                                                              start=(tl == 0), stop=(tl == 31)),
                  reads=[onesf.d, R.mask.d], writes=[ps.d])
        cnt = T(kb, es, [128, NE], F32, "cnt")
        nsg = T(kb, es, [128, NE], F32, "nsg")
        pend = T(kb, es, [128, NE], F32, "pend")
        pst = T(kb, es, [128, NE], F32, "pst")
        t32 = T(kb, es, [128, NE], F32, "t32")
        kb.op("dve", lambda ps=ps: nc.vector.tensor_copy(cnt[:], ps[:, 0:NE]), reads=[ps.d], writes=[cnt.d])
        kb.op("dve", lambda: nc.vector.memset(nsg[:], 0.0), writes=[nsg.d])
        for k in range(NL // SEG):
            kb.op("dve", lambda k=k: nc.vector.tensor_scalar(t32[:], cnt[:], float(SEG * k) + 0.5, None, op0=ALU.is_ge),
                  reads=[cnt.d], writes=[t32.d])
            kb.op("dve", lambda: nc.vector.tensor_tensor(nsg[:], nsg[:], t32[:], op=ALU.add), reads=[nsg.d, t32.d], writes=[nsg.d])
        kb.op("dve", lambda: nc.vector.tensor_tensor_scan(pend[:], onesf[:, 0:NE], nsg[:], 0.0, ALU.mult, ALU.add),
              reads=[onesf.d, nsg.d], writes=[pend.d])
        kb.op("dve", lambda: nc.vector.tensor_tensor(pst[:], pend[:], nsg[:], op=ALU.subtract), reads=[pend.d, nsg.d], writes=[pst.d])
        kb.op("dve", lambda: nc.vector.tensor_scalar(pst[:], pst[:], float(SEG), None, op0=ALU.mult), reads=[pst.d], writes=[pst.d])
        sidx = T(kb, es, [128, NSEG], F32, "sidx")
        kb.op("pool", lambda: nc.gpsimd.iota(sidx[:], pattern=[[1, NSEG]], base=0, channel_multiplier=0,
                                              allow_small_or_imprecise_dtypes=True), writes=[sidx.d])
        cmp3 = T(kb, es, [128, NSEG, NE], F32, "cmp3")
        kb.op("dve", lambda: nc.vector.tensor_tensor(cmp3[:], pend[:].unsqueeze(1).to_broadcast([128, NSEG, NE]),
                                                     sidx[:].unsqueeze(2).to_broadcast([128, NSEG, NE]), op=ALU.is_le),
              reads=[pend.d, sidx.d], writes=[cmp3.d])
        sef = T(kb, es, [128, NSEG], F32, "sef")
        kb.op("dve", lambda: nc.vector.tensor_reduce(sef[:], cmp3[:], axis=AX.X, op=ALU.add), reads=[cmp3.d], writes=[sef.d])
        kb.op("dve", lambda: nc.vector.tensor_scalar(sef[:], sef[:], float(NE - 1), None, op0=ALU.min), reads=[sef.d], writes=[sef.d])
        kb.op("dve", lambda: nc.vector.tensor_copy(R.segexp[:], sef[:]), reads=[sef.d], writes=[R.segexp.d])
        used = T(kb, es, [128, NSEG], F32, "used")
        kb.op("dve", lambda: nc.vector.tensor_scalar(used[:], sidx[:], pend[:, NE - 1:NE], None, op0=ALU.is_lt),
              reads=[sidx.d, pend.d], writes=[used.d])
        pcf = T(kb, es, [128, 1], F32, "pcf")
        kb.op("pool", lambda: nc.gpsimd.iota(pcf[:], pattern=[[0, 1]], base=0, channel_multiplier=1,
                                              allow_small_or_imprecise_dtypes=True), writes=[pcf.d])
        OOB = 1000000.0
        sgf = T(kb, es, [128, NSEG], F32, "sgf")
        kb.op("dve", lambda: nc.vector.tensor_scalar(sgf[:], sef[:], 128.0, pcf[:, 0:1], op0=ALU.mult, op1=ALU.add),
              reads=[sef.d, pcf.d], writes=[sgf.d])
        for src, dst in ((sgf, R.segidx), (sef, R.segrow)):
            kb.op("dve", lambda src=src: nc.vector.tensor_scalar(src[:], src[:], -OOB, None, op0=ALU.add),
                  reads=[src.d], writes=[src.d])
            kb.op("dve", lambda src=src: nc.vector.tensor_tensor(src[:], src[:], used[:], op=ALU.mult),
                  reads=[src.d, used.d], writes=[src.d])
            kb.op("dve", lambda src=src: nc.vector.tensor_scalar(src[:], src[:], OOB, None, op0=ALU.add),
                  reads=[src.d], writes=[src.d])
            kb.op("dve", lambda src=src, dst=dst: nc.vector.tensor_copy(dst[:], src[:]), reads=[src.d], writes=[dst.d])
        if "logits" in g.dbg:
            dbg_out(g, "cnt", cnt[:], [cnt.d], [128, NE])
            dbg_out(g, "segexp", R.segexp[:], [R.segexp.d], [128, NSEG], I32)
        carry = T(kb, es, [128, NE], F32, "carry")
        kb.op("dve", lambda: nc.vector.tensor_copy(carry[:], pst[:]), reads=[pst.d], writes=[carry.d])
        ief = T(kb, es, [128, NE], F32, "ief")
        kb.op("pool", lambda: nc.gpsimd.iota(ief[:], pattern=[[1, NE]], base=0, channel_multiplier=0,
                                              allow_small_or_imprecise_dtypes=True), writes=[ief.d])
        slf = T(kb, es, [128, NE], F32, "slf")
        slk = T(kb, es, [128, 4], F32, "slk")
        hld = [T(kb, es, [128, D], BF16, f"hld{i}") for i in range(2)]
        for tl in range(32):
            t0 = tl * 128
            psA = next_ps(g)
            kb.op("pe", lambda tl=tl, psA=psA: nc.tensor.matmul(psA[:, 0:NE], lhsT=triu[:], rhs=R.mask[:, tl, :],
                                                                start=True, stop=True),
                  reads=[triu.d, R.mask.d], writes=[psA.d])
            kb.op("dve", lambda psA=psA: nc.vector.tensor_tensor(slf[:], psA[:, 0:NE], carry[:], op=ALU.add),
                  reads=[psA.d, carry.d], writes=[slf.d])
            psB = next_ps(g)
            kb.op("pe", lambda tl=tl, psB=psB: nc.tensor.matmul(psB[:, 0:NE], lhsT=onesf[:], rhs=R.mask[:, tl, :],
                                                                start=True, stop=True),
                  reads=[onesf.d, R.mask.d], writes=[psB.d])
            kb.op("dve", lambda psB=psB: nc.vector.tensor_tensor(carry[:], carry[:], psB[:, 0:NE], op=ALU.add),
                  reads=[psB.d, carry.d, slf.d], writes=[carry.d])
            for k in range(4):
                kb.op("dve", lambda k=k, tl=tl: nc.vector.scalar_tensor_tensor(
                    t32[:], ief[:], R.idxf[:, tl, k:k + 1], slf[:], op0=ALU.is_equal, op1=ALU.mult,
                    accum_out=slk[:, k:k + 1]), reads=[ief.d, R.idxf.d, slf.d], writes=[t32.d, slk.d])
            kb.op("dve", lambda tl=tl: nc.vector.tensor_copy(R.slot[:, tl, :], slk[:]), reads=[slk.d], writes=[R.slot.ds[tl]])
            hl = hld[tl % 2]
            kb.dma("sp", lambda hl=hl, t0=t0: nc.sync.dma_start(out=hl[:], in_=S.h2[t0:t0 + 128, :]),
                   reads=[S.h2_d], writes=[hl.d])
            for k in range(4):
                kb.dma("pool", lambda hl=hl, tl=tl, k=k: nc.gpsimd.indirect_dma_start(
                    out=S.xg[:, :], out_offset=bass.IndirectOffsetOnAxis(ap=R.slot[:, tl, k:k + 1], axis=0),
                    in_=hl[:, :], in_offset=None), reads=[hl.d, R.slot.ds[tl]], writes=[S.xg_d])
        if "logits" in g.dbg:
            dbg_out(g, "slot", R.slot[:], R.slot.ds, [128, 32, 4], I32)
        bg = T(kb, es, [NE, 2 * D], F32, "bgrow")
        kb.dma("sp", lambda: nc.sync.dma_start(out=bg[:], in_=H.exp_b_gu[0]), writes=[bg.d])
        bgt = T(kb, es, [128, 16, NE], F32, "bgt")
        for c4 in range(0, 16, 4):
            ps = next_ps(g)
            for cc in range(4):
                kb.op("pe", lambda cc=cc, c4=c4, ps=ps: nc.tensor.transpose(
                    ps[:, cc * NE:(cc + 1) * NE], bg[:, (c4 + cc) * 128:(c4 + cc + 1) * 128], g.ident[0:NE, 0:NE]),
                    reads=[bg.d, g.ident.d], writes=[ps.d])
            kb.op("act", lambda c4=c4, ps=ps: nc.scalar.copy(
                bgt[:, c4:c4 + 4, :], ps[:, 0:4 * NE].rearrange("p (a e) -> p a e", a=4)), reads=[ps.d], writes=[bgt.d])
        kb.dma("sp", lambda: nc.sync.dma_start(out=S.bguT.ap().rearrange("e p c -> p c e"), in_=bgt[:],
                                               allow_slow_non_contiguous=True), reads=[bgt.d], writes=[S.bguT_d])


def phase_f(g):
    nc, kb, H, S = g.nc, g.kb, g.H, g.S
    R = g.R
    with ExitStack() as es:
        wgu = [T(kb, es, [128, 8, 2 * D], BF16, f"wgu{i}") for i in range(2)]
        wdn = [T(kb, es, [128, 8, D], BF16, f"wdn{i}") for i in range(2)]
        bgu = [T(kb, es, [128, 16], F32, f"bgu{i}") for i in range(2)]
        bdn = [T(kb, es, [128, D], F32, f"bdn{i}") for i in range(2)]
        bgs = [T(kb, es, [128, 8], F32, f"bgs{i}") for i in range(2)]
        xrow = [T(kb, es, [128, JB, D], BF16, f"xrow{i}") for i in range(2)]
        xT = [T(kb, es, [128, 8, SEG], BF16, f"xT{i}") for i in range(2)]
        actT = [T(kb, es, [128, 8, SEG], BF16, f"actT{i}", nd=8) for i in range(2)]
        yrow = [T(kb, es, [128, JB, D], BF16, f"yrow{i}") for i in range(2)]
        L0 = [T(kb, es, [128, SEG], F32, f"fL0_{i}") for i in range(2)]
        Gc = [T(kb, es, [128, SEG], F32, f"fGc_{i}") for i in range(2)]
        sg = [T(kb, es, [128, SEG], F32, f"fsg_{i}") for i in range(2)]
        tt = [T(kb, es, [128, SEG], F32, f"ftt_{i}") for i in range(2)]
        bc_rows = nc.gpsimd.to_reg(NE * 128 - 1)
        bc_e = nc.gpsimd.to_reg(NE - 1)
        wg_rows = S.wg.ap().rearrange("e p f -> (e p) f")
        wd_rows = S.wd.ap().rearrange("e p f -> (e p) f")
        bgu_rows = S.bguT.ap().rearrange("e p c -> (e p) c")
        nseg = g.nseg_limit if getattr(g, "nseg_limit", None) else NSEG
        bench = getattr(g, "bench", "") or ""
        for s in range(nseg):
            b = s % 2
            if "nodma" in bench and s >= 2:
                KB.dead = True
            kb.dma("pool", lambda: nc.gpsimd.indirect_dma_start(
                out=wgu[b][:, :, :].rearrange("p k n -> p (k n)"), out_offset=None, in_=wg_rows,
                in_offset=bass.IndirectOffsetOnAxis(ap=R.segidx[:, s:s + 1], axis=0),
                bounds_check=bc_rows, oob_is_err=False),
                reads=[R.segidx.d, S.wg_d], writes=[wgu[b].d])
            kb.dma("pool", lambda: nc.gpsimd.indirect_dma_start(
                out=wdn[b][:, :, :].rearrange("p k n -> p (k n)"), out_offset=None, in_=wd_rows,
                in_offset=bass.IndirectOffsetOnAxis(ap=R.segidx[:, s:s + 1], axis=0),
                bounds_check=bc_rows, oob_is_err=False),
                reads=[R.segidx.d, S.wd_d], writes=[wdn[b].d])
            kb.dma("pool", lambda: nc.gpsimd.indirect_dma_start(
                out=bgu[b][:, :], out_offset=None, in_=bgu_rows,
                in_offset=bass.IndirectOffsetOnAxis(ap=R.segidx[:, s:s + 1], axis=0),
                bounds_check=bc_rows, oob_is_err=False),
                reads=[R.segidx.d, S.bguT_d], writes=[bgu[b].d])
            kb.dma("pool", lambda: nc.gpsimd.indirect_dma_start(
                out=bdn[b][:, :], out_offset=None, in_=H.exp_b_down[0],
                in_offset=bass.IndirectOffsetOnAxis(ap=R.segrow[:, s:s + 1], axis=0),
                bounds_check=bc_e, oob_is_err=False),
                reads=[R.segrow.d], writes=[bdn[b].d])
            if bench:
                KB.dead = False
            kb.op("act", lambda: nc.scalar.mul(bgs[b][:], bgu[b][:, 8:16], 1.0 / 1.702), reads=[bgu[b].d], writes=[bgs[b].d])
            kb.dma("sp", lambda: nc.sync.dma_start(
                out=xrow[b][:], in_=S.xg[s * SEG:(s + 1) * SEG, :].rearrange("(j p) d -> p j d", p=128)),
                reads=[S.xg_d], writes=[xrow[b].d])
            if "nocomp" in bench:
                KB.dead = True
            for kc in range(8):
                pb = g.psb[kc % 2]
                for j in range(JB):
                    kb.op("pe", lambda j=j, kc=kc, pb=pb: nc.tensor.transpose(
                        pb[:, j * 128:(j + 1) * 128], xrow[b][:, j, kc * 128:(kc + 1) * 128], g.identb[:]),
                        reads=[xrow[b].d, g.identb.d], writes=[pb.d])
                if kc % 2 == 0:
                    kb.op("act", lambda kc=kc, pb=pb: nc.scalar.copy(xT[b][:, kc, :], pb[:, 0:SEG]),
                          reads=[pb.d], writes=[xT[b].d])
                else:
                    kb.op("dve", lambda kc=kc, pb=pb: nc.vector.tensor_copy(xT[b][:, kc, :], pb[:, 0:SEG]),
                          reads=[pb.d], writes=[xT[b].d])
            for c in range(8):
                i2 = c % 2
                pg = next_ps(g)
                for kc in range(8):
                    kb.op("pe", lambda kc=kc, c=c, pg=pg: nc.tensor.matmul(
                        pg[:, 0:SEG], lhsT=wgu[b][:, kc, c * 128:(c + 1) * 128], rhs=xT[b][:, kc, :],
                        start=(kc == 0), stop=(kc == 7)), reads=[wgu[b].d, xT[b].d], writes=[pg.d])
                pl_ = next_ps(g)
                for kc in range(8):
                    kb.op("pe", lambda kc=kc, c=c, pl_=pl_: nc.tensor.matmul(
                        pl_[:, 0:SEG], lhsT=wgu[b][:, kc, D + c * 128:D + (c + 1) * 128], rhs=xT[b][:, kc, :],
                        start=(kc == 0), stop=(kc == 7)), reads=[wgu[b].d, xT[b].d], writes=[pl_.d])
                kb.op("dve", lambda c=c, pg=pg: nc.vector.tensor_scalar(Gc[i2][:], pg[:, 0:SEG], bgu[b][:, c:c + 1], 7.0,
                                                                      op0=ALU.add, op1=ALU.min),
                      reads=[pg.d, bgu[b].d], writes=[Gc[i2].d])
                kb.op("act", lambda: nc.scalar.activation(sg[i2][:], Gc[i2][:], ACT.Silu, scale=1.702),
                      reads=[Gc[i2].d], writes=[sg[i2].d])
                kb.op("act", lambda c=c, pl_=pl_: nc.scalar.activation(L0[i2][:], pl_[:, 0:SEG], ACT.Identity,
                                                                      bias=bgs[b][:, c:c + 1], scale=1.0 / 1.702),
                      reads=[pl_.d, bgs[b].d], writes=[L0[i2].d])
                kb.op("dve", lambda: nc.vector.tensor_scalar(L0[i2][:], L0[i2][:], 7.0 / 1.702, -7.0 / 1.702,
                                                             op0=ALU.min, op1=ALU.max),
                      reads=[L0[i2].d], writes=[L0[i2].d])
                kb.op("dve", lambda c=c: nc.vector.scalar_tensor_tensor(actT[b][:, c, :], L0[i2][:], 1.0 / 1.702, sg[i2][:],
                                                                       op0=ALU.add, op1=ALU.mult),
                      reads=[L0[i2].d, sg[i2].d], writes=[actT[b].ds[c]])
            for j in range(JB):
                for nh in range(2):
                    ps = next_ps(g)
                    for c in range(8):
                        kb.op("pe", lambda c=c, j=j, nh=nh, ps=ps: nc.tensor.matmul(
                            ps[:, :], lhsT=actT[b][:, c, j * 128:(j + 1) * 128], rhs=wdn[b][:, c, nh * 512:(nh + 1) * 512],
                            start=(c == 0), stop=(c == 7)), reads=[actT[b].ds[c], wdn[b].d], writes=[ps.d])
                    kb.op("dve", lambda j=j, nh=nh, ps=ps: nc.vector.tensor_tensor(
                        yrow[b][:, j, nh * 512:(nh + 1) * 512], ps[:, :], bdn[b][:, nh * 512:(nh + 1) * 512], op=ALU.add),
                        reads=[ps.d, bdn[b].d], writes=[yrow[b].d])
            kb.dma("act", lambda: nc.scalar.dma_start(
                out=S.yg[s * SEG:(s + 1) * SEG, :].rearrange("(j p) d -> p j d", p=128), in_=yrow[b][:]),
                reads=[yrow[b].d], writes=[S.yg_d])
            if bench:
                KB.dead = False


def phase_g(g):
    nc, kb, H, S = g.nc, g.kb, g.H, g.S
    R = g.R
    with ExitStack() as es:
        g2b = bc_tile(g, es, "g2b", S.mod[0:1, 5 * D:6 * D], D, [S.mod_d])
        fnw = bc_tile(g, es, "fnw", H.final_norm_w.ap().rearrange("(o d) -> o d", o=1), D)
        yk = [[T(kb, es, [128, D], BF16, f"yk{i}_{k}") for k in range(4)] for i in range(2)]
        x2t = [T(kb, es, [128, D], F32, f"gx2{i}") for i in range(2)]
        acc_ = [T(kb, es, [128, D], F32, f"gacc{i}") for i in range(2)]
        x3_ = [T(kb, es, [128, D], F32, f"gx3{i}") for i in range(2)]
        sm_ = [T(kb, es, [128, 4], F32, f"gsm{i}") for i in range(2)]
        ot = [T(kb, es, [128, D], F32, f"got{i}") for i in range(2)]
        junk = T(kb, es, [128, D], BF16, "gjunk")
        sm1 = T(kb, es, [128, 4], F32, "gsm")
        for tl in range(NL // 128):
            t0 = tl * 128
            b = tl % 2
            acc, x3, sm1 = acc_[b], x3_[b], sm_[b]
            for k in range(4):
                kb.dma("pool", lambda k=k: nc.gpsimd.indirect_dma_start(
                    out=yk[b][k][:, :], out_offset=None, in_=S.yg[:, :],
                    in_offset=bass.IndirectOffsetOnAxis(ap=R.slot[:, tl, k:k + 1], axis=0)),
                    reads=[S.yg_d, R.slot.ds[tl]], writes=[yk[b][k].d])
            kb.dma("sp", lambda: nc.sync.dma_start(out=x2t[b][:], in_=S.x2[t0:t0 + 128, :]), reads=[S.x2_d], writes=[x2t[b].d])
            kb.op("act", lambda: nc.scalar.activation(acc[:], yk[b][0][:], ACT.Identity, scale=R.gate[:, tl, 0:1]),
                  reads=[yk[b][0].d, R.gate.d], writes=[acc.d])
            for k in range(1, 4):
                kb.op("dve", lambda k=k: nc.vector.scalar_tensor_tensor(acc[:], yk[b][k][:], R.gate[:, tl, k:k + 1], acc[:],
                                                                       op0=ALU.mult, op1=ALU.add),
                      reads=[yk[b][k].d, R.gate.d, acc.d], writes=[acc.d])
            kb.op("dve", lambda: nc.vector.tensor_tensor(acc[:], acc[:], g2b[:], op=ALU.mult), reads=[acc.d, g2b.d], writes=[acc.d])
            kb.op("dve", lambda: nc.vector.tensor_tensor(x3[:], acc[:], x2t[b][:], op=ALU.add), reads=[acc.d, x2t[b].d], writes=[x3.d])
            kb.op("act", lambda: nc.scalar.activation(junk[:], x3[:], ACT.Square, accum_out=sm1[:, 0:1]),
                  reads=[x3.d], writes=[junk.d, sm1.d])
            kb.op("dve", lambda: nc.vector.tensor_scalar(sm1[:, 1:2], sm1[:, 0:1], 1.0 / D, EPS, op0=ALU.mult, op1=ALU.add),
                  reads=[sm1.d], writes=[sm1.d])
            kb.op("act", lambda: nc.scalar.activation(sm1[:, 1:2], sm1[:, 1:2], ACT.Sqrt), reads=[sm1.d], writes=[sm1.d])
            kb.op("dve", lambda: nc.vector.reciprocal(sm1[:, 2:3], sm1[:, 1:2]), reads=[sm1.d], writes=[sm1.d])
            kb.op("dve", lambda: nc.vector.scalar_tensor_tensor(ot[b][:], x3[:], sm1[:, 2:3], fnw[:], op0=ALU.mult, op1=ALU.mult),
                  reads=[x3.d, sm1.d, fnw.d], writes=[ot[b].d])
            kb.dma("act", lambda: nc.scalar.dma_start(out=H.out[t0:t0 + 128, :], in_=ot[b][:]), reads=[ot[b].d], writes=[g.out_d])


_SHARED = ("c_ctx", "ada_w", "ada_b", "norm_mix_w", "w_in", "gla_lr_up", "gla_lr_bias", "gla_norm_w",
           "s5_lam_re", "s5_lam_im", "s5_log_dt", "s5_b_re", "s5_b_im", "s5_c_re", "s5_c_im", "s5_d",
           "glu_w", "glu_b", "w_out", "norm_ffn_w", "router_w", "router_b", "exp_w_gu", "exp_b_gu",
           "exp_w_down", "exp_b_down", "final_norm_w")


def kernel(x, c, ctx, c_ctx, ada_w, ada_b, norm_mix_w, w_in, gla_lr_up, gla_lr_bias, gla_norm_w,
           s5_lam_re, s5_lam_im, s5_log_dt, s5_b_re, s5_b_im, s5_c_re, s5_c_im, s5_d, glu_w, glu_b,
           w_out, norm_ffn_w, router_w, router_b, exp_w_gu, exp_b_gu, exp_w_down, exp_b_down,
           final_norm_w):
    loc = locals()
    shared = {k: np.ascontiguousarray(np.asarray(loc[k], dtype=np.float32)) for k in _SHARED}
    x = np.asarray(x, dtype=np.float32)
    c = np.asarray(c, dtype=np.float32)
    ctx = np.asarray(ctx, dtype=np.float32)
    nb = x.shape[0]
    nc, _ = build()
    in_maps = []
    for b in range(nb):
        m = dict(shared)
        m["x"] = np.ascontiguousarray(x[b])
        m["ctx"] = np.ascontiguousarray(ctx[b])
        m["c"] = np.ascontiguousarray(c[b:b + 1])
        in_maps.append(m)
    res = run_bass_kernel_spmd(nc, in_maps, core_ids=list(range(nb)))
    return np.stack([np.asarray(r["out"], dtype=np.float32) for r in res.results], axis=0)
```
